# Optimizing a Trainium2 kernel written in Bass

```python
import jax, jax.numpy as jnp
from jax import lax
import numpy as np

D_MODEL = 1024
BATCH = 2
SEQ = 16384
DEPTH = 1

MLA_HEADS = 8
QK_NOPE_DIM = 64
QK_ROPE_DIM = 32
QK_DIM = QK_NOPE_DIM + QK_ROPE_DIM
V_HEAD_DIM = 64
MLA_WIDTH = MLA_HEADS * V_HEAD_DIM
Q_LORA_RANK = 256
KV_LORA_RANK = 128
ROPE_THETA = 10000.0
Q_BLOCK = 128
SGU_GROUPS = 8
SGU_GROUP_DIM = 64
SGU_WIDTH = SGU_GROUPS * SGU_GROUP_DIM
SGU_CHUNK = 128
N_GROUPS = 4
EXPERTS_PER_GROUP = 8
N_EXPERTS = N_GROUPS * EXPERTS_PER_GROUP
TOP_K = 2
EXPERT_FF = 512
MOE_BLOCK = 256
N_BRANCHES = 2
N_MOD = 6
EPS = 1e-6
IN_WIDTH = Q_LORA_RANK + KV_LORA_RANK + QK_ROPE_DIM + 2 * SGU_WIDTH + N_BRANCHES * D_MODEL

kernel_name = "hybrid_mla_sgu_hmoe_adaln_block"


def rmsnorm(x, g):
    xf = x.astype(jnp.float32)
    y = xf * lax.rsqrt(jnp.mean(xf * xf, axis=-1, keepdims=True) + EPS)
    return (y * g.astype(jnp.float32)).astype(x.dtype)


def layernorm(x, g, b):
    xf = x.astype(jnp.float32)
    mu = jnp.mean(xf, axis=-1, keepdims=True)
    var = jnp.mean(jnp.square(xf - mu), axis=-1, keepdims=True)
    y = (xf - mu) * lax.rsqrt(var + EPS)
    return (y * g.astype(jnp.float32) + b.astype(jnp.float32)).astype(x.dtype)


def rope(x, positions):
    half = x.shape[-1] // 2
    freqs = ROPE_THETA ** (-jnp.arange(half, dtype=jnp.float32) / half)
    ang = positions.astype(jnp.float32)[..., None] * freqs
    cos = jnp.cos(ang)[:, :, None, :]
    sin = jnp.sin(ang)[:, :, None, :]
    xf = x.astype(jnp.float32)
    x1, x2 = xf[..., :half], xf[..., half:]
    out = jnp.concatenate([x1 * cos - x2 * sin, x1 * sin + x2 * cos], axis=-1)
    return out.astype(x.dtype)


def mla(c_q, c_kv, k_rope, positions, q_norm_g, w_uq, kv_norm_g, w_ukv):
    B, S, _ = c_q.shape
    q = (rmsnorm(c_q, q_norm_g) @ w_uq).reshape(B, S, MLA_HEADS, QK_DIM)
    q = jnp.concatenate([q[..., :QK_NOPE_DIM], rope(q[..., QK_NOPE_DIM:], positions)], axis=-1)
    kv = (rmsnorm(c_kv, kv_norm_g) @ w_ukv).reshape(B, S, MLA_HEADS, QK_NOPE_DIM + V_HEAD_DIM)
    k_nope, v = kv[..., :QK_NOPE_DIM], kv[..., QK_NOPE_DIM:]
    k_pe = rope(k_rope[:, :, None, :], positions)
    k = jnp.concatenate([k_nope, jnp.broadcast_to(k_pe, (B, S, MLA_HEADS, QK_ROPE_DIM))], axis=-1)
    scale = QK_DIM ** -0.5
    nb = S // Q_BLOCK
    qb = q.reshape(B, nb, Q_BLOCK, MLA_HEADS, QK_DIM).transpose(1, 0, 2, 3, 4)
    k_idx = jnp.arange(S)

    def attend_block(args):
        q_blk, i = args
        s = jnp.einsum('bqhd,bkhd->bhqk', q_blk, k).astype(jnp.float32) * scale
        q_idx = i * Q_BLOCK + jnp.arange(Q_BLOCK)
        causal = k_idx[None, :] <= q_idx[:, None]
        s = jnp.where(causal[None, None], s, -jnp.inf)
        p = jax.nn.softmax(s, axis=-1).astype(v.dtype)
        return jnp.einsum('bhqk,bkhd->bqhd', p, v)

    o = lax.map(attend_block, (qb, jnp.arange(nb)))
    return o.transpose(1, 0, 2, 3, 4).reshape(B, S, MLA_WIDTH)


def spatial_gating(u, v, v_norm_g, v_norm_b, w_s, b_s):
    u = jax.nn.gelu(u)
    v = layernorm(jax.nn.gelu(v), v_norm_g, v_norm_b)
    B, S, _ = v.shape
    nc = S // SGU_CHUNK
    vc = v.reshape(B, nc, SGU_CHUNK, SGU_GROUPS, SGU_GROUP_DIM)
    tril = jnp.tril(jnp.ones((SGU_CHUNK, SGU_CHUNK), dtype=bool))
    ws = jnp.where(tril[None], w_s, jnp.zeros_like(w_s))
    s = jnp.einsum('gts,bnsgc->bntgc', ws, vc) + b_s.T[None, None, :, :, None]
    return u * s.reshape(B, S, SGU_WIDTH)


def hierarchical_moe(h, w_rg, b_rg, w_re, b_re, w1, w3, w2):
    B, S, D = h.shape
    xf = h.reshape(-1, D)
    N = xf.shape[0]
    g_logits = (xf @ w_rg).astype(jnp.float32) + b_rg.astype(jnp.float32)
    g_prob = jax.nn.softmax(g_logits, axis=-1)
    grp = jnp.argmax(g_logits, axis=-1)
    p_grp = jnp.take_along_axis(g_prob, grp[:, None], axis=-1)
    e_logits = ((xf @ w_re).astype(jnp.float32) + b_re.astype(jnp.float32)).reshape(N, N_GROUPS, EXPERTS_PER_GROUP)
    e_in = jnp.take_along_axis(e_logits, grp[:, None, None], axis=1)[:, 0]
    e_prob = jax.nn.softmax(e_in, axis=-1)
    top_p, top_i = lax.top_k(e_prob, TOP_K)
    top_p = top_p / jnp.sum(top_p, axis=-1, keepdims=True)
    weights = p_grp * top_p
    experts = grp[:, None] * EXPERTS_PER_GROUP + top_i
    A = N * TOP_K
    flat_e = experts.reshape(-1)
    flat_tok = jnp.arange(A, dtype=jnp.int32) // TOP_K
    flat_w = weights.reshape(-1)
    order = jnp.argsort(flat_e)
    sorted_e = flat_e[order]
    counts = jnp.zeros((N_EXPERTS,), jnp.int32).at[flat_e].add(1)
    starts = jnp.cumsum(counts) - counts
    padded = ((counts + MOE_BLOCK - 1) // MOE_BLOCK) * MOE_BLOCK
    pad_ends = jnp.cumsum(padded)
    pad_starts = pad_ends - padded
    rank = jnp.arange(A, dtype=jnp.int32) - starts[sorted_e]
    dest = pad_starts[sorted_e] + rank
    P = ((A + MOE_BLOCK - 1) // MOE_BLOCK + N_EXPERTS) * MOE_BLOCK
    nblk = P // MOE_BLOCK
    slot_tok = jnp.zeros((P,), jnp.int32).at[dest].set(flat_tok[order])
    slot_w = jnp.zeros((P,), jnp.float32).at[dest].set(flat_w[order])
    blk_e = jnp.minimum(jnp.searchsorted(pad_ends, jnp.arange(nblk) * MOE_BLOCK, side='right'), N_EXPERTS - 1)
    x_blk = xf[slot_tok].reshape(nblk, MOE_BLOCK, D)

    def expert_block(args):
        xb, e = args
        return (jax.nn.silu(xb @ w1[e]) * (xb @ w3[e])) @ w2[e]

    y = lax.map(expert_block, (x_blk, blk_e)).reshape(P, D)
    out = jnp.zeros((N, D), jnp.float32).at[slot_tok].add(y.astype(jnp.float32) * slot_w[:, None])
    return out.astype(h.dtype).reshape(B, S, D)


def setup_inputs(seed: int = 0) -> dict:
    key = jax.random.key(seed)
    ks = jax.random.split(key, 32)
    f32 = jnp.float32

    def nrm(k, shape, fan_in, mult=1.0):
        return jax.random.normal(k, shape, f32) * (mult * fan_in ** -0.5)

    def gain(k, shape):
        return 1.0 + 0.02 * jax.random.normal(k, shape, f32)

    L = DEPTH
    x = jax.random.normal(ks[0], (BATCH, SEQ, D_MODEL), f32)
    c = jax.random.normal(ks[1], (BATCH, D_MODEL), f32)
    offs = jax.random.randint(ks[2], (BATCH, 1), 0, 4096, dtype=jnp.int32)
    positions = jnp.arange(SEQ, dtype=jnp.int32)[None, :] + offs
    return {
        "x": x,
        "c": c,
        "positions": positions,
        "w_ada": nrm(ks[3], (L, D_MODEL, N_MOD * D_MODEL), D_MODEL),
        "b_ada": 0.01 * jax.random.normal(ks[4], (L, N_MOD * D_MODEL), f32),
        "norm1_g": gain(ks[5], (L, D_MODEL)),
        "w_in": nrm(ks[6], (L, D_MODEL, IN_WIDTH), D_MODEL),
        "q_norm_g": gain(ks[7], (L, Q_LORA_RANK)),
        "w_uq": nrm(ks[8], (L, Q_LORA_RANK, MLA_HEADS * QK_DIM), Q_LORA_RANK),
        "kv_norm_g": gain(ks[9], (L, KV_LORA_RANK)),
        "w_ukv": nrm(ks[10], (L, KV_LORA_RANK, MLA_HEADS * (QK_NOPE_DIM + V_HEAD_DIM)), KV_LORA_RANK),
        "v_norm_g": gain(ks[11], (L, SGU_WIDTH)),
        "v_norm_b": 0.01 * jax.random.normal(ks[12], (L, SGU_WIDTH), f32),
        "w_s": nrm(ks[13], (L, SGU_GROUPS, SGU_CHUNK, SGU_CHUNK), SGU_CHUNK),
        "b_s": gain(ks[14], (L, SGU_GROUPS, SGU_CHUNK)),
        "w_br_mla": nrm(ks[15], (L, MLA_WIDTH, D_MODEL), MLA_WIDTH),
        "w_br_sgu": nrm(ks[16], (L, SGU_WIDTH, D_MODEL), SGU_WIDTH),
        "w_out": nrm(ks[17], (L, D_MODEL, D_MODEL), D_MODEL),
        "norm2_g": gain(ks[18], (L, D_MODEL)),
        "w_rg": nrm(ks[19], (L, D_MODEL, N_GROUPS), D_MODEL),
        "b_rg": 0.01 * jax.random.normal(ks[20], (L, N_GROUPS), f32),
        "w_re": nrm(ks[21], (L, D_MODEL, N_EXPERTS), D_MODEL),
        "b_re": 0.01 * jax.random.normal(ks[22], (L, N_EXPERTS), f32),
        "w1": nrm(ks[23], (L, N_EXPERTS, D_MODEL, EXPERT_FF), D_MODEL),
        "w3": nrm(ks[24], (L, N_EXPERTS, D_MODEL, EXPERT_FF), D_MODEL),
        "w2": nrm(ks[25], (L, N_EXPERTS, EXPERT_FF, D_MODEL), EXPERT_FF),
        "final_g": gain(ks[26], (D_MODEL,)),
    }


def reference(x, c, positions, w_ada, b_ada, norm1_g, w_in, q_norm_g, w_uq, kv_norm_g, w_ukv,
              v_norm_g, v_norm_b, w_s, b_s, w_br_mla, w_br_sgu, w_out, norm2_g,
              w_rg, b_rg, w_re, b_re, w1, w3, w2, final_g):
    split_idx = list(np.cumsum([Q_LORA_RANK, KV_LORA_RANK, QK_ROPE_DIM, SGU_WIDTH, SGU_WIDTH, D_MODEL])[:])
    for l in range(DEPTH):
        mod = jax.nn.silu(c) @ w_ada[l] + b_ada[l]
        shift1, scale1, gate1, shift2, scale2, gate2 = [m[:, None, :] for m in jnp.split(mod, N_MOD, axis=-1)]

        h1 = rmsnorm(x, norm1_g[l]) * (1.0 + scale1) + shift1
        proj = h1 @ w_in[l]
        c_q, c_kv, k_rope, sgu_u, sgu_v, gl_mla, gl_sgu = jnp.split(proj, split_idx, axis=-1)
        y_mla = mla(c_q, c_kv, k_rope, positions, q_norm_g[l], w_uq[l], kv_norm_g[l], w_ukv[l]) @ w_br_mla[l]
        y_sgu = spatial_gating(sgu_u, sgu_v, v_norm_g[l], v_norm_b[l], w_s[l], b_s[l]) @ w_br_sgu[l]
        merged = jax.nn.sigmoid(gl_mla) * y_mla + jax.nn.sigmoid(gl_sgu) * y_sgu
        x = x + gate1 * (merged @ w_out[l])

        h2 = rmsnorm(x, norm2_g[l]) * (1.0 + scale2) + shift2
        x = x + gate2 * hierarchical_moe(h2, w_rg[l], b_rg[l], w_re[l], b_re[l], w1[l], w3[l], w2[l])
    return rmsnorm(x, final_g)
```

```python
import numpy as np
from contextlib import ExitStack
import concourse.bass as bass
import concourse.mybir as mybir
from concourse.bass_utils import run_bass_kernel_spmd

F32 = mybir.dt.float32
BF16 = mybir.dt.bfloat16
I32 = mybir.dt.int32
AF = mybir.ActivationFunctionType
ALU = mybir.AluOpType
AX = mybir.AxisListType

D = 1024
NH = 8
EPS = 1e-6
NEXP = 32
FF = 512
BLK = 256
IN_W = 3488
SCALE = 96 ** -0.5
GELU_C = 1.5957691216057308


class Res:
    __slots__ = ("name", "w", "r")

    def __init__(self, name=""):
        self.name = name
        self.w = None
        self.r = {}


class Lane:
    def __init__(self, T, name):
        self.sem = T.newsem(name)
        self.count = 0


class Eng:
    def __init__(self, T, name, eng, is_pe=False):
        self.name = name
        self.eng = eng
        self.is_pe = is_pe
        self.sem = T.newsem(name + "_s0")
        self.own = [self.sem]
        self.count = 0
        self.waited = {}


class Tracker:
    CAP = 12000

    def __init__(self, nc, es):
        self.nc = nc
        self.es = es
        self.nsems = 0
        self.lanes = []
        self.pe = Eng(self, "pe", nc.tensor, True)
        self.act = Eng(self, "act", nc.scalar)
        self.dve = Eng(self, "dve", nc.vector)
        self.pool = Eng(self, "pool", nc.gpsimd)
        self.sp = Eng(self, "sp", nc.sync)
        self.engs = [self.pe, self.act, self.dve, self.pool, self.sp]

    def newsem(self, name):
        self.nsems += 1
        return self.es.enter_context(self.nc.semaphore(f"{name}_{self.nsems}"))

    def lane(self, name="l"):
        l = Lane(self, name)
        self.lanes.append(l)
        return l

    def _wait(self, E, deps):
        for sem, val in deps:
            if E.is_pe and any(sem is s for s in E.own):
                continue
            k = id(sem)
            if E.waited.get(k, 0) < val:
                E.eng.wait_ge(sem, val)
                E.waited[k] = val

    @staticmethod
    def _deps(reads, writes):
        deps = []
        for r in reads:
            if r.w is not None:
                deps.append(r.w)
        for w in writes:
            if w.w is not None:
                deps.append(w.w)
            deps.extend(w.r.values())
        return deps

    @staticmethod
    def _record(ev, reads, writes):
        for r in reads:
            r.r[id(ev[0])] = ev
        for w in writes:
            w.w = ev
            w.r = {}

    def op(self, E, fn, reads=(), writes=()):
        self._wait(E, self._deps(reads, writes))
        if E.count >= self.CAP:
            E.sem = self.newsem(E.name + "_s")
            E.own.append(E.sem)
            E.count = 0
        ins = fn()
        ins.then_inc(E.sem, 1)
        E.count += 1
        self._record((E.sem, E.count), reads, writes)

    def dma(self, E, lane, fn, reads=(), writes=()):
        self._wait(E, self._deps(reads, writes))
        ins = fn()
        ins.then_inc(lane.sem, 16)
        lane.count += 16
        self._record((lane.sem, lane.count), reads, writes)

    def barrier(self):
        evs = [(e.sem, e.count) for e in self.engs if e.count > 0]
        evs += [(l.sem, l.count) for l in self.lanes if l.count > 0]
        for E in self.engs:
            self._wait(E, evs)


class Buf:
    def __init__(self, T, tile, name, lane=False):
        self.t = tile
        self.r = Res(name)
        self.l = T.lane(name) if lane else None


def build_nc(S, stop=99):
    NT = S // 128
    NP = NT // 8
    NOWN = 2 * NP
    TOK = NOWN * 128
    NG = NT // 4
    NGO = max(NOWN // 4, 1)
    GW = min(512, TOK)
    NBLK = (TOK * 2) // BLK + NEXP
    NSLOT = NBLK * BLK

    nc = bass.Bass("TRN2", target_bir_lowering=False)

    def din(name, shape, dt=F32):
        return nc.dram_tensor(name, list(shape), dt, kind="ExternalInput").ap()

    xb = din("xb", [S, D]); xo = din("xo", [TOK, D])
    posb = din("posb", [1, S], I32); poso = din("poso", [1, TOK], I32)
    ccol = din("ccol", [128, 8])
    w_ada = din("w_ada", [D, 6 * D]); b_adaT = din("b_adaT", [128, 48]); b_ada_row = din("b_ada_row", [1, 6 * D])
    g1col = din("g1col", [128, 8]); g1row = din("g1row", [1, D]); w_in = din("w_in", [D, IN_W])
    gqcol = din("gqcol", [128, 2]); w_uq = din("w_uq", [256, 768])
    gkvcol = din("gkvcol", [128, 1]); w_ukv = din("w_ukv", [128, 1024])
    gv_row = din("gv_row", [1, 512]); bv_row = din("bv_row", [1, 512])
    w_s = din("w_s", [8, 128, 128]); bsT = din("bsT", [128, 8])
    w_br_mla = din("w_br_mla", [512, D]); w_br_sgu = din("w_br_sgu", [512, D]); w_out = din("w_out", [D, D])
    g2col = din("g2col", [128, 8]); g2row = din("g2row", [1, D])
    wr = din("wr", [D, 36]); br_row = din("br_row", [1, 36])
    w1 = din("w1", [NEXP * 128, 4096]); w3 = din("w3", [NEXP * 128, 4096]); w2 = din("w2", [NEXP * 128, 4096])
    gf_row = din("gf_row", [1, D])
    c_ident = din("c_ident", [128, 128]); c_tril = din("c_tril", [128, 128]); c_lstrict = din("c_lstrict", [128, 128])
    c_kq = din("c_kq", [128, 128]); c_fq = din("c_fq", [128, 1]); c_dtab = din("c_dtab", [128, 16])
    c_thr = din("c_thr", [128, NBLK]); c_pidx = din("c_pidx", [128, 1])

    out_d = nc.dram_tensor("out", [TOK, D], F32, kind="ExternalOutput").ap()
    X1 = nc.dram_tensor("X1s", [TOK, D], F32, kind="Internal").ap()
    H2 = nc.dram_tensor("H2s", [TOK, D], BF16, kind="Internal").ap()
    XS = nc.dram_tensor("XSs", [NSLOT, D], BF16, kind="Internal").ap()
    YS = nc.dram_tensor("YSs", [NSLOT, D], F32, kind="Internal").ap()
    OS = nc.dram_tensor("OSs", [TOK, 512], BF16, kind="Internal").ap()

    with ExitStack() as es:
        T = Tracker(nc, es)
        PE, ACT, DVE, POOL, SP = T.pe, T.act, T.dve, T.pool, T.sp

        def sbt(stack, name, shape, dt, lane=False):
            t = stack.enter_context(nc.sbuf_tensor(name, list(shape), dt))
            return Buf(T, t, name, lane)

        banks = []
        for i in range(8):
            t = es.enter_context(nc.psum_tensor(f"pb{i}", [128, 512], F32))
            banks.append(Buf(T, t, f"pb{i}"))
        rot = {"i": 0, "lst": list(range(8))}

        def pnext():
            b = banks[rot["lst"][rot["i"] % len(rot["lst"])]]
            rot["i"] += 1
            return b

        def set_rot(lst):
            rot["lst"] = list(lst)
            rot["i"] = 0

        def V(fn, reads=(), writes=()):
            T.op(DVE, fn, reads, writes)

        def A(fn, reads=(), writes=()):
            T.op(ACT, fn, reads, writes)

        def G(fn, reads=(), writes=()):
            T.op(POOL, fn, reads, writes)

        def P(fn, reads=(), writes=()):
            T.op(PE, fn, reads, writes)

        def load(buf, src, eng=None):
            E = SP if eng is None else eng
            T.dma(E, buf.l, lambda: E.eng.dma_start(out=buf.t[:], in_=src), writes=[buf.r])

        def load_to(buf, dst_ap, src, eng=None):
            E = SP if eng is None else eng
            T.dma(E, buf.l, lambda: E.eng.dma_start(out=dst_ap, in_=src), writes=[buf.r])

        identf = sbt(es, "identf", [128, 128], F32, True); load(identf, c_ident)
        identb = sbt(es, "identb", [128, 128], BF16)
        V(lambda: nc.vector.tensor_copy(out=identb.t[:], in_=identf.t[:]), [identf.r], [identb.r])
        onesb = sbt(es, "onesb", [128, 128], BF16)
        V(lambda: nc.vector.memset(onesb.t[:], 1.0), [], [onesb.r])
        onesf = sbt(es, "onesf", [128, 128], F32)
        V(lambda: nc.vector.memset(onesf.t[:], 1.0), [], [onesf.r])
        kqf = sbt(es, "kqf", [128, 128], F32, True); load(kqf, c_kq)
        kqb = sbt(es, "kqb", [128, 128], BF16)
        V(lambda: nc.vector.tensor_copy(out=kqb.t[:], in_=kqf.t[:]), [kqf.r], [kqb.r])
        fq = sbt(es, "fq", [128, 1], F32, True); load(fq, c_fq)
        dtab = sbt(es, "dtab", [128, 16], F32, True); load(dtab, c_dtab)
        g1c = sbt(es, "g1c", [128, 8], F32, True); load(g1c, g1col)
        g2c = sbt(es, "g2c", [128, 8], F32, True); load(g2c, g2col)
        gqc = sbt(es, "gqc", [128, 2], F32, True); load(gqc, gqcol)
        gkvc = sbt(es, "gkvc", [128, 1], F32, True); load(gkvc, gkvcol)
        cc = sbt(es, "cc", [128, 8], F32, True); load(cc, ccol)
        badT = sbt(es, "badT", [128, 48], F32, True); load(badT, b_adaT)
        modT = sbt(es, "modT", [128, 48], F32)
        gate1b = sbt(es, "gate1b", [128, D], F32)
        s2b = sbt(es, "s2b", [128, D], F32)
        sh2b = sbt(es, "sh2b", [128, D], F32)
        gate2b = sbt(es, "gate2b", [128, D], F32)
        s1b = sbt(es, "s1b", [128, D], F32)
        sh1b = sbt(es, "sh1b", [128, 8], BF16)
        ones512 = sbt(es, "ones512", [1, 512], BF16)
        V(lambda: nc.vector.memset(ones512.t[:], 1.0), [], [ones512.r])
        s1c = sbt(es, "s1c", [128, 8], F32); s2c = sbt(es, "s2c", [128, 8], F32)

        with ExitStack() as ph:
            sc = sbt(ph, "sc", [128, 8], F32)
            A(lambda: nc.scalar.activation(out=sc.t[:], in_=cc.t[:], func=AF.Silu), [cc.r], [sc.r])
            brow = [sbt(ph, f"brow{i}", [1, 512], F32, True) for i in range(2)]
            rowdst = {2: (s1b, 0), 3: (s1b, 512), 4: (gate1b, 0), 5: (gate1b, 512), 6: (sh2b, 0), 7: (sh2b, 512), 8: (s2b, 0), 9: (s2b, 512),
                      10: (gate2b, 0), 11: (gate2b, 512)}
            wp = [sbt(ph, f"wada{i}", [128, 8, 512], F32, True) for i in range(2)]
            pcol = banks[7]
            set_rot(range(7))
            rowtmp = sbt(ph, "rowtmp", [1, 512], F32)
            for pc in range(12):
                wb_ = wp[pc % 2]
                load(wb_, w_ada[:, pc * 512:(pc + 1) * 512].rearrange("(kc p) n -> p kc n", p=128))
                if pc in rowdst:
                    prow = pnext()
                    br_ = brow[pc % 2]
                    load(br_, b_ada_row[0:1, pc * 512:(pc + 1) * 512])
                    for kc in range(8):
                        P(lambda: nc.tensor.matmul(prow.t[0:1, :], lhsT=sc.t[:, kc:kc + 1], rhs=wb_.t[:, kc, :],
                                                   start=(kc == 0), stop=(kc == 7)), [sc.r, wb_.r], [prow.r])
                    V(lambda: nc.vector.tensor_tensor(out=rowtmp.t[0:1, :], in0=prow.t[0:1, :], in1=br_.t[0:1, :], op=ALU.add),
                      [prow.r, br_.r], [rowtmp.r])
                    pbc = pnext()
                    P(lambda: nc.tensor.matmul(pbc.t[:], lhsT=onesf.t[0:1, :], rhs=rowtmp.t[0:1, :], start=True, stop=True),
                      [onesf.r, rowtmp.r], [pbc.r])
                    dtile, dc0 = rowdst[pc]
                    V(lambda: nc.vector.tensor_copy(out=dtile.t[:, dc0:dc0 + 512], in_=pbc.t[:]), [pbc.r], [dtile.r])
                for oc in range(4):
                    col = pc * 4 + oc
                    for kc in range(8):
                        P(lambda: nc.tensor.matmul(pcol.t[:, col:col + 1], lhsT=wb_.t[:, kc, oc * 128:(oc + 1) * 128],
                                                   rhs=sc.t[:, kc:kc + 1], start=(kc == 0), stop=(kc == 7)),
                          [sc.r, wb_.r], [pcol.r])
            V(lambda: nc.vector.tensor_tensor(out=modT.t[:], in0=pcol.t[:, 0:48], in1=badT.t[:], op=ALU.add),
              [pcol.r, badT.r], [modT.r])
            V(lambda: nc.vector.scalar_tensor_tensor(out=s1c.t[:], in0=modT.t[:, 8:16], scalar=1.0, in1=g1c.t[:],
                                                     op0=ALU.add, op1=ALU.mult), [modT.r, g1c.r], [s1c.r])
            V(lambda: nc.vector.scalar_tensor_tensor(out=s2c.t[:], in0=modT.t[:, 32:40], scalar=1.0, in1=g2c.t[:],
                                                     op0=ALU.add, op1=ALU.mult), [modT.r, g2c.r], [s2c.r])
            g1rb = sbt(ph, "g1rb", [128, D], F32, True); load(g1rb, g1row.partition_broadcast(128))
            V(lambda: nc.vector.scalar_tensor_tensor(out=s1b.t[:], in0=s1b.t[:], scalar=1.0, in1=g1rb.t[:], op0=ALU.add,
                                                     op1=ALU.mult), [s1b.r, g1rb.r], [s1b.r])
            V(lambda: nc.vector.tensor_copy(out=sh1b.t[:], in_=modT.t[:, 0:8]), [modT.r], [sh1b.r])
            T.barrier()
        if stop <= 0:
            return nc
        sh1 = lambda kc: modT.t[:, kc:kc + 1]
        sh2 = lambda kc: modT.t[:, 24 + kc:25 + kc]

        FE = {}
        cnt = {"x": 0, "fe": 0, "st": 0, "xn": 0, "cp": 0}

        def make_fe(stack, nx=3, nxn=2):
            k = cnt["fe"]; cnt["fe"] += 1
            FE["nx"] = nx
            FE["xts"] = [sbt(stack, f"xt{k}_{i}", [128, D], F32, True) for i in range(nx)]
            FE["junk"] = sbt(stack, f"junk{k}", [128, D], BF16)
            FE["stat"] = [sbt(stack, f"stat{k}_{i}", [128, 16], F32) for i in range(3)]
            FE["xns"] = [sbt(stack, f"xn{k}_{i}", [128, D], BF16) for i in range(nxn)]

        def fe_a(tile_aps):
            n = len(tile_aps)
            st = FE["stat"][cnt["st"] % 3]; cnt["st"] += 1
            junk = FE["junk"]
            xl = []
            for j in range(n):
                xt = FE["xts"][cnt["x"] % FE["nx"]]; cnt["x"] += 1
                load(xt, tile_aps[j])
                xl.append(xt)
            for j in range(n):
                A(lambda: nc.scalar.activation(out=junk.t[:], in_=xl[j].t[:], func=AF.Square, accum_out=st.t[:, j:j + 1]),
                  [xl[j].r], [junk.r, st.r])
            V(lambda: nc.vector.tensor_scalar(out=st.t[:, 4:4 + n], in0=st.t[:, 0:n], scalar1=1.0 / D, scalar2=EPS,
                                              op0=ALU.mult, op1=ALU.add), [st.r], [st.r])
            A(lambda: nc.scalar.activation(out=st.t[:, 8:8 + n], in_=st.t[:, 4:4 + n], func=AF.Sqrt), [st.r], [st.r])
            V(lambda: nc.vector.reciprocal(out=st.t[:, 12:12 + n], in_=st.t[:, 8:8 + n]), [st.r], [st.r])
            xnl = []
            for j in range(n):
                xn = FE["xns"][cnt["xn"] % len(FE["xns"])]; cnt["xn"] += 1
                V(lambda: nc.vector.scalar_tensor_tensor(out=xn.t[:], in0=xl[j].t[:], scalar=st.t[:, 12 + j:13 + j], in1=s1b.t[:],
                                                         op0=ALU.mult, op1=ALU.mult), [xl[j].r, st.r, s1b.r], [xn.r])
                xnl.append(xn)
            return xl, xnl

        def fe_b(state, dst, dst_res, c0s):
            xl, xnl = state
            for j, xn in enumerate(xnl):
                tp = pnext()
                tpb = tp.t[:].bitcast(BF16)
                for kc in range(8):
                    P(lambda: nc.tensor.transpose(out=tpb[:, kc * 128:(kc + 1) * 128], in_=xn.t[:, kc * 128:(kc + 1) * 128],
                                                  identity=identb.t[:]), [xn.r, identb.r], [tp.r])
                c0 = c0s[j]
                src3 = tpb.rearrange("p (k t) -> p k t", k=8)
                if cnt["cp"] % 2 == 0:
                    A(lambda: nc.scalar.copy(out=dst[:, :, c0:c0 + 128], in_=src3), [tp.r], [dst_res])
                else:
                    V(lambda: nc.vector.tensor_copy(out=dst[:, :, c0:c0 + 128], in_=src3), [tp.r], [dst_res])
                cnt["cp"] += 1
            return xl

        def front_end_multi(tile_aps, dst, dst_res, c0s):
            return fe_b(fe_a(tile_aps), dst, dst_res, c0s)

        def bias_row(dst_ap, wfn, M, wres, dres):
            ps = pnext()
            for kc in range(8):
                P(lambda: nc.tensor.matmul(ps.t[0:1, 0:M], lhsT=sh1b.t[:, kc:kc + 1], rhs=wfn(kc), start=(kc == 0), stop=(kc == 7)),
                  [sh1b.r, wres], [ps.r])
            V(lambda: nc.vector.tensor_copy(out=dst_ap, in_=ps.t[0:1, 0:M]), [ps.r], [dres])

        s1col = lambda kc: s1c.t[:, kc:kc + 1]

        Aall = sbt(es, "Aall", [128, NOWN, 32], BF16)
        A0all = sbt(es, "A0all", [128, NOWN, 32], BF16)
        A1all = sbt(es, "A1all", [128, NOWN, 32], BF16)
        wts = sbt(es, "wts", [128, NOWN, 2], F32)
        att = es.enter_context(ExitStack())
        ckvnT = sbt(att, "ckvnT", [128, S], BF16); ckvn_r = [Res() for _ in range(NG)]
        KT = sbt(att, "KT", [128, S], BF16); ktn_r = [Res() for _ in range(NG)]; ktr_r = [Res() for _ in range(NG)]
        vh_r = [Res() for _ in range(NG)]
        cqnT = sbt(att, "cqnT", [128, 2, TOK], BF16); cqn_r = [Res() for _ in range(NGO)]
        qT_r = [Res() for _ in range(NGO)]
        CQ = sbt(att, "CQ", [128, TOK], BF16); SQ = sbt(att, "SQ", [128, TOK], BF16)
        cs_r = [Res() for _ in range(NGO)]

        def rope_tables(pos_ap_512, Cdst, Sdst, wres, lp, width):
            pi_ = lp["posi"]; R = slice(64, 96)
            T.dma(SP, pi_.l, lambda: nc.sync.dma_start(out=pi_.t[R, 0:width], in_=pos_ap_512.partition_broadcast(32)),
                  writes=[pi_.r])
            pf = lp["pf"]; u = lp["u"]; ki = lp["ki"]; kf = lp["kf"]; f2 = lp["f2"]
            V(lambda: nc.vector.tensor_copy(out=pf.t[R, 0:width], in_=pi_.t[R, 0:width]), [pi_.r], [pf.r])
            for which, off, dstb in ((0, 0.5, Sdst), (1, 0.75, Cdst)):
                V(lambda: nc.vector.tensor_scalar(out=u.t[R, 0:width], in0=pf.t[R, 0:width], scalar1=fq.t[R, 0:1],
                                                  scalar2=off, op0=ALU.mult, op1=ALU.add), [pf.r, fq.r], [u.r])
                V(lambda: nc.vector.tensor_copy(out=ki.t[R, 0:width], in_=u.t[R, 0:width]), [u.r], [ki.r])
                V(lambda: nc.vector.tensor_copy(out=kf.t[R, 0:width], in_=ki.t[R, 0:width]), [ki.r], [kf.r])
                V(lambda: nc.vector.scalar_tensor_tensor(out=f2.t[R, 0:width], in0=u.t[R, 0:width], scalar=-0.5,
                                                         in1=kf.t[R, 0:width], op0=ALU.add, op1=ALU.subtract),
                  [u.r, kf.r], [f2.r])
                V(lambda: nc.vector.scalar_tensor_tensor(out=u.t[R, 0:width], in0=f2.t[R, 0:width], scalar=-0.5,
                                                         in1=f2.t[R, 0:width], op0=ALU.is_lt, op1=ALU.add),
                  [f2.r], [u.r])
                A(lambda: nc.scalar.activation(out=dstb, in_=u.t[R, 0:width], func=AF.Sin, scale=6.28318),
                  [u.r], [wres])

        with ExitStack() as lp_:
            make_fe(lp_, 4, 4)
            lp = {}
            lp["posi"] = sbt(lp_, "posi", [128, 512], I32, True)
            for nm in ("pf", "u", "kf", "f2"):
                lp[nm] = sbt(lp_, "lp_" + nm, [128, 512], F32)
            lp["t1"] = lp["pf"]; lp["t2"] = lp["kf"]
            lp["ki"] = sbt(lp_, "lp_ki", [128, 512], I32)
            Ctmp = sbt(lp_, "Ctmp", [128, 512], F32); Stmp = sbt(lp_, "Stmp", [128, 512], F32)
            h1Ts = [sbt(lp_, f"h1T{i}", [128, 8, 512], BF16) for i in range(2)]
            sq = sbt(lp_, "sq", [128, 2, 512], BF16)
            rs = sbt(lp_, "rs", [128, 512], F32); rs2 = sbt(lp_, "rs2", [128, 512], F32)
            wkv = sbt(lp_, "wkv", [128, 8, 128], BF16, True)
            wkr = sbt(lp_, "wkr", [128, 8, 96], BF16, True)
            wkrr = sbt(lp_, "wkrr", [128, 8, 96], BF16)
            wq = sbt(lp_, "wq", [128, 8, 256], BF16, True)
            win_v = w_in.rearrange("(kc p) n -> p kc n", p=128)
            load(wkv, win_v[:, :, 256:384], POOL)
            V(lambda: nc.vector.memset(wkr.t[:], 0.0), [], [wkr.r])
            V(lambda: nc.vector.memset(wkrr.t[:], 0.0), [], [wkrr.r])
            load_to(wkr, wkr.t[:, :, 64:96], win_v[:, :, 384:416], POOL)
            load(wq, win_v[:, :, 0:256], POOL)
            V(lambda: nc.vector.tensor_scalar(out=wkrr.t[:, :, 64:80], in0=wkr.t[:, :, 80:96], scalar1=-1.0, scalar2=None,
                                              op0=ALU.mult), [wkr.r, wkrr.r], [wkrr.r])
            V(lambda: nc.vector.tensor_copy(out=wkrr.t[:, :, 80:96], in_=wkr.t[:, :, 64:80]), [wkr.r, wkrr.r], [wkrr.r])
            set_rot(range(8))
            brows = sbt(lp_, "brows", [1, 640], BF16)
            bias_row(brows.t[0:1, 0:128], lambda kc: wkv.t[:, kc, :], 128, wkv.r, brows.r)
            bias_row(brows.t[0:1, 128:224], lambda kc: wkr.t[:, kc, :], 96, wkr.r, brows.r)
            bias_row(brows.t[0:1, 224:320], lambda kc: wkrr.t[:, kc, :], 96, wkrr.r, brows.r)
            bias_row(brows.t[0:1, 320:576], lambda kc: wq.t[:, kc, :], 256, wq.r, brows.r)
            hcnt = {"i": 0}
            h1_of = {}

            fe_state = {}

            def latent_fe_a(mode, g, width):
                ntl = width // 128
                src = xb if mode == "k" else xo
                fe_state[(mode, g)] = fe_a([src[(g * 4 + tl) * 128:(g * 4 + tl + 1) * 128, :] for tl in range(ntl)])

            def latent_fe_b(mode, g, width):
                ntl = width // 128
                h1 = h1Ts[hcnt["i"] % 2]; hcnt["i"] += 1
                h1_of[(mode, g)] = h1
                fe_b(fe_state.pop((mode, g)), h1.t, h1.r, [tl * 128 for tl in range(ntl)])

            mm_out = {}

            def latent_mm(mode, g, width):
                h1 = h1_of.pop((mode, g))
                W = slice(0, width)
                if mode == "k":
                    pk = pnext(); pr = pnext(); prr = pnext()
                    for kc in range(8):
                        P(lambda: nc.tensor.matmul(pk.t[:, W], lhsT=wkv.t[:, kc, :], rhs=h1.t[:, kc, W],
                                                   start=(kc == 0), stop=False), [wkv.r, h1.r], [pk.r])
                    P(lambda: nc.tensor.matmul(pk.t[:, W], lhsT=brows.t[0:1, 0:128], rhs=ones512.t[0:1, W], start=False, stop=True),
                      [brows.r, ones512.r], [pk.r])
                    for kc in range(8):
                        P(lambda: nc.tensor.matmul(pr.t[0:96, W], lhsT=wkr.t[:, kc, :], rhs=h1.t[:, kc, W],
                                                   start=(kc == 0), stop=False), [wkr.r, h1.r], [pr.r])
                    P(lambda: nc.tensor.matmul(pr.t[0:96, W], lhsT=brows.t[0:1, 128:224], rhs=ones512.t[0:1, W], start=False, stop=True),
                      [brows.r, ones512.r], [pr.r])
                    for kc in range(8):
                        P(lambda: nc.tensor.matmul(prr.t[0:96, W], lhsT=wkrr.t[:, kc, :], rhs=h1.t[:, kc, W],
                                                   start=(kc == 0), stop=False), [wkrr.r, h1.r], [prr.r])
                    P(lambda: nc.tensor.matmul(prr.t[0:96, W], lhsT=brows.t[0:1, 224:320], rhs=ones512.t[0:1, W], start=False, stop=True),
                      [brows.r, ones512.r], [prr.r])
                    chunks = [pk]
                    gcol = [gkvc.t[:, 0:1]]
                    nfeat = 128
                else:
                    p0 = pnext(); p1 = pnext()
                    for ci, pp in enumerate((p0, p1)):
                        for kc in range(8):
                            P(lambda: nc.tensor.matmul(pp.t[:, W], lhsT=wq.t[:, kc, ci * 128:(ci + 1) * 128],
                                                       rhs=h1.t[:, kc, W], start=(kc == 0), stop=False),
                              [wq.r, h1.r], [pp.r])
                        P(lambda: nc.tensor.matmul(pp.t[:, W], lhsT=brows.t[0:1, 320 + ci * 128:320 + (ci + 1) * 128],
                                                   rhs=ones512.t[0:1, W], start=False, stop=True), [brows.r, ones512.r], [pp.r])
                    chunks = [p0, p1]
                    gcol = [gqc.t[:, 0:1], gqc.t[:, 1:2]]
                    nfeat = 256
                mm_out[(mode, g)] = (chunks, gcol, nfeat, (pr, prr) if mode == "k" else None)

            def latent_rest(mode, g, width):
                W = slice(0, width)
                chunks, gcol, nfeat, prs = mm_out.pop((mode, g))
                if prs is not None:
                    pr, prr = prs
                for ci, pp in enumerate(chunks):
                    A(lambda: nc.scalar.activation(out=sq.t[:, ci, W], in_=pp.t[:, W], func=AF.Square), [pp.r], [sq.r])
                pss = pnext()
                for ci in range(len(chunks)):
                    P(lambda: nc.tensor.matmul(pss.t[:, W], lhsT=onesb.t[:], rhs=sq.t[:, ci, W], start=(ci == 0),
                                               stop=(ci == len(chunks) - 1)), [onesb.r, sq.r], [pss.r])
                V(lambda: nc.vector.tensor_scalar(out=rs.t[:, W], in0=pss.t[:, W], scalar1=1.0 / nfeat, scalar2=EPS,
                                                  op0=ALU.mult, op1=ALU.add), [pss.r], [rs.r])
                A(lambda: nc.scalar.activation(out=rs2.t[:, W], in_=rs.t[:, W], func=AF.Sqrt), [rs.r], [rs2.r])
                V(lambda: nc.vector.reciprocal(out=rs.t[:, W], in_=rs2.t[:, W]), [rs2.r], [rs.r])
                c0 = g * 512
                for ci, pp in enumerate(chunks):
                    if mode == "k":
                        dst = ckvnT.t[:, c0:c0 + width]; dres = ckvn_r[g]
                    else:
                        dst = cqnT.t[:, ci, c0:c0 + width]; dres = cqn_r[g]
                    V(lambda: nc.vector.scalar_tensor_tensor(out=dst, in0=pp.t[:, W], scalar=gcol[ci], in1=rs.t[:, W],
                                                             op0=ALU.mult, op1=ALU.mult),
                      [pp.r, rs.r, gkvc.r, gqc.r], [dres])
                R = slice(64, 96)
                if mode == "k":
                    rope_tables(posb[0:1, c0:c0 + width], Ctmp.t[R, W], Stmp.t[R, W], Ctmp.r, lp, width)
                    t1 = lp["t1"]; t2 = lp["t2"]
                    V(lambda: nc.vector.tensor_tensor(out=t1.t[R, W], in0=pr.t[R, W], in1=Ctmp.t[R, W], op=ALU.mult),
                      [pr.r, Ctmp.r], [t1.r])
                    V(lambda: nc.vector.tensor_tensor(out=t2.t[R, W], in0=prr.t[R, W], in1=Stmp.t[R, W], op=ALU.mult),
                      [prr.r, Ctmp.r], [t2.r])
                    V(lambda: nc.vector.tensor_tensor(out=KT.t[R, c0:c0 + width], in0=t1.t[R, W], in1=t2.t[R, W],
                                                      op=ALU.add), [t1.r, t2.r], [ktr_r[g]])
                else:
                    rope_tables(poso[0:1, c0:c0 + width], CQ.t[R, c0:c0 + width], SQ.t[R, c0:c0 + width], cs_r[g], lp, width)

            work = [("k", g, 512) for g in range(NG)] + [("q", g, GW) for g in range(NGO)]
            latent_fe_a(*work[0]); latent_fe_b(*work[0])
            for wi, w_ in enumerate(work):
                nxt = work[wi + 1] if wi + 1 < len(work) else None
                if nxt:
                    latent_fe_a(*nxt)
                latent_mm(*w_)
                if nxt:
                    latent_fe_b(*nxt)
                latent_rest(*w_)
            T.barrier()
            if stop <= 1:
                return nc

        with ExitStack() as ap_:
            qT = sbt(ap_, "qT", [128, TOK], BF16)
            Vh = sbt(ap_, "Vh", [128, NT, 65], BF16)
            V(lambda: nc.vector.memset(Vh.t[:, :, 64:65], 1.0), [], vh_r)
            wuqb = sbt(ap_, "wuqb", [128, 2, 768], BF16, True)
            wuqr = sbt(ap_, "wuqr", [128, 2, NH, 96], BF16)
            wukvb = sbt(ap_, "wukvb", [128, 1024], BF16, True)
            load(wuqb, w_uq.rearrange("(kc p) n -> p kc n", p=128), POOL)
            load(wukvb, w_ukv, POOL)
            V(lambda: nc.vector.memset(wuqr.t[:], 0.0), [], [wuqr.r])
            wv = wuqb.t[:].rearrange("p k (h d) -> p k h d", h=NH)
            V(lambda: nc.vector.tensor_scalar(out=wuqr.t[:, :, :, 64:80], in0=wv[:, :, :, 80:96], scalar1=-1.0, scalar2=None,
                                              op0=ALU.mult), [wuqb.r, wuqr.r], [wuqr.r])
            V(lambda: nc.vector.tensor_copy(out=wuqr.t[:, :, :, 80:96], in_=wv[:, :, :, 64:80]), [wuqb.r, wuqr.r], [wuqr.r])

            PTs = [sbt(ap_, f"PT{i}", [128, 512], BF16) for i in range(4)]
            rec = [sbt(ap_, f"rec{i}", [128, 1], F32) for i in range(4)]
            ost = [sbt(ap_, f"ost{i}", [128, 64], BF16, True) for i in range(4)]
            osc = [0]
            t1q = sbt(ap_, "t1q", [128, 512], F32); t2q = sbt(ap_, "t2q", [128, 512], F32)
            set_rot([4, 5, 6, 7])
            oacc_i = 0
            ptc = 0
            for h in range(NH):
                for g in range(NG):
                    ps = pnext()
                    P(lambda: nc.tensor.matmul(ps.t[0:64, :], lhsT=wukvb.t[:, h * 128:h * 128 + 64],
                                               rhs=ckvnT.t[:, g * 512:(g + 1) * 512], start=True, stop=True),
                      [wukvb.r, ckvn_r[g]], [ps.r])
                    if g % 2 == 0:
                        V(lambda: nc.vector.tensor_copy(out=KT.t[0:64, g * 512:(g + 1) * 512], in_=ps.t[0:64, :]),
                          [ps.r], [ktn_r[g]])
                    else:
                        A(lambda: nc.scalar.copy(out=KT.t[0:64, g * 512:(g + 1) * 512], in_=ps.t[0:64, :]),
                          [ps.r], [ktn_r[g]])
                for g8 in range(NT // 8):
                    ps = pnext()
                    for k8 in range(8):
                        kt = g8 * 8 + k8
                        P(lambda: nc.tensor.matmul(ps.t[:, k8 * 64:(k8 + 1) * 64], lhsT=ckvnT.t[:, kt * 128:(kt + 1) * 128],
                                                   rhs=wukvb.t[:, h * 128 + 64:h * 128 + 128], start=True, stop=True),
                          [wukvb.r, ckvn_r[kt // 4]], [ps.r])
                    wr_ = [vh_r[2 * g8], vh_r[2 * g8 + 1]]
                    src3 = ps.t[:].rearrange("p (a d) -> p a d", a=8)
                    if g8 % 2 == 0:
                        A(lambda: nc.scalar.copy(out=Vh.t[:, g8 * 8:(g8 + 1) * 8, 0:64], in_=src3), [ps.r], wr_)
                    else:
                        V(lambda: nc.vector.tensor_copy(out=Vh.t[:, g8 * 8:(g8 + 1) * 8, 0:64], in_=src3), [ps.r], wr_)
                for g in range(NGO):
                    W = slice(0, GW); c0 = g * 512; R = slice(64, 96)
                    pa = pnext(); pr = pnext()
                    for kc in range(2):
                        P(lambda: nc.tensor.matmul(pa.t[0:96, W], lhsT=wuqb.t[:, kc, h * 96:(h + 1) * 96],
                                                   rhs=cqnT.t[:, kc, c0:c0 + GW], start=(kc == 0), stop=(kc == 1)),
                          [wuqb.r, cqn_r[g]], [pa.r])
                    for kc in range(2):
                        P(lambda: nc.tensor.matmul(pr.t[0:96, W], lhsT=wuqr.t[:, kc, h, :],
                                                   rhs=cqnT.t[:, kc, c0:c0 + GW], start=(kc == 0), stop=(kc == 1)),
                          [wuqr.r, cqn_r[g]], [pr.r])
                    V(lambda: nc.vector.tensor_copy(out=qT.t[0:64, c0:c0 + GW], in_=pa.t[0:64, W]), [pa.r], [qT_r[g]])
                    V(lambda: nc.vector.tensor_tensor(out=t1q.t[R, W], in0=pa.t[R, W], in1=CQ.t[R, c0:c0 + GW], op=ALU.mult),
                      [pa.r, cs_r[g]], [t1q.r])
                    V(lambda: nc.vector.tensor_tensor(out=t2q.t[R, W], in0=pr.t[R, W], in1=SQ.t[R, c0:c0 + GW], op=ALU.mult),
                      [pr.r, cs_r[g]], [t2q.r])
                    V(lambda: nc.vector.tensor_tensor(out=qT.t[R, c0:c0 + GW], in0=t1q.t[R, W], in1=t2q.t[R, W], op=ALU.add),
                      [t1q.r, t2q.r], [qT_r[g]])
                steps = [(m, kp) for m in range(NP) for kp in range((8 * m + 8) // 2)]
                oa_of = {}
                for m in range(NP):
                    oa_of[m] = [banks[(oacc_i * 2) % 4], banks[(oacc_i * 2 + 1) % 4]]
                    oacc_i += 1
                pt_of = {}

                def emit_qk(si):
                    m, kp = steps[si]
                    qg = qT_r[(m * 256) // 512]
                    qs = qT.t[0:96, m * 256:(m + 1) * 256]
                    ps = pnext()
                    for kl in range(2):
                        kt = 2 * kp + kl
                        P(lambda: nc.tensor.matmul(ps.t[:, kl * 256:(kl + 1) * 256], lhsT=KT.t[0:96, kt * 128:(kt + 1) * 128],
                                                   rhs=qs, start=True, stop=True),
                          [ktn_r[kt // 4], ktr_r[kt // 4], qg], [ps.r])
                    PT = PTs[si % len(PTs)]
                    pt_of[si] = PT
                    A(lambda: nc.scalar.activation(out=PT.t[:], in_=ps.t[:], func=AF.Exp, scale=SCALE), [ps.r], [PT.r])
                    for kl in range(2):
                        kt = 2 * kp + kl
                        o = kt - 8 * m
                        if o >= 0:
                            for a in range(2):
                                cs_ = kl * 256 + a * 128
                                V(lambda: nc.vector.scalar_tensor_tensor(
                                    out=PT.t[:, cs_:cs_ + 128], in0=kqb.t[:], scalar=dtab.t[:, a * 8 + o:a * 8 + o + 1],
                                    in1=PT.t[:, cs_:cs_ + 128], op0=ALU.is_le, op1=ALU.mult),
                                  [PT.r, kqb.r, dtab.r], [PT.r])

                def emit_pv(si):
                    nonlocal_osc = None
                    m, kp = steps[si]
                    nk = 8 * m + 8
                    oa = oa_of[m]
                    PT = pt_of.pop(si)
                    for kl in range(2):
                        kt = 2 * kp + kl
                        for a in range(2):
                            cs_ = kl * 256 + a * 128
                            P(lambda: nc.tensor.matmul(oa[a].t[:, 0:65], lhsT=PT.t[:, cs_:cs_ + 128], rhs=Vh.t[:, kt, :],
                                                       start=(kt == 0), stop=(kt == nk - 1)),
                              [PT.r, vh_r[kt // 4]], [oa[a].r])
                    if kp == nk // 2 - 1:
                        for a in range(2):
                            i_own = 2 * m + a
                            rc = rec[(2 * m + a) % 4]
                            V(lambda: nc.vector.reciprocal(out=rc.t[:], in_=oa[a].t[:, 64:65]), [oa[a].r], [rc.r])
                            os_ = ost[osc[0] % 4]; osc[0] += 1
                            V(lambda: nc.vector.tensor_scalar(out=os_.t[:], in0=oa[a].t[:, 0:64],
                                                              scalar1=rc.t[:, 0:1], scalar2=None, op0=ALU.mult),
                              [oa[a].r, rc.r], [os_.r])
                            T.dma(SP, os_.l, lambda: nc.sync.dma_start(out=OS[i_own * 128:(i_own + 1) * 128, h * 64:(h + 1) * 64],
                                                                       in_=os_.t[:]), reads=[os_.r])

                LA = 2
                for si in range(len(steps) + LA):
                    if si < len(steps):
                        emit_qk(si)
                    if si - LA >= 0:
                        emit_pv(si - LA)
            T.barrier()
            if stop <= 2:
                return nc
        att.close()

        pc_ = es.enter_context(ExitStack())
        set_rot(range(8))
        win_v = w_in.rearrange("(kc p) n -> p kc n", p=128)
        wsg = sbt(pc_, "wsg", [128, 8, 1024], BF16, True); load(wsg, win_v[:, :, 416:1440], POOL)
        wgl = sbt(pc_, "wgl", [128, 8, 2048], BF16, True)
        for kc in range(8):
            load_to(wgl, wgl.t[:, kc, :], win_v[:, kc, 1440:3488], POOL)
        wbm = sbt(pc_, "wbm", [128, 4, D], BF16, True); load(wbm, w_br_mla.rearrange("(kc p) n -> p kc n", p=128), POOL)
        wbs = sbt(pc_, "wbs", [128, 4, D], BF16, True); load(wbs, w_br_sgu.rearrange("(kc p) n -> p kc n", p=128), POOL)
        wo = sbt(pc_, "wo", [128, 8, D], BF16, True); load(wo, w_out.rearrange("(kc p) n -> p kc n", p=128), POOL)
        wrf = sbt(pc_, "wrf", [128, 8, 36], F32, True); load(wrf, wr.rearrange("(kc p) n -> p kc n", p=128))
        brb = sbt(pc_, "brb", [128, 36], F32, True); load(brb, br_row.partition_broadcast(128))
        gvb = sbt(pc_, "gvb", [128, 512], F32, True); load(gvb, gv_row.partition_broadcast(128))
        bvb = sbt(pc_, "bvb", [128, 512], F32, True); load(bvb, bv_row.partition_broadcast(128))
        bst = sbt(pc_, "bst", [128, 8], F32, True); load(bst, bsT)
        make_fe(pc_, 2)
        browc = sbt(pc_, "browc", [1, 3072], BF16)
        for q_ in range(2):
            bias_row(browc.t[0:1, q_ * 512:(q_ + 1) * 512], lambda kc: wsg.t[:, kc, q_ * 512:(q_ + 1) * 512], 512, wsg.r, browc.r)
        for q_ in range(4):
            bias_row(browc.t[0:1, 1024 + q_ * 512:1024 + (q_ + 1) * 512], lambda kc: wgl.t[:, kc, q_ * 512:(q_ + 1) * 512], 512,
                     wgl.r, browc.r)
        wsT = sbt(pc_, "wsT", [128, 8, 128], BF16)
        with ExitStack() as tmp_:
            g2rb = sbt(tmp_, "g2rb", [128, D], F32, True); load(g2rb, g2row.partition_broadcast(128))
            V(lambda: nc.vector.scalar_tensor_tensor(out=s2b.t[:], in0=s2b.t[:], scalar=1.0, in1=g2rb.t[:], op0=ALU.add,
                                                     op1=ALU.mult), [s2b.r, g2rb.r], [s2b.r])
            wsf = sbt(tmp_, "wsf", [128, 8, 128], F32, True); load(wsf, w_s.rearrange("g t s -> t g s"))
            trl = sbt(tmp_, "trl", [128, 128], F32, True); load(trl, c_tril)
            wsm = sbt(tmp_, "wsm", [128, 8, 128], BF16)
            V(lambda: nc.vector.tensor_tensor(out=wsm.t[:], in0=wsf.t[:], in1=trl.t[:].unsqueeze(1).to_broadcast([128, 8, 128]),
                                              op=ALU.mult), [wsf.r, trl.r], [wsm.r])
            tp = pnext(); tpb = tp.t[:].bitcast(BF16)
            for g in range(8):
                P(lambda: nc.tensor.transpose(out=tpb[:, g * 128:(g + 1) * 128], in_=wsm.t[:, g, :], identity=identb.t[:]),
                  [wsm.r, identb.r], [tp.r])
            V(lambda: nc.vector.tensor_copy(out=wsT.t[:].rearrange("p g t -> p (g t)"), in_=tpb[:, :]), [tp.r], [wsT.r])
            T.barrier()


        with ExitStack() as cw:
            h1c = [sbt(cw, f"h1c{i}", [128, 8, 128], BF16) for i in range(2)]
            uv = sbt(cw, "uv", [128, 1024], F32)
            x2 = sbt(cw, "x2t", [128, 1024], F32)
            inner = sbt(cw, "inner", [128, 1024], F32)
            gel = sbt(cw, "gel", [128, 1024], F32)
            otile = [sbt(cw, f"otile{i}", [128, 512], BF16, True) for i in range(2)]
            vnb = sbt(cw, "vnb", [128, 512], BF16)
            sgoB = [sbt(cw, f"sgo{i}", [128, 512], BF16) for i in range(2)]
            lst = sbt(cw, "lst", [128, 8], F32)
            sigB = [sbt(cw, f"sig{i}", [128, 2048], BF16) for i in range(2)]
            OT = sbt(cw, "OT", [128, 4, 128], BF16)
            SGT = sbt(cw, "SGT", [128, 4, 128], BF16)
            m1 = sbt(cw, "m1", [128, D], F32); m2 = sbt(cw, "m2", [128, D], F32)
            mrg = sbt(cw, "mrg", [128, D], BF16)
            mT = sbt(cw, "mT", [128, 8, 128], BF16)
            x1 = [sbt(cw, f"x1_{i}", [128, D], F32, True) for i in range(2)]
            x1n = m1
            h2T = sbt(cw, "h2T", [128, 8, 128], F32)
            h2f = m2
            h2p = [sbt(cw, f"h2p{i}", [128, D], BF16, True) for i in range(2)]
            st2 = sbt(cw, "st2", [128, 4], F32)
            lgall = sbt(cw, "lgall", [128, NOWN, 36], F32)
            rtb = sbt(cw, "rtb", [128, 8, NOWN], F32)
            s1out = {}

            def S1(i):
                h1 = h1c[i % 2]; sgo = sgoB[i % 2]; sig = sigB[i % 2]
                xt = front_end_multi([xo[i * 128:(i + 1) * 128, :]], h1.t, h1.r, [0])[0]
                s1out[i] = (xt, sgo, sig)
                load(otile[i % 2], OS[i * 128:(i + 1) * 128, :])
                pu = pnext(); pv = pnext()
                for hh, pp in enumerate((pu, pv)):
                    for kc in range(8):
                        P(lambda: nc.tensor.matmul(pp.t[:], lhsT=h1.t[:, kc, :], rhs=wsg.t[:, kc, hh * 512:(hh + 1) * 512],
                                                   start=(kc == 0), stop=False), [h1.r, wsg.r], [pp.r])
                    P(lambda: nc.tensor.matmul(pp.t[:], lhsT=onesb.t[0:1, :], rhs=browc.t[0:1, hh * 512:(hh + 1) * 512],
                                               start=False, stop=True), [onesb.r, browc.r], [pp.r])
                pg = [pnext() for _ in range(4)]
                for q4 in range(4):
                    for kc in range(8):
                        P(lambda: nc.tensor.matmul(pg[q4].t[:], lhsT=h1.t[:, kc, :], rhs=wgl.t[:, kc, q4 * 512:(q4 + 1) * 512],
                                                   start=(kc == 0), stop=False), [h1.r, wgl.r], [pg[q4].r])
                    P(lambda: nc.tensor.matmul(pg[q4].t[:], lhsT=onesb.t[0:1, :], rhs=browc.t[0:1, 1024 + q4 * 512:1024 + (q4 + 1) * 512],
                                               start=False, stop=True), [onesb.r, browc.r], [pg[q4].r])
                for hh, pp in enumerate((pu, pv)):
                    cs_ = slice(hh * 512, (hh + 1) * 512)
                    A(lambda: nc.scalar.copy(out=uv.t[:, cs_], in_=pp.t[:]), [pp.r], [uv.r])
                    A(lambda: nc.scalar.activation(out=x2.t[:, cs_], in_=pp.t[:], func=AF.Square, scale=0.044715 ** 0.5),
                      [pp.r], [x2.r])
                V(lambda: nc.vector.scalar_tensor_tensor(out=inner.t[:], in0=x2.t[:], scalar=1.0, in1=uv.t[:], op0=ALU.add,
                                                         op1=ALU.mult), [x2.r, uv.r], [inner.r])
                for q4 in range(4):
                    A(lambda: nc.scalar.activation(out=sig.t[:, q4 * 512:(q4 + 1) * 512], in_=pg[q4].t[:], func=AF.Sigmoid),
                      [pg[q4].r], [sig.r])
                A(lambda: nc.scalar.activation(out=inner.t[:], in_=inner.t[:], func=AF.Sigmoid, scale=GELU_C), [inner.r], [inner.r])
                G(lambda: nc.gpsimd.tensor_tensor(out=gel.t[:], in0=inner.t[:], in1=uv.t[:], op=ALU.mult), [inner.r, uv.r], [gel.r])
                gv_ = gel.t[:, 512:1024]
                V(lambda: nc.vector.tensor_reduce(out=lst.t[:, 0:1], in_=gv_, axis=AX.X, op=ALU.add), [gel.r], [lst.r])
                V(lambda: nc.vector.tensor_scalar(out=lst.t[:, 1:2], in0=lst.t[:, 0:1], scalar1=1.0 / 512, scalar2=None,
                                                  op0=ALU.mult), [lst.r], [lst.r])
                V(lambda: nc.vector.tensor_scalar(out=x2.t[:, 0:512], in0=gv_, scalar1=lst.t[:, 1:2], scalar2=None,
                                                  op0=ALU.subtract), [gel.r, lst.r], [x2.r])
                A(lambda: nc.scalar.activation(out=x2.t[:, 512:1024], in_=x2.t[:, 0:512], func=AF.Square, accum_out=lst.t[:, 2:3]),
                  [x2.r], [x2.r, lst.r])
                V(lambda: nc.vector.tensor_scalar(out=lst.t[:, 3:4], in0=lst.t[:, 2:3], scalar1=1.0 / 512, scalar2=EPS,
                                                  op0=ALU.mult, op1=ALU.add), [lst.r], [lst.r])
                A(lambda: nc.scalar.activation(out=lst.t[:, 4:5], in_=lst.t[:, 3:4], func=AF.Sqrt), [lst.r], [lst.r])
                V(lambda: nc.vector.reciprocal(out=lst.t[:, 5:6], in_=lst.t[:, 4:5]), [lst.r], [lst.r])
                V(lambda: nc.vector.scalar_tensor_tensor(out=x2.t[:, 0:512], in0=x2.t[:, 0:512], scalar=lst.t[:, 5:6],
                                                         in1=gvb.t[:], op0=ALU.mult, op1=ALU.mult), [x2.r, lst.r, gvb.r], [x2.r])
                V(lambda: nc.vector.tensor_tensor(out=vnb.t[:], in0=x2.t[:, 0:512], in1=bvb.t[:], op=ALU.add),
                  [x2.r, bvb.r], [vnb.r])
                psg = pnext()
                for g in range(8):
                    P(lambda: nc.tensor.matmul(psg.t[:, g * 64:(g + 1) * 64], lhsT=wsT.t[:, g, :], rhs=vnb.t[:, g * 64:(g + 1) * 64],
                                               start=True, stop=True), [wsT.r, vnb.r], [psg.r])
                V(lambda: nc.vector.tensor_tensor(out=x2.t[:, 0:512].rearrange("p (g c) -> p g c", g=8),
                                                  in0=psg.t[:].rearrange("p (g c) -> p g c", g=8),
                                                  in1=bst.t[:].unsqueeze(2).to_broadcast([128, 8, 64]), op=ALU.add),
                  [psg.r, bst.r], [x2.r])
                V(lambda: nc.vector.tensor_tensor(out=sgo.t[:], in0=x2.t[:, 0:512], in1=gel.t[:, 0:512], op=ALU.mult),
                  [x2.r, gel.r], [sgo.r])

            def S2(i):
                xt, sgo, sig = s1out.pop(i)
                junk = FE["junk"]
                ot_ = otile[i % 2]
                tp = pnext(); tpb = tp.t[:].bitcast(BF16)
                for kc in range(4):
                    P(lambda: nc.tensor.transpose(out=tpb[:, kc * 128:(kc + 1) * 128], in_=ot_.t[:, kc * 128:(kc + 1) * 128],
                                                  identity=identb.t[:]), [ot_.r, identb.r], [tp.r])
                for kc in range(4):
                    P(lambda: nc.tensor.transpose(out=tpb[:, 512 + kc * 128:512 + (kc + 1) * 128], in_=sgo.t[:, kc * 128:(kc + 1) * 128],
                                                  identity=identb.t[:]), [sgo.r, identb.r], [tp.r])
                A(lambda: nc.scalar.copy(out=OT.t[:].rearrange("p k t -> p (k t)"), in_=tpb[:, 0:512]), [tp.r], [OT.r])
                A(lambda: nc.scalar.copy(out=SGT.t[:].rearrange("p k t -> p (k t)"), in_=tpb[:, 512:1024]), [tp.r], [SGT.r])
                for br, (srcT, wbr, sc0, mm) in enumerate(((OT, wbm, 0, m1), (SGT, wbs, 1024, m2))):
                    for hh in range(2):
                        ps = pnext()
                        for kc in range(4):
                            P(lambda: nc.tensor.matmul(ps.t[:], lhsT=srcT.t[:, kc, :], rhs=wbr.t[:, kc, hh * 512:(hh + 1) * 512],
                                                       start=(kc == 0), stop=(kc == 3)), [srcT.r, wbr.r], [ps.r])
                        V(lambda: nc.vector.tensor_tensor(out=mm.t[:, hh * 512:(hh + 1) * 512], in0=ps.t[:],
                                                          in1=sig.t[:, sc0 + hh * 512:sc0 + (hh + 1) * 512], op=ALU.mult),
                          [ps.r, sig.r], [mm.r])
                G(lambda: nc.gpsimd.tensor_tensor(out=mrg.t[:], in0=m1.t[:], in1=m2.t[:], op=ALU.add), [m1.r, m2.r], [mrg.r])
                tp = pnext(); tpb = tp.t[:].bitcast(BF16)
                for kc in range(8):
                    P(lambda: nc.tensor.transpose(out=tpb[:, kc * 128:(kc + 1) * 128], in_=mrg.t[:, kc * 128:(kc + 1) * 128],
                                                  identity=identb.t[:]), [mrg.r, identb.r], [tp.r])
                A(lambda: nc.scalar.copy(out=mT.t[:].rearrange("p k t -> p (k t)"), in_=tpb[:, :]), [tp.r], [mT.r])
                xx = x1[i % 2]
                for hh in range(2):
                    ps = pnext()
                    for kc in range(8):
                        P(lambda: nc.tensor.matmul(ps.t[:], lhsT=mT.t[:, kc, :], rhs=wo.t[:, kc, hh * 512:(hh + 1) * 512],
                                                   start=(kc == 0), stop=(kc == 7)), [mT.r, wo.r], [ps.r])
                    cs_ = slice(hh * 512, (hh + 1) * 512)
                    V(lambda: nc.vector.tensor_tensor(out=m1.t[:, cs_], in0=ps.t[:], in1=gate1b.t[:, cs_], op=ALU.mult),
                      [ps.r, gate1b.r], [m1.r])
                V(lambda: nc.vector.tensor_tensor(out=xx.t[:], in0=m1.t[:], in1=xt.t[:], op=ALU.add), [m1.r, xt.r], [xx.r])
                T.dma(SP, xx.l, lambda: nc.sync.dma_start(out=X1[i * 128:(i + 1) * 128, :], in_=xx.t[:]), reads=[xx.r])

            def S3(i):
                xx = x1[i % 2]
                junk = FE["junk"]
                A(lambda: nc.scalar.activation(out=junk.t[:], in_=xx.t[:], func=AF.Square, accum_out=st2.t[:, 0:1]),
                  [xx.r], [junk.r, st2.r])
                V(lambda: nc.vector.tensor_scalar(out=st2.t[:, 1:2], in0=st2.t[:, 0:1], scalar1=1.0 / D, scalar2=EPS,
                                                  op0=ALU.mult, op1=ALU.add), [st2.r], [st2.r])
                A(lambda: nc.scalar.activation(out=st2.t[:, 2:3], in_=st2.t[:, 1:2], func=AF.Sqrt), [st2.r], [st2.r])
                V(lambda: nc.vector.reciprocal(out=st2.t[:, 3:4], in_=st2.t[:, 2:3]), [st2.r], [st2.r])
                V(lambda: nc.vector.tensor_scalar(out=x1n.t[:], in0=xx.t[:], scalar1=st2.t[:, 3:4], scalar2=None, op0=ALU.mult),
                  [xx.r, st2.r], [x1n.r])
                G(lambda: nc.gpsimd.tensor_tensor(out=h2f.t[:], in0=x1n.t[:], in1=s2b.t[:], op=ALU.mult), [x1n.r, s2b.r], [h2f.r])
                hp = h2p[i % 2]
                G(lambda: nc.gpsimd.tensor_tensor(out=hp.t[:].rearrange("t (kc p) -> t p kc", kc=8),
                                                  in0=h2f.t[:].rearrange("t (p kc) -> t p kc", kc=8),
                                                  in1=sh2b.t[:].rearrange("t (p kc) -> t p kc", kc=8), op=ALU.add),
                  [h2f.r, sh2b.r], [hp.r])
                T.dma(SP, hp.l, lambda: nc.sync.dma_start(out=H2[i * 128:(i + 1) * 128, :], in_=hp.t[:]), reads=[hp.r])
                tA = pnext(); tB = pnext()
                for kc in range(8):
                    tt = tA if kc < 4 else tB
                    P(lambda: nc.tensor.transpose(out=tt.t[:, (kc % 4) * 128:(kc % 4 + 1) * 128], in_=x1n.t[:, kc * 128:(kc + 1) * 128],
                                                  identity=identf.t[:]), [x1n.r, identf.r], [tt.r])
                for kc in range(8):
                    tt = tA if kc < 4 else tB
                    V(lambda: nc.vector.tensor_scalar(out=h2T.t[:, kc, :], in0=tt.t[:, (kc % 4) * 128:(kc % 4 + 1) * 128],
                                                      scalar1=s2c.t[:, kc:kc + 1], scalar2=sh2(kc), op0=ALU.mult, op1=ALU.add),
                      [tt.r, s2c.r, modT.r], [h2T.r])
                pl = pnext()
                for kc in range(8):
                    P(lambda: nc.tensor.matmul(pl.t[:, 0:36], lhsT=h2T.t[:, kc, :], rhs=wrf.t[:, kc, :], start=(kc == 0), stop=(kc == 7)),
                      [h2T.r, wrf.r], [pl.r])
                V(lambda: nc.vector.tensor_tensor(out=lgall.t[:, i, :], in0=pl.t[:, 0:36], in1=brb.t[:], op=ALU.add), [pl.r, brb.r], [lgall.r])

            for step in range(NOWN + 2):
                if step < NOWN:
                    S1(step)
                if 0 <= step - 1 < NOWN:
                    S2(step - 1)
                if 0 <= step - 2 < NOWN:
                    S3(step - 2)

            TT = NOWN
            Lg = lgall.t[:, :, 0:4]
            Le4 = lgall.t[:, :, 4:36].rearrange("p t (g e) -> p t g e", g=4)
            gm = rtb.t[:, 0, :]; se = rtb.t[:, 1, :]; pgr = rtb.t[:, 2, :]; mv1 = rtb.t[:, 3, :]; mv2 = rtb.t[:, 4, :]
            dd_ = rtb.t[:, 5, :]; ee_ = rtb.t[:, 6, :]; rr_ = rtb.t[:, 7, :]
            ex = uv.t[:, 0:TT * 4].rearrange("p (t g) -> p t g", g=4)
            pen3 = x2.t[:, 0:TT * 4].rearrange("p (t g) -> p t g", g=4)
            Mx3 = inner.t[:, 0:TT * 32].rearrange("p (t e) -> p t e", e=32)
            Mx4 = inner.t[:, 0:TT * 32].rearrange("p (t g e) -> p t g e", g=4, e=8)
            My3 = gel.t[:, 0:TT * 32].rearrange("p (t e) -> p t e", e=32)
            bc3 = lambda ap2, n: ap2.unsqueeze(2).to_broadcast([128, TT, n])
            V(lambda: nc.vector.tensor_reduce(out=gm, in_=Lg, axis=AX.X, op=ALU.max), [lgall.r], [rtb.r])
            V(lambda: nc.vector.tensor_tensor(out=ex, in0=Lg, in1=bc3(gm, 4), op=ALU.subtract), [lgall.r, rtb.r], [uv.r])
            A(lambda: nc.scalar.activation(out=ex, in_=ex, func=AF.Exp), [uv.r], [uv.r])
            V(lambda: nc.vector.tensor_reduce(out=se, in_=ex, axis=AX.X, op=ALU.add), [uv.r], [rtb.r])
            V(lambda: nc.vector.reciprocal(out=pgr, in_=se), [rtb.r], [rtb.r])
            V(lambda: nc.vector.tensor_tensor(out=pen3, in0=Lg, in1=bc3(gm, 4), op=ALU.is_equal), [lgall.r, rtb.r], [x2.r])
            V(lambda: nc.vector.tensor_scalar(out=pen3, in0=pen3, scalar1=-1.0, scalar2=1e30, op0=ALU.add, op1=ALU.mult),
              [x2.r], [x2.r])
            V(lambda: nc.vector.tensor_tensor(out=Mx4, in0=Le4, in1=pen3.unsqueeze(3).to_broadcast([128, TT, 4, 8]), op=ALU.add),
              [lgall.r, x2.r], [inner.r])
            V(lambda: nc.vector.tensor_reduce(out=mv1, in_=Mx3, axis=AX.X, op=ALU.max), [inner.r], [rtb.r])
            V(lambda: nc.vector.tensor_tensor(out=A0all.t[:], in0=Mx3, in1=bc3(mv1, 32), op=ALU.is_equal), [inner.r, rtb.r], [A0all.r])
            V(lambda: nc.vector.scalar_tensor_tensor(out=My3, in0=A0all.t[:], scalar=-1e30, in1=Mx3, op0=ALU.mult, op1=ALU.add),
              [A0all.r, inner.r], [gel.r])
            V(lambda: nc.vector.tensor_reduce(out=mv2, in_=My3, axis=AX.X, op=ALU.max), [gel.r], [rtb.r])
            V(lambda: nc.vector.tensor_tensor(out=A1all.t[:], in0=My3, in1=bc3(mv2, 32), op=ALU.is_equal), [gel.r, rtb.r], [A1all.r])
            V(lambda: nc.vector.tensor_tensor(out=Aall.t[:], in0=A0all.t[:], in1=A1all.t[:], op=ALU.add), [A0all.r, A1all.r], [Aall.r])
            V(lambda: nc.vector.tensor_tensor(out=dd_, in0=mv2, in1=mv1, op=ALU.subtract), [rtb.r], [rtb.r])
            A(lambda: nc.scalar.activation(out=ee_, in_=dd_, func=AF.Exp), [rtb.r], [rtb.r])
            V(lambda: nc.vector.tensor_scalar(out=ee_, in0=ee_, scalar1=1.0, scalar2=None, op0=ALU.add), [rtb.r], [rtb.r])
            V(lambda: nc.vector.reciprocal(out=rr_, in_=ee_), [rtb.r], [rtb.r])
            V(lambda: nc.vector.tensor_tensor(out=wts.t[:, :, 0], in0=rr_, in1=pgr, op=ALU.mult), [rtb.r], [wts.r])
            V(lambda: nc.vector.tensor_tensor(out=wts.t[:, :, 1], in0=pgr, in1=wts.t[:, :, 0], op=ALU.subtract), [rtb.r, wts.r], [wts.r])
            T.barrier()
            if stop <= 3:
                return nc
        pc_.close()

        NA = NOWN * 32
        REG_SLOT = nc.gpsimd.to_reg(NSLOT - 1)
        REG_W = nc.gpsimd.to_reg(NEXP * 128 - 1)
        d0i = sbt(es, "d0i", [128, NOWN], I32); d1i = sbt(es, "d1i", [128, NOWN], I32)
        idxw = sbt(es, "idxw", [128, NBLK], I32)
        with ExitStack() as dp:
            set_rot(range(8))
            Ab_t = Aall.t[:].rearrange("p t e -> p (t e)")
            lsf = sbt(dp, "lsf", [128, 128], F32, True); load(lsf, c_lstrict)
            lsb = sbt(dp, "lsb", [128, 128], BF16)
            thr = sbt(dp, "thr", [128, NBLK], F32, True); load(thr, c_thr)
            pidx = sbt(dp, "pidx", [128, 1], F32, True); load(pidx, c_pidx)
            V(lambda: nc.vector.tensor_copy(out=lsb.t[:], in_=lsf.t[:]), [lsf.r], [lsb.r])
            tsum = sbt(dp, "tsum", [128, NOWN, 32], F32)
            rloc = sbt(dp, "rloc", [128, NOWN, 32], F32)
            for c0 in range(0, NA, 512):
                w_ = min(512, NA - c0)
                ps = pnext()
                P(lambda: nc.tensor.matmul(ps.t[:, 0:w_], lhsT=onesb.t[:], rhs=Ab_t[:, c0:c0 + w_], start=True, stop=True),
                  [onesb.r, Aall.r], [ps.r])
                V(lambda: nc.vector.tensor_copy(out=tsum.t[:].rearrange("p t e -> p (t e)")[:, c0:c0 + w_], in_=ps.t[:, 0:w_]),
                  [ps.r], [tsum.r])
                ps2 = pnext()
                P(lambda: nc.tensor.matmul(ps2.t[:, 0:w_], lhsT=lsb.t[:], rhs=Ab_t[:, c0:c0 + w_], start=True, stop=True),
                  [lsb.r, Aall.r], [ps2.r])
                V(lambda: nc.vector.tensor_copy(out=rloc.t[:].rearrange("p t e -> p (t e)")[:, c0:c0 + w_], in_=ps2.t[:, 0:w_]),
                  [ps2.r], [rloc.r])
            cum = sbt(dp, "cum", [128, NOWN + 1, 32], F32)
            V(lambda: nc.vector.memset(cum.t[:, 0, :], 0.0), [], [cum.r])
            for t in range(NOWN):
                V(lambda: nc.vector.tensor_tensor(out=cum.t[:, t + 1, :], in0=cum.t[:, t, :], in1=tsum.t[:, t, :], op=ALU.add),
                  [cum.r, tsum.r], [cum.r])
            ci = sbt(dp, "ci", [128, 32], I32)
            pad = sbt(dp, "pad", [128, 32], F32)
            pend = sbt(dp, "pend", [128, 32], F32)
            pst = sbt(dp, "pst", [128, 32], F32)
            V(lambda: nc.vector.tensor_scalar(out=pad.t[:], in0=cum.t[:, NOWN, :], scalar1=float(BLK - 1), scalar2=None,
                                              op0=ALU.add), [cum.r], [pad.r])
            V(lambda: nc.vector.tensor_copy(out=ci.t[:], in_=pad.t[:]), [pad.r], [ci.r])
            V(lambda: nc.vector.tensor_scalar(out=ci.t[:], in0=ci.t[:], scalar1=8, scalar2=8, op0=ALU.arith_shift_right,
                                              op1=ALU.logical_shift_left), [ci.r], [ci.r])
            V(lambda: nc.vector.tensor_copy(out=pad.t[:], in_=ci.t[:]), [ci.r], [pad.r])
            V(lambda: nc.vector.tensor_tensor_scan(out=pend.t[:], data0=onesf.t[:, 0:32], data1=pad.t[:], initial=0.0,
                                                   op0=ALU.mult, op1=ALU.add), [onesf.r, pad.r], [pend.r])
            V(lambda: nc.vector.tensor_tensor(out=pst.t[:], in0=pend.t[:], in1=pad.t[:], op=ALU.subtract), [pend.r, pad.r], [pst.r])
            V(lambda: nc.vector.tensor_tensor(out=rloc.t[:], in0=rloc.t[:], in1=cum.t[:, 0:NOWN, :], op=ALU.add),
              [rloc.r, cum.r], [rloc.r])
            V(lambda: nc.vector.tensor_tensor(out=rloc.t[:], in0=rloc.t[:], in1=pst.t[:].unsqueeze(1).to_broadcast([128, NOWN, 32]),
                                              op=ALU.add), [rloc.r, pst.r], [rloc.r])
            df = sbt(dp, "df", [128, NOWN], F32)
            for Ak, dk in ((A0all, d0i), (A1all, d1i)):
                V(lambda: nc.vector.tensor_tensor(out=tsum.t[:], in0=Ak.t[:], in1=rloc.t[:], op=ALU.mult), [Ak.r, rloc.r, tsum.r], [tsum.r])
                V(lambda: nc.vector.tensor_reduce(out=df.t[:], in_=tsum.t[:], axis=AX.X, op=ALU.add), [tsum.r], [df.r])
                V(lambda: nc.vector.tensor_copy(out=dk.t[:], in_=df.t[:]), [df.r], [dk.r])
            cmp_ = sbt(dp, "cmp", [128, NBLK, 32], F32)
            bef = sbt(dp, "bef", [128, NBLK], F32)
            V(lambda: nc.vector.tensor_tensor(out=cmp_.t[:], in0=pend.t[:].unsqueeze(1).to_broadcast([128, NBLK, 32]),
                                              in1=thr.t[:].unsqueeze(2).to_broadcast([128, NBLK, 32]), op=ALU.is_le),
              [pend.r, thr.r], [cmp_.r])
            V(lambda: nc.vector.tensor_reduce(out=bef.t[:], in_=cmp_.t[:], axis=AX.X, op=ALU.add), [cmp_.r], [bef.r])
            V(lambda: nc.vector.tensor_scalar(out=bef.t[:], in0=bef.t[:], scalar1=float(NEXP - 1), scalar2=128.0, op0=ALU.min,
                                              op1=ALU.mult), [bef.r], [bef.r])
            V(lambda: nc.vector.tensor_scalar(out=bef.t[:], in0=bef.t[:], scalar1=pidx.t[:, 0:1], scalar2=None, op0=ALU.add),
              [bef.r, pidx.r], [bef.r])
            skp = sbt(dp, "skp", [128, NBLK], F32)
            V(lambda: nc.vector.tensor_scalar(out=skp.t[:], in0=thr.t[:], scalar1=pend.t[:, 31:32], scalar2=1.0e6, op0=ALU.is_ge,
                                              op1=ALU.mult), [thr.r, pend.r], [skp.r])
            V(lambda: nc.vector.tensor_tensor(out=bef.t[:], in0=bef.t[:], in1=skp.t[:], op=ALU.add), [bef.r, skp.r], [bef.r])
            V(lambda: nc.vector.tensor_copy(out=idxw.t[:], in_=bef.t[:]), [bef.r], [idxw.r])
            hb = [sbt(dp, f"hb{i}", [128, D], BF16, True) for i in range(3)]
            xs_res = Res("XS")
            for t in range(NOWN):
                b_ = hb[t % 3]
                load(b_, H2[t * 128:(t + 1) * 128, :])
                for dk in (d0i, d1i):
                    T.dma(POOL, b_.l, lambda: nc.gpsimd.indirect_dma_start(
                        out=XS, out_offset=bass.IndirectOffsetOnAxis(ap=dk.t[:, t:t + 1], axis=0), in_=b_.t[:], in_offset=None,
                        bounds_check=REG_SLOT, oob_is_err=False),
                        reads=[b_.r, dk.r])
            T.barrier()
            if stop <= 4:
                return nc

        with ExitStack() as dd:
            set_rot(range(8))
            stg = [[sbt(dd, f"stg{i}_{k}", [128, 4096], F32, True) for k in range(3)] for i in range(2)]
            w1b = [sbt(dd, f"w1b{i}", [128, 8, 4, 128], BF16) for i in range(2)]
            w3b = [sbt(dd, f"w3b{i}", [128, 8, 4, 128], BF16) for i in range(2)]
            w2b = [sbt(dd, f"w2b{i}", [128, 4, D], BF16) for i in range(2)]
            xbk = [sbt(dd, f"xbk{i}", [128, 2, D], BF16, True) for i in range(2)]
            xTb = [sbt(dd, f"xTb{i}", [128, 8, 256], BF16) for i in range(1)]
            sil = sbt(dd, "sil", [128, 256], F32)
            gTb = [sbt(dd, f"gTb{i}", [128, 4, 256], BF16) for i in range(2)]
            ysb = [sbt(dd, f"ysb{i}", [128, D], F32, True) for i in range(2)]
            yc = 0

            def issue_w(j):
                s_ = stg[j % 2]
                for k, wsrc in enumerate((w1, w3, w2)):
                    T.dma(POOL, s_[k].l, lambda: nc.gpsimd.indirect_dma_start(
                        out=s_[k].t[:], out_offset=None, in_=wsrc,
                        in_offset=bass.IndirectOffsetOnAxis(ap=idxw.t[:, j:j + 1], axis=0),
                        bounds_check=REG_W, oob_is_err=False),
                        reads=[idxw.r], writes=[s_[k].r])

            def issue_x(j):
                load(xbk[j % 2], XS[j * BLK:(j + 1) * BLK, :].rearrange("(a p) d -> p a d", p=128))

            issue_w(0)
            issue_x(0)
            for j in range(NBLK):
                if j + 1 < NBLK:
                    issue_w(j + 1)
                    issue_x(j + 1)
                s_ = stg[j % 2]; b1 = w1b[j % 2]; b3 = w3b[j % 2]; b2 = w2b[j % 2]
                V(lambda: nc.vector.tensor_copy(out=b1.t[:], in_=s_[0].t[:].rearrange("p (kc m fc) -> p kc fc m", kc=8, m=128, fc=4)),
                  [s_[0].r], [b1.r])
                V(lambda: nc.vector.tensor_copy(out=b3.t[:], in_=s_[1].t[:].rearrange("p (kc m fc) -> p kc fc m", kc=8, m=128, fc=4)),
                  [s_[1].r], [b3.r])
                A(lambda: nc.scalar.copy(out=b2.t[:].rearrange("p f n -> p (f n)"), in_=s_[2].t[:]), [s_[2].r], [b2.r])
                xk = xbk[j % 2]; xT_ = xTb[0]; gT_ = gTb[j % 2]
                for st_ in range(2):
                    tp = pnext(); tpb = tp.t[:].bitcast(BF16)
                    for kc in range(8):
                        P(lambda: nc.tensor.transpose(out=tpb[:, kc * 128:(kc + 1) * 128], in_=xk.t[:, st_, kc * 128:(kc + 1) * 128],
                                                      identity=identb.t[:]), [xk.r, identb.r], [tp.r])
                    cp = (lambda f: A(f, [tp.r], [xT_.r])) if st_ == 0 else (lambda f: V(f, [tp.r], [xT_.r]))
                    if st_ == 0:
                        A(lambda: nc.scalar.copy(out=xT_.t[:, :, 0:128], in_=tpb.rearrange("p (k t) -> p k t", k=8)), [tp.r], [xT_.r])
                    else:
                        V(lambda: nc.vector.tensor_copy(out=xT_.t[:, :, 128:256], in_=tpb.rearrange("p (k t) -> p k t", k=8)),
                          [tp.r], [xT_.r])
                for fc in range(4):
                    pa = pnext(); pb_ = pnext()
                    for kc in range(8):
                        P(lambda: nc.tensor.matmul(pa.t[:, 0:256], lhsT=b1.t[:, kc, fc, :], rhs=xT_.t[:, kc, :], start=(kc == 0),
                                                   stop=(kc == 7)), [b1.r, xT_.r], [pa.r])
                    for kc in range(8):
                        P(lambda: nc.tensor.matmul(pb_.t[:, 0:256], lhsT=b3.t[:, kc, fc, :], rhs=xT_.t[:, kc, :], start=(kc == 0),
                                                   stop=(kc == 7)), [b3.r, xT_.r], [pb_.r])
                    A(lambda: nc.scalar.activation(out=sil.t[:], in_=pa.t[:, 0:256], func=AF.Silu), [pa.r], [sil.r])
                    V(lambda: nc.vector.tensor_tensor(out=gT_.t[:, fc, :], in0=pb_.t[:, 0:256], in1=sil.t[:], op=ALU.mult),
                      [pb_.r, sil.r], [gT_.r])
                for st_ in range(2):
                    y_ = ysb[yc % 2]; yc += 1
                    for hh in range(2):
                        ps = pnext()
                        for fc in range(4):
                            P(lambda: nc.tensor.matmul(ps.t[:], lhsT=gT_.t[:, fc, st_ * 128:(st_ + 1) * 128],
                                                       rhs=b2.t[:, fc, hh * 512:(hh + 1) * 512], start=(fc == 0), stop=(fc == 3)),
                              [gT_.r, b2.r], [ps.r])
                        if hh == 0:
                            A(lambda: nc.scalar.copy(out=y_.t[:, 0:512], in_=ps.t[:]), [ps.r], [y_.r])
                        else:
                            V(lambda: nc.vector.tensor_copy(out=y_.t[:, 512:1024], in_=ps.t[:]), [ps.r], [y_.r])
                    r0 = j * BLK + st_ * 128
                    T.dma(SP, y_.l, lambda: nc.sync.dma_start(out=YS[r0:r0 + 128, :], in_=y_.t[:]), reads=[y_.r])
            T.barrier()
            if stop <= 5:
                return nc

        with ExitStack() as ee:
            set_rot(range(8))
            gfb = sbt(ee, "gfb", [128, D], F32, True); load(gfb, gf_row.partition_broadcast(128))
            y0 = [sbt(ee, f"y0_{i}", [128, D], F32, True) for i in range(2)]
            y1 = [sbt(ee, f"y1_{i}", [128, D], F32, True) for i in range(2)]
            xr = [sbt(ee, f"xr{i}", [128, D], F32, True) for i in range(2)]
            acc = sbt(ee, "acc", [128, D], F32)
            jk = sbt(ee, "jk", [128, D], F32)
            ob = [sbt(ee, f"ob{i}", [128, D], F32, True) for i in range(2)]
            st3 = sbt(ee, "st3", [128, 4], F32)
            def issue_e(t):
                a0 = y0[t % 2]; a1 = y1[t % 2]; xx = xr[t % 2]
                T.dma(POOL, a0.l, lambda: nc.gpsimd.indirect_dma_start(
                    out=a0.t[:], out_offset=None, in_=YS, in_offset=bass.IndirectOffsetOnAxis(ap=d0i.t[:, t:t + 1], axis=0),
                    bounds_check=REG_SLOT, oob_is_err=False),
                    reads=[d0i.r], writes=[a0.r])
                T.dma(POOL, a1.l, lambda: nc.gpsimd.indirect_dma_start(
                    out=a1.t[:], out_offset=None, in_=YS, in_offset=bass.IndirectOffsetOnAxis(ap=d1i.t[:, t:t + 1], axis=0),
                    bounds_check=REG_SLOT, oob_is_err=False),
                    reads=[d1i.r], writes=[a1.r])
                load(xx, X1[t * 128:(t + 1) * 128, :])

            issue_e(0)
            for t in range(NOWN):
                a0 = y0[t % 2]; a1 = y1[t % 2]; xx = xr[t % 2]; o_ = ob[t % 2]
                if t + 1 < NOWN:
                    issue_e(t + 1)
                V(lambda: nc.vector.tensor_scalar(out=acc.t[:], in0=a0.t[:], scalar1=wts.t[:, t, 0:1], scalar2=None, op0=ALU.mult),
                  [a0.r, wts.r], [acc.r])
                V(lambda: nc.vector.scalar_tensor_tensor(out=acc.t[:], in0=a1.t[:], scalar=wts.t[:, t, 1:2], in1=acc.t[:],
                                                         op0=ALU.mult, op1=ALU.add), [a1.r, wts.r, acc.r], [acc.r])
                G(lambda: nc.gpsimd.tensor_tensor(out=acc.t[:], in0=acc.t[:], in1=gate2b.t[:], op=ALU.mult), [acc.r, gate2b.r], [acc.r])
                G(lambda: nc.gpsimd.tensor_tensor(out=acc.t[:], in0=acc.t[:], in1=xx.t[:], op=ALU.add), [acc.r, xx.r], [acc.r])
                A(lambda: nc.scalar.activation(out=jk.t[:], in_=acc.t[:], func=AF.Square, accum_out=st3.t[:, 0:1]),
                  [acc.r], [jk.r, st3.r])
                V(lambda: nc.vector.tensor_scalar(out=st3.t[:, 1:2], in0=st3.t[:, 0:1], scalar1=1.0 / D, scalar2=EPS,
                                                  op0=ALU.mult, op1=ALU.add), [st3.r], [st3.r])
                A(lambda: nc.scalar.activation(out=st3.t[:, 2:3], in_=st3.t[:, 1:2], func=AF.Sqrt), [st3.r], [st3.r])
                V(lambda: nc.vector.reciprocal(out=st3.t[:, 3:4], in_=st3.t[:, 2:3]), [st3.r], [st3.r])
                V(lambda: nc.vector.scalar_tensor_tensor(out=o_.t[:], in0=acc.t[:], scalar=st3.t[:, 3:4], in1=gfb.t[:],
                                                         op0=ALU.mult, op1=ALU.mult), [acc.r, st3.r, gfb.r], [o_.r])
                T.dma(SP, o_.l, lambda: nc.sync.dma_start(out=out_d[t * 128:(t + 1) * 128, :], in_=o_.t[:]), reads=[o_.r])
            T.barrier()
    return nc


def _own_tiles(j, NP):
    t = []
    for m in range(NP):
        t += [8 * m + j, 8 * m + 7 - j]
    return t


def make_in_maps(inp, S):
    f32 = np.float32
    NT = S // 128; NP = NT // 8; NOWN = 2 * NP; TOK = NOWN * 128
    NBLK = (TOK * 2) // BLK + NEXP
    g = lambda k: np.asarray(inp[k])
    col = lambda v, n: np.ascontiguousarray(np.asarray(v, f32).reshape(n, 128).T)
    x = g("x"); c = g("c"); pos = g("positions")
    p = np.arange(128)
    fq = np.zeros((128, 1), f32)
    freqs = (np.float32(10000.0) ** (-np.arange(16, dtype=f32) / np.float32(16))).astype(f32)
    fq[64:96, 0] = np.tile(freqs, 2) / np.float32(2 * np.pi)
    shared = dict(
        w_ada=np.ascontiguousarray(g("w_ada")[0]), b_adaT=col(g("b_ada")[0], 48), b_ada_row=g("b_ada")[0].reshape(1, -1).astype(f32),
        g1col=col(g("norm1_g")[0], 8), g1row=g("norm1_g")[0].reshape(1, -1).astype(f32), w_in=np.ascontiguousarray(g("w_in")[0]),
        gqcol=col(g("q_norm_g")[0], 2), w_uq=np.ascontiguousarray(g("w_uq")[0]),
        gkvcol=col(g("kv_norm_g")[0], 1), w_ukv=np.ascontiguousarray(g("w_ukv")[0]),
        gv_row=g("v_norm_g")[0].reshape(1, -1).astype(f32), bv_row=g("v_norm_b")[0].reshape(1, -1).astype(f32),
        w_s=np.ascontiguousarray(g("w_s")[0]), bsT=np.ascontiguousarray(g("b_s")[0].T),
        w_br_mla=np.ascontiguousarray(g("w_br_mla")[0]), w_br_sgu=np.ascontiguousarray(g("w_br_sgu")[0]),
        w_out=np.ascontiguousarray(g("w_out")[0]),
        g2col=col(g("norm2_g")[0], 8), g2row=g("norm2_g")[0].reshape(1, -1).astype(f32),
        wr=np.ascontiguousarray(np.concatenate([g("w_rg")[0], g("w_re")[0]], axis=1)),
        br_row=np.concatenate([g("b_rg")[0], g("b_re")[0]]).reshape(1, -1).astype(f32),
        w1=np.ascontiguousarray(g("w1")[0]).reshape(NEXP * 128, 4096),
        w3=np.ascontiguousarray(g("w3")[0]).reshape(NEXP * 128, 4096),
        w2=np.ascontiguousarray(g("w2")[0]).reshape(NEXP * 128, 4096),
        gf_row=g("final_g").reshape(1, -1).astype(f32),
        c_ident=np.eye(128, dtype=f32), c_tril=np.tril(np.ones((128, 128), f32)),
        c_lstrict=(p[:, None] < p[None, :]).astype(f32), c_kq=(p[:, None] - p[None, :]).astype(f32),
        c_fq=fq, c_thr=np.tile((np.arange(NBLK, dtype=f32) * BLK)[None, :], (128, 1)).astype(f32),
        c_pidx=p.astype(f32).reshape(128, 1),
    )
    maps = []
    owns = []
    for core in range(8):
        b, j = core // 4, core % 4
        tiles = _own_tiles(j, NP)
        rows = np.concatenate([np.arange(t * 128, (t + 1) * 128) for t in tiles])
        owns.append((b, rows))
        dt_ = np.zeros((128, 16), f32)
        for o in range(8):
            dt_[:, o] = (j - o) * 128
            dt_[:, 8 + o] = (7 - j - o) * 128
        m = dict(shared)
        m.update(
            xb=np.ascontiguousarray(x[b, :S]), xo=np.ascontiguousarray(x[b][rows]),
            posb=np.ascontiguousarray(pos[b, :S].reshape(1, -1).astype(np.int32)),
            poso=np.ascontiguousarray(pos[b][rows].reshape(1, -1).astype(np.int32)),
            ccol=col(c[b], 8), c_dtab=dt_,
        )
        maps.append(m)
    return maps, owns


_NC_CACHE = {}


def kernel(**inputs):
    S = int(np.asarray(inputs["x"]).shape[1])
    if S not in _NC_CACHE:
        _NC_CACHE[S] = build_nc(S)
    nc = _NC_CACHE[S]
    maps, owns = make_in_maps(inputs, S)
    res = run_bass_kernel_spmd(nc, maps, core_ids=list(range(8)))
    B = np.asarray(inputs["x"]).shape[0]
    out = np.zeros((B, S, D), np.float32)
    for core, (b, rows) in enumerate(owns):
        out[b, rows] = np.asarray(res.results[core]["out"], dtype=np.float32)
    return out
```

```python
import numpy as np
from contextlib import ExitStack
import concourse.bass as bass
import concourse.mybir as mybir
from concourse.bass_utils import run_bass_kernel_spmd

F32 = mybir.dt.float32
BF16 = mybir.dt.bfloat16
I32 = mybir.dt.int32
AF = mybir.ActivationFunctionType
ALU = mybir.AluOpType
AX = mybir.AxisListType

D = 1024
NH = 8
EPS = 1e-6
NEXP = 32
FF = 512
BLK = 256
IN_W = 3488
SCALE = 96 ** -0.5
GELU_C = 1.5957691216057308


class Res:
    __slots__ = ("name", "w", "r")

    def __init__(self, name=""):
        self.name = name
        self.w = None
        self.r = {}


class Lane:
    def __init__(self, T, name):
        self.sem = T.newsem(name)
        self.count = 0


class Eng:
    def __init__(self, T, name, eng, is_pe=False):
        self.name = name
        self.eng = eng
        self.is_pe = is_pe
        self.sem = T.newsem(name + "_s0")
        self.own = [self.sem]
        self.count = 0
        self.waited = {}


class Tracker:
    CAP = 12000

    def __init__(self, nc, es):
        self.nc = nc
        self.es = es
        self.nsems = 0
        self.lanes = []
        self.pe = Eng(self, "pe", nc.tensor, True)
        self.act = Eng(self, "act", nc.scalar)
        self.dve = Eng(self, "dve", nc.vector)
        self.pool = Eng(self, "pool", nc.gpsimd)
        self.sp = Eng(self, "sp", nc.sync)
        self.engs = [self.pe, self.act, self.dve, self.pool, self.sp]

    def newsem(self, name):
        self.nsems += 1
        return self.es.enter_context(self.nc.semaphore(f"{name}_{self.nsems}"))

    def lane(self, name="l"):
        l = Lane(self, name)
        self.lanes.append(l)
        return l

    def _wait(self, E, deps):
        for sem, val in deps:
            if E.is_pe and any(sem is s for s in E.own):
                continue
            k = id(sem)
            if E.waited.get(k, 0) < val:
                E.eng.wait_ge(sem, val)
                E.waited[k] = val

    @staticmethod
    def _deps(reads, writes):
        deps = []
        for r in reads:
            if r.w is not None:
                deps.append(r.w)
        for w in writes:
            if w.w is not None:
                deps.append(w.w)
            deps.extend(w.r.values())
        return deps

    @staticmethod
    def _record(ev, reads, writes):
        for r in reads:
            r.r[id(ev[0])] = ev
        for w in writes:
            w.w = ev
            w.r = {}

    def op(self, E, fn, reads=(), writes=()):
        self._wait(E, self._deps(reads, writes))
        if E.count >= self.CAP:
            E.sem = self.newsem(E.name + "_s")
            E.own.append(E.sem)
            E.count = 0
        ins = fn()
        ins.then_inc(E.sem, 1)
        E.count += 1
        self._record((E.sem, E.count), reads, writes)

    def dma(self, E, lane, fn, reads=(), writes=()):
        self._wait(E, self._deps(reads, writes))
        ins = fn()
        ins.then_inc(lane.sem, 16)
        lane.count += 16
        self._record((lane.sem, lane.count), reads, writes)

    def barrier(self):
        evs = [(e.sem, e.count) for e in self.engs if e.count > 0]
        evs += [(l.sem, l.count) for l in self.lanes if l.count > 0]
        for E in self.engs:
            self._wait(E, evs)


class Buf:
    def __init__(self, T, tile, name, lane=False):
        self.t = tile
        self.r = Res(name)
        self.l = T.lane(name) if lane else None


def build_nc(S, stop=99):
    NT = S // 128
    NP = NT // 8
    NOWN = 2 * NP
    TOK = NOWN * 128
    NG = NT // 4
    NGO = max(NOWN // 4, 1)
    GW = min(512, TOK)
    NBLK = (TOK * 2) // BLK + NEXP
    NSLOT = NBLK * BLK

    nc = bass.Bass("TRN2", target_bir_lowering=False)

    def din(name, shape, dt=F32):
        return nc.dram_tensor(name, list(shape), dt, kind="ExternalInput").ap()

    xb = din("xb", [S, D]); xo = din("xo", [TOK, D])
    posb = din("posb", [1, S], I32); poso = din("poso", [1, TOK], I32)
    ccol = din("ccol", [128, 8])
    w_ada = din("w_ada", [D, 6 * D]); b_adaT = din("b_adaT", [128, 48]); b_ada_row = din("b_ada_row", [1, 6 * D])
    g1col = din("g1col", [128, 8]); g1row = din("g1row", [1, D]); w_in = din("w_in", [D, IN_W])
    gqcol = din("gqcol", [128, 2]); w_uq = din("w_uq", [256, 768])
    gkvcol = din("gkvcol", [128, 1]); w_ukv = din("w_ukv", [128, 1024])
    gv_row = din("gv_row", [1, 512]); bv_row = din("bv_row", [1, 512])
    w_s = din("w_s", [8, 128, 128]); bsT = din("bsT", [128, 8])
    w_br_mla = din("w_br_mla", [512, D]); w_br_sgu = din("w_br_sgu", [512, D]); w_out = din("w_out", [D, D])
    g2col = din("g2col", [128, 8]); g2row = din("g2row", [1, D])
    wr = din("wr", [D, 36]); br_row = din("br_row", [1, 36])
    w1 = din("w1", [NEXP * 128, 4096]); w3 = din("w3", [NEXP * 128, 4096]); w2 = din("w2", [NEXP * 128, 4096])
    gf_row = din("gf_row", [1, D])
    c_ident = din("c_ident", [128, 128]); c_tril = din("c_tril", [128, 128]); c_lstrict = din("c_lstrict", [128, 128])
    c_kq = din("c_kq", [128, 128]); c_fq = din("c_fq", [128, 1]); c_dtab = din("c_dtab", [128, 16])
    c_thr = din("c_thr", [128, NBLK]); c_pidx = din("c_pidx", [128, 1])

    out_d = nc.dram_tensor("out", [TOK, D], F32, kind="ExternalOutput").ap()
    X1 = nc.dram_tensor("X1s", [TOK, D], F32, kind="Internal").ap()
    H2 = nc.dram_tensor("H2s", [TOK, D], BF16, kind="Internal").ap()
    XS = nc.dram_tensor("XSs", [NSLOT, D], BF16, kind="Internal").ap()
    YS = nc.dram_tensor("YSs", [NSLOT, D], F32, kind="Internal").ap()
    OS = nc.dram_tensor("OSs", [TOK, 512], BF16, kind="Internal").ap()

    with ExitStack() as es:
        T = Tracker(nc, es)
        PE, ACT, DVE, POOL, SP = T.pe, T.act, T.dve, T.pool, T.sp

        def sbt(stack, name, shape, dt, lane=False):
            t = stack.enter_context(nc.sbuf_tensor(name, list(shape), dt))
            return Buf(T, t, name, lane)

        banks = []
        for i in range(8):
            t = es.enter_context(nc.psum_tensor(f"pb{i}", [128, 512], F32))
            banks.append(Buf(T, t, f"pb{i}"))
        rot = {"i": 0, "lst": list(range(8))}

        def pnext():
            b = banks[rot["lst"][rot["i"] % len(rot["lst"])]]
            rot["i"] += 1
            return b

        def set_rot(lst):
            rot["lst"] = list(lst)
            rot["i"] = 0

        def V(fn, reads=(), writes=()):
            T.op(DVE, fn, reads, writes)

        def A(fn, reads=(), writes=()):
            T.op(ACT, fn, reads, writes)

        def G(fn, reads=(), writes=()):
            T.op(POOL, fn, reads, writes)

        def P(fn, reads=(), writes=()):
            T.op(PE, fn, reads, writes)

        def load(buf, src, eng=None):
            E = SP if eng is None else eng
            T.dma(E, buf.l, lambda: E.eng.dma_start(out=buf.t[:], in_=src), writes=[buf.r])

        def load_to(buf, dst_ap, src, eng=None):
            E = SP if eng is None else eng
            T.dma(E, buf.l, lambda: E.eng.dma_start(out=dst_ap, in_=src), writes=[buf.r])

        identf = sbt(es, "identf", [128, 128], F32, True); load(identf, c_ident)
        identb = sbt(es, "identb", [128, 128], BF16)
        V(lambda: nc.vector.tensor_copy(out=identb.t[:], in_=identf.t[:]), [identf.r], [identb.r])
        onesb = sbt(es, "onesb", [128, 128], BF16)
        V(lambda: nc.vector.memset(onesb.t[:], 1.0), [], [onesb.r])
        onesf = sbt(es, "onesf", [128, 128], F32)
        V(lambda: nc.vector.memset(onesf.t[:], 1.0), [], [onesf.r])
        kqf = sbt(es, "kqf", [128, 128], F32, True); load(kqf, c_kq)
        kqb = sbt(es, "kqb", [128, 128], BF16)
        V(lambda: nc.vector.tensor_copy(out=kqb.t[:], in_=kqf.t[:]), [kqf.r], [kqb.r])
        fq = sbt(es, "fq", [128, 1], F32, True); load(fq, c_fq)
        dtab = sbt(es, "dtab", [128, 16], F32, True); load(dtab, c_dtab)
        g1c = sbt(es, "g1c", [128, 8], F32, True); load(g1c, g1col)
        g2c = sbt(es, "g2c", [128, 8], F32, True); load(g2c, g2col)
        gqc = sbt(es, "gqc", [128, 2], F32, True); load(gqc, gqcol)
        gkvc = sbt(es, "gkvc", [128, 1], F32, True); load(gkvc, gkvcol)
        cc = sbt(es, "cc", [128, 8], F32, True); load(cc, ccol)
        badT = sbt(es, "badT", [128, 48], F32, True); load(badT, b_adaT)
        modT = sbt(es, "modT", [128, 48], F32)
        gate1b = sbt(es, "gate1b", [128, D], F32)
        s2b = sbt(es, "s2b", [128, D], F32)
        sh2b = sbt(es, "sh2b", [128, D], F32)
        gate2b = sbt(es, "gate2b", [128, D], F32)
        s1b = sbt(es, "s1b", [128, D], F32)
        sh1b = sbt(es, "sh1b", [128, 8], BF16)
        ones512 = sbt(es, "ones512", [1, 512], BF16)
        V(lambda: nc.vector.memset(ones512.t[:], 1.0), [], [ones512.r])
        s1c = sbt(es, "s1c", [128, 8], F32); s2c = sbt(es, "s2c", [128, 8], F32)

        with ExitStack() as ph:
            sc = sbt(ph, "sc", [128, 8], F32)
            A(lambda: nc.scalar.activation(out=sc.t[:], in_=cc.t[:], func=AF.Silu), [cc.r], [sc.r])
            brow = [sbt(ph, f"brow{i}", [1, 512], F32, True) for i in range(2)]
            rowdst = {2: (s1b, 0), 3: (s1b, 512), 4: (gate1b, 0), 5: (gate1b, 512), 6: (sh2b, 0), 7: (sh2b, 512), 8: (s2b, 0), 9: (s2b, 512),
                      10: (gate2b, 0), 11: (gate2b, 512)}
            wp = [sbt(ph, f"wada{i}", [128, 8, 512], F32, True) for i in range(2)]
            pcol = banks[7]
            set_rot(range(7))
            rowtmp = sbt(ph, "rowtmp", [1, 512], F32)
            for pc in range(12):
                wb_ = wp[pc % 2]
                load(wb_, w_ada[:, pc * 512:(pc + 1) * 512].rearrange("(kc p) n -> p kc n", p=128))
                if pc in rowdst:
                    prow = pnext()
                    br_ = brow[pc % 2]
                    load(br_, b_ada_row[0:1, pc * 512:(pc + 1) * 512])
                    for kc in range(8):
                        P(lambda: nc.tensor.matmul(prow.t[0:1, :], lhsT=sc.t[:, kc:kc + 1], rhs=wb_.t[:, kc, :],
                                                   start=(kc == 0), stop=(kc == 7)), [sc.r, wb_.r], [prow.r])
                    V(lambda: nc.vector.tensor_tensor(out=rowtmp.t[0:1, :], in0=prow.t[0:1, :], in1=br_.t[0:1, :], op=ALU.add),
                      [prow.r, br_.r], [rowtmp.r])
                    pbc = pnext()
                    P(lambda: nc.tensor.matmul(pbc.t[:], lhsT=onesf.t[0:1, :], rhs=rowtmp.t[0:1, :], start=True, stop=True),
                      [onesf.r, rowtmp.r], [pbc.r])
                    dtile, dc0 = rowdst[pc]
                    V(lambda: nc.vector.tensor_copy(out=dtile.t[:, dc0:dc0 + 512], in_=pbc.t[:]), [pbc.r], [dtile.r])
                for oc in range(4):
                    col = pc * 4 + oc
                    for kc in range(8):
                        P(lambda: nc.tensor.matmul(pcol.t[:, col:col + 1], lhsT=wb_.t[:, kc, oc * 128:(oc + 1) * 128],
                                                   rhs=sc.t[:, kc:kc + 1], start=(kc == 0), stop=(kc == 7)),
                          [sc.r, wb_.r], [pcol.r])
            V(lambda: nc.vector.tensor_tensor(out=modT.t[:], in0=pcol.t[:, 0:48], in1=badT.t[:], op=ALU.add),
              [pcol.r, badT.r], [modT.r])
            V(lambda: nc.vector.scalar_tensor_tensor(out=s1c.t[:], in0=modT.t[:, 8:16], scalar=1.0, in1=g1c.t[:],
                                                     op0=ALU.add, op1=ALU.mult), [modT.r, g1c.r], [s1c.r])
            V(lambda: nc.vector.scalar_tensor_tensor(out=s2c.t[:], in0=modT.t[:, 32:40], scalar=1.0, in1=g2c.t[:],
                                                     op0=ALU.add, op1=ALU.mult), [modT.r, g2c.r], [s2c.r])
            g1rb = sbt(ph, "g1rb", [128, D], F32, True); load(g1rb, g1row.partition_broadcast(128))
            V(lambda: nc.vector.scalar_tensor_tensor(out=s1b.t[:], in0=s1b.t[:], scalar=1.0, in1=g1rb.t[:], op0=ALU.add,
                                                     op1=ALU.mult), [s1b.r, g1rb.r], [s1b.r])
            V(lambda: nc.vector.tensor_copy(out=sh1b.t[:], in_=modT.t[:, 0:8]), [modT.r], [sh1b.r])
            T.barrier()
        if stop <= 0:
            return nc
        sh1 = lambda kc: modT.t[:, kc:kc + 1]
        sh2 = lambda kc: modT.t[:, 24 + kc:25 + kc]

        FE = {}
        cnt = {"x": 0, "fe": 0, "st": 0, "xn": 0, "cp": 0}

        def make_fe(stack, nx=3, nxn=2):
            k = cnt["fe"]; cnt["fe"] += 1
            FE["nx"] = nx
            FE["xts"] = [sbt(stack, f"xt{k}_{i}", [128, D], F32, True) for i in range(nx)]
            FE["junk"] = sbt(stack, f"junk{k}", [128, D], BF16)
            FE["stat"] = [sbt(stack, f"stat{k}_{i}", [128, 16], F32) for i in range(3)]
            FE["xns"] = [sbt(stack, f"xn{k}_{i}", [128, D], BF16) for i in range(nxn)]

        def fe_a(tile_aps):
            n = len(tile_aps)
            st = FE["stat"][cnt["st"] % 3]; cnt["st"] += 1
            junk = FE["junk"]
            xl = []
            for j in range(n):
                xt = FE["xts"][cnt["x"] % FE["nx"]]; cnt["x"] += 1
                load(xt, tile_aps[j])
                xl.append(xt)
            for j in range(n):
                A(lambda: nc.scalar.activation(out=junk.t[:], in_=xl[j].t[:], func=AF.Square, accum_out=st.t[:, j:j + 1]),
                  [xl[j].r], [junk.r, st.r])
            V(lambda: nc.vector.tensor_scalar(out=st.t[:, 4:4 + n], in0=st.t[:, 0:n], scalar1=1.0 / D, scalar2=EPS,
                                              op0=ALU.mult, op1=ALU.add), [st.r], [st.r])
            A(lambda: nc.scalar.activation(out=st.t[:, 8:8 + n], in_=st.t[:, 4:4 + n], func=AF.Sqrt), [st.r], [st.r])
            V(lambda: nc.vector.reciprocal(out=st.t[:, 12:12 + n], in_=st.t[:, 8:8 + n]), [st.r], [st.r])
            xnl = []
            for j in range(n):
                xn = FE["xns"][cnt["xn"] % len(FE["xns"])]; cnt["xn"] += 1
                V(lambda: nc.vector.scalar_tensor_tensor(out=xn.t[:], in0=xl[j].t[:], scalar=st.t[:, 12 + j:13 + j], in1=s1b.t[:],
                                                         op0=ALU.mult, op1=ALU.mult), [xl[j].r, st.r, s1b.r], [xn.r])
                xnl.append(xn)
            return xl, xnl

        def fe_b(state, dst, dst_res, c0s):
            xl, xnl = state
            for j, xn in enumerate(xnl):
                tp = pnext()
                tpb = tp.t[:].bitcast(BF16)
                for kc in range(8):
                    P(lambda: nc.tensor.transpose(out=tpb[:, kc * 128:(kc + 1) * 128], in_=xn.t[:, kc * 128:(kc + 1) * 128],
                                                  identity=identb.t[:]), [xn.r, identb.r], [tp.r])
                c0 = c0s[j]
                src3 = tpb.rearrange("p (k t) -> p k t", k=8)
                if cnt["cp"] % 2 == 0:
                    A(lambda: nc.scalar.copy(out=dst[:, :, c0:c0 + 128], in_=src3), [tp.r], [dst_res])
                else:
                    V(lambda: nc.vector.tensor_copy(out=dst[:, :, c0:c0 + 128], in_=src3), [tp.r], [dst_res])
                cnt["cp"] += 1
            return xl

        def front_end_multi(tile_aps, dst, dst_res, c0s):
            return fe_b(fe_a(tile_aps), dst, dst_res, c0s)

        def bias_row(dst_ap, wfn, M, wres, dres):
            ps = pnext()
            for kc in range(8):
                P(lambda: nc.tensor.matmul(ps.t[0:1, 0:M], lhsT=sh1b.t[:, kc:kc + 1], rhs=wfn(kc), start=(kc == 0), stop=(kc == 7)),
                  [sh1b.r, wres], [ps.r])
            V(lambda: nc.vector.tensor_copy(out=dst_ap, in_=ps.t[0:1, 0:M]), [ps.r], [dres])

        s1col = lambda kc: s1c.t[:, kc:kc + 1]

        Aall = sbt(es, "Aall", [128, NOWN, 32], BF16)
        A0all = sbt(es, "A0all", [128, NOWN, 32], BF16)
        A1all = sbt(es, "A1all", [128, NOWN, 32], BF16)
        wts = sbt(es, "wts", [128, NOWN, 2], F32)
        att = es.enter_context(ExitStack())
        ckvnT = sbt(att, "ckvnT", [128, S], BF16); ckvn_r = [Res() for _ in range(NG)]
        KT = sbt(att, "KT", [128, S], BF16); ktn_r = [Res() for _ in range(NG)]; ktr_r = [Res() for _ in range(NG)]
        vh_r = [Res() for _ in range(NG)]
        cqnT = sbt(att, "cqnT", [128, 2, TOK], BF16); cqn_r = [Res() for _ in range(NGO)]
        qT_r = [Res() for _ in range(NGO)]
        CQ = sbt(att, "CQ", [128, TOK], BF16); SQ = sbt(att, "SQ", [128, TOK], BF16)
        cs_r = [Res() for _ in range(NGO)]

        def rope_tables(pos_ap_512, Cdst, Sdst, wres, lp, width):
            pi_ = lp["posi"]; R = slice(64, 96)
            T.dma(SP, pi_.l, lambda: nc.sync.dma_start(out=pi_.t[R, 0:width], in_=pos_ap_512.partition_broadcast(32)),
                  writes=[pi_.r])
            pf = lp["pf"]; u = lp["u"]; ki = lp["ki"]; kf = lp["kf"]; f2 = lp["f2"]
            V(lambda: nc.vector.tensor_copy(out=pf.t[R, 0:width], in_=pi_.t[R, 0:width]), [pi_.r], [pf.r])
            for which, off, dstb in ((0, 0.5, Sdst), (1, 0.75, Cdst)):
                V(lambda: nc.vector.tensor_scalar(out=u.t[R, 0:width], in0=pf.t[R, 0:width], scalar1=fq.t[R, 0:1],
                                                  scalar2=off, op0=ALU.mult, op1=ALU.add), [pf.r, fq.r], [u.r])
                V(lambda: nc.vector.tensor_copy(out=ki.t[R, 0:width], in_=u.t[R, 0:width]), [u.r], [ki.r])
                V(lambda: nc.vector.tensor_copy(out=kf.t[R, 0:width], in_=ki.t[R, 0:width]), [ki.r], [kf.r])
                V(lambda: nc.vector.scalar_tensor_tensor(out=f2.t[R, 0:width], in0=u.t[R, 0:width], scalar=-0.5,
                                                         in1=kf.t[R, 0:width], op0=ALU.add, op1=ALU.subtract),
                  [u.r, kf.r], [f2.r])
                V(lambda: nc.vector.scalar_tensor_tensor(out=u.t[R, 0:width], in0=f2.t[R, 0:width], scalar=-0.5,
                                                         in1=f2.t[R, 0:width], op0=ALU.is_lt, op1=ALU.add),
                  [f2.r], [u.r])
                A(lambda: nc.scalar.activation(out=dstb, in_=u.t[R, 0:width], func=AF.Sin, scale=6.28318),
                  [u.r], [wres])

        with ExitStack() as lp_:
            make_fe(lp_, 4, 4)
            lp = {}
            lp["posi"] = sbt(lp_, "posi", [128, 512], I32, True)
            for nm in ("pf", "u", "kf", "f2"):
                lp[nm] = sbt(lp_, "lp_" + nm, [128, 512], F32)
            lp["t1"] = lp["pf"]; lp["t2"] = lp["kf"]
            lp["ki"] = sbt(lp_, "lp_ki", [128, 512], I32)
            Ctmp = sbt(lp_, "Ctmp", [128, 512], F32); Stmp = sbt(lp_, "Stmp", [128, 512], F32)
            h1Ts = [sbt(lp_, f"h1T{i}", [128, 8, 512], BF16) for i in range(2)]
            sq = sbt(lp_, "sq", [128, 2, 512], BF16)
            rs = sbt(lp_, "rs", [128, 512], F32); rs2 = sbt(lp_, "rs2", [128, 512], F32)
            wkv = sbt(lp_, "wkv", [128, 8, 128], BF16, True)
            wkr = sbt(lp_, "wkr", [128, 8, 96], BF16, True)
            wkrr = sbt(lp_, "wkrr", [128, 8, 96], BF16)
            wq = sbt(lp_, "wq", [128, 8, 256], BF16, True)
            win_v = w_in.rearrange("(kc p) n -> p kc n", p=128)
            load(wkv, win_v[:, :, 256:384], POOL)
            V(lambda: nc.vector.memset(wkr.t[:], 0.0), [], [wkr.r])
            V(lambda: nc.vector.memset(wkrr.t[:], 0.0), [], [wkrr.r])
            load_to(wkr, wkr.t[:, :, 64:96], win_v[:, :, 384:416], POOL)
            load(wq, win_v[:, :, 0:256], POOL)
            V(lambda: nc.vector.tensor_scalar(out=wkrr.t[:, :, 64:80], in0=wkr.t[:, :, 80:96], scalar1=-1.0, scalar2=None,
                                              op0=ALU.mult), [wkr.r, wkrr.r], [wkrr.r])
            V(lambda: nc.vector.tensor_copy(out=wkrr.t[:, :, 80:96], in_=wkr.t[:, :, 64:80]), [wkr.r, wkrr.r], [wkrr.r])
            set_rot(range(8))
            brows = sbt(lp_, "brows", [1, 640], BF16)
            bias_row(brows.t[0:1, 0:128], lambda kc: wkv.t[:, kc, :], 128, wkv.r, brows.r)
            bias_row(brows.t[0:1, 128:224], lambda kc: wkr.t[:, kc, :], 96, wkr.r, brows.r)
            bias_row(brows.t[0:1, 224:320], lambda kc: wkrr.t[:, kc, :], 96, wkrr.r, brows.r)
            bias_row(brows.t[0:1, 320:576], lambda kc: wq.t[:, kc, :], 256, wq.r, brows.r)
            hcnt = {"i": 0}
            h1_of = {}

            fe_state = {}

            def latent_fe_a(mode, g, width):
                ntl = width // 128
                src = xb if mode == "k" else xo
                fe_state[(mode, g)] = fe_a([src[(g * 4 + tl) * 128:(g * 4 + tl + 1) * 128, :] for tl in range(ntl)])

            def latent_fe_b(mode, g, width):
                ntl = width // 128
                h1 = h1Ts[hcnt["i"] % 2]; hcnt["i"] += 1
                h1_of[(mode, g)] = h1
                fe_b(fe_state.pop((mode, g)), h1.t, h1.r, [tl * 128 for tl in range(ntl)])

            mm_out = {}

            def latent_mm(mode, g, width):
                h1 = h1_of.pop((mode, g))
                W = slice(0, width)
                if mode == "k":
                    pk = pnext(); pr = pnext(); prr = pnext()
                    for kc in range(8):
                        P(lambda: nc.tensor.matmul(pk.t[:, W], lhsT=wkv.t[:, kc, :], rhs=h1.t[:, kc, W],
                                                   start=(kc == 0), stop=False), [wkv.r, h1.r], [pk.r])
                    P(lambda: nc.tensor.matmul(pk.t[:, W], lhsT=brows.t[0:1, 0:128], rhs=ones512.t[0:1, W], start=False, stop=True),
                      [brows.r, ones512.r], [pk.r])
                    for kc in range(8):
                        P(lambda: nc.tensor.matmul(pr.t[0:96, W], lhsT=wkr.t[:, kc, :], rhs=h1.t[:, kc, W],
                                                   start=(kc == 0), stop=False), [wkr.r, h1.r], [pr.r])
                    P(lambda: nc.tensor.matmul(pr.t[0:96, W], lhsT=brows.t[0:1, 128:224], rhs=ones512.t[0:1, W], start=False, stop=True),
                      [brows.r, ones512.r], [pr.r])
                    for kc in range(8):
                        P(lambda: nc.tensor.matmul(prr.t[0:96, W], lhsT=wkrr.t[:, kc, :], rhs=h1.t[:, kc, W],
                                                   start=(kc == 0), stop=False), [wkrr.r, h1.r], [prr.r])
                    P(lambda: nc.tensor.matmul(prr.t[0:96, W], lhsT=brows.t[0:1, 224:320], rhs=ones512.t[0:1, W], start=False, stop=True),
                      [brows.r, ones512.r], [prr.r])
                    chunks = [pk]
                    gcol = [gkvc.t[:, 0:1]]
                    nfeat = 128
                else:
                    p0 = pnext(); p1 = pnext()
                    for ci, pp in enumerate((p0, p1)):
                        for kc in range(8):
                            P(lambda: nc.tensor.matmul(pp.t[:, W], lhsT=wq.t[:, kc, ci * 128:(ci + 1) * 128],
                                                       rhs=h1.t[:, kc, W], start=(kc == 0), stop=False),
                              [wq.r, h1.r], [pp.r])
                        P(lambda: nc.tensor.matmul(pp.t[:, W], lhsT=brows.t[0:1, 320 + ci * 128:320 + (ci + 1) * 128],
                                                   rhs=ones512.t[0:1, W], start=False, stop=True), [brows.r, ones512.r], [pp.r])
                    chunks = [p0, p1]
                    gcol = [gqc.t[:, 0:1], gqc.t[:, 1:2]]
                    nfeat = 256
                mm_out[(mode, g)] = (chunks, gcol, nfeat, (pr, prr) if mode == "k" else None)

            def latent_rest(mode, g, width):
                W = slice(0, width)
                chunks, gcol, nfeat, prs = mm_out.pop((mode, g))
                if prs is not None:
                    pr, prr = prs
                for ci, pp in enumerate(chunks):
                    A(lambda: nc.scalar.activation(out=sq.t[:, ci, W], in_=pp.t[:, W], func=AF.Square), [pp.r], [sq.r])
                pss = pnext()
                for ci in range(len(chunks)):
                    P(lambda: nc.tensor.matmul(pss.t[:, W], lhsT=onesb.t[:], rhs=sq.t[:, ci, W], start=(ci == 0),
                                               stop=(ci == len(chunks) - 1)), [onesb.r, sq.r], [pss.r])
                V(lambda: nc.vector.tensor_scalar(out=rs.t[:, W], in0=pss.t[:, W], scalar1=1.0 / nfeat, scalar2=EPS,
                                                  op0=ALU.mult, op1=ALU.add), [pss.r], [rs.r])
                A(lambda: nc.scalar.activation(out=rs2.t[:, W], in_=rs.t[:, W], func=AF.Sqrt), [rs.r], [rs2.r])
                V(lambda: nc.vector.reciprocal(out=rs.t[:, W], in_=rs2.t[:, W]), [rs2.r], [rs.r])
                c0 = g * 512
                for ci, pp in enumerate(chunks):
                    if mode == "k":
                        dst = ckvnT.t[:, c0:c0 + width]; dres = ckvn_r[g]
                    else:
                        dst = cqnT.t[:, ci, c0:c0 + width]; dres = cqn_r[g]
                    V(lambda: nc.vector.scalar_tensor_tensor(out=dst, in0=pp.t[:, W], scalar=gcol[ci], in1=rs.t[:, W],
                                                             op0=ALU.mult, op1=ALU.mult),
                      [pp.r, rs.r, gkvc.r, gqc.r], [dres])
                R = slice(64, 96)
                if mode == "k":
                    rope_tables(posb[0:1, c0:c0 + width], Ctmp.t[R, W], Stmp.t[R, W], Ctmp.r, lp, width)
                    t1 = lp["t1"]; t2 = lp["t2"]
                    V(lambda: nc.vector.tensor_tensor(out=t1.t[R, W], in0=pr.t[R, W], in1=Ctmp.t[R, W], op=ALU.mult),
                      [pr.r, Ctmp.r], [t1.r])
                    V(lambda: nc.vector.tensor_tensor(out=t2.t[R, W], in0=prr.t[R, W], in1=Stmp.t[R, W], op=ALU.mult),
                      [prr.r, Ctmp.r], [t2.r])
                    V(lambda: nc.vector.tensor_tensor(out=KT.t[R, c0:c0 + width], in0=t1.t[R, W], in1=t2.t[R, W],
                                                      op=ALU.add), [t1.r, t2.r], [ktr_r[g]])
                else:
                    rope_tables(poso[0:1, c0:c0 + width], CQ.t[R, c0:c0 + width], SQ.t[R, c0:c0 + width], cs_r[g], lp, width)

            work = [("k", g, 512) for g in range(NG)] + [("q", g, GW) for g in range(NGO)]
            latent_fe_a(*work[0]); latent_fe_b(*work[0])
            for wi, w_ in enumerate(work):
                nxt = work[wi + 1] if wi + 1 < len(work) else None
                if nxt:
                    latent_fe_a(*nxt)
                latent_mm(*w_)
                if nxt:
                    latent_fe_b(*nxt)
                latent_rest(*w_)
            T.barrier()
            if stop <= 1:
                return nc

        with ExitStack() as ap_:
            qT = sbt(ap_, "qT", [128, TOK], BF16)
            Vh = sbt(ap_, "Vh", [128, NT, 65], BF16)
            V(lambda: nc.vector.memset(Vh.t[:, :, 64:65], 1.0), [], vh_r)
            wuqb = sbt(ap_, "wuqb", [128, 2, 768], BF16, True)
            wuqr = sbt(ap_, "wuqr", [128, 2, NH, 96], BF16)
            wukvb = sbt(ap_, "wukvb", [128, 1024], BF16, True)
            load(wuqb, w_uq.rearrange("(kc p) n -> p kc n", p=128), POOL)
            load(wukvb, w_ukv, POOL)
            V(lambda: nc.vector.memset(wuqr.t[:], 0.0), [], [wuqr.r])
            wv = wuqb.t[:].rearrange("p k (h d) -> p k h d", h=NH)
            V(lambda: nc.vector.tensor_scalar(out=wuqr.t[:, :, :, 64:80], in0=wv[:, :, :, 80:96], scalar1=-1.0, scalar2=None,
                                              op0=ALU.mult), [wuqb.r, wuqr.r], [wuqr.r])
            V(lambda: nc.vector.tensor_copy(out=wuqr.t[:, :, :, 80:96], in_=wv[:, :, :, 64:80]), [wuqb.r, wuqr.r], [wuqr.r])

            PTs = [sbt(ap_, f"PT{i}", [128, 512], BF16) for i in range(4)]
            rec = [sbt(ap_, f"rec{i}", [128, 1], F32) for i in range(4)]
            ost = [sbt(ap_, f"ost{i}", [128, 64], BF16, True) for i in range(4)]
            osc = [0]
            t1q = sbt(ap_, "t1q", [128, 512], F32); t2q = sbt(ap_, "t2q", [128, 512], F32)
            set_rot([4, 5, 6, 7])
            oacc_i = 0
            ptc = 0
            for h in range(NH):
                for g in range(NG):
                    ps = pnext()
                    P(lambda: nc.tensor.matmul(ps.t[0:64, :], lhsT=wukvb.t[:, h * 128:h * 128 + 64],
                                               rhs=ckvnT.t[:, g * 512:(g + 1) * 512], start=True, stop=True),
                      [wukvb.r, ckvn_r[g]], [ps.r])
                    if g % 2 == 0:
                        V(lambda: nc.vector.tensor_copy(out=KT.t[0:64, g * 512:(g + 1) * 512], in_=ps.t[0:64, :]),
                          [ps.r], [ktn_r[g]])
                    else:
                        A(lambda: nc.scalar.copy(out=KT.t[0:64, g * 512:(g + 1) * 512], in_=ps.t[0:64, :]),
                          [ps.r], [ktn_r[g]])
                for g8 in range(NT // 8):
                    ps = pnext()
                    for k8 in range(8):
                        kt = g8 * 8 + k8
                        P(lambda: nc.tensor.matmul(ps.t[:, k8 * 64:(k8 + 1) * 64], lhsT=ckvnT.t[:, kt * 128:(kt + 1) * 128],
                                                   rhs=wukvb.t[:, h * 128 + 64:h * 128 + 128], start=True, stop=True),
                          [wukvb.r, ckvn_r[kt // 4]], [ps.r])
                    wr_ = [vh_r[2 * g8], vh_r[2 * g8 + 1]]
                    src3 = ps.t[:].rearrange("p (a d) -> p a d", a=8)
                    if g8 % 2 == 0:
                        A(lambda: nc.scalar.copy(out=Vh.t[:, g8 * 8:(g8 + 1) * 8, 0:64], in_=src3), [ps.r], wr_)
                    else:
                        V(lambda: nc.vector.tensor_copy(out=Vh.t[:, g8 * 8:(g8 + 1) * 8, 0:64], in_=src3), [ps.r], wr_)
                for g in range(NGO):
                    W = slice(0, GW); c0 = g * 512; R = slice(64, 96)
                    pa = pnext(); pr = pnext()
                    for kc in range(2):
                        P(lambda: nc.tensor.matmul(pa.t[0:96, W], lhsT=wuqb.t[:, kc, h * 96:(h + 1) * 96],
                                                   rhs=cqnT.t[:, kc, c0:c0 + GW], start=(kc == 0), stop=(kc == 1)),
                          [wuqb.r, cqn_r[g]], [pa.r])
                    for kc in range(2):
                        P(lambda: nc.tensor.matmul(pr.t[0:96, W], lhsT=wuqr.t[:, kc, h, :],
                                                   rhs=cqnT.t[:, kc, c0:c0 + GW], start=(kc == 0), stop=(kc == 1)),
                          [wuqr.r, cqn_r[g]], [pr.r])
                    V(lambda: nc.vector.tensor_copy(out=qT.t[0:64, c0:c0 + GW], in_=pa.t[0:64, W]), [pa.r], [qT_r[g]])
                    V(lambda: nc.vector.tensor_tensor(out=t1q.t[R, W], in0=pa.t[R, W], in1=CQ.t[R, c0:c0 + GW], op=ALU.mult),
                      [pa.r, cs_r[g]], [t1q.r])
                    V(lambda: nc.vector.tensor_tensor(out=t2q.t[R, W], in0=pr.t[R, W], in1=SQ.t[R, c0:c0 + GW], op=ALU.mult),
                      [pr.r, cs_r[g]], [t2q.r])
                    V(lambda: nc.vector.tensor_tensor(out=qT.t[R, c0:c0 + GW], in0=t1q.t[R, W], in1=t2q.t[R, W], op=ALU.add),
                      [t1q.r, t2q.r], [qT_r[g]])
                steps = [(m, kp) for m in range(NP) for kp in range((8 * m + 8) // 2)]
                oa_of = {}
                for m in range(NP):
                    oa_of[m] = [banks[(oacc_i * 2) % 4], banks[(oacc_i * 2 + 1) % 4]]
                    oacc_i += 1
                pt_of = {}

                def emit_qk(si):
                    m, kp = steps[si]
                    qg = qT_r[(m * 256) // 512]
                    qs = qT.t[0:96, m * 256:(m + 1) * 256]
                    ps = pnext()
                    for kl in range(2):
                        kt = 2 * kp + kl
                        P(lambda: nc.tensor.matmul(ps.t[:, kl * 256:(kl + 1) * 256], lhsT=KT.t[0:96, kt * 128:(kt + 1) * 128],
                                                   rhs=qs, start=True, stop=True),
                          [ktn_r[kt // 4], ktr_r[kt // 4], qg], [ps.r])
                    PT = PTs[si % len(PTs)]
                    pt_of[si] = PT
                    A(lambda: nc.scalar.activation(out=PT.t[:], in_=ps.t[:], func=AF.Exp, scale=SCALE), [ps.r], [PT.r])
                    for kl in range(2):
                        kt = 2 * kp + kl
                        o = kt - 8 * m
                        if o >= 0:
                            for a in range(2):
                                cs_ = kl * 256 + a * 128
                                V(lambda: nc.vector.scalar_tensor_tensor(
                                    out=PT.t[:, cs_:cs_ + 128], in0=kqb.t[:], scalar=dtab.t[:, a * 8 + o:a * 8 + o + 1],
                                    in1=PT.t[:, cs_:cs_ + 128], op0=ALU.is_le, op1=ALU.mult),
                                  [PT.r, kqb.r, dtab.r], [PT.r])

                def emit_pv(si):
                    nonlocal_osc = None
                    m, kp = steps[si]
                    nk = 8 * m + 8
                    oa = oa_of[m]
                    PT = pt_of.pop(si)
                    for kl in range(2):
                        kt = 2 * kp + kl
                        for a in range(2):
                            cs_ = kl * 256 + a * 128
                            P(lambda: nc.tensor.matmul(oa[a].t[:, 0:65], lhsT=PT.t[:, cs_:cs_ + 128], rhs=Vh.t[:, kt, :],
                                                       start=(kt == 0), stop=(kt == nk - 1)),
                              [PT.r, vh_r[kt // 4]], [oa[a].r])
                    if kp == nk // 2 - 1:
                        for a in range(2):
                            i_own = 2 * m + a
                            rc = rec[(2 * m + a) % 4]
                            V(lambda: nc.vector.reciprocal(out=rc.t[:], in_=oa[a].t[:, 64:65]), [oa[a].r], [rc.r])
                            os_ = ost[osc[0] % 4]; osc[0] += 1
                            V(lambda: nc.vector.tensor_scalar(out=os_.t[:], in0=oa[a].t[:, 0:64],
                                                              scalar1=rc.t[:, 0:1], scalar2=None, op0=ALU.mult),
                              [oa[a].r, rc.r], [os_.r])
                            T.dma(SP, os_.l, lambda: nc.sync.dma_start(out=OS[i_own * 128:(i_own + 1) * 128, h * 64:(h + 1) * 64],
                                                                       in_=os_.t[:]), reads=[os_.r])

                LA = 2
                for si in range(len(steps) + LA):
                    if si < len(steps):
                        emit_qk(si)
                    if si - LA >= 0:
                        emit_pv(si - LA)
            T.barrier()
            if stop <= 2:
                return nc
        att.close()

        pc_ = es.enter_context(ExitStack())
        set_rot(range(8))
        win_v = w_in.rearrange("(kc p) n -> p kc n", p=128)
        wsg = sbt(pc_, "wsg", [128, 8, 1024], BF16, True); load(wsg, win_v[:, :, 416:1440], POOL)
        wgl = sbt(pc_, "wgl", [128, 8, 2048], BF16, True)
        for kc in range(8):
            load_to(wgl, wgl.t[:, kc, :], win_v[:, kc, 1440:3488], POOL)
        wbm = sbt(pc_, "wbm", [128, 4, D], BF16, True); load(wbm, w_br_mla.rearrange("(kc p) n -> p kc n", p=128), POOL)
        wbs = sbt(pc_, "wbs", [128, 4, D], BF16, True); load(wbs, w_br_sgu.rearrange("(kc p) n -> p kc n", p=128), POOL)
        wo = sbt(pc_, "wo", [128, 8, D], BF16, True); load(wo, w_out.rearrange("(kc p) n -> p kc n", p=128), POOL)
        wrf = sbt(pc_, "wrf", [128, 8, 36], F32, True); load(wrf, wr.rearrange("(kc p) n -> p kc n", p=128))
        brb = sbt(pc_, "brb", [128, 36], F32, True); load(brb, br_row.partition_broadcast(128))
        gvb = sbt(pc_, "gvb", [128, 512], F32, True); load(gvb, gv_row.partition_broadcast(128))
        bvb = sbt(pc_, "bvb", [128, 512], F32, True); load(bvb, bv_row.partition_broadcast(128))
        bst = sbt(pc_, "bst", [128, 8], F32, True); load(bst, bsT)
        make_fe(pc_, 3)
        browc = sbt(pc_, "browc", [1, 3072], BF16)
        for q_ in range(2):
            bias_row(browc.t[0:1, q_ * 512:(q_ + 1) * 512], lambda kc: wsg.t[:, kc, q_ * 512:(q_ + 1) * 512], 512, wsg.r, browc.r)
        for q_ in range(4):
            bias_row(browc.t[0:1, 1024 + q_ * 512:1024 + (q_ + 1) * 512], lambda kc: wgl.t[:, kc, q_ * 512:(q_ + 1) * 512], 512,
                     wgl.r, browc.r)
        wsT = sbt(pc_, "wsT", [128, 8, 128], BF16)
        with ExitStack() as tmp_:
            g2rb = sbt(tmp_, "g2rb", [128, D], F32, True); load(g2rb, g2row.partition_broadcast(128))
            V(lambda: nc.vector.scalar_tensor_tensor(out=s2b.t[:], in0=s2b.t[:], scalar=1.0, in1=g2rb.t[:], op0=ALU.add,
                                                     op1=ALU.mult), [s2b.r, g2rb.r], [s2b.r])
            wsf = sbt(tmp_, "wsf", [128, 8, 128], F32, True); load(wsf, w_s.rearrange("g t s -> t g s"))
            trl = sbt(tmp_, "trl", [128, 128], F32, True); load(trl, c_tril)
            wsm = sbt(tmp_, "wsm", [128, 8, 128], BF16)
            V(lambda: nc.vector.tensor_tensor(out=wsm.t[:], in0=wsf.t[:], in1=trl.t[:].unsqueeze(1).to_broadcast([128, 8, 128]),
                                              op=ALU.mult), [wsf.r, trl.r], [wsm.r])
            tp = pnext(); tpb = tp.t[:].bitcast(BF16)
            for g in range(8):
                P(lambda: nc.tensor.transpose(out=tpb[:, g * 128:(g + 1) * 128], in_=wsm.t[:, g, :], identity=identb.t[:]),
                  [wsm.r, identb.r], [tp.r])
            V(lambda: nc.vector.tensor_copy(out=wsT.t[:].rearrange("p g t -> p (g t)"), in_=tpb[:, :]), [tp.r], [wsT.r])
            T.barrier()


        with ExitStack() as cw:
            h1c = [sbt(cw, f"h1c{i}", [128, 8, 128], BF16) for i in range(1)]
            uv = sbt(cw, "uv", [128, 1024], F32)
            x2 = sbt(cw, "x2t", [128, 1024], F32)
            inner = sbt(cw, "inner", [128, 1024], F32)
            gel = sbt(cw, "gel", [128, 512], F32)
            otile = [sbt(cw, f"otile{i}", [128, 512], BF16, True) for i in range(2)]
            sgoB = [sbt(cw, f"sgo{i}", [128, 512], BF16) for i in range(1)]
            lst = sbt(cw, "lst", [128, 8], F32)
            sigB = [sbt(cw, f"sig{i}", [128, 2048], BF16) for i in range(2)]
            OT = sbt(cw, "OT", [128, 4, 128], BF16)
            SGT = sbt(cw, "SGT", [128, 4, 128], BF16)
            m1 = sbt(cw, "m1", [128, D], F32); m2 = sbt(cw, "m2", [128, D], F32)
            mrg = sbt(cw, "mrg", [128, D], BF16)
            mT = sbt(cw, "mT", [128, 8, 128], BF16)
            x1 = [sbt(cw, f"x1_{i}", [128, D], F32, True) for i in range(2)]
            x1n = m1
            h2T = sbt(cw, "h2T", [128, 8, 128], F32)
            h2f = m2
            h2p = [sbt(cw, f"h2p{i}", [128, D], BF16, True) for i in range(1)]
            st2 = sbt(cw, "st2", [128, 4], F32)
            lgall = sbt(cw, "lgall", [128, NOWN, 36], F32)
            rtb = sbt(cw, "rtb", [128, 8, NOWN], F32)
            s1out = {}

            vnbB = [sbt(cw, f"vnbB{i}", [128, 512], BF16) for i in range(2)]
            gubB = [sbt(cw, f"gubB{i}", [128, 512], BF16) for i in range(2)]
            fe_st = {}

            def S1(i):
                h1 = h1c[0]; sig = sigB[i % 2]; vnb_ = vnbB[i % 2]; gub = gubB[i % 2]
                if i not in fe_st:
                    fe_st[i] = fe_a([xo[i * 128:(i + 1) * 128, :]])
                xt = fe_b(fe_st.pop(i), h1.t, h1.r, [0])[0]
                s1out[i] = (xt, vnb_, gub, sig)
                load(otile[i % 2], OS[i * 128:(i + 1) * 128, :])
                pu = pnext(); pv = pnext()
                for hh, pp in enumerate((pu, pv)):
                    for kc in range(8):
                        P(lambda: nc.tensor.matmul(pp.t[:], lhsT=h1.t[:, kc, :], rhs=wsg.t[:, kc, hh * 512:(hh + 1) * 512],
                                                   start=(kc == 0), stop=False), [h1.r, wsg.r], [pp.r])
                    P(lambda: nc.tensor.matmul(pp.t[:], lhsT=onesb.t[0:1, :], rhs=browc.t[0:1, hh * 512:(hh + 1) * 512],
                                               start=False, stop=True), [onesb.r, browc.r], [pp.r])
                pg = [pnext() for _ in range(4)]
                for q4 in range(4):
                    for kc in range(8):
                        P(lambda: nc.tensor.matmul(pg[q4].t[:], lhsT=h1.t[:, kc, :], rhs=wgl.t[:, kc, q4 * 512:(q4 + 1) * 512],
                                                   start=(kc == 0), stop=False), [h1.r, wgl.r], [pg[q4].r])
                    P(lambda: nc.tensor.matmul(pg[q4].t[:], lhsT=onesb.t[0:1, :], rhs=browc.t[0:1, 1024 + q4 * 512:1024 + (q4 + 1) * 512],
                                               start=False, stop=True), [onesb.r, browc.r], [pg[q4].r])
                for hh, pp in enumerate((pu, pv)):
                    cs_ = slice(hh * 512, (hh + 1) * 512)
                    A(lambda: nc.scalar.copy(out=uv.t[:, cs_], in_=pp.t[:]), [pp.r], [uv.r])
                    A(lambda: nc.scalar.activation(out=x2.t[:, cs_], in_=pp.t[:], func=AF.Square), [pp.r], [x2.r])
                G(lambda: nc.gpsimd.tensor_scalar(out=x2.t[:], in0=x2.t[:], scalar1=0.044715, scalar2=1.0, op0=ALU.mult,
                                                  op1=ALU.add), [x2.r], [x2.r])
                G(lambda: nc.gpsimd.tensor_tensor(out=inner.t[:], in0=x2.t[:], in1=uv.t[:], op=ALU.mult), [x2.r, uv.r], [inner.r])
                A(lambda: nc.scalar.activation(out=inner.t[:], in_=inner.t[:], func=AF.Sigmoid, scale=GELU_C), [inner.r], [inner.r])
                for q4 in range(4):
                    A(lambda: nc.scalar.activation(out=sig.t[:, q4 * 512:(q4 + 1) * 512], in_=pg[q4].t[:], func=AF.Sigmoid),
                      [pg[q4].r], [sig.r])
                if i + 1 < NOWN:
                    fe_st[i + 1] = fe_a([xo[(i + 1) * 128:(i + 2) * 128, :]])
                G(lambda: nc.gpsimd.tensor_tensor(out=gel.t[:, 0:512], in0=inner.t[:, 512:1024], in1=uv.t[:, 512:1024], op=ALU.mult),
                  [inner.r, uv.r], [gel.r])
                G(lambda: nc.gpsimd.tensor_tensor(out=gub.t[:], in0=inner.t[:, 0:512], in1=uv.t[:, 0:512], op=ALU.mult),
                  [inner.r, uv.r], [gub.r])
                gv_ = gel.t[:, 0:512]
                V(lambda: nc.vector.tensor_reduce(out=lst.t[:, 0:1], in_=gv_, axis=AX.X, op=ALU.add), [gel.r], [lst.r])
                V(lambda: nc.vector.tensor_scalar(out=lst.t[:, 1:2], in0=lst.t[:, 0:1], scalar1=1.0 / 512, scalar2=None,
                                                  op0=ALU.mult), [lst.r], [lst.r])
                V(lambda: nc.vector.tensor_scalar(out=x2.t[:, 0:512], in0=gv_, scalar1=lst.t[:, 1:2], scalar2=None,
                                                  op0=ALU.subtract), [gel.r, lst.r], [x2.r])
                A(lambda: nc.scalar.activation(out=x2.t[:, 512:1024], in_=x2.t[:, 0:512], func=AF.Square, accum_out=lst.t[:, 2:3]),
                  [x2.r], [x2.r, lst.r])
                V(lambda: nc.vector.tensor_scalar(out=lst.t[:, 3:4], in0=lst.t[:, 2:3], scalar1=1.0 / 512, scalar2=EPS,
                                                  op0=ALU.mult, op1=ALU.add), [lst.r], [lst.r])
                A(lambda: nc.scalar.activation(out=lst.t[:, 4:5], in_=lst.t[:, 3:4], func=AF.Sqrt), [lst.r], [lst.r])
                V(lambda: nc.vector.reciprocal(out=lst.t[:, 5:6], in_=lst.t[:, 4:5]), [lst.r], [lst.r])
                V(lambda: nc.vector.scalar_tensor_tensor(out=x2.t[:, 0:512], in0=x2.t[:, 0:512], scalar=lst.t[:, 5:6],
                                                         in1=gvb.t[:], op0=ALU.mult, op1=ALU.mult), [x2.r, lst.r, gvb.r], [x2.r])
                V(lambda: nc.vector.tensor_tensor(out=vnb_.t[:], in0=x2.t[:, 0:512], in1=bvb.t[:], op=ALU.add),
                  [x2.r, bvb.r], [vnb_.r])

            def S2(i):
                xt, vnb_, gub, sig = s1out.pop(i)
                sgo = sgoB[0]
                psg = pnext()
                for g in range(8):
                    P(lambda: nc.tensor.matmul(psg.t[:, g * 64:(g + 1) * 64], lhsT=wsT.t[:, g, :], rhs=vnb_.t[:, g * 64:(g + 1) * 64],
                                               start=True, stop=True), [wsT.r, vnb_.r], [psg.r])
                ot_ = otile[i % 2]
                tp = pnext(); tpb = tp.t[:].bitcast(BF16)
                for kc in range(4):
                    P(lambda: nc.tensor.transpose(out=tpb[:, kc * 128:(kc + 1) * 128], in_=ot_.t[:, kc * 128:(kc + 1) * 128],
                                                  identity=identb.t[:]), [ot_.r, identb.r], [tp.r])
                A(lambda: nc.scalar.copy(out=OT.t[:].rearrange("p k t -> p (k t)"), in_=tpb[:, 0:512]), [tp.r], [OT.r])
                V(lambda: nc.vector.tensor_tensor(out=m2.t[:, 0:512].rearrange("p (g c) -> p g c", g=8),
                                                  in0=psg.t[:].rearrange("p (g c) -> p g c", g=8),
                                                  in1=bst.t[:].unsqueeze(2).to_broadcast([128, 8, 64]), op=ALU.add),
                  [psg.r, bst.r], [m2.r])
                V(lambda: nc.vector.tensor_tensor(out=sgo.t[:], in0=m2.t[:, 0:512], in1=gub.t[:], op=ALU.mult),
                  [m2.r, gub.r], [sgo.r])
                for hh in range(2):
                    ps = pnext()
                    for kc in range(4):
                        P(lambda: nc.tensor.matmul(ps.t[:], lhsT=OT.t[:, kc, :], rhs=wbm.t[:, kc, hh * 512:(hh + 1) * 512],
                                                   start=(kc == 0), stop=(kc == 3)), [OT.r, wbm.r], [ps.r])
                    V(lambda: nc.vector.tensor_tensor(out=m1.t[:, hh * 512:(hh + 1) * 512], in0=ps.t[:],
                                                      in1=sig.t[:, hh * 512:(hh + 1) * 512], op=ALU.mult), [ps.r, sig.r], [m1.r])
                tp2 = pnext(); tpb2 = tp2.t[:].bitcast(BF16)
                for kc in range(4):
                    P(lambda: nc.tensor.transpose(out=tpb2[:, kc * 128:(kc + 1) * 128], in_=sgo.t[:, kc * 128:(kc + 1) * 128],
                                                  identity=identb.t[:]), [sgo.r, identb.r], [tp2.r])
                A(lambda: nc.scalar.copy(out=SGT.t[:].rearrange("p k t -> p (k t)"), in_=tpb2[:, 0:512]), [tp2.r], [SGT.r])
                for hh in range(2):
                    ps = pnext()
                    for kc in range(4):
                        P(lambda: nc.tensor.matmul(ps.t[:], lhsT=SGT.t[:, kc, :], rhs=wbs.t[:, kc, hh * 512:(hh + 1) * 512],
                                                   start=(kc == 0), stop=(kc == 3)), [SGT.r, wbs.r], [ps.r])
                    V(lambda: nc.vector.tensor_tensor(out=m2.t[:, hh * 512:(hh + 1) * 512], in0=ps.t[:],
                                                      in1=sig.t[:, 1024 + hh * 512:1024 + (hh + 1) * 512], op=ALU.mult),
                      [ps.r, sig.r], [m2.r])
                    V(lambda: nc.vector.tensor_tensor(out=mrg.t[:, hh * 512:(hh + 1) * 512], in0=m1.t[:, hh * 512:(hh + 1) * 512],
                                                      in1=m2.t[:, hh * 512:(hh + 1) * 512], op=ALU.add), [m1.r, m2.r], [mrg.r])
                tp = pnext(); tpb = tp.t[:].bitcast(BF16)
                for kc in range(8):
                    P(lambda: nc.tensor.transpose(out=tpb[:, kc * 128:(kc + 1) * 128], in_=mrg.t[:, kc * 128:(kc + 1) * 128],
                                                  identity=identb.t[:]), [mrg.r, identb.r], [tp.r])
                A(lambda: nc.scalar.copy(out=mT.t[:].rearrange("p k t -> p (k t)"), in_=tpb[:, :]), [tp.r], [mT.r])
                xx = x1[i % 2]
                for hh in range(2):
                    ps = pnext()
                    for kc in range(8):
                        P(lambda: nc.tensor.matmul(ps.t[:], lhsT=mT.t[:, kc, :], rhs=wo.t[:, kc, hh * 512:(hh + 1) * 512],
                                                   start=(kc == 0), stop=(kc == 7)), [mT.r, wo.r], [ps.r])
                    cs_ = slice(hh * 512, (hh + 1) * 512)
                    V(lambda: nc.vector.tensor_tensor(out=m1.t[:, cs_], in0=ps.t[:], in1=gate1b.t[:, cs_], op=ALU.mult),
                      [ps.r, gate1b.r], [m1.r])
                V(lambda: nc.vector.tensor_tensor(out=xx.t[:], in0=m1.t[:], in1=xt.t[:], op=ALU.add), [m1.r, xt.r], [xx.r])
                T.dma(SP, xx.l, lambda: nc.sync.dma_start(out=X1[i * 128:(i + 1) * 128, :], in_=xx.t[:]), reads=[xx.r])

            def S3(i):
                xx = x1[i % 2]
                junk = FE["junk"]
                A(lambda: nc.scalar.activation(out=junk.t[:], in_=xx.t[:], func=AF.Square, accum_out=st2.t[:, 0:1]),
                  [xx.r], [junk.r, st2.r])
                V(lambda: nc.vector.tensor_scalar(out=st2.t[:, 1:2], in0=st2.t[:, 0:1], scalar1=1.0 / D, scalar2=EPS,
                                                  op0=ALU.mult, op1=ALU.add), [st2.r], [st2.r])
                A(lambda: nc.scalar.activation(out=st2.t[:, 2:3], in_=st2.t[:, 1:2], func=AF.Sqrt), [st2.r], [st2.r])
                V(lambda: nc.vector.reciprocal(out=st2.t[:, 3:4], in_=st2.t[:, 2:3]), [st2.r], [st2.r])
                V(lambda: nc.vector.tensor_scalar(out=x1n.t[:], in0=xx.t[:], scalar1=st2.t[:, 3:4], scalar2=None, op0=ALU.mult),
                  [xx.r, st2.r], [x1n.r])
                G(lambda: nc.gpsimd.tensor_tensor(out=h2f.t[:], in0=x1n.t[:], in1=s2b.t[:], op=ALU.mult), [x1n.r, s2b.r], [h2f.r])
                hp = h2p[0]
                G(lambda: nc.gpsimd.tensor_tensor(out=hp.t[:].rearrange("t (kc p) -> t p kc", kc=8),
                                                  in0=h2f.t[:].rearrange("t (p kc) -> t p kc", kc=8),
                                                  in1=sh2b.t[:].rearrange("t (p kc) -> t p kc", kc=8), op=ALU.add),
                  [h2f.r, sh2b.r], [hp.r])
                T.dma(SP, hp.l, lambda: nc.sync.dma_start(out=H2[i * 128:(i + 1) * 128, :], in_=hp.t[:]), reads=[hp.r])
                tA = pnext(); tB = pnext()
                for kc in range(8):
                    tt = tA if kc < 4 else tB
                    P(lambda: nc.tensor.transpose(out=tt.t[:, (kc % 4) * 128:(kc % 4 + 1) * 128], in_=x1n.t[:, kc * 128:(kc + 1) * 128],
                                                  identity=identf.t[:]), [x1n.r, identf.r], [tt.r])
                for kc in range(8):
                    tt = tA if kc < 4 else tB
                    V(lambda: nc.vector.tensor_scalar(out=h2T.t[:, kc, :], in0=tt.t[:, (kc % 4) * 128:(kc % 4 + 1) * 128],
                                                      scalar1=s2c.t[:, kc:kc + 1], scalar2=sh2(kc), op0=ALU.mult, op1=ALU.add),
                      [tt.r, s2c.r, modT.r], [h2T.r])
                pl = pnext()
                for kc in range(8):
                    P(lambda: nc.tensor.matmul(pl.t[:, 0:36], lhsT=h2T.t[:, kc, :], rhs=wrf.t[:, kc, :], start=(kc == 0), stop=(kc == 7)),
                      [h2T.r, wrf.r], [pl.r])
                V(lambda: nc.vector.tensor_tensor(out=lgall.t[:, i, :], in0=pl.t[:, 0:36], in1=brb.t[:], op=ALU.add), [pl.r, brb.r], [lgall.r])

            for step in range(NOWN + 2):
                if step < NOWN:
                    S1(step)
                if 0 <= step - 1 < NOWN:
                    S2(step - 1)
                if 0 <= step - 2 < NOWN:
                    S3(step - 2)

            TT = NOWN
            Lg = lgall.t[:, :, 0:4]
            Le4 = lgall.t[:, :, 4:36].rearrange("p t (g e) -> p t g e", g=4)
            gm = rtb.t[:, 0, :]; se = rtb.t[:, 1, :]; pgr = rtb.t[:, 2, :]; mv1 = rtb.t[:, 3, :]; mv2 = rtb.t[:, 4, :]
            dd_ = rtb.t[:, 5, :]; ee_ = rtb.t[:, 6, :]; rr_ = rtb.t[:, 7, :]
            ex = uv.t[:, 0:TT * 4].rearrange("p (t g) -> p t g", g=4)
            pen3 = x2.t[:, 0:TT * 4].rearrange("p (t g) -> p t g", g=4)
            Mx3 = inner.t[:, 0:TT * 32].rearrange("p (t e) -> p t e", e=32)
            Mx4 = inner.t[:, 0:TT * 32].rearrange("p (t g e) -> p t g e", g=4, e=8)
            My3 = m1.t[:, 0:TT * 32].rearrange("p (t e) -> p t e", e=32)
            bc3 = lambda ap2, n: ap2.unsqueeze(2).to_broadcast([128, TT, n])
            V(lambda: nc.vector.tensor_reduce(out=gm, in_=Lg, axis=AX.X, op=ALU.max), [lgall.r], [rtb.r])
            V(lambda: nc.vector.tensor_tensor(out=ex, in0=Lg, in1=bc3(gm, 4), op=ALU.subtract), [lgall.r, rtb.r], [uv.r])
            A(lambda: nc.scalar.activation(out=ex, in_=ex, func=AF.Exp), [uv.r], [uv.r])
            V(lambda: nc.vector.tensor_reduce(out=se, in_=ex, axis=AX.X, op=ALU.add), [uv.r], [rtb.r])
            V(lambda: nc.vector.reciprocal(out=pgr, in_=se), [rtb.r], [rtb.r])
            V(lambda: nc.vector.tensor_tensor(out=pen3, in0=Lg, in1=bc3(gm, 4), op=ALU.is_equal), [lgall.r, rtb.r], [x2.r])
            V(lambda: nc.vector.tensor_scalar(out=pen3, in0=pen3, scalar1=-1.0, scalar2=1e30, op0=ALU.add, op1=ALU.mult),
              [x2.r], [x2.r])
            V(lambda: nc.vector.tensor_tensor(out=Mx4, in0=Le4, in1=pen3.unsqueeze(3).to_broadcast([128, TT, 4, 8]), op=ALU.add),
              [lgall.r, x2.r], [inner.r])
            V(lambda: nc.vector.tensor_reduce(out=mv1, in_=Mx3, axis=AX.X, op=ALU.max), [inner.r], [rtb.r])
            V(lambda: nc.vector.tensor_tensor(out=A0all.t[:], in0=Mx3, in1=bc3(mv1, 32), op=ALU.is_equal), [inner.r, rtb.r], [A0all.r])
            V(lambda: nc.vector.scalar_tensor_tensor(out=My3, in0=A0all.t[:], scalar=-1e30, in1=Mx3, op0=ALU.mult, op1=ALU.add),
              [A0all.r, inner.r], [m1.r])
            V(lambda: nc.vector.tensor_reduce(out=mv2, in_=My3, axis=AX.X, op=ALU.max), [m1.r], [rtb.r])
            V(lambda: nc.vector.tensor_tensor(out=A1all.t[:], in0=My3, in1=bc3(mv2, 32), op=ALU.is_equal), [m1.r, rtb.r], [A1all.r])
            V(lambda: nc.vector.tensor_tensor(out=Aall.t[:], in0=A0all.t[:], in1=A1all.t[:], op=ALU.add), [A0all.r, A1all.r], [Aall.r])
            V(lambda: nc.vector.tensor_tensor(out=dd_, in0=mv2, in1=mv1, op=ALU.subtract), [rtb.r], [rtb.r])
            A(lambda: nc.scalar.activation(out=ee_, in_=dd_, func=AF.Exp), [rtb.r], [rtb.r])
            V(lambda: nc.vector.tensor_scalar(out=ee_, in0=ee_, scalar1=1.0, scalar2=None, op0=ALU.add), [rtb.r], [rtb.r])
            V(lambda: nc.vector.reciprocal(out=rr_, in_=ee_), [rtb.r], [rtb.r])
            V(lambda: nc.vector.tensor_tensor(out=wts.t[:, :, 0], in0=rr_, in1=pgr, op=ALU.mult), [rtb.r], [wts.r])
            V(lambda: nc.vector.tensor_tensor(out=wts.t[:, :, 1], in0=pgr, in1=wts.t[:, :, 0], op=ALU.subtract), [rtb.r, wts.r], [wts.r])
            T.barrier()
            if stop <= 3:
                return nc
        pc_.close()

        NA = NOWN * 32
        REG_SLOT = nc.gpsimd.to_reg(NSLOT - 1)
        REG_W = nc.gpsimd.to_reg(NEXP * 128 - 1)
        d0i = sbt(es, "d0i", [128, NOWN], I32); d1i = sbt(es, "d1i", [128, NOWN], I32)
        idxw = sbt(es, "idxw", [128, NBLK], I32)
        with ExitStack() as dp:
            set_rot(range(8))
            Ab_t = Aall.t[:].rearrange("p t e -> p (t e)")
            lsf = sbt(dp, "lsf", [128, 128], F32, True); load(lsf, c_lstrict)
            lsb = sbt(dp, "lsb", [128, 128], BF16)
            thr = sbt(dp, "thr", [128, NBLK], F32, True); load(thr, c_thr)
            pidx = sbt(dp, "pidx", [128, 1], F32, True); load(pidx, c_pidx)
            V(lambda: nc.vector.tensor_copy(out=lsb.t[:], in_=lsf.t[:]), [lsf.r], [lsb.r])
            tsum = sbt(dp, "tsum", [128, NOWN, 32], F32)
            rloc = sbt(dp, "rloc", [128, NOWN, 32], F32)
            for c0 in range(0, NA, 512):
                w_ = min(512, NA - c0)
                ps = pnext()
                P(lambda: nc.tensor.matmul(ps.t[:, 0:w_], lhsT=onesb.t[:], rhs=Ab_t[:, c0:c0 + w_], start=True, stop=True),
                  [onesb.r, Aall.r], [ps.r])
                V(lambda: nc.vector.tensor_copy(out=tsum.t[:].rearrange("p t e -> p (t e)")[:, c0:c0 + w_], in_=ps.t[:, 0:w_]),
                  [ps.r], [tsum.r])
                ps2 = pnext()
                P(lambda: nc.tensor.matmul(ps2.t[:, 0:w_], lhsT=lsb.t[:], rhs=Ab_t[:, c0:c0 + w_], start=True, stop=True),
                  [lsb.r, Aall.r], [ps2.r])
                V(lambda: nc.vector.tensor_copy(out=rloc.t[:].rearrange("p t e -> p (t e)")[:, c0:c0 + w_], in_=ps2.t[:, 0:w_]),
                  [ps2.r], [rloc.r])
            cum = sbt(dp, "cum", [128, NOWN + 1, 32], F32)
            V(lambda: nc.vector.memset(cum.t[:, 0, :], 0.0), [], [cum.r])
            for t in range(NOWN):
                V(lambda: nc.vector.tensor_tensor(out=cum.t[:, t + 1, :], in0=cum.t[:, t, :], in1=tsum.t[:, t, :], op=ALU.add),
                  [cum.r, tsum.r], [cum.r])
            ci = sbt(dp, "ci", [128, 32], I32)
            pad = sbt(dp, "pad", [128, 32], F32)
            pend = sbt(dp, "pend", [128, 32], F32)
            pst = sbt(dp, "pst", [128, 32], F32)
            V(lambda: nc.vector.tensor_scalar(out=pad.t[:], in0=cum.t[:, NOWN, :], scalar1=float(BLK - 1), scalar2=None,
                                              op0=ALU.add), [cum.r], [pad.r])
            V(lambda: nc.vector.tensor_copy(out=ci.t[:], in_=pad.t[:]), [pad.r], [ci.r])
            V(lambda: nc.vector.tensor_scalar(out=ci.t[:], in0=ci.t[:], scalar1=8, scalar2=8, op0=ALU.arith_shift_right,
                                              op1=ALU.logical_shift_left), [ci.r], [ci.r])
            V(lambda: nc.vector.tensor_copy(out=pad.t[:], in_=ci.t[:]), [ci.r], [pad.r])
            V(lambda: nc.vector.tensor_tensor_scan(out=pend.t[:], data0=onesf.t[:, 0:32], data1=pad.t[:], initial=0.0,
                                                   op0=ALU.mult, op1=ALU.add), [onesf.r, pad.r], [pend.r])
            V(lambda: nc.vector.tensor_tensor(out=pst.t[:], in0=pend.t[:], in1=pad.t[:], op=ALU.subtract), [pend.r, pad.r], [pst.r])
            V(lambda: nc.vector.tensor_tensor(out=rloc.t[:], in0=rloc.t[:], in1=cum.t[:, 0:NOWN, :], op=ALU.add),
              [rloc.r, cum.r], [rloc.r])
            V(lambda: nc.vector.tensor_tensor(out=rloc.t[:], in0=rloc.t[:], in1=pst.t[:].unsqueeze(1).to_broadcast([128, NOWN, 32]),
                                              op=ALU.add), [rloc.r, pst.r], [rloc.r])
            df = sbt(dp, "df", [128, NOWN], F32)
            for Ak, dk in ((A0all, d0i), (A1all, d1i)):
                V(lambda: nc.vector.tensor_tensor(out=tsum.t[:], in0=Ak.t[:], in1=rloc.t[:], op=ALU.mult), [Ak.r, rloc.r, tsum.r], [tsum.r])
                V(lambda: nc.vector.tensor_reduce(out=df.t[:], in_=tsum.t[:], axis=AX.X, op=ALU.add), [tsum.r], [df.r])
                V(lambda: nc.vector.tensor_copy(out=dk.t[:], in_=df.t[:]), [df.r], [dk.r])
            cmp_ = sbt(dp, "cmp", [128, NBLK, 32], F32)
            bef = sbt(dp, "bef", [128, NBLK], F32)
            V(lambda: nc.vector.tensor_tensor(out=cmp_.t[:], in0=pend.t[:].unsqueeze(1).to_broadcast([128, NBLK, 32]),
                                              in1=thr.t[:].unsqueeze(2).to_broadcast([128, NBLK, 32]), op=ALU.is_le),
              [pend.r, thr.r], [cmp_.r])
            V(lambda: nc.vector.tensor_reduce(out=bef.t[:], in_=cmp_.t[:], axis=AX.X, op=ALU.add), [cmp_.r], [bef.r])
            V(lambda: nc.vector.tensor_scalar(out=bef.t[:], in0=bef.t[:], scalar1=float(NEXP - 1), scalar2=128.0, op0=ALU.min,
                                              op1=ALU.mult), [bef.r], [bef.r])
            V(lambda: nc.vector.tensor_scalar(out=bef.t[:], in0=bef.t[:], scalar1=pidx.t[:, 0:1], scalar2=None, op0=ALU.add),
              [bef.r, pidx.r], [bef.r])
            skp = sbt(dp, "skp", [128, NBLK], F32)
            V(lambda: nc.vector.tensor_scalar(out=skp.t[:], in0=thr.t[:], scalar1=pend.t[:, 31:32], scalar2=1.0e6, op0=ALU.is_ge,
                                              op1=ALU.mult), [thr.r, pend.r], [skp.r])
            V(lambda: nc.vector.tensor_tensor(out=bef.t[:], in0=bef.t[:], in1=skp.t[:], op=ALU.add), [bef.r, skp.r], [bef.r])
            V(lambda: nc.vector.tensor_copy(out=idxw.t[:], in_=bef.t[:]), [bef.r], [idxw.r])
            hb = [sbt(dp, f"hb{i}", [128, D], BF16, True) for i in range(3)]
            xs_res = Res("XS")
            for t in range(NOWN):
                b_ = hb[t % 3]
                load(b_, H2[t * 128:(t + 1) * 128, :])
                for dk in (d0i, d1i):
                    T.dma(POOL, b_.l, lambda: nc.gpsimd.indirect_dma_start(
                        out=XS, out_offset=bass.IndirectOffsetOnAxis(ap=dk.t[:, t:t + 1], axis=0), in_=b_.t[:], in_offset=None,
                        bounds_check=REG_SLOT, oob_is_err=False),
                        reads=[b_.r, dk.r])
            T.barrier()
            if stop <= 4:
                return nc

        with ExitStack() as dd:
            set_rot(range(8))
            stg = [[sbt(dd, f"stg{i}_{k}", [128, 4096], F32, True) for k in range(3)] for i in range(2)]
            w1b = [sbt(dd, f"w1b{i}", [128, 8, 4, 128], BF16) for i in range(2)]
            w3b = [sbt(dd, f"w3b{i}", [128, 8, 4, 128], BF16) for i in range(2)]
            w2b = [sbt(dd, f"w2b{i}", [128, 4, D], BF16) for i in range(2)]
            xbk = [sbt(dd, f"xbk{i}", [128, 2, D], BF16, True) for i in range(2)]
            xTb = [sbt(dd, f"xTb{i}", [128, 8, 256], BF16) for i in range(1)]
            sil = sbt(dd, "sil", [128, 256], F32)
            gTb = [sbt(dd, f"gTb{i}", [128, 4, 256], BF16) for i in range(2)]
            ysb = [sbt(dd, f"ysb{i}", [128, D], F32, True) for i in range(2)]
            yc = 0

            def issue_w(j):
                s_ = stg[j % 2]
                for k, wsrc in enumerate((w1, w3, w2)):
                    T.dma(POOL, s_[k].l, lambda: nc.gpsimd.indirect_dma_start(
                        out=s_[k].t[:], out_offset=None, in_=wsrc,
                        in_offset=bass.IndirectOffsetOnAxis(ap=idxw.t[:, j:j + 1], axis=0),
                        bounds_check=REG_W, oob_is_err=False),
                        reads=[idxw.r], writes=[s_[k].r])

            def issue_x(j):
                load(xbk[j % 2], XS[j * BLK:(j + 1) * BLK, :].rearrange("(a p) d -> p a d", p=128))

            issue_w(0)
            issue_x(0)
            for j in range(NBLK):
                if j + 1 < NBLK:
                    issue_w(j + 1)
                    issue_x(j + 1)
                s_ = stg[j % 2]; b1 = w1b[j % 2]; b3 = w3b[j % 2]; b2 = w2b[j % 2]
                V(lambda: nc.vector.tensor_copy(out=b1.t[:], in_=s_[0].t[:].rearrange("p (kc m fc) -> p kc fc m", kc=8, m=128, fc=4)),
                  [s_[0].r], [b1.r])
                V(lambda: nc.vector.tensor_copy(out=b3.t[:], in_=s_[1].t[:].rearrange("p (kc m fc) -> p kc fc m", kc=8, m=128, fc=4)),
                  [s_[1].r], [b3.r])
                A(lambda: nc.scalar.copy(out=b2.t[:].rearrange("p f n -> p (f n)"), in_=s_[2].t[:]), [s_[2].r], [b2.r])
                xk = xbk[j % 2]; xT_ = xTb[0]; gT_ = gTb[j % 2]
                for st_ in range(2):
                    tp = pnext(); tpb = tp.t[:].bitcast(BF16)
                    for kc in range(8):
                        P(lambda: nc.tensor.transpose(out=tpb[:, kc * 128:(kc + 1) * 128], in_=xk.t[:, st_, kc * 128:(kc + 1) * 128],
                                                      identity=identb.t[:]), [xk.r, identb.r], [tp.r])
                    cp = (lambda f: A(f, [tp.r], [xT_.r])) if st_ == 0 else (lambda f: V(f, [tp.r], [xT_.r]))
                    if st_ == 0:
                        A(lambda: nc.scalar.copy(out=xT_.t[:, :, 0:128], in_=tpb.rearrange("p (k t) -> p k t", k=8)), [tp.r], [xT_.r])
                    else:
                        V(lambda: nc.vector.tensor_copy(out=xT_.t[:, :, 128:256], in_=tpb.rearrange("p (k t) -> p k t", k=8)),
                          [tp.r], [xT_.r])
                for fc in range(4):
                    pa = pnext(); pb_ = pnext()
                    for kc in range(8):
                        P(lambda: nc.tensor.matmul(pa.t[:, 0:256], lhsT=b1.t[:, kc, fc, :], rhs=xT_.t[:, kc, :], start=(kc == 0),
                                                   stop=(kc == 7)), [b1.r, xT_.r], [pa.r])
                    for kc in range(8):
                        P(lambda: nc.tensor.matmul(pb_.t[:, 0:256], lhsT=b3.t[:, kc, fc, :], rhs=xT_.t[:, kc, :], start=(kc == 0),
                                                   stop=(kc == 7)), [b3.r, xT_.r], [pb_.r])
                    A(lambda: nc.scalar.activation(out=sil.t[:], in_=pa.t[:, 0:256], func=AF.Silu), [pa.r], [sil.r])
                    V(lambda: nc.vector.tensor_tensor(out=gT_.t[:, fc, :], in0=pb_.t[:, 0:256], in1=sil.t[:], op=ALU.mult),
                      [pb_.r, sil.r], [gT_.r])
                for st_ in range(2):
                    y_ = ysb[yc % 2]; yc += 1
                    for hh in range(2):
                        ps = pnext()
                        for fc in range(4):
                            P(lambda: nc.tensor.matmul(ps.t[:], lhsT=gT_.t[:, fc, st_ * 128:(st_ + 1) * 128],
                                                       rhs=b2.t[:, fc, hh * 512:(hh + 1) * 512], start=(fc == 0), stop=(fc == 3)),
                              [gT_.r, b2.r], [ps.r])
                        if hh == 0:
                            A(lambda: nc.scalar.copy(out=y_.t[:, 0:512], in_=ps.t[:]), [ps.r], [y_.r])
                        else:
                            V(lambda: nc.vector.tensor_copy(out=y_.t[:, 512:1024], in_=ps.t[:]), [ps.r], [y_.r])
                    r0 = j * BLK + st_ * 128
                    T.dma(SP, y_.l, lambda: nc.sync.dma_start(out=YS[r0:r0 + 128, :], in_=y_.t[:]), reads=[y_.r])
            T.barrier()
            if stop <= 5:
                return nc

        with ExitStack() as ee:
            set_rot(range(8))
            gfb = sbt(ee, "gfb", [128, D], F32, True); load(gfb, gf_row.partition_broadcast(128))
            y0 = [sbt(ee, f"y0_{i}", [128, D], F32, True) for i in range(2)]
            y1 = [sbt(ee, f"y1_{i}", [128, D], F32, True) for i in range(2)]
            xr = [sbt(ee, f"xr{i}", [128, D], F32, True) for i in range(2)]
            acc = sbt(ee, "acc", [128, D], F32)
            jk = sbt(ee, "jk", [128, D], F32)
            ob = [sbt(ee, f"ob{i}", [128, D], F32, True) for i in range(2)]
            st3 = sbt(ee, "st3", [128, 4], F32)
            def issue_e(t):
                a0 = y0[t % 2]; a1 = y1[t % 2]; xx = xr[t % 2]
                T.dma(POOL, a0.l, lambda: nc.gpsimd.indirect_dma_start(
                    out=a0.t[:], out_offset=None, in_=YS, in_offset=bass.IndirectOffsetOnAxis(ap=d0i.t[:, t:t + 1], axis=0),
                    bounds_check=REG_SLOT, oob_is_err=False),
                    reads=[d0i.r], writes=[a0.r])
                T.dma(POOL, a1.l, lambda: nc.gpsimd.indirect_dma_start(
                    out=a1.t[:], out_offset=None, in_=YS, in_offset=bass.IndirectOffsetOnAxis(ap=d1i.t[:, t:t + 1], axis=0),
                    bounds_check=REG_SLOT, oob_is_err=False),
                    reads=[d1i.r], writes=[a1.r])
                load(xx, X1[t * 128:(t + 1) * 128, :])

            issue_e(0)
            for t in range(NOWN):
                a0 = y0[t % 2]; a1 = y1[t % 2]; xx = xr[t % 2]; o_ = ob[t % 2]
                if t + 1 < NOWN:
                    issue_e(t + 1)
                V(lambda: nc.vector.tensor_scalar(out=acc.t[:], in0=a0.t[:], scalar1=wts.t[:, t, 0:1], scalar2=None, op0=ALU.mult),
                  [a0.r, wts.r], [acc.r])
                V(lambda: nc.vector.scalar_tensor_tensor(out=acc.t[:], in0=a1.t[:], scalar=wts.t[:, t, 1:2], in1=acc.t[:],
                                                         op0=ALU.mult, op1=ALU.add), [a1.r, wts.r, acc.r], [acc.r])
                G(lambda: nc.gpsimd.tensor_tensor(out=acc.t[:], in0=acc.t[:], in1=gate2b.t[:], op=ALU.mult), [acc.r, gate2b.r], [acc.r])
                G(lambda: nc.gpsimd.tensor_tensor(out=acc.t[:], in0=acc.t[:], in1=xx.t[:], op=ALU.add), [acc.r, xx.r], [acc.r])
                A(lambda: nc.scalar.activation(out=jk.t[:], in_=acc.t[:], func=AF.Square, accum_out=st3.t[:, 0:1]),
                  [acc.r], [jk.r, st3.r])
                V(lambda: nc.vector.tensor_scalar(out=st3.t[:, 1:2], in0=st3.t[:, 0:1], scalar1=1.0 / D, scalar2=EPS,
                                                  op0=ALU.mult, op1=ALU.add), [st3.r], [st3.r])
                A(lambda: nc.scalar.activation(out=st3.t[:, 2:3], in_=st3.t[:, 1:2], func=AF.Sqrt), [st3.r], [st3.r])
                V(lambda: nc.vector.reciprocal(out=st3.t[:, 3:4], in_=st3.t[:, 2:3]), [st3.r], [st3.r])
                V(lambda: nc.vector.scalar_tensor_tensor(out=o_.t[:], in0=acc.t[:], scalar=st3.t[:, 3:4], in1=gfb.t[:],
                                                         op0=ALU.mult, op1=ALU.mult), [acc.r, st3.r, gfb.r], [o_.r])
                T.dma(SP, o_.l, lambda: nc.sync.dma_start(out=out_d[t * 128:(t + 1) * 128, :], in_=o_.t[:]), reads=[o_.r])
            T.barrier()
    return nc


def _own_tiles(j, NP):
    t = []
    for m in range(NP):
        t += [8 * m + j, 8 * m + 7 - j]
    return t


def make_in_maps(inp, S):
    f32 = np.float32
    NT = S // 128; NP = NT // 8; NOWN = 2 * NP; TOK = NOWN * 128
    NBLK = (TOK * 2) // BLK + NEXP
    g = lambda k: np.asarray(inp[k])
    col = lambda v, n: np.ascontiguousarray(np.asarray(v, f32).reshape(n, 128).T)
    x = g("x"); c = g("c"); pos = g("positions")
    p = np.arange(128)
    fq = np.zeros((128, 1), f32)
    freqs = (np.float32(10000.0) ** (-np.arange(16, dtype=f32) / np.float32(16))).astype(f32)
    fq[64:96, 0] = np.tile(freqs, 2) / np.float32(2 * np.pi)
    shared = dict(
        w_ada=np.ascontiguousarray(g("w_ada")[0]), b_adaT=col(g("b_ada")[0], 48), b_ada_row=g("b_ada")[0].reshape(1, -1).astype(f32),
        g1col=col(g("norm1_g")[0], 8), g1row=g("norm1_g")[0].reshape(1, -1).astype(f32), w_in=np.ascontiguousarray(g("w_in")[0]),
        gqcol=col(g("q_norm_g")[0], 2), w_uq=np.ascontiguousarray(g("w_uq")[0]),
        gkvcol=col(g("kv_norm_g")[0], 1), w_ukv=np.ascontiguousarray(g("w_ukv")[0]),
        gv_row=g("v_norm_g")[0].reshape(1, -1).astype(f32), bv_row=g("v_norm_b")[0].reshape(1, -1).astype(f32),
        w_s=np.ascontiguousarray(g("w_s")[0]), bsT=np.ascontiguousarray(g("b_s")[0].T),
        w_br_mla=np.ascontiguousarray(g("w_br_mla")[0]), w_br_sgu=np.ascontiguousarray(g("w_br_sgu")[0]),
        w_out=np.ascontiguousarray(g("w_out")[0]),
        g2col=col(g("norm2_g")[0], 8), g2row=g("norm2_g")[0].reshape(1, -1).astype(f32),
        wr=np.ascontiguousarray(np.concatenate([g("w_rg")[0], g("w_re")[0]], axis=1)),
        br_row=np.concatenate([g("b_rg")[0], g("b_re")[0]]).reshape(1, -1).astype(f32),
        w1=np.ascontiguousarray(g("w1")[0]).reshape(NEXP * 128, 4096),
        w3=np.ascontiguousarray(g("w3")[0]).reshape(NEXP * 128, 4096),
        w2=np.ascontiguousarray(g("w2")[0]).reshape(NEXP * 128, 4096),
        gf_row=g("final_g").reshape(1, -1).astype(f32),
        c_ident=np.eye(128, dtype=f32), c_tril=np.tril(np.ones((128, 128), f32)),
        c_lstrict=(p[:, None] < p[None, :]).astype(f32), c_kq=(p[:, None] - p[None, :]).astype(f32),
        c_fq=fq, c_thr=np.tile((np.arange(NBLK, dtype=f32) * BLK)[None, :], (128, 1)).astype(f32),
        c_pidx=p.astype(f32).reshape(128, 1),
    )
    maps = []
    owns = []
    for core in range(8):
        b, j = core // 4, core % 4
        tiles = _own_tiles(j, NP)
        rows = np.concatenate([np.arange(t * 128, (t + 1) * 128) for t in tiles])
        owns.append((b, rows))
        dt_ = np.zeros((128, 16), f32)
        for o in range(8):
            dt_[:, o] = (j - o) * 128
            dt_[:, 8 + o] = (7 - j - o) * 128
        m = dict(shared)
        m.update(
            xb=np.ascontiguousarray(x[b, :S]), xo=np.ascontiguousarray(x[b][rows]),
            posb=np.ascontiguousarray(pos[b, :S].reshape(1, -1).astype(np.int32)),
            poso=np.ascontiguousarray(pos[b][rows].reshape(1, -1).astype(np.int32)),
            ccol=col(c[b], 8), c_dtab=dt_,
        )
        maps.append(m)
    return maps, owns


_NC_CACHE = {}


def kernel(**inputs):
    S = int(np.asarray(inputs["x"]).shape[1])
    if S not in _NC_CACHE:
        _NC_CACHE[S] = build_nc(S)
    nc = _NC_CACHE[S]
    maps, owns = make_in_maps(inputs, S)
    res = run_bass_kernel_spmd(nc, maps, core_ids=list(range(8)))
    B = np.asarray(inputs["x"]).shape[0]
    out = np.zeros((B, S, D), np.float32)
    for core, (b, rows) in enumerate(owns):
        out[b, rows] = np.asarray(res.results[core]["out"], dtype=np.float32)
    return out
```

```python
import numpy as np
from contextlib import ExitStack
import concourse.bass as bass
import concourse.mybir as mybir
from concourse.bass_utils import run_bass_kernel_spmd

F32 = mybir.dt.float32
BF16 = mybir.dt.bfloat16
I32 = mybir.dt.int32
AF = mybir.ActivationFunctionType
ALU = mybir.AluOpType
AX = mybir.AxisListType

D = 1024
NH = 8
EPS = 1e-6
NEXP = 32
FF = 512
BLK = 256
IN_W = 3488
SCALE = 96 ** -0.5
GELU_C = 1.5957691216057308


class Res:
    __slots__ = ("name", "w", "r")

    def __init__(self, name=""):
        self.name = name
        self.w = None
        self.r = {}


class Lane:
    def __init__(self, T, name):
        self.sem = T.newsem(name)
        self.count = 0


class Eng:
    def __init__(self, T, name, eng, is_pe=False):
        self.name = name
        self.eng = eng
        self.is_pe = is_pe
        self.sem = T.newsem(name + "_s0")
        self.own = [self.sem]
        self.count = 0
        self.waited = {}


class Tracker:
    CAP = 12000

    def __init__(self, nc, es):
        self.nc = nc
        self.es = es
        self.nsems = 0
        self.lanes = []
        self.pe = Eng(self, "pe", nc.tensor, True)
        self.act = Eng(self, "act", nc.scalar)
        self.dve = Eng(self, "dve", nc.vector)
        self.pool = Eng(self, "pool", nc.gpsimd)
        self.sp = Eng(self, "sp", nc.sync)
        self.engs = [self.pe, self.act, self.dve, self.pool, self.sp]

    def newsem(self, name):
        self.nsems += 1
        return self.es.enter_context(self.nc.semaphore(f"{name}_{self.nsems}"))

    def lane(self, name="l"):
        l = Lane(self, name)
        self.lanes.append(l)
        return l

    def _wait(self, E, deps):
        for sem, val in deps:
            if E.is_pe and any(sem is s for s in E.own):
                continue
            k = id(sem)
            if E.waited.get(k, 0) < val:
                E.eng.wait_ge(sem, val)
                E.waited[k] = val

    @staticmethod
    def _deps(reads, writes):
        deps = []
        for r in reads:
            if r.w is not None:
                deps.append(r.w)
        for w in writes:
            if w.w is not None:
                deps.append(w.w)
            deps.extend(w.r.values())
        return deps

    @staticmethod
    def _record(ev, reads, writes):
        for r in reads:
            r.r[id(ev[0])] = ev
        for w in writes:
            w.w = ev
            w.r = {}

    def op(self, E, fn, reads=(), writes=()):
        self._wait(E, self._deps(reads, writes))
        if E.count >= self.CAP:
            E.sem = self.newsem(E.name + "_s")
            E.own.append(E.sem)
            E.count = 0
        ins = fn()
        ins.then_inc(E.sem, 1)
        E.count += 1
        self._record((E.sem, E.count), reads, writes)

    def dma(self, E, lane, fn, reads=(), writes=()):
        self._wait(E, self._deps(reads, writes))
        ins = fn()
        ins.then_inc(lane.sem, 16)
        lane.count += 16
        self._record((lane.sem, lane.count), reads, writes)

    def barrier(self):
        evs = [(e.sem, e.count) for e in self.engs if e.count > 0]
        evs += [(l.sem, l.count) for l in self.lanes if l.count > 0]
        for E in self.engs:
            self._wait(E, evs)


class Buf:
    def __init__(self, T, tile, name, lane=False):
        self.t = tile
        self.r = Res(name)
        self.l = T.lane(name) if lane else None


def build_nc(S, stop=99):
    NT = S // 128
    NP = NT // 8
    NOWN = 2 * NP
    TOK = NOWN * 128
    NG = NT // 4
    NGO = max(NOWN // 4, 1)
    GW = min(512, TOK)
    NBLK = (TOK * 2) // BLK + NEXP
    NSLOT = NBLK * BLK

    nc = bass.Bass("TRN2", target_bir_lowering=False)

    def din(name, shape, dt=F32):
        return nc.dram_tensor(name, list(shape), dt, kind="ExternalInput").ap()

    xb = din("xb", [S, D]); xo = din("xo", [TOK, D])
    posb = din("posb", [1, S], I32); poso = din("poso", [1, TOK], I32)
    ccol = din("ccol", [128, 8])
    w_ada = din("w_ada", [D, 6 * D]); b_adaT = din("b_adaT", [128, 48]); b_ada_row = din("b_ada_row", [1, 6 * D])
    g1col = din("g1col", [128, 8]); g1row = din("g1row", [1, D]); w_in = din("w_in", [D, IN_W])
    gqcol = din("gqcol", [128, 2]); w_uq = din("w_uq", [256, 768])
    gkvcol = din("gkvcol", [128, 1]); w_ukv = din("w_ukv", [128, 1024])
    gv_row = din("gv_row", [1, 512]); bv_row = din("bv_row", [1, 512])
    w_s = din("w_s", [8, 128, 128]); bsT = din("bsT", [128, 8])
    w_br_mla = din("w_br_mla", [512, D]); w_br_sgu = din("w_br_sgu", [512, D]); w_out = din("w_out", [D, D])
    g2col = din("g2col", [128, 8]); g2row = din("g2row", [1, D])
    wr = din("wr", [D, 36]); br_row = din("br_row", [1, 36])
    w1 = din("w1", [NEXP * 128, 4096]); w3 = din("w3", [NEXP * 128, 4096]); w2 = din("w2", [NEXP * 128, 4096])
    gf_row = din("gf_row", [1, D])
    c_ident = din("c_ident", [128, 128]); c_tril = din("c_tril", [128, 128]); c_lstrict = din("c_lstrict", [128, 128])
    c_kq = din("c_kq", [128, 128]); c_fq = din("c_fq", [128, 1]); c_dtab = din("c_dtab", [128, 16])
    c_thr = din("c_thr", [128, NBLK]); c_pidx = din("c_pidx", [128, 1])

    out_d = nc.dram_tensor("out", [TOK, D], F32, kind="ExternalOutput").ap()
    X1 = nc.dram_tensor("X1s", [TOK, D], F32, kind="Internal").ap()
    H2 = nc.dram_tensor("H2s", [TOK, D], BF16, kind="Internal").ap()
    XS = nc.dram_tensor("XSs", [NSLOT, D], BF16, kind="Internal").ap()
    YS = nc.dram_tensor("YSs", [NSLOT, D], F32, kind="Internal").ap()
    OS = nc.dram_tensor("OSs", [TOK, 512], BF16, kind="Internal").ap()

    with ExitStack() as es:
        T = Tracker(nc, es)
        PE, ACT, DVE, POOL, SP = T.pe, T.act, T.dve, T.pool, T.sp

        def sbt(stack, name, shape, dt, lane=False):
            t = stack.enter_context(nc.sbuf_tensor(name, list(shape), dt))
            return Buf(T, t, name, lane)

        banks = []
        for i in range(8):
            t = es.enter_context(nc.psum_tensor(f"pb{i}", [128, 512], F32))
            banks.append(Buf(T, t, f"pb{i}"))
        rot = {"i": 0, "lst": list(range(8))}

        def pnext():
            b = banks[rot["lst"][rot["i"] % len(rot["lst"])]]
            rot["i"] += 1
            return b

        def set_rot(lst):
            rot["lst"] = list(lst)
            rot["i"] = 0

        def V(fn, reads=(), writes=()):
            T.op(DVE, fn, reads, writes)

        def A(fn, reads=(), writes=()):
            T.op(ACT, fn, reads, writes)

        def G(fn, reads=(), writes=()):
            T.op(POOL, fn, reads, writes)

        def P(fn, reads=(), writes=()):
            T.op(PE, fn, reads, writes)

        def load(buf, src, eng=None):
            E = SP if eng is None else eng
            T.dma(E, buf.l, lambda: E.eng.dma_start(out=buf.t[:], in_=src), writes=[buf.r])

        def load_to(buf, dst_ap, src, eng=None):
            E = SP if eng is None else eng
            T.dma(E, buf.l, lambda: E.eng.dma_start(out=dst_ap, in_=src), writes=[buf.r])

        identf = sbt(es, "identf", [128, 128], F32, True); load(identf, c_ident)
        identb = sbt(es, "identb", [128, 128], BF16)
        V(lambda: nc.vector.tensor_copy(out=identb.t[:], in_=identf.t[:]), [identf.r], [identb.r])
        onesb = sbt(es, "onesb", [128, 128], BF16)
        V(lambda: nc.vector.memset(onesb.t[:], 1.0), [], [onesb.r])
        onesf = sbt(es, "onesf", [128, 128], F32)
        V(lambda: nc.vector.memset(onesf.t[:], 1.0), [], [onesf.r])
        kqf = sbt(es, "kqf", [128, 128], F32, True); load(kqf, c_kq)
        kqb = sbt(es, "kqb", [128, 128], BF16)
        V(lambda: nc.vector.tensor_copy(out=kqb.t[:], in_=kqf.t[:]), [kqf.r], [kqb.r])
        fq = sbt(es, "fq", [128, 1], F32, True); load(fq, c_fq)
        dtab = sbt(es, "dtab", [128, 16], F32, True); load(dtab, c_dtab)
        g1c = sbt(es, "g1c", [128, 8], F32, True); load(g1c, g1col)
        g2c = sbt(es, "g2c", [128, 8], F32, True); load(g2c, g2col)
        gqc = sbt(es, "gqc", [128, 2], F32, True); load(gqc, gqcol)
        gkvc = sbt(es, "gkvc", [128, 1], F32, True); load(gkvc, gkvcol)
        cc = sbt(es, "cc", [128, 8], F32, True); load(cc, ccol)
        badT = sbt(es, "badT", [128, 48], F32, True); load(badT, b_adaT)
        modT = sbt(es, "modT", [128, 48], F32)
        gate1b = sbt(es, "gate1b", [128, D], F32)
        s2b = sbt(es, "s2b", [128, D], F32)
        sh2b = sbt(es, "sh2b", [128, D], F32)
        gate2b = sbt(es, "gate2b", [128, D], F32)
        s1b = sbt(es, "s1b", [128, D], F32)
        sh1b = sbt(es, "sh1b", [128, 8], BF16)
        ones512 = sbt(es, "ones512", [1, 512], BF16)
        V(lambda: nc.vector.memset(ones512.t[:], 1.0), [], [ones512.r])
        s1c = sbt(es, "s1c", [128, 8], F32); s2c = sbt(es, "s2c", [128, 8], F32)

        with ExitStack() as ph:
            sc = sbt(ph, "sc", [128, 8], F32)
            A(lambda: nc.scalar.activation(out=sc.t[:], in_=cc.t[:], func=AF.Silu), [cc.r], [sc.r])
            brow = [sbt(ph, f"brow{i}", [1, 512], F32, True) for i in range(2)]
            rowdst = {2: (s1b, 0), 3: (s1b, 512), 4: (gate1b, 0), 5: (gate1b, 512), 6: (sh2b, 0), 7: (sh2b, 512), 8: (s2b, 0), 9: (s2b, 512),
                      10: (gate2b, 0), 11: (gate2b, 512)}
            wp = [sbt(ph, f"wada{i}", [128, 8, 512], F32, True) for i in range(2)]
            pcol = banks[7]
            set_rot(range(7))
            rowtmp = sbt(ph, "rowtmp", [1, 512], F32)
            for pc in range(12):
                wb_ = wp[pc % 2]
                load(wb_, w_ada[:, pc * 512:(pc + 1) * 512].rearrange("(kc p) n -> p kc n", p=128))
                if pc in rowdst:
                    prow = pnext()
                    br_ = brow[pc % 2]
                    load(br_, b_ada_row[0:1, pc * 512:(pc + 1) * 512])
                    for kc in range(8):
                        P(lambda: nc.tensor.matmul(prow.t[0:1, :], lhsT=sc.t[:, kc:kc + 1], rhs=wb_.t[:, kc, :],
                                                   start=(kc == 0), stop=(kc == 7)), [sc.r, wb_.r], [prow.r])
                    V(lambda: nc.vector.tensor_tensor(out=rowtmp.t[0:1, :], in0=prow.t[0:1, :], in1=br_.t[0:1, :], op=ALU.add),
                      [prow.r, br_.r], [rowtmp.r])
                    pbc = pnext()
                    P(lambda: nc.tensor.matmul(pbc.t[:], lhsT=onesf.t[0:1, :], rhs=rowtmp.t[0:1, :], start=True, stop=True),
                      [onesf.r, rowtmp.r], [pbc.r])
                    dtile, dc0 = rowdst[pc]
                    V(lambda: nc.vector.tensor_copy(out=dtile.t[:, dc0:dc0 + 512], in_=pbc.t[:]), [pbc.r], [dtile.r])
                for oc in range(4):
                    col = pc * 4 + oc
                    for kc in range(8):
                        P(lambda: nc.tensor.matmul(pcol.t[:, col:col + 1], lhsT=wb_.t[:, kc, oc * 128:(oc + 1) * 128],
                                                   rhs=sc.t[:, kc:kc + 1], start=(kc == 0), stop=(kc == 7)),
                          [sc.r, wb_.r], [pcol.r])
            V(lambda: nc.vector.tensor_tensor(out=modT.t[:], in0=pcol.t[:, 0:48], in1=badT.t[:], op=ALU.add),
              [pcol.r, badT.r], [modT.r])
            V(lambda: nc.vector.scalar_tensor_tensor(out=s1c.t[:], in0=modT.t[:, 8:16], scalar=1.0, in1=g1c.t[:],
                                                     op0=ALU.add, op1=ALU.mult), [modT.r, g1c.r], [s1c.r])
            V(lambda: nc.vector.scalar_tensor_tensor(out=s2c.t[:], in0=modT.t[:, 32:40], scalar=1.0, in1=g2c.t[:],
                                                     op0=ALU.add, op1=ALU.mult), [modT.r, g2c.r], [s2c.r])
            g1rb = sbt(ph, "g1rb", [128, D], F32, True); load(g1rb, g1row.partition_broadcast(128))
            V(lambda: nc.vector.scalar_tensor_tensor(out=s1b.t[:], in0=s1b.t[:], scalar=1.0, in1=g1rb.t[:], op0=ALU.add,
                                                     op1=ALU.mult), [s1b.r, g1rb.r], [s1b.r])
            V(lambda: nc.vector.tensor_copy(out=sh1b.t[:], in_=modT.t[:, 0:8]), [modT.r], [sh1b.r])
            T.barrier()
        if stop <= 0:
            return nc
        sh1 = lambda kc: modT.t[:, kc:kc + 1]
        sh2 = lambda kc: modT.t[:, 24 + kc:25 + kc]

        FE = {}
        cnt = {"x": 0, "fe": 0, "st": 0, "xn": 0, "cp": 0}

        def make_fe(stack, nx=3, nxn=2):
            k = cnt["fe"]; cnt["fe"] += 1
            FE["nx"] = nx
            FE["xts"] = [sbt(stack, f"xt{k}_{i}", [128, D], F32, True) for i in range(nx)]
            FE["junk"] = sbt(stack, f"junk{k}", [128, D], BF16)
            FE["stat"] = [sbt(stack, f"stat{k}_{i}", [128, 16], F32) for i in range(3)]
            FE["xns"] = [sbt(stack, f"xn{k}_{i}", [128, D], BF16) for i in range(nxn)]

        def fe_a(tile_aps):
            n = len(tile_aps)
            st = FE["stat"][cnt["st"] % 3]; cnt["st"] += 1
            junk = FE["junk"]
            xl = []
            for j in range(n):
                xt = FE["xts"][cnt["x"] % FE["nx"]]; cnt["x"] += 1
                load(xt, tile_aps[j])
                xl.append(xt)
            for j in range(n):
                A(lambda: nc.scalar.activation(out=junk.t[:], in_=xl[j].t[:], func=AF.Square, accum_out=st.t[:, j:j + 1]),
                  [xl[j].r], [junk.r, st.r])
            V(lambda: nc.vector.tensor_scalar(out=st.t[:, 4:4 + n], in0=st.t[:, 0:n], scalar1=1.0 / D, scalar2=EPS,
                                              op0=ALU.mult, op1=ALU.add), [st.r], [st.r])
            A(lambda: nc.scalar.activation(out=st.t[:, 8:8 + n], in_=st.t[:, 4:4 + n], func=AF.Sqrt), [st.r], [st.r])
            V(lambda: nc.vector.reciprocal(out=st.t[:, 12:12 + n], in_=st.t[:, 8:8 + n]), [st.r], [st.r])
            xnl = []
            for j in range(n):
                xn = FE["xns"][cnt["xn"] % len(FE["xns"])]; cnt["xn"] += 1
                V(lambda: nc.vector.scalar_tensor_tensor(out=xn.t[:], in0=xl[j].t[:], scalar=st.t[:, 12 + j:13 + j], in1=s1b.t[:],
                                                         op0=ALU.mult, op1=ALU.mult), [xl[j].r, st.r, s1b.r], [xn.r])
                xnl.append(xn)
            return xl, xnl

        def fe_b(state, dst, dst_res, c0s):
            xl, xnl = state
            for j, xn in enumerate(xnl):
                tp = pnext()
                tpb = tp.t[:].bitcast(BF16)
                for kc in range(8):
                    P(lambda: nc.tensor.transpose(out=tpb[:, kc * 128:(kc + 1) * 128], in_=xn.t[:, kc * 128:(kc + 1) * 128],
                                                  identity=identb.t[:]), [xn.r, identb.r], [tp.r])
                c0 = c0s[j]
                src3 = tpb.rearrange("p (k t) -> p k t", k=8)
                if cnt["cp"] % 2 == 0:
                    A(lambda: nc.scalar.copy(out=dst[:, :, c0:c0 + 128], in_=src3), [tp.r], [dst_res])
                else:
                    V(lambda: nc.vector.tensor_copy(out=dst[:, :, c0:c0 + 128], in_=src3), [tp.r], [dst_res])
                cnt["cp"] += 1
            return xl

        def front_end_multi(tile_aps, dst, dst_res, c0s):
            return fe_b(fe_a(tile_aps), dst, dst_res, c0s)

        def bias_row(dst_ap, wfn, M, wres, dres):
            ps = pnext()
            for kc in range(8):
                P(lambda: nc.tensor.matmul(ps.t[0:1, 0:M], lhsT=sh1b.t[:, kc:kc + 1], rhs=wfn(kc), start=(kc == 0), stop=(kc == 7)),
                  [sh1b.r, wres], [ps.r])
            V(lambda: nc.vector.tensor_copy(out=dst_ap, in_=ps.t[0:1, 0:M]), [ps.r], [dres])

        s1col = lambda kc: s1c.t[:, kc:kc + 1]

        Aall = sbt(es, "Aall", [128, NOWN, 32], BF16)
        A0all = sbt(es, "A0all", [128, NOWN, 32], BF16)
        A1all = sbt(es, "A1all", [128, NOWN, 32], BF16)
        wts = sbt(es, "wts", [128, NOWN, 2], F32)
        att = es.enter_context(ExitStack())
        ckvnT = sbt(att, "ckvnT", [128, S], BF16); ckvn_r = [Res() for _ in range(NG)]
        KT = sbt(att, "KT", [128, S], BF16); ktn_r = [Res() for _ in range(NG)]; ktr_r = [Res() for _ in range(NG)]
        vh_r = [Res() for _ in range(NG)]
        cqnT = sbt(att, "cqnT", [128, 2, TOK], BF16); cqn_r = [Res() for _ in range(NGO)]
        qT_r = [Res() for _ in range(NGO)]
        CQ = sbt(att, "CQ", [128, TOK], BF16); SQ = sbt(att, "SQ", [128, TOK], BF16)
        cs_r = [Res() for _ in range(NGO)]

        def rope_tables(pos_ap_512, Cdst, Sdst, wres, lp, width):
            pi_ = lp["posi"]; R = slice(64, 96)
            T.dma(SP, pi_.l, lambda: nc.sync.dma_start(out=pi_.t[R, 0:width], in_=pos_ap_512.partition_broadcast(32)),
                  writes=[pi_.r])
            pf = lp["pf"]; u = lp["u"]; ki = lp["ki"]; kf = lp["kf"]; f2 = lp["f2"]
            V(lambda: nc.vector.tensor_copy(out=pf.t[R, 0:width], in_=pi_.t[R, 0:width]), [pi_.r], [pf.r])
            for which, off, dstb in ((0, 0.5, Sdst), (1, 0.75, Cdst)):
                V(lambda: nc.vector.tensor_scalar(out=u.t[R, 0:width], in0=pf.t[R, 0:width], scalar1=fq.t[R, 0:1],
                                                  scalar2=off, op0=ALU.mult, op1=ALU.add), [pf.r, fq.r], [u.r])
                V(lambda: nc.vector.tensor_copy(out=ki.t[R, 0:width], in_=u.t[R, 0:width]), [u.r], [ki.r])
                V(lambda: nc.vector.tensor_copy(out=kf.t[R, 0:width], in_=ki.t[R, 0:width]), [ki.r], [kf.r])
                V(lambda: nc.vector.scalar_tensor_tensor(out=f2.t[R, 0:width], in0=u.t[R, 0:width], scalar=-0.5,
                                                         in1=kf.t[R, 0:width], op0=ALU.add, op1=ALU.subtract),
                  [u.r, kf.r], [f2.r])
                V(lambda: nc.vector.scalar_tensor_tensor(out=u.t[R, 0:width], in0=f2.t[R, 0:width], scalar=-0.5,
                                                         in1=f2.t[R, 0:width], op0=ALU.is_lt, op1=ALU.add),
                  [f2.r], [u.r])
                A(lambda: nc.scalar.activation(out=dstb, in_=u.t[R, 0:width], func=AF.Sin, scale=6.28318),
                  [u.r], [wres])

        with ExitStack() as lp_:
            make_fe(lp_, 4, 4)
            lp = {}
            lp["posi"] = sbt(lp_, "posi", [128, 512], I32, True)
            for nm in ("pf", "u", "kf", "f2"):
                lp[nm] = sbt(lp_, "lp_" + nm, [128, 512], F32)
            lp["t1"] = lp["pf"]; lp["t2"] = lp["kf"]
            lp["ki"] = sbt(lp_, "lp_ki", [128, 512], I32)
            Ctmp = sbt(lp_, "Ctmp", [128, 512], F32); Stmp = sbt(lp_, "Stmp", [128, 512], F32)
            h1Ts = [sbt(lp_, f"h1T{i}", [128, 8, 512], BF16) for i in range(2)]
            sq = sbt(lp_, "sq", [128, 2, 512], BF16)
            rs = sbt(lp_, "rs", [128, 512], F32); rs2 = sbt(lp_, "rs2", [128, 512], F32)
            wkv = sbt(lp_, "wkv", [128, 8, 128], BF16, True)
            wkr = sbt(lp_, "wkr", [128, 8, 96], BF16, True)
            wkrr = sbt(lp_, "wkrr", [128, 8, 96], BF16)
            wq = sbt(lp_, "wq", [128, 8, 256], BF16, True)
            win_v = w_in.rearrange("(kc p) n -> p kc n", p=128)
            load(wkv, win_v[:, :, 256:384], POOL)
            V(lambda: nc.vector.memset(wkr.t[:], 0.0), [], [wkr.r])
            V(lambda: nc.vector.memset(wkrr.t[:], 0.0), [], [wkrr.r])
            load_to(wkr, wkr.t[:, :, 64:96], win_v[:, :, 384:416], POOL)
            load(wq, win_v[:, :, 0:256], POOL)
            V(lambda: nc.vector.tensor_scalar(out=wkrr.t[:, :, 64:80], in0=wkr.t[:, :, 80:96], scalar1=-1.0, scalar2=None,
                                              op0=ALU.mult), [wkr.r, wkrr.r], [wkrr.r])
            V(lambda: nc.vector.tensor_copy(out=wkrr.t[:, :, 80:96], in_=wkr.t[:, :, 64:80]), [wkr.r, wkrr.r], [wkrr.r])
            set_rot(range(8))
            brows = sbt(lp_, "brows", [1, 640], BF16)
            bias_row(brows.t[0:1, 0:128], lambda kc: wkv.t[:, kc, :], 128, wkv.r, brows.r)
            bias_row(brows.t[0:1, 128:224], lambda kc: wkr.t[:, kc, :], 96, wkr.r, brows.r)
            bias_row(brows.t[0:1, 224:320], lambda kc: wkrr.t[:, kc, :], 96, wkrr.r, brows.r)
            bias_row(brows.t[0:1, 320:576], lambda kc: wq.t[:, kc, :], 256, wq.r, brows.r)
            hcnt = {"i": 0}
            h1_of = {}

            fe_state = {}

            def latent_fe_a(mode, g, width):
                ntl = width // 128
                src = xb if mode == "k" else xo
                fe_state[(mode, g)] = fe_a([src[(g * 4 + tl) * 128:(g * 4 + tl + 1) * 128, :] for tl in range(ntl)])

            def latent_fe_b(mode, g, width):
                ntl = width // 128
                h1 = h1Ts[hcnt["i"] % 2]; hcnt["i"] += 1
                h1_of[(mode, g)] = h1
                fe_b(fe_state.pop((mode, g)), h1.t, h1.r, [tl * 128 for tl in range(ntl)])

            mm_out = {}

            def latent_mm(mode, g, width):
                h1 = h1_of.pop((mode, g))
                W = slice(0, width)
                if mode == "k":
                    pk = pnext(); pr = pnext(); prr = pnext()
                    for kc in range(8):
                        P(lambda: nc.tensor.matmul(pk.t[:, W], lhsT=wkv.t[:, kc, :], rhs=h1.t[:, kc, W],
                                                   start=(kc == 0), stop=False), [wkv.r, h1.r], [pk.r])
                    P(lambda: nc.tensor.matmul(pk.t[:, W], lhsT=brows.t[0:1, 0:128], rhs=ones512.t[0:1, W], start=False, stop=True),
                      [brows.r, ones512.r], [pk.r])
                    for kc in range(8):
                        P(lambda: nc.tensor.matmul(pr.t[0:96, W], lhsT=wkr.t[:, kc, :], rhs=h1.t[:, kc, W],
                                                   start=(kc == 0), stop=False), [wkr.r, h1.r], [pr.r])
                    P(lambda: nc.tensor.matmul(pr.t[0:96, W], lhsT=brows.t[0:1, 128:224], rhs=ones512.t[0:1, W], start=False, stop=True),
                      [brows.r, ones512.r], [pr.r])
                    for kc in range(8):
                        P(lambda: nc.tensor.matmul(prr.t[0:96, W], lhsT=wkrr.t[:, kc, :], rhs=h1.t[:, kc, W],
                                                   start=(kc == 0), stop=False), [wkrr.r, h1.r], [prr.r])
                    P(lambda: nc.tensor.matmul(prr.t[0:96, W], lhsT=brows.t[0:1, 224:320], rhs=ones512.t[0:1, W], start=False, stop=True),
                      [brows.r, ones512.r], [prr.r])
                    chunks = [pk]
                    gcol = [gkvc.t[:, 0:1]]
                    nfeat = 128
                else:
                    p0 = pnext(); p1 = pnext()
                    for ci, pp in enumerate((p0, p1)):
                        for kc in range(8):
                            P(lambda: nc.tensor.matmul(pp.t[:, W], lhsT=wq.t[:, kc, ci * 128:(ci + 1) * 128],
                                                       rhs=h1.t[:, kc, W], start=(kc == 0), stop=False),
                              [wq.r, h1.r], [pp.r])
                        P(lambda: nc.tensor.matmul(pp.t[:, W], lhsT=brows.t[0:1, 320 + ci * 128:320 + (ci + 1) * 128],
                                                   rhs=ones512.t[0:1, W], start=False, stop=True), [brows.r, ones512.r], [pp.r])
                    chunks = [p0, p1]
                    gcol = [gqc.t[:, 0:1], gqc.t[:, 1:2]]
                    nfeat = 256
                mm_out[(mode, g)] = (chunks, gcol, nfeat, (pr, prr) if mode == "k" else None)

            def latent_rest(mode, g, width):
                W = slice(0, width)
                chunks, gcol, nfeat, prs = mm_out.pop((mode, g))
                if prs is not None:
                    pr, prr = prs
                for ci, pp in enumerate(chunks):
                    A(lambda: nc.scalar.activation(out=sq.t[:, ci, W], in_=pp.t[:, W], func=AF.Square), [pp.r], [sq.r])
                pss = pnext()
                for ci in range(len(chunks)):
                    P(lambda: nc.tensor.matmul(pss.t[:, W], lhsT=onesb.t[:], rhs=sq.t[:, ci, W], start=(ci == 0),
                                               stop=(ci == len(chunks) - 1)), [onesb.r, sq.r], [pss.r])
                V(lambda: nc.vector.tensor_scalar(out=rs.t[:, W], in0=pss.t[:, W], scalar1=1.0 / nfeat, scalar2=EPS,
                                                  op0=ALU.mult, op1=ALU.add), [pss.r], [rs.r])
                A(lambda: nc.scalar.activation(out=rs2.t[:, W], in_=rs.t[:, W], func=AF.Sqrt), [rs.r], [rs2.r])
                V(lambda: nc.vector.reciprocal(out=rs.t[:, W], in_=rs2.t[:, W]), [rs2.r], [rs.r])
                c0 = g * 512
                for ci, pp in enumerate(chunks):
                    if mode == "k":
                        dst = ckvnT.t[:, c0:c0 + width]; dres = ckvn_r[g]
                    else:
                        dst = cqnT.t[:, ci, c0:c0 + width]; dres = cqn_r[g]
                    V(lambda: nc.vector.scalar_tensor_tensor(out=dst, in0=pp.t[:, W], scalar=gcol[ci], in1=rs.t[:, W],
                                                             op0=ALU.mult, op1=ALU.mult),
                      [pp.r, rs.r, gkvc.r, gqc.r], [dres])
                R = slice(64, 96)
                if mode == "k":
                    rope_tables(posb[0:1, c0:c0 + width], Ctmp.t[R, W], Stmp.t[R, W], Ctmp.r, lp, width)
                    t1 = lp["t1"]; t2 = lp["t2"]
                    V(lambda: nc.vector.tensor_tensor(out=t1.t[R, W], in0=pr.t[R, W], in1=Ctmp.t[R, W], op=ALU.mult),
                      [pr.r, Ctmp.r], [t1.r])
                    V(lambda: nc.vector.tensor_tensor(out=t2.t[R, W], in0=prr.t[R, W], in1=Stmp.t[R, W], op=ALU.mult),
                      [prr.r, Ctmp.r], [t2.r])
                    V(lambda: nc.vector.tensor_tensor(out=KT.t[R, c0:c0 + width], in0=t1.t[R, W], in1=t2.t[R, W],
                                                      op=ALU.add), [t1.r, t2.r], [ktr_r[g]])
                else:
                    rope_tables(poso[0:1, c0:c0 + width], CQ.t[R, c0:c0 + width], SQ.t[R, c0:c0 + width], cs_r[g], lp, width)

            work = [("k", g, 512) for g in range(NG)] + [("q", g, GW) for g in range(NGO)]
            latent_fe_a(*work[0]); latent_fe_b(*work[0])
            for wi, w_ in enumerate(work):
                nxt = work[wi + 1] if wi + 1 < len(work) else None
                if nxt:
                    latent_fe_a(*nxt)
                latent_mm(*w_)
                if nxt:
                    latent_fe_b(*nxt)
                latent_rest(*w_)
            T.barrier()
            if stop <= 1:
                return nc

        with ExitStack() as ap_:
            qT = sbt(ap_, "qT", [128, TOK], BF16)
            Vh = sbt(ap_, "Vh", [128, NT, 65], BF16)
            V(lambda: nc.vector.memset(Vh.t[:, :, 64:65], 1.0), [], vh_r)
            wuqb = sbt(ap_, "wuqb", [128, 2, 768], BF16, True)
            wuqr = sbt(ap_, "wuqr", [128, 2, NH, 96], BF16)
            wukvb = sbt(ap_, "wukvb", [128, 1024], BF16, True)
            load(wuqb, w_uq.rearrange("(kc p) n -> p kc n", p=128), POOL)
            load(wukvb, w_ukv, POOL)
            V(lambda: nc.vector.memset(wuqr.t[:], 0.0), [], [wuqr.r])
            wv = wuqb.t[:].rearrange("p k (h d) -> p k h d", h=NH)
            V(lambda: nc.vector.tensor_scalar(out=wuqr.t[:, :, :, 64:80], in0=wv[:, :, :, 80:96], scalar1=-1.0, scalar2=None,
                                              op0=ALU.mult), [wuqb.r, wuqr.r], [wuqr.r])
            V(lambda: nc.vector.tensor_copy(out=wuqr.t[:, :, :, 80:96], in_=wv[:, :, :, 64:80]), [wuqb.r, wuqr.r], [wuqr.r])

            PTs = [sbt(ap_, f"PT{i}", [128, 512], BF16) for i in range(4)]
            rec = [sbt(ap_, f"rec{i}", [128, 1], F32) for i in range(4)]
            ost = [sbt(ap_, f"ost{i}", [128, 64], BF16, True) for i in range(4)]
            osc = [0]
            t1q = sbt(ap_, "t1q", [128, 512], F32); t2q = sbt(ap_, "t2q", [128, 512], F32)
            set_rot([4, 5, 6, 7])
            oacc_i = 0
            ptc = 0
            for h in range(NH):
                for g in range(NG):
                    ps = pnext()
                    P(lambda: nc.tensor.matmul(ps.t[0:64, :], lhsT=wukvb.t[:, h * 128:h * 128 + 64],
                                               rhs=ckvnT.t[:, g * 512:(g + 1) * 512], start=True, stop=True),
                      [wukvb.r, ckvn_r[g]], [ps.r])
                    if g % 2 == 0:
                        V(lambda: nc.vector.tensor_copy(out=KT.t[0:64, g * 512:(g + 1) * 512], in_=ps.t[0:64, :]),
                          [ps.r], [ktn_r[g]])
                    else:
                        A(lambda: nc.scalar.copy(out=KT.t[0:64, g * 512:(g + 1) * 512], in_=ps.t[0:64, :]),
                          [ps.r], [ktn_r[g]])
                for g8 in range(NT // 8):
                    ps = pnext()
                    for k8 in range(8):
                        kt = g8 * 8 + k8
                        P(lambda: nc.tensor.matmul(ps.t[:, k8 * 64:(k8 + 1) * 64], lhsT=ckvnT.t[:, kt * 128:(kt + 1) * 128],
                                                   rhs=wukvb.t[:, h * 128 + 64:h * 128 + 128], start=True, stop=True),
                          [wukvb.r, ckvn_r[kt // 4]], [ps.r])
                    wr_ = [vh_r[2 * g8], vh_r[2 * g8 + 1]]
                    src3 = ps.t[:].rearrange("p (a d) -> p a d", a=8)
                    if g8 % 2 == 0:
                        A(lambda: nc.scalar.copy(out=Vh.t[:, g8 * 8:(g8 + 1) * 8, 0:64], in_=src3), [ps.r], wr_)
                    else:
                        V(lambda: nc.vector.tensor_copy(out=Vh.t[:, g8 * 8:(g8 + 1) * 8, 0:64], in_=src3), [ps.r], wr_)
                for g in range(NGO):
                    W = slice(0, GW); c0 = g * 512; R = slice(64, 96)
                    pa = pnext(); pr = pnext()
                    for kc in range(2):
                        P(lambda: nc.tensor.matmul(pa.t[0:96, W], lhsT=wuqb.t[:, kc, h * 96:(h + 1) * 96],
                                                   rhs=cqnT.t[:, kc, c0:c0 + GW], start=(kc == 0), stop=(kc == 1)),
                          [wuqb.r, cqn_r[g]], [pa.r])
                    for kc in range(2):
                        P(lambda: nc.tensor.matmul(pr.t[0:96, W], lhsT=wuqr.t[:, kc, h, :],
                                                   rhs=cqnT.t[:, kc, c0:c0 + GW], start=(kc == 0), stop=(kc == 1)),
                          [wuqr.r, cqn_r[g]], [pr.r])
                    V(lambda: nc.vector.tensor_copy(out=qT.t[0:64, c0:c0 + GW], in_=pa.t[0:64, W]), [pa.r], [qT_r[g]])
                    V(lambda: nc.vector.tensor_tensor(out=t1q.t[R, W], in0=pa.t[R, W], in1=CQ.t[R, c0:c0 + GW], op=ALU.mult),
                      [pa.r, cs_r[g]], [t1q.r])
                    V(lambda: nc.vector.tensor_tensor(out=t2q.t[R, W], in0=pr.t[R, W], in1=SQ.t[R, c0:c0 + GW], op=ALU.mult),
                      [pr.r, cs_r[g]], [t2q.r])
                    V(lambda: nc.vector.tensor_tensor(out=qT.t[R, c0:c0 + GW], in0=t1q.t[R, W], in1=t2q.t[R, W], op=ALU.add),
                      [t1q.r, t2q.r], [qT_r[g]])
                steps = [(m, kp) for m in range(NP) for kp in range((8 * m + 8) // 2)]
                oa_of = {}
                for m in range(NP):
                    oa_of[m] = [banks[(oacc_i * 2) % 4], banks[(oacc_i * 2 + 1) % 4]]
                    oacc_i += 1
                pt_of = {}

                def emit_qk(si):
                    m, kp = steps[si]
                    qg = qT_r[(m * 256) // 512]
                    qs = qT.t[0:96, m * 256:(m + 1) * 256]
                    ps = pnext()
                    for kl in range(2):
                        kt = 2 * kp + kl
                        P(lambda: nc.tensor.matmul(ps.t[:, kl * 256:(kl + 1) * 256], lhsT=KT.t[0:96, kt * 128:(kt + 1) * 128],
                                                   rhs=qs, start=True, stop=True),
                          [ktn_r[kt // 4], ktr_r[kt // 4], qg], [ps.r])
                    PT = PTs[si % len(PTs)]
                    pt_of[si] = PT
                    A(lambda: nc.scalar.activation(out=PT.t[:], in_=ps.t[:], func=AF.Exp, scale=SCALE), [ps.r], [PT.r])
                    for kl in range(2):
                        kt = 2 * kp + kl
                        o = kt - 8 * m
                        if o >= 0:
                            for a in range(2):
                                cs_ = kl * 256 + a * 128
                                V(lambda: nc.vector.scalar_tensor_tensor(
                                    out=PT.t[:, cs_:cs_ + 128], in0=kqb.t[:], scalar=dtab.t[:, a * 8 + o:a * 8 + o + 1],
                                    in1=PT.t[:, cs_:cs_ + 128], op0=ALU.is_le, op1=ALU.mult),
                                  [PT.r, kqb.r, dtab.r], [PT.r])

                def emit_pv(si):
                    nonlocal_osc = None
                    m, kp = steps[si]
                    nk = 8 * m + 8
                    oa = oa_of[m]
                    PT = pt_of.pop(si)
                    for kl in range(2):
                        kt = 2 * kp + kl
                        for a in range(2):
                            cs_ = kl * 256 + a * 128
                            P(lambda: nc.tensor.matmul(oa[a].t[:, 0:65], lhsT=PT.t[:, cs_:cs_ + 128], rhs=Vh.t[:, kt, :],
                                                       start=(kt == 0), stop=(kt == nk - 1)),
                              [PT.r, vh_r[kt // 4]], [oa[a].r])
                    if kp == nk // 2 - 1:
                        for a in range(2):
                            i_own = 2 * m + a
                            rc = rec[(2 * m + a) % 4]
                            V(lambda: nc.vector.reciprocal(out=rc.t[:], in_=oa[a].t[:, 64:65]), [oa[a].r], [rc.r])
                            os_ = ost[osc[0] % 4]; osc[0] += 1
                            V(lambda: nc.vector.tensor_scalar(out=os_.t[:], in0=oa[a].t[:, 0:64],
                                                              scalar1=rc.t[:, 0:1], scalar2=None, op0=ALU.mult),
                              [oa[a].r, rc.r], [os_.r])
                            T.dma(SP, os_.l, lambda: nc.sync.dma_start(out=OS[i_own * 128:(i_own + 1) * 128, h * 64:(h + 1) * 64],
                                                                       in_=os_.t[:]), reads=[os_.r])

                LA = 2
                for si in range(len(steps) + LA):
                    if si < len(steps):
                        emit_qk(si)
                    if si - LA >= 0:
                        emit_pv(si - LA)
            T.barrier()
            if stop <= 2:
                return nc
        att.close()

        pc_ = es.enter_context(ExitStack())
        set_rot(range(8))
        win_v = w_in.rearrange("(kc p) n -> p kc n", p=128)
        wsg = sbt(pc_, "wsg", [128, 8, 1024], BF16, True); load(wsg, win_v[:, :, 416:1440], POOL)
        wgl = sbt(pc_, "wgl", [128, 8, 2048], BF16, True)
        for kc in range(8):
            load_to(wgl, wgl.t[:, kc, :], win_v[:, kc, 1440:3488], POOL)
        wbm = sbt(pc_, "wbm", [128, 4, D], BF16, True); load(wbm, w_br_mla.rearrange("(kc p) n -> p kc n", p=128), POOL)
        wbs = sbt(pc_, "wbs", [128, 4, D], BF16, True); load(wbs, w_br_sgu.rearrange("(kc p) n -> p kc n", p=128), POOL)
        wo = sbt(pc_, "wo", [128, 8, D], BF16, True); load(wo, w_out.rearrange("(kc p) n -> p kc n", p=128), POOL)
        wrf = sbt(pc_, "wrf", [128, 8, 36], F32, True); load(wrf, wr.rearrange("(kc p) n -> p kc n", p=128))
        brb = sbt(pc_, "brb", [128, 36], F32, True); load(brb, br_row.partition_broadcast(128))
        gvb = sbt(pc_, "gvb", [128, 512], F32, True); load(gvb, gv_row.partition_broadcast(128))
        bvb = sbt(pc_, "bvb", [128, 512], F32, True); load(bvb, bv_row.partition_broadcast(128))
        bst = sbt(pc_, "bst", [128, 8], F32, True); load(bst, bsT)
        make_fe(pc_, 3)
        browc = sbt(pc_, "browc", [1, 3072], BF16)
        for q_ in range(2):
            bias_row(browc.t[0:1, q_ * 512:(q_ + 1) * 512], lambda kc: wsg.t[:, kc, q_ * 512:(q_ + 1) * 512], 512, wsg.r, browc.r)
        for q_ in range(4):
            bias_row(browc.t[0:1, 1024 + q_ * 512:1024 + (q_ + 1) * 512], lambda kc: wgl.t[:, kc, q_ * 512:(q_ + 1) * 512], 512,
                     wgl.r, browc.r)
        wsT = sbt(pc_, "wsT", [128, 8, 128], BF16)
        with ExitStack() as tmp_:
            g2rb = sbt(tmp_, "g2rb", [128, D], F32, True); load(g2rb, g2row.partition_broadcast(128))
            V(lambda: nc.vector.scalar_tensor_tensor(out=s2b.t[:], in0=s2b.t[:], scalar=1.0, in1=g2rb.t[:], op0=ALU.add,
                                                     op1=ALU.mult), [s2b.r, g2rb.r], [s2b.r])
            wsf = sbt(tmp_, "wsf", [128, 8, 128], F32, True); load(wsf, w_s.rearrange("g t s -> t g s"))
            trl = sbt(tmp_, "trl", [128, 128], F32, True); load(trl, c_tril)
            wsm = sbt(tmp_, "wsm", [128, 8, 128], BF16)
            V(lambda: nc.vector.tensor_tensor(out=wsm.t[:], in0=wsf.t[:], in1=trl.t[:].unsqueeze(1).to_broadcast([128, 8, 128]),
                                              op=ALU.mult), [wsf.r, trl.r], [wsm.r])
            tp = pnext(); tpb = tp.t[:].bitcast(BF16)
            for g in range(8):
                P(lambda: nc.tensor.transpose(out=tpb[:, g * 128:(g + 1) * 128], in_=wsm.t[:, g, :], identity=identb.t[:]),
                  [wsm.r, identb.r], [tp.r])
            V(lambda: nc.vector.tensor_copy(out=wsT.t[:].rearrange("p g t -> p (g t)"), in_=tpb[:, :]), [tp.r], [wsT.r])
            T.barrier()


        with ExitStack() as cw:
            h1c = [sbt(cw, f"h1c{i}", [128, 8, 128], BF16) for i in range(1)]
            uv = sbt(cw, "uv", [128, 1024], F32)
            x2 = sbt(cw, "x2t", [128, 1024], F32)
            inner = sbt(cw, "inner", [128, 1024], F32)
            gel = sbt(cw, "gel", [128, 512], F32)
            otile = [sbt(cw, f"otile{i}", [128, 512], BF16, True) for i in range(2)]
            sgoB = [sbt(cw, f"sgo{i}", [128, 512], BF16) for i in range(1)]
            lst = sbt(cw, "lst", [128, 8], F32)
            sigB = [sbt(cw, f"sig{i}", [128, 2048], BF16) for i in range(2)]
            OT = sbt(cw, "OT", [128, 4, 128], BF16)
            SGT = sbt(cw, "SGT", [128, 4, 128], BF16)
            m1 = sbt(cw, "m1", [128, D], F32); m2 = sbt(cw, "m2", [128, D], F32)
            mrg = sbt(cw, "mrg", [128, D], BF16)
            mT = sbt(cw, "mT", [128, 8, 128], BF16)
            x1 = [sbt(cw, f"x1_{i}", [128, D], F32, True) for i in range(2)]
            x1n = m1
            h2T = sbt(cw, "h2T", [128, 8, 128], F32)
            h2f = m2
            h2p = [sbt(cw, f"h2p{i}", [128, D], BF16, True) for i in range(1)]
            st2 = sbt(cw, "st2", [128, 4], F32)
            lgall = sbt(cw, "lgall", [128, NOWN, 36], F32)
            rtb = sbt(cw, "rtb", [128, 8, NOWN], F32)
            s1out = {}

            vnbB = [sbt(cw, f"vnbB{i}", [128, 512], BF16) for i in range(2)]
            gubB = [sbt(cw, f"gubB{i}", [128, 512], BF16) for i in range(2)]
            fe_st = {}

            def S1(i):
                h1 = h1c[0]; sig = sigB[i % 2]; vnb_ = vnbB[i % 2]; gub = gubB[i % 2]
                if i not in fe_st:
                    fe_st[i] = fe_a([xo[i * 128:(i + 1) * 128, :]])
                xt = fe_b(fe_st.pop(i), h1.t, h1.r, [0])[0]
                s1out[i] = (xt, vnb_, gub, sig)
                load(otile[i % 2], OS[i * 128:(i + 1) * 128, :])
                pu = pnext(); pv = pnext()
                for hh, pp in enumerate((pu, pv)):
                    for kc in range(8):
                        P(lambda: nc.tensor.matmul(pp.t[:], lhsT=h1.t[:, kc, :], rhs=wsg.t[:, kc, hh * 512:(hh + 1) * 512],
                                                   start=(kc == 0), stop=False), [h1.r, wsg.r], [pp.r])
                    P(lambda: nc.tensor.matmul(pp.t[:], lhsT=onesb.t[0:1, :], rhs=browc.t[0:1, hh * 512:(hh + 1) * 512],
                                               start=False, stop=True), [onesb.r, browc.r], [pp.r])
                pg = [pnext() for _ in range(4)]
                for q4 in range(4):
                    for kc in range(8):
                        P(lambda: nc.tensor.matmul(pg[q4].t[:], lhsT=h1.t[:, kc, :], rhs=wgl.t[:, kc, q4 * 512:(q4 + 1) * 512],
                                                   start=(kc == 0), stop=False), [h1.r, wgl.r], [pg[q4].r])
                    P(lambda: nc.tensor.matmul(pg[q4].t[:], lhsT=onesb.t[0:1, :], rhs=browc.t[0:1, 1024 + q4 * 512:1024 + (q4 + 1) * 512],
                                               start=False, stop=True), [onesb.r, browc.r], [pg[q4].r])
                for hh, pp in enumerate((pu, pv)):
                    cs_ = slice(hh * 512, (hh + 1) * 512)
                    A(lambda: nc.scalar.copy(out=uv.t[:, cs_], in_=pp.t[:]), [pp.r], [uv.r])
                    A(lambda: nc.scalar.activation(out=x2.t[:, cs_], in_=pp.t[:], func=AF.Square, scale=0.044715 ** 0.5),
                      [pp.r], [x2.r])
                V(lambda: nc.vector.scalar_tensor_tensor(out=inner.t[:], in0=x2.t[:], scalar=1.0, in1=uv.t[:], op0=ALU.add,
                                                         op1=ALU.mult), [x2.r, uv.r], [inner.r])
                for q4 in range(4):
                    A(lambda: nc.scalar.activation(out=sig.t[:, q4 * 512:(q4 + 1) * 512], in_=pg[q4].t[:], func=AF.Sigmoid),
                      [pg[q4].r], [sig.r])
                A(lambda: nc.scalar.activation(out=inner.t[:], in_=inner.t[:], func=AF.Sigmoid, scale=GELU_C), [inner.r], [inner.r])
                if i + 1 < NOWN:
                    fe_st[i + 1] = fe_a([xo[(i + 1) * 128:(i + 2) * 128, :]])
                G(lambda: nc.gpsimd.tensor_tensor(out=gel.t[:, 0:512], in0=inner.t[:, 512:1024], in1=uv.t[:, 512:1024], op=ALU.mult),
                  [inner.r, uv.r], [gel.r])
                G(lambda: nc.gpsimd.tensor_tensor(out=gub.t[:], in0=inner.t[:, 0:512], in1=uv.t[:, 0:512], op=ALU.mult),
                  [inner.r, uv.r], [gub.r])
                gv_ = gel.t[:, 0:512]
                V(lambda: nc.vector.tensor_reduce(out=lst.t[:, 0:1], in_=gv_, axis=AX.X, op=ALU.add), [gel.r], [lst.r])
                V(lambda: nc.vector.tensor_scalar(out=lst.t[:, 1:2], in0=lst.t[:, 0:1], scalar1=1.0 / 512, scalar2=None,
                                                  op0=ALU.mult), [lst.r], [lst.r])
                V(lambda: nc.vector.tensor_scalar(out=x2.t[:, 0:512], in0=gv_, scalar1=lst.t[:, 1:2], scalar2=None,
                                                  op0=ALU.subtract), [gel.r, lst.r], [x2.r])
                A(lambda: nc.scalar.activation(out=x2.t[:, 512:1024], in_=x2.t[:, 0:512], func=AF.Square, accum_out=lst.t[:, 2:3]),
                  [x2.r], [x2.r, lst.r])
                V(lambda: nc.vector.tensor_scalar(out=lst.t[:, 3:4], in0=lst.t[:, 2:3], scalar1=1.0 / 512, scalar2=EPS,
                                                  op0=ALU.mult, op1=ALU.add), [lst.r], [lst.r])
                A(lambda: nc.scalar.activation(out=lst.t[:, 4:5], in_=lst.t[:, 3:4], func=AF.Sqrt), [lst.r], [lst.r])
                V(lambda: nc.vector.reciprocal(out=lst.t[:, 5:6], in_=lst.t[:, 4:5]), [lst.r], [lst.r])
                V(lambda: nc.vector.scalar_tensor_tensor(out=x2.t[:, 0:512], in0=x2.t[:, 0:512], scalar=lst.t[:, 5:6],
                                                         in1=gvb.t[:], op0=ALU.mult, op1=ALU.mult), [x2.r, lst.r, gvb.r], [x2.r])
                V(lambda: nc.vector.tensor_tensor(out=vnb_.t[:], in0=x2.t[:, 0:512], in1=bvb.t[:], op=ALU.add),
                  [x2.r, bvb.r], [vnb_.r])

            def S2(i):
                xt, vnb_, gub, sig = s1out.pop(i)
                sgo = sgoB[0]
                psg = pnext()
                for g in range(8):
                    P(lambda: nc.tensor.matmul(psg.t[:, g * 64:(g + 1) * 64], lhsT=wsT.t[:, g, :], rhs=vnb_.t[:, g * 64:(g + 1) * 64],
                                               start=True, stop=True), [wsT.r, vnb_.r], [psg.r])
                ot_ = otile[i % 2]
                tp = pnext(); tpb = tp.t[:].bitcast(BF16)
                for kc in range(4):
                    P(lambda: nc.tensor.transpose(out=tpb[:, kc * 128:(kc + 1) * 128], in_=ot_.t[:, kc * 128:(kc + 1) * 128],
                                                  identity=identb.t[:]), [ot_.r, identb.r], [tp.r])
                A(lambda: nc.scalar.copy(out=OT.t[:].rearrange("p k t -> p (k t)"), in_=tpb[:, 0:512]), [tp.r], [OT.r])
                V(lambda: nc.vector.tensor_tensor(out=m2.t[:, 0:512].rearrange("p (g c) -> p g c", g=8),
                                                  in0=psg.t[:].rearrange("p (g c) -> p g c", g=8),
                                                  in1=bst.t[:].unsqueeze(2).to_broadcast([128, 8, 64]), op=ALU.add),
                  [psg.r, bst.r], [m2.r])
                V(lambda: nc.vector.tensor_tensor(out=sgo.t[:], in0=m2.t[:, 0:512], in1=gub.t[:], op=ALU.mult),
                  [m2.r, gub.r], [sgo.r])
                for hh in range(2):
                    ps = pnext()
                    for kc in range(4):
                        P(lambda: nc.tensor.matmul(ps.t[:], lhsT=OT.t[:, kc, :], rhs=wbm.t[:, kc, hh * 512:(hh + 1) * 512],
                                                   start=(kc == 0), stop=(kc == 3)), [OT.r, wbm.r], [ps.r])
                    V(lambda: nc.vector.tensor_tensor(out=m1.t[:, hh * 512:(hh + 1) * 512], in0=ps.t[:],
                                                      in1=sig.t[:, hh * 512:(hh + 1) * 512], op=ALU.mult), [ps.r, sig.r], [m1.r])
                tp2 = pnext(); tpb2 = tp2.t[:].bitcast(BF16)
                for kc in range(4):
                    P(lambda: nc.tensor.transpose(out=tpb2[:, kc * 128:(kc + 1) * 128], in_=sgo.t[:, kc * 128:(kc + 1) * 128],
                                                  identity=identb.t[:]), [sgo.r, identb.r], [tp2.r])
                A(lambda: nc.scalar.copy(out=SGT.t[:].rearrange("p k t -> p (k t)"), in_=tpb2[:, 0:512]), [tp2.r], [SGT.r])
                for hh in range(2):
                    ps = pnext()
                    for kc in range(4):
                        P(lambda: nc.tensor.matmul(ps.t[:], lhsT=SGT.t[:, kc, :], rhs=wbs.t[:, kc, hh * 512:(hh + 1) * 512],
                                                   start=(kc == 0), stop=(kc == 3)), [SGT.r, wbs.r], [ps.r])
                    V(lambda: nc.vector.tensor_tensor(out=m2.t[:, hh * 512:(hh + 1) * 512], in0=ps.t[:],
                                                      in1=sig.t[:, 1024 + hh * 512:1024 + (hh + 1) * 512], op=ALU.mult),
                      [ps.r, sig.r], [m2.r])
                    V(lambda: nc.vector.tensor_tensor(out=mrg.t[:, hh * 512:(hh + 1) * 512], in0=m1.t[:, hh * 512:(hh + 1) * 512],
                                                      in1=m2.t[:, hh * 512:(hh + 1) * 512], op=ALU.add), [m1.r, m2.r], [mrg.r])
                tp = pnext(); tpb = tp.t[:].bitcast(BF16)
                for kc in range(8):
                    P(lambda: nc.tensor.transpose(out=tpb[:, kc * 128:(kc + 1) * 128], in_=mrg.t[:, kc * 128:(kc + 1) * 128],
                                                  identity=identb.t[:]), [mrg.r, identb.r], [tp.r])
                A(lambda: nc.scalar.copy(out=mT.t[:].rearrange("p k t -> p (k t)"), in_=tpb[:, :]), [tp.r], [mT.r])
                xx = x1[i % 2]
                for hh in range(2):
                    ps = pnext()
                    for kc in range(8):
                        P(lambda: nc.tensor.matmul(ps.t[:], lhsT=mT.t[:, kc, :], rhs=wo.t[:, kc, hh * 512:(hh + 1) * 512],
                                                   start=(kc == 0), stop=(kc == 7)), [mT.r, wo.r], [ps.r])
                    cs_ = slice(hh * 512, (hh + 1) * 512)
                    V(lambda: nc.vector.tensor_tensor(out=m1.t[:, cs_], in0=ps.t[:], in1=gate1b.t[:, cs_], op=ALU.mult),
                      [ps.r, gate1b.r], [m1.r])
                V(lambda: nc.vector.tensor_tensor(out=xx.t[:], in0=m1.t[:], in1=xt.t[:], op=ALU.add), [m1.r, xt.r], [xx.r])
                T.dma(SP, xx.l, lambda: nc.sync.dma_start(out=X1[i * 128:(i + 1) * 128, :], in_=xx.t[:]), reads=[xx.r])

            def S3(i):
                xx = x1[i % 2]
                junk = FE["junk"]
                A(lambda: nc.scalar.activation(out=junk.t[:], in_=xx.t[:], func=AF.Square, accum_out=st2.t[:, 0:1]),
                  [xx.r], [junk.r, st2.r])
                V(lambda: nc.vector.tensor_scalar(out=st2.t[:, 1:2], in0=st2.t[:, 0:1], scalar1=1.0 / D, scalar2=EPS,
                                                  op0=ALU.mult, op1=ALU.add), [st2.r], [st2.r])
                A(lambda: nc.scalar.activation(out=st2.t[:, 2:3], in_=st2.t[:, 1:2], func=AF.Sqrt), [st2.r], [st2.r])
                V(lambda: nc.vector.reciprocal(out=st2.t[:, 3:4], in_=st2.t[:, 2:3]), [st2.r], [st2.r])
                V(lambda: nc.vector.tensor_scalar(out=x1n.t[:], in0=xx.t[:], scalar1=st2.t[:, 3:4], scalar2=None, op0=ALU.mult),
                  [xx.r, st2.r], [x1n.r])
                G(lambda: nc.gpsimd.tensor_tensor(out=h2f.t[:], in0=x1n.t[:], in1=s2b.t[:], op=ALU.mult), [x1n.r, s2b.r], [h2f.r])
                hp = h2p[0]
                G(lambda: nc.gpsimd.tensor_tensor(out=hp.t[:].rearrange("t (kc p) -> t p kc", kc=8),
                                                  in0=h2f.t[:].rearrange("t (p kc) -> t p kc", kc=8),
                                                  in1=sh2b.t[:].rearrange("t (p kc) -> t p kc", kc=8), op=ALU.add),
                  [h2f.r, sh2b.r], [hp.r])
                T.dma(SP, hp.l, lambda: nc.sync.dma_start(out=H2[i * 128:(i + 1) * 128, :], in_=hp.t[:]), reads=[hp.r])
                tA = pnext(); tB = pnext()
                for kc in range(8):
                    tt = tA if kc < 4 else tB
                    P(lambda: nc.tensor.transpose(out=tt.t[:, (kc % 4) * 128:(kc % 4 + 1) * 128], in_=x1n.t[:, kc * 128:(kc + 1) * 128],
                                                  identity=identf.t[:]), [x1n.r, identf.r], [tt.r])
                for kc in range(8):
                    tt = tA if kc < 4 else tB
                    V(lambda: nc.vector.tensor_scalar(out=h2T.t[:, kc, :], in0=tt.t[:, (kc % 4) * 128:(kc % 4 + 1) * 128],
                                                      scalar1=s2c.t[:, kc:kc + 1], scalar2=sh2(kc), op0=ALU.mult, op1=ALU.add),
                      [tt.r, s2c.r, modT.r], [h2T.r])
                pl = pnext()
                for kc in range(8):
                    P(lambda: nc.tensor.matmul(pl.t[:, 0:36], lhsT=h2T.t[:, kc, :], rhs=wrf.t[:, kc, :], start=(kc == 0), stop=(kc == 7)),
                      [h2T.r, wrf.r], [pl.r])
                V(lambda: nc.vector.tensor_tensor(out=lgall.t[:, i, :], in0=pl.t[:, 0:36], in1=brb.t[:], op=ALU.add), [pl.r, brb.r], [lgall.r])

            for step in range(NOWN + 2):
                if step < NOWN:
                    S1(step)
                if 0 <= step - 1 < NOWN:
                    S2(step - 1)
                if 0 <= step - 2 < NOWN:
                    S3(step - 2)

            TT = NOWN
            Lg = lgall.t[:, :, 0:4]
            Le4 = lgall.t[:, :, 4:36].rearrange("p t (g e) -> p t g e", g=4)
            gm = rtb.t[:, 0, :]; se = rtb.t[:, 1, :]; pgr = rtb.t[:, 2, :]; mv1 = rtb.t[:, 3, :]; mv2 = rtb.t[:, 4, :]
            dd_ = rtb.t[:, 5, :]; ee_ = rtb.t[:, 6, :]; rr_ = rtb.t[:, 7, :]
            ex = uv.t[:, 0:TT * 4].rearrange("p (t g) -> p t g", g=4)
            pen3 = x2.t[:, 0:TT * 4].rearrange("p (t g) -> p t g", g=4)
            Mx3 = inner.t[:, 0:TT * 32].rearrange("p (t e) -> p t e", e=32)
            Mx4 = inner.t[:, 0:TT * 32].rearrange("p (t g e) -> p t g e", g=4, e=8)
            My3 = m1.t[:, 0:TT * 32].rearrange("p (t e) -> p t e", e=32)
            bc3 = lambda ap2, n: ap2.unsqueeze(2).to_broadcast([128, TT, n])
            V(lambda: nc.vector.tensor_reduce(out=gm, in_=Lg, axis=AX.X, op=ALU.max), [lgall.r], [rtb.r])
            V(lambda: nc.vector.tensor_tensor(out=ex, in0=Lg, in1=bc3(gm, 4), op=ALU.subtract), [lgall.r, rtb.r], [uv.r])
            A(lambda: nc.scalar.activation(out=ex, in_=ex, func=AF.Exp), [uv.r], [uv.r])
            V(lambda: nc.vector.tensor_reduce(out=se, in_=ex, axis=AX.X, op=ALU.add), [uv.r], [rtb.r])
            V(lambda: nc.vector.reciprocal(out=pgr, in_=se), [rtb.r], [rtb.r])
            V(lambda: nc.vector.tensor_tensor(out=pen3, in0=Lg, in1=bc3(gm, 4), op=ALU.is_equal), [lgall.r, rtb.r], [x2.r])
            V(lambda: nc.vector.tensor_scalar(out=pen3, in0=pen3, scalar1=-1.0, scalar2=1e30, op0=ALU.add, op1=ALU.mult),
              [x2.r], [x2.r])
            V(lambda: nc.vector.tensor_tensor(out=Mx4, in0=Le4, in1=pen3.unsqueeze(3).to_broadcast([128, TT, 4, 8]), op=ALU.add),
              [lgall.r, x2.r], [inner.r])
            V(lambda: nc.vector.tensor_reduce(out=mv1, in_=Mx3, axis=AX.X, op=ALU.max), [inner.r], [rtb.r])
            V(lambda: nc.vector.tensor_tensor(out=A0all.t[:], in0=Mx3, in1=bc3(mv1, 32), op=ALU.is_equal), [inner.r, rtb.r], [A0all.r])
            V(lambda: nc.vector.scalar_tensor_tensor(out=My3, in0=A0all.t[:], scalar=-1e30, in1=Mx3, op0=ALU.mult, op1=ALU.add),
              [A0all.r, inner.r], [m1.r])
            V(lambda: nc.vector.tensor_reduce(out=mv2, in_=My3, axis=AX.X, op=ALU.max), [m1.r], [rtb.r])
            V(lambda: nc.vector.tensor_tensor(out=A1all.t[:], in0=My3, in1=bc3(mv2, 32), op=ALU.is_equal), [m1.r, rtb.r], [A1all.r])
            V(lambda: nc.vector.tensor_tensor(out=Aall.t[:], in0=A0all.t[:], in1=A1all.t[:], op=ALU.add), [A0all.r, A1all.r], [Aall.r])
            V(lambda: nc.vector.tensor_tensor(out=dd_, in0=mv2, in1=mv1, op=ALU.subtract), [rtb.r], [rtb.r])
            A(lambda: nc.scalar.activation(out=ee_, in_=dd_, func=AF.Exp), [rtb.r], [rtb.r])
            V(lambda: nc.vector.tensor_scalar(out=ee_, in0=ee_, scalar1=1.0, scalar2=None, op0=ALU.add), [rtb.r], [rtb.r])
            V(lambda: nc.vector.reciprocal(out=rr_, in_=ee_), [rtb.r], [rtb.r])
            V(lambda: nc.vector.tensor_tensor(out=wts.t[:, :, 0], in0=rr_, in1=pgr, op=ALU.mult), [rtb.r], [wts.r])
            V(lambda: nc.vector.tensor_tensor(out=wts.t[:, :, 1], in0=pgr, in1=wts.t[:, :, 0], op=ALU.subtract), [rtb.r, wts.r], [wts.r])
            T.barrier()
            if stop <= 3:
                return nc
        pc_.close()

        NA = NOWN * 32
        REG_SLOT = nc.gpsimd.to_reg(NSLOT - 1)
        REG_W = nc.gpsimd.to_reg(NEXP * 128 - 1)
        d0i = sbt(es, "d0i", [128, NOWN], I32); d1i = sbt(es, "d1i", [128, NOWN], I32)
        idxw = sbt(es, "idxw", [128, NBLK], I32)
        with ExitStack() as dp:
            set_rot(range(8))
            Ab_t = Aall.t[:].rearrange("p t e -> p (t e)")
            lsf = sbt(dp, "lsf", [128, 128], F32, True); load(lsf, c_lstrict)
            lsb = sbt(dp, "lsb", [128, 128], BF16)
            thr = sbt(dp, "thr", [128, NBLK], F32, True); load(thr, c_thr)
            pidx = sbt(dp, "pidx", [128, 1], F32, True); load(pidx, c_pidx)
            V(lambda: nc.vector.tensor_copy(out=lsb.t[:], in_=lsf.t[:]), [lsf.r], [lsb.r])
            tsum = sbt(dp, "tsum", [128, NOWN, 32], F32)
            rloc = sbt(dp, "rloc", [128, NOWN, 32], F32)
            for c0 in range(0, NA, 512):
                w_ = min(512, NA - c0)
                ps = pnext()
                P(lambda: nc.tensor.matmul(ps.t[:, 0:w_], lhsT=onesb.t[:], rhs=Ab_t[:, c0:c0 + w_], start=True, stop=True),
                  [onesb.r, Aall.r], [ps.r])
                V(lambda: nc.vector.tensor_copy(out=tsum.t[:].rearrange("p t e -> p (t e)")[:, c0:c0 + w_], in_=ps.t[:, 0:w_]),
                  [ps.r], [tsum.r])
                ps2 = pnext()
                P(lambda: nc.tensor.matmul(ps2.t[:, 0:w_], lhsT=lsb.t[:], rhs=Ab_t[:, c0:c0 + w_], start=True, stop=True),
                  [lsb.r, Aall.r], [ps2.r])
                V(lambda: nc.vector.tensor_copy(out=rloc.t[:].rearrange("p t e -> p (t e)")[:, c0:c0 + w_], in_=ps2.t[:, 0:w_]),
                  [ps2.r], [rloc.r])
            cum = sbt(dp, "cum", [128, NOWN + 1, 32], F32)
            V(lambda: nc.vector.memset(cum.t[:, 0, :], 0.0), [], [cum.r])
            for t in range(NOWN):
                V(lambda: nc.vector.tensor_tensor(out=cum.t[:, t + 1, :], in0=cum.t[:, t, :], in1=tsum.t[:, t, :], op=ALU.add),
                  [cum.r, tsum.r], [cum.r])
            ci = sbt(dp, "ci", [128, 32], I32)
            pad = sbt(dp, "pad", [128, 32], F32)
            pend = sbt(dp, "pend", [128, 32], F32)
            pst = sbt(dp, "pst", [128, 32], F32)
            V(lambda: nc.vector.tensor_scalar(out=pad.t[:], in0=cum.t[:, NOWN, :], scalar1=float(BLK - 1), scalar2=None,
                                              op0=ALU.add), [cum.r], [pad.r])
            V(lambda: nc.vector.tensor_copy(out=ci.t[:], in_=pad.t[:]), [pad.r], [ci.r])
            V(lambda: nc.vector.tensor_scalar(out=ci.t[:], in0=ci.t[:], scalar1=8, scalar2=8, op0=ALU.arith_shift_right,
                                              op1=ALU.logical_shift_left), [ci.r], [ci.r])
            V(lambda: nc.vector.tensor_copy(out=pad.t[:], in_=ci.t[:]), [ci.r], [pad.r])
            V(lambda: nc.vector.tensor_tensor_scan(out=pend.t[:], data0=onesf.t[:, 0:32], data1=pad.t[:], initial=0.0,
                                                   op0=ALU.mult, op1=ALU.add), [onesf.r, pad.r], [pend.r])
            V(lambda: nc.vector.tensor_tensor(out=pst.t[:], in0=pend.t[:], in1=pad.t[:], op=ALU.subtract), [pend.r, pad.r], [pst.r])
            V(lambda: nc.vector.tensor_tensor(out=rloc.t[:], in0=rloc.t[:], in1=cum.t[:, 0:NOWN, :], op=ALU.add),
              [rloc.r, cum.r], [rloc.r])
            V(lambda: nc.vector.tensor_tensor(out=rloc.t[:], in0=rloc.t[:], in1=pst.t[:].unsqueeze(1).to_broadcast([128, NOWN, 32]),
                                              op=ALU.add), [rloc.r, pst.r], [rloc.r])
            df = sbt(dp, "df", [128, NOWN], F32)
            for Ak, dk in ((A0all, d0i), (A1all, d1i)):
                V(lambda: nc.vector.tensor_tensor(out=tsum.t[:], in0=Ak.t[:], in1=rloc.t[:], op=ALU.mult), [Ak.r, rloc.r, tsum.r], [tsum.r])
                V(lambda: nc.vector.tensor_reduce(out=df.t[:], in_=tsum.t[:], axis=AX.X, op=ALU.add), [tsum.r], [df.r])
                V(lambda: nc.vector.tensor_copy(out=dk.t[:], in_=df.t[:]), [df.r], [dk.r])
            cmp_ = sbt(dp, "cmp", [128, NBLK, 32], F32)
            bef = sbt(dp, "bef", [128, NBLK], F32)
            V(lambda: nc.vector.tensor_tensor(out=cmp_.t[:], in0=pend.t[:].unsqueeze(1).to_broadcast([128, NBLK, 32]),
                                              in1=thr.t[:].unsqueeze(2).to_broadcast([128, NBLK, 32]), op=ALU.is_le),
              [pend.r, thr.r], [cmp_.r])
            V(lambda: nc.vector.tensor_reduce(out=bef.t[:], in_=cmp_.t[:], axis=AX.X, op=ALU.add), [cmp_.r], [bef.r])
            V(lambda: nc.vector.tensor_scalar(out=bef.t[:], in0=bef.t[:], scalar1=float(NEXP - 1), scalar2=128.0, op0=ALU.min,
                                              op1=ALU.mult), [bef.r], [bef.r])
            V(lambda: nc.vector.tensor_scalar(out=bef.t[:], in0=bef.t[:], scalar1=pidx.t[:, 0:1], scalar2=None, op0=ALU.add),
              [bef.r, pidx.r], [bef.r])
            skp = sbt(dp, "skp", [128, NBLK], F32)
            V(lambda: nc.vector.tensor_scalar(out=skp.t[:], in0=thr.t[:], scalar1=pend.t[:, 31:32], scalar2=1.0e6, op0=ALU.is_ge,
                                              op1=ALU.mult), [thr.r, pend.r], [skp.r])
            V(lambda: nc.vector.tensor_tensor(out=bef.t[:], in0=bef.t[:], in1=skp.t[:], op=ALU.add), [bef.r, skp.r], [bef.r])
            V(lambda: nc.vector.tensor_copy(out=idxw.t[:], in_=bef.t[:]), [bef.r], [idxw.r])
            hb = [sbt(dp, f"hb{i}", [128, D], BF16, True) for i in range(3)]
            xs_res = Res("XS")
            for t in range(NOWN):
                b_ = hb[t % 3]
                load(b_, H2[t * 128:(t + 1) * 128, :])
                for dk in (d0i, d1i):
                    T.dma(POOL, b_.l, lambda: nc.gpsimd.indirect_dma_start(
                        out=XS, out_offset=bass.IndirectOffsetOnAxis(ap=dk.t[:, t:t + 1], axis=0), in_=b_.t[:], in_offset=None,
                        bounds_check=REG_SLOT, oob_is_err=False),
                        reads=[b_.r, dk.r])
            T.barrier()
            if stop <= 4:
                return nc

        with ExitStack() as dd:
            set_rot(range(8))
            stg = [[sbt(dd, f"stg{i}_{k}", [128, 4096], F32, True) for k in range(3)] for i in range(2)]
            w1b = [sbt(dd, f"w1b{i}", [128, 8, 4, 128], BF16) for i in range(2)]
            w3b = [sbt(dd, f"w3b{i}", [128, 8, 4, 128], BF16) for i in range(2)]
            w2b = [sbt(dd, f"w2b{i}", [128, 4, D], BF16) for i in range(2)]
            xbk = [sbt(dd, f"xbk{i}", [128, 2, D], BF16, True) for i in range(2)]
            xTb = [sbt(dd, f"xTb{i}", [128, 8, 256], BF16) for i in range(1)]
            sil = sbt(dd, "sil", [128, 256], F32)
            gTb = [sbt(dd, f"gTb{i}", [128, 4, 256], BF16) for i in range(2)]
            ysb = [sbt(dd, f"ysb{i}", [128, D], F32, True) for i in range(2)]
            yc = 0

            def issue_w(j):
                s_ = stg[j % 2]
                for k, wsrc in enumerate((w1, w3, w2)):
                    T.dma(POOL, s_[k].l, lambda: nc.gpsimd.indirect_dma_start(
                        out=s_[k].t[:], out_offset=None, in_=wsrc,
                        in_offset=bass.IndirectOffsetOnAxis(ap=idxw.t[:, j:j + 1], axis=0),
                        bounds_check=REG_W, oob_is_err=False),
                        reads=[idxw.r], writes=[s_[k].r])

            def issue_x(j):
                load(xbk[j % 2], XS[j * BLK:(j + 1) * BLK, :].rearrange("(a p) d -> p a d", p=128))

            issue_w(0)
            issue_x(0)
            for j in range(NBLK):
                if j + 1 < NBLK:
                    issue_w(j + 1)
                    issue_x(j + 1)
                s_ = stg[j % 2]; b1 = w1b[j % 2]; b3 = w3b[j % 2]; b2 = w2b[j % 2]
                V(lambda: nc.vector.tensor_copy(out=b1.t[:], in_=s_[0].t[:].rearrange("p (kc m fc) -> p kc fc m", kc=8, m=128, fc=4)),
                  [s_[0].r], [b1.r])
                V(lambda: nc.vector.tensor_copy(out=b3.t[:], in_=s_[1].t[:].rearrange("p (kc m fc) -> p kc fc m", kc=8, m=128, fc=4)),
                  [s_[1].r], [b3.r])
                A(lambda: nc.scalar.copy(out=b2.t[:].rearrange("p f n -> p (f n)"), in_=s_[2].t[:]), [s_[2].r], [b2.r])
                xk = xbk[j % 2]; xT_ = xTb[0]; gT_ = gTb[j % 2]
                for st_ in range(2):
                    tp = pnext(); tpb = tp.t[:].bitcast(BF16)
                    for kc in range(8):
                        P(lambda: nc.tensor.transpose(out=tpb[:, kc * 128:(kc + 1) * 128], in_=xk.t[:, st_, kc * 128:(kc + 1) * 128],
                                                      identity=identb.t[:]), [xk.r, identb.r], [tp.r])
                    cp = (lambda f: A(f, [tp.r], [xT_.r])) if st_ == 0 else (lambda f: V(f, [tp.r], [xT_.r]))
                    if st_ == 0:
                        A(lambda: nc.scalar.copy(out=xT_.t[:, :, 0:128], in_=tpb.rearrange("p (k t) -> p k t", k=8)), [tp.r], [xT_.r])
                    else:
                        V(lambda: nc.vector.tensor_copy(out=xT_.t[:, :, 128:256], in_=tpb.rearrange("p (k t) -> p k t", k=8)),
                          [tp.r], [xT_.r])
                for fc in range(4):
                    pa = pnext(); pb_ = pnext()
                    for kc in range(8):
                        P(lambda: nc.tensor.matmul(pa.t[:, 0:256], lhsT=b1.t[:, kc, fc, :], rhs=xT_.t[:, kc, :], start=(kc == 0),
                                                   stop=(kc == 7)), [b1.r, xT_.r], [pa.r])
                    for kc in range(8):
                        P(lambda: nc.tensor.matmul(pb_.t[:, 0:256], lhsT=b3.t[:, kc, fc, :], rhs=xT_.t[:, kc, :], start=(kc == 0),
                                                   stop=(kc == 7)), [b3.r, xT_.r], [pb_.r])
                    A(lambda: nc.scalar.activation(out=sil.t[:], in_=pa.t[:, 0:256], func=AF.Silu), [pa.r], [sil.r])
                    V(lambda: nc.vector.tensor_tensor(out=gT_.t[:, fc, :], in0=pb_.t[:, 0:256], in1=sil.t[:], op=ALU.mult),
                      [pb_.r, sil.r], [gT_.r])
                for st_ in range(2):
                    y_ = ysb[yc % 2]; yc += 1
                    for hh in range(2):
                        ps = pnext()
                        for fc in range(4):
                            P(lambda: nc.tensor.matmul(ps.t[:], lhsT=gT_.t[:, fc, st_ * 128:(st_ + 1) * 128],
                                                       rhs=b2.t[:, fc, hh * 512:(hh + 1) * 512], start=(fc == 0), stop=(fc == 3)),
                              [gT_.r, b2.r], [ps.r])
                        if hh == 0:
                            A(lambda: nc.scalar.copy(out=y_.t[:, 0:512], in_=ps.t[:]), [ps.r], [y_.r])
                        else:
                            V(lambda: nc.vector.tensor_copy(out=y_.t[:, 512:1024], in_=ps.t[:]), [ps.r], [y_.r])
                    r0 = j * BLK + st_ * 128
                    T.dma(SP, y_.l, lambda: nc.sync.dma_start(out=YS[r0:r0 + 128, :], in_=y_.t[:]), reads=[y_.r])
            T.barrier()
            if stop <= 5:
                return nc

        with ExitStack() as ee:
            set_rot(range(8))
            gfb = sbt(ee, "gfb", [128, D], F32, True); load(gfb, gf_row.partition_broadcast(128))
            y0 = [sbt(ee, f"y0_{i}", [128, D], F32, True) for i in range(2)]
            y1 = [sbt(ee, f"y1_{i}", [128, D], F32, True) for i in range(2)]
            xr = [sbt(ee, f"xr{i}", [128, D], F32, True) for i in range(2)]
            acc = sbt(ee, "acc", [128, D], F32)
            jk = sbt(ee, "jk", [128, D], F32)
            ob = [sbt(ee, f"ob{i}", [128, D], F32, True) for i in range(2)]
            st3 = sbt(ee, "st3", [128, 4], F32)
            def issue_e(t):
                a0 = y0[t % 2]; a1 = y1[t % 2]; xx = xr[t % 2]
                T.dma(POOL, a0.l, lambda: nc.gpsimd.indirect_dma_start(
                    out=a0.t[:], out_offset=None, in_=YS, in_offset=bass.IndirectOffsetOnAxis(ap=d0i.t[:, t:t + 1], axis=0),
                    bounds_check=REG_SLOT, oob_is_err=False),
                    reads=[d0i.r], writes=[a0.r])
                T.dma(POOL, a1.l, lambda: nc.gpsimd.indirect_dma_start(
                    out=a1.t[:], out_offset=None, in_=YS, in_offset=bass.IndirectOffsetOnAxis(ap=d1i.t[:, t:t + 1], axis=0),
                    bounds_check=REG_SLOT, oob_is_err=False),
                    reads=[d1i.r], writes=[a1.r])
                load(xx, X1[t * 128:(t + 1) * 128, :])

            issue_e(0)
            for t in range(NOWN):
                a0 = y0[t % 2]; a1 = y1[t % 2]; xx = xr[t % 2]; o_ = ob[t % 2]
                if t + 1 < NOWN:
                    issue_e(t + 1)
                V(lambda: nc.vector.tensor_scalar(out=acc.t[:], in0=a0.t[:], scalar1=wts.t[:, t, 0:1], scalar2=None, op0=ALU.mult),
                  [a0.r, wts.r], [acc.r])
                V(lambda: nc.vector.scalar_tensor_tensor(out=acc.t[:], in0=a1.t[:], scalar=wts.t[:, t, 1:2], in1=acc.t[:],
                                                         op0=ALU.mult, op1=ALU.add), [a1.r, wts.r, acc.r], [acc.r])
                G(lambda: nc.gpsimd.tensor_tensor(out=acc.t[:], in0=acc.t[:], in1=gate2b.t[:], op=ALU.mult), [acc.r, gate2b.r], [acc.r])
                G(lambda: nc.gpsimd.tensor_tensor(out=acc.t[:], in0=acc.t[:], in1=xx.t[:], op=ALU.add), [acc.r, xx.r], [acc.r])
                A(lambda: nc.scalar.activation(out=jk.t[:], in_=acc.t[:], func=AF.Square, accum_out=st3.t[:, 0:1]),
                  [acc.r], [jk.r, st3.r])
                V(lambda: nc.vector.tensor_scalar(out=st3.t[:, 1:2], in0=st3.t[:, 0:1], scalar1=1.0 / D, scalar2=EPS,
                                                  op0=ALU.mult, op1=ALU.add), [st3.r], [st3.r])
                A(lambda: nc.scalar.activation(out=st3.t[:, 2:3], in_=st3.t[:, 1:2], func=AF.Sqrt), [st3.r], [st3.r])
                V(lambda: nc.vector.reciprocal(out=st3.t[:, 3:4], in_=st3.t[:, 2:3]), [st3.r], [st3.r])
                V(lambda: nc.vector.scalar_tensor_tensor(out=o_.t[:], in0=acc.t[:], scalar=st3.t[:, 3:4], in1=gfb.t[:],
                                                         op0=ALU.mult, op1=ALU.mult), [acc.r, st3.r, gfb.r], [o_.r])
                T.dma(SP, o_.l, lambda: nc.sync.dma_start(out=out_d[t * 128:(t + 1) * 128, :], in_=o_.t[:]), reads=[o_.r])
            T.barrier()
    return nc


def _own_tiles(j, NP):
    t = []
    for m in range(NP):
        t += [8 * m + j, 8 * m + 7 - j]
    return t


def make_in_maps(inp, S):
    f32 = np.float32
    NT = S // 128; NP = NT // 8; NOWN = 2 * NP; TOK = NOWN * 128
    NBLK = (TOK * 2) // BLK + NEXP
    g = lambda k: np.asarray(inp[k])
    col = lambda v, n: np.ascontiguousarray(np.asarray(v, f32).reshape(n, 128).T)
    x = g("x"); c = g("c"); pos = g("positions")
    p = np.arange(128)
    fq = np.zeros((128, 1), f32)
    freqs = (np.float32(10000.0) ** (-np.arange(16, dtype=f32) / np.float32(16))).astype(f32)
    fq[64:96, 0] = np.tile(freqs, 2) / np.float32(2 * np.pi)
    shared = dict(
        w_ada=np.ascontiguousarray(g("w_ada")[0]), b_adaT=col(g("b_ada")[0], 48), b_ada_row=g("b_ada")[0].reshape(1, -1).astype(f32),
        g1col=col(g("norm1_g")[0], 8), g1row=g("norm1_g")[0].reshape(1, -1).astype(f32), w_in=np.ascontiguousarray(g("w_in")[0]),
        gqcol=col(g("q_norm_g")[0], 2), w_uq=np.ascontiguousarray(g("w_uq")[0]),
        gkvcol=col(g("kv_norm_g")[0], 1), w_ukv=np.ascontiguousarray(g("w_ukv")[0]),
        gv_row=g("v_norm_g")[0].reshape(1, -1).astype(f32), bv_row=g("v_norm_b")[0].reshape(1, -1).astype(f32),
        w_s=np.ascontiguousarray(g("w_s")[0]), bsT=np.ascontiguousarray(g("b_s")[0].T),
        w_br_mla=np.ascontiguousarray(g("w_br_mla")[0]), w_br_sgu=np.ascontiguousarray(g("w_br_sgu")[0]),
        w_out=np.ascontiguousarray(g("w_out")[0]),
        g2col=col(g("norm2_g")[0], 8), g2row=g("norm2_g")[0].reshape(1, -1).astype(f32),
        wr=np.ascontiguousarray(np.concatenate([g("w_rg")[0], g("w_re")[0]], axis=1)),
        br_row=np.concatenate([g("b_rg")[0], g("b_re")[0]]).reshape(1, -1).astype(f32),
        w1=np.ascontiguousarray(g("w1")[0]).reshape(NEXP * 128, 4096),
        w3=np.ascontiguousarray(g("w3")[0]).reshape(NEXP * 128, 4096),
        w2=np.ascontiguousarray(g("w2")[0]).reshape(NEXP * 128, 4096),
        gf_row=g("final_g").reshape(1, -1).astype(f32),
        c_ident=np.eye(128, dtype=f32), c_tril=np.tril(np.ones((128, 128), f32)),
        c_lstrict=(p[:, None] < p[None, :]).astype(f32), c_kq=(p[:, None] - p[None, :]).astype(f32),
        c_fq=fq, c_thr=np.tile((np.arange(NBLK, dtype=f32) * BLK)[None, :], (128, 1)).astype(f32),
        c_pidx=p.astype(f32).reshape(128, 1),
    )
    maps = []
    owns = []
    for core in range(8):
        b, j = core // 4, core % 4
        tiles = _own_tiles(j, NP)
        rows = np.concatenate([np.arange(t * 128, (t + 1) * 128) for t in tiles])
        owns.append((b, rows))
        dt_ = np.zeros((128, 16), f32)
        for o in range(8):
            dt_[:, o] = (j - o) * 128
            dt_[:, 8 + o] = (7 - j - o) * 128
        m = dict(shared)
        m.update(
            xb=np.ascontiguousarray(x[b, :S]), xo=np.ascontiguousarray(x[b][rows]),
            posb=np.ascontiguousarray(pos[b, :S].reshape(1, -1).astype(np.int32)),
            poso=np.ascontiguousarray(pos[b][rows].reshape(1, -1).astype(np.int32)),
            ccol=col(c[b], 8), c_dtab=dt_,
        )
        maps.append(m)
    return maps, owns


_NC_CACHE = {}


def kernel(**inputs):
    S = int(np.asarray(inputs["x"]).shape[1])
    if S not in _NC_CACHE:
        _NC_CACHE[S] = build_nc(S)
    nc = _NC_CACHE[S]
    maps, owns = make_in_maps(inputs, S)
    res = run_bass_kernel_spmd(nc, maps, core_ids=list(range(8)))
    B = np.asarray(inputs["x"]).shape[0]
    out = np.zeros((B, S, D), np.float32)
    for core, (b, rows) in enumerate(owns):
        out[b, rows] = np.asarray(res.results[core]["out"], dtype=np.float32)
    return out
```

```python
import numpy as np
from contextlib import ExitStack
import concourse.bass as bass
import concourse.mybir as mybir
from concourse.bass_utils import run_bass_kernel_spmd

F32 = mybir.dt.float32
BF16 = mybir.dt.bfloat16
I32 = mybir.dt.int32
AF = mybir.ActivationFunctionType
ALU = mybir.AluOpType
AX = mybir.AxisListType

D = 1024
NH = 8
EPS = 1e-6
NEXP = 32
FF = 512
BLK = 256
IN_W = 3488
SCALE = 96 ** -0.5
GELU_C = 1.5957691216057308


class Res:
    __slots__ = ("name", "w", "r")

    def __init__(self, name=""):
        self.name = name
        self.w = None
        self.r = {}


class Lane:
    def __init__(self, T, name):
        self.sem = T.newsem(name)
        self.count = 0


class Eng:
    def __init__(self, T, name, eng, is_pe=False):
        self.name = name
        self.eng = eng
        self.is_pe = is_pe
        self.sem = T.newsem(name + "_s0")
        self.own = [self.sem]
        self.count = 0
        self.waited = {}


class Tracker:
    CAP = 12000

    def __init__(self, nc, es):
        self.nc = nc
        self.es = es
        self.nsems = 0
        self.lanes = []
        self.pe = Eng(self, "pe", nc.tensor, True)
        self.act = Eng(self, "act", nc.scalar)
        self.dve = Eng(self, "dve", nc.vector)
        self.pool = Eng(self, "pool", nc.gpsimd)
        self.sp = Eng(self, "sp", nc.sync)
        self.engs = [self.pe, self.act, self.dve, self.pool, self.sp]

    def newsem(self, name):
        self.nsems += 1
        return self.es.enter_context(self.nc.semaphore(f"{name}_{self.nsems}"))

    def lane(self, name="l"):
        l = Lane(self, name)
        self.lanes.append(l)
        return l

    def _wait(self, E, deps):
        for sem, val in deps:
            if E.is_pe and any(sem is s for s in E.own):
                continue
            k = id(sem)
            if E.waited.get(k, 0) < val:
                E.eng.wait_ge(sem, val)
                E.waited[k] = val

    @staticmethod
    def _deps(reads, writes):
        deps = []
        for r in reads:
            if r.w is not None:
                deps.append(r.w)
        for w in writes:
            if w.w is not None:
                deps.append(w.w)
            deps.extend(w.r.values())
        return deps

    @staticmethod
    def _record(ev, reads, writes):
        for r in reads:
            r.r[id(ev[0])] = ev
        for w in writes:
            w.w = ev
            w.r = {}

    def op(self, E, fn, reads=(), writes=()):
        self._wait(E, self._deps(reads, writes))
        if E.count >= self.CAP:
            E.sem = self.newsem(E.name + "_s")
            E.own.append(E.sem)
            E.count = 0
        ins = fn()
        ins.then_inc(E.sem, 1)
        E.count += 1
        self._record((E.sem, E.count), reads, writes)

    def dma(self, E, lane, fn, reads=(), writes=()):
        self._wait(E, self._deps(reads, writes))
        ins = fn()
        ins.then_inc(lane.sem, 16)
        lane.count += 16
        self._record((lane.sem, lane.count), reads, writes)

    def barrier(self):
        evs = [(e.sem, e.count) for e in self.engs if e.count > 0]
        evs += [(l.sem, l.count) for l in self.lanes if l.count > 0]
        for E in self.engs:
            self._wait(E, evs)


class Buf:
    def __init__(self, T, tile, name, lane=False):
        self.t = tile
        self.r = Res(name)
        self.l = T.lane(name) if lane else None


def build_nc(S, stop=99):
    NT = S // 128
    NP = NT // 8
    NOWN = 2 * NP
    TOK = NOWN * 128
    NG = NT // 4
    NGO = max(NOWN // 4, 1)
    GW = min(512, TOK)
    NBLK = (TOK * 2) // BLK + NEXP
    NSLOT = NBLK * BLK

    nc = bass.Bass("TRN2", target_bir_lowering=False)

    def din(name, shape, dt=F32):
        return nc.dram_tensor(name, list(shape), dt, kind="ExternalInput").ap()

    xb = din("xb", [S, D]); xo = din("xo", [TOK, D])
    posb = din("posb", [1, S], I32); poso = din("poso", [1, TOK], I32)
    ccol = din("ccol", [128, 8])
    w_ada = din("w_ada", [D, 6 * D]); b_adaT = din("b_adaT", [128, 48]); b_ada_row = din("b_ada_row", [1, 6 * D])
    g1col = din("g1col", [128, 8]); g1row = din("g1row", [1, D]); w_in = din("w_in", [D, IN_W])
    gqcol = din("gqcol", [128, 2]); w_uq = din("w_uq", [256, 768])
    gkvcol = din("gkvcol", [128, 1]); w_ukv = din("w_ukv", [128, 1024])
    gv_row = din("gv_row", [1, 512]); bv_row = din("bv_row", [1, 512])
    w_s = din("w_s", [8, 128, 128]); bsT = din("bsT", [128, 8])
    w_br_mla = din("w_br_mla", [512, D]); w_br_sgu = din("w_br_sgu", [512, D]); w_out = din("w_out", [D, D])
    g2col = din("g2col", [128, 8]); g2row = din("g2row", [1, D])
    wr = din("wr", [D, 36]); br_row = din("br_row", [1, 36])
    w1 = din("w1", [NEXP * 128, 4096]); w3 = din("w3", [NEXP * 128, 4096]); w2 = din("w2", [NEXP * 128, 4096])
    gf_row = din("gf_row", [1, D])
    c_ident = din("c_ident", [128, 128]); c_tril = din("c_tril", [128, 128]); c_lstrict = din("c_lstrict", [128, 128])
    c_kq = din("c_kq", [128, 128]); c_fq = din("c_fq", [128, 1]); c_dtab = din("c_dtab", [128, 16])
    c_thr = din("c_thr", [128, NBLK]); c_pidx = din("c_pidx", [128, 1])

    out_d = nc.dram_tensor("out", [TOK, D], F32, kind="ExternalOutput").ap()
    X1 = nc.dram_tensor("X1s", [TOK, D], F32, kind="Internal").ap()
    H2 = nc.dram_tensor("H2s", [TOK, D], BF16, kind="Internal").ap()
    XS = nc.dram_tensor("XSs", [NSLOT, D], BF16, kind="Internal").ap()
    YS = nc.dram_tensor("YSs", [NSLOT, D], F32, kind="Internal").ap()
    OS = nc.dram_tensor("OSs", [TOK, 512], BF16, kind="Internal").ap()

    with ExitStack() as es:
        T = Tracker(nc, es)
        PE, ACT, DVE, POOL, SP = T.pe, T.act, T.dve, T.pool, T.sp

        def sbt(stack, name, shape, dt, lane=False):
            t = stack.enter_context(nc.sbuf_tensor(name, list(shape), dt))
            return Buf(T, t, name, lane)

        banks = []
        for i in range(8):
            t = es.enter_context(nc.psum_tensor(f"pb{i}", [128, 512], F32))
            banks.append(Buf(T, t, f"pb{i}"))
        rot = {"i": 0, "lst": list(range(8))}

        def pnext():
            b = banks[rot["lst"][rot["i"] % len(rot["lst"])]]
            rot["i"] += 1
            return b

        def set_rot(lst):
            rot["lst"] = list(lst)
            rot["i"] = 0

        def V(fn, reads=(), writes=()):
            T.op(DVE, fn, reads, writes)

        def A(fn, reads=(), writes=()):
            T.op(ACT, fn, reads, writes)

        def G(fn, reads=(), writes=()):
            T.op(POOL, fn, reads, writes)

        def P(fn, reads=(), writes=()):
            T.op(PE, fn, reads, writes)

        def load(buf, src, eng=None):
            E = SP if eng is None else eng
            T.dma(E, buf.l, lambda: E.eng.dma_start(out=buf.t[:], in_=src), writes=[buf.r])

        def load_to(buf, dst_ap, src, eng=None):
            E = SP if eng is None else eng
            T.dma(E, buf.l, lambda: E.eng.dma_start(out=dst_ap, in_=src), writes=[buf.r])

        identf = sbt(es, "identf", [128, 128], F32, True); load(identf, c_ident)
        identb = sbt(es, "identb", [128, 128], BF16)
        V(lambda: nc.vector.tensor_copy(out=identb.t[:], in_=identf.t[:]), [identf.r], [identb.r])
        onesb = sbt(es, "onesb", [128, 128], BF16)
        V(lambda: nc.vector.memset(onesb.t[:], 1.0), [], [onesb.r])
        onesf = sbt(es, "onesf", [128, 128], F32)
        V(lambda: nc.vector.memset(onesf.t[:], 1.0), [], [onesf.r])
        kqf = sbt(es, "kqf", [128, 128], F32, True); load(kqf, c_kq)
        kqb = sbt(es, "kqb", [128, 128], BF16)
        V(lambda: nc.vector.tensor_copy(out=kqb.t[:], in_=kqf.t[:]), [kqf.r], [kqb.r])
        fq = sbt(es, "fq", [128, 1], F32, True); load(fq, c_fq)
        dtab = sbt(es, "dtab", [128, 16], F32, True); load(dtab, c_dtab)
        g1c = sbt(es, "g1c", [128, 8], F32, True); load(g1c, g1col)
        g2c = sbt(es, "g2c", [128, 8], F32, True); load(g2c, g2col)
        gqc = sbt(es, "gqc", [128, 2], F32, True); load(gqc, gqcol)
        gkvc = sbt(es, "gkvc", [128, 1], F32, True); load(gkvc, gkvcol)
        cc = sbt(es, "cc", [128, 8], F32, True); load(cc, ccol)
        badT = sbt(es, "badT", [128, 48], F32, True); load(badT, b_adaT)
        modT = sbt(es, "modT", [128, 48], F32)
        gate1b = sbt(es, "gate1b", [128, D], F32)
        s2b = sbt(es, "s2b", [128, D], F32)
        sh2b = sbt(es, "sh2b", [128, D], F32)
        gate2b = sbt(es, "gate2b", [128, D], F32)
        s1b = sbt(es, "s1b", [128, D], F32)
        sh1b = sbt(es, "sh1b", [128, 8], BF16)
        ones512 = sbt(es, "ones512", [1, 512], BF16)
        V(lambda: nc.vector.memset(ones512.t[:], 1.0), [], [ones512.r])
        s1c = sbt(es, "s1c", [128, 8], F32); s2c = sbt(es, "s2c", [128, 8], F32)

        with ExitStack() as ph:
            sc = sbt(ph, "sc", [128, 8], F32)
            A(lambda: nc.scalar.activation(out=sc.t[:], in_=cc.t[:], func=AF.Silu), [cc.r], [sc.r])
            brow = [sbt(ph, f"brow{i}", [1, 512], F32, True) for i in range(2)]
            rowdst = {2: (s1b, 0), 3: (s1b, 512), 4: (gate1b, 0), 5: (gate1b, 512), 6: (sh2b, 0), 7: (sh2b, 512), 8: (s2b, 0), 9: (s2b, 512),
                      10: (gate2b, 0), 11: (gate2b, 512)}
            wp = [sbt(ph, f"wada{i}", [128, 8, 512], F32, True) for i in range(2)]
            pcol = banks[7]
            set_rot(range(7))
            rowtmp = sbt(ph, "rowtmp", [1, 512], F32)
            for pc in range(12):
                wb_ = wp[pc % 2]
                load(wb_, w_ada[:, pc * 512:(pc + 1) * 512].rearrange("(kc p) n -> p kc n", p=128))
                if pc in rowdst:
                    prow = pnext()
                    br_ = brow[pc % 2]
                    load(br_, b_ada_row[0:1, pc * 512:(pc + 1) * 512])
                    for kc in range(8):
                        P(lambda: nc.tensor.matmul(prow.t[0:1, :], lhsT=sc.t[:, kc:kc + 1], rhs=wb_.t[:, kc, :],
                                                   start=(kc == 0), stop=(kc == 7)), [sc.r, wb_.r], [prow.r])
                    V(lambda: nc.vector.tensor_tensor(out=rowtmp.t[0:1, :], in0=prow.t[0:1, :], in1=br_.t[0:1, :], op=ALU.add),
                      [prow.r, br_.r], [rowtmp.r])
                    pbc = pnext()
                    P(lambda: nc.tensor.matmul(pbc.t[:], lhsT=onesf.t[0:1, :], rhs=rowtmp.t[0:1, :], start=True, stop=True),
                      [onesf.r, rowtmp.r], [pbc.r])
                    dtile, dc0 = rowdst[pc]
                    V(lambda: nc.vector.tensor_copy(out=dtile.t[:, dc0:dc0 + 512], in_=pbc.t[:]), [pbc.r], [dtile.r])
                for oc in range(4):
                    col = pc * 4 + oc
                    for kc in range(8):
                        P(lambda: nc.tensor.matmul(pcol.t[:, col:col + 1], lhsT=wb_.t[:, kc, oc * 128:(oc + 1) * 128],
                                                   rhs=sc.t[:, kc:kc + 1], start=(kc == 0), stop=(kc == 7)),
                          [sc.r, wb_.r], [pcol.r])
            V(lambda: nc.vector.tensor_tensor(out=modT.t[:], in0=pcol.t[:, 0:48], in1=badT.t[:], op=ALU.add),
              [pcol.r, badT.r], [modT.r])
            V(lambda: nc.vector.scalar_tensor_tensor(out=s1c.t[:], in0=modT.t[:, 8:16], scalar=1.0, in1=g1c.t[:],
                                                     op0=ALU.add, op1=ALU.mult), [modT.r, g1c.r], [s1c.r])
            V(lambda: nc.vector.scalar_tensor_tensor(out=s2c.t[:], in0=modT.t[:, 32:40], scalar=1.0, in1=g2c.t[:],
                                                     op0=ALU.add, op1=ALU.mult), [modT.r, g2c.r], [s2c.r])
            g1rb = sbt(ph, "g1rb", [128, D], F32, True); load(g1rb, g1row.partition_broadcast(128))
            V(lambda: nc.vector.scalar_tensor_tensor(out=s1b.t[:], in0=s1b.t[:], scalar=1.0, in1=g1rb.t[:], op0=ALU.add,
                                                     op1=ALU.mult), [s1b.r, g1rb.r], [s1b.r])
            V(lambda: nc.vector.tensor_copy(out=sh1b.t[:], in_=modT.t[:, 0:8]), [modT.r], [sh1b.r])
            T.barrier()
        if stop <= 0:
            return nc
        sh1 = lambda kc: modT.t[:, kc:kc + 1]
        sh2 = lambda kc: modT.t[:, 24 + kc:25 + kc]

        FE = {}
        cnt = {"x": 0, "fe": 0, "st": 0, "xn": 0, "cp": 0}

        def make_fe(stack, nx=3, nxn=2):
            k = cnt["fe"]; cnt["fe"] += 1
            FE["nx"] = nx
            FE["xts"] = [sbt(stack, f"xt{k}_{i}", [128, D], F32, True) for i in range(nx)]
            FE["junk"] = sbt(stack, f"junk{k}", [128, D], BF16)
            FE["stat"] = [sbt(stack, f"stat{k}_{i}", [128, 16], F32) for i in range(3)]
            FE["xns"] = [sbt(stack, f"xn{k}_{i}", [128, D], BF16) for i in range(nxn)]

        def fe_a(tile_aps):
            n = len(tile_aps)
            st = FE["stat"][cnt["st"] % 3]; cnt["st"] += 1
            junk = FE["junk"]
            xl = []
            for j in range(n):
                xt = FE["xts"][cnt["x"] % FE["nx"]]; cnt["x"] += 1
                load(xt, tile_aps[j])
                xl.append(xt)
            for j in range(n):
                A(lambda: nc.scalar.activation(out=junk.t[:], in_=xl[j].t[:], func=AF.Square, accum_out=st.t[:, j:j + 1]),
                  [xl[j].r], [junk.r, st.r])
            V(lambda: nc.vector.tensor_scalar(out=st.t[:, 4:4 + n], in0=st.t[:, 0:n], scalar1=1.0 / D, scalar2=EPS,
                                              op0=ALU.mult, op1=ALU.add), [st.r], [st.r])
            A(lambda: nc.scalar.activation(out=st.t[:, 8:8 + n], in_=st.t[:, 4:4 + n], func=AF.Sqrt), [st.r], [st.r])
            V(lambda: nc.vector.reciprocal(out=st.t[:, 12:12 + n], in_=st.t[:, 8:8 + n]), [st.r], [st.r])
            xnl = []
            for j in range(n):
                xn = FE["xns"][cnt["xn"] % len(FE["xns"])]; cnt["xn"] += 1
                V(lambda: nc.vector.scalar_tensor_tensor(out=xn.t[:], in0=xl[j].t[:], scalar=st.t[:, 12 + j:13 + j], in1=s1b.t[:],
                                                         op0=ALU.mult, op1=ALU.mult), [xl[j].r, st.r, s1b.r], [xn.r])
                xnl.append(xn)
            return xl, xnl

        def fe_b(state, dst, dst_res, c0s):
            xl, xnl = state
            for j, xn in enumerate(xnl):
                tp = pnext()
                tpb = tp.t[:].bitcast(BF16)
                for kc in range(8):
                    P(lambda: nc.tensor.transpose(out=tpb[:, kc * 128:(kc + 1) * 128], in_=xn.t[:, kc * 128:(kc + 1) * 128],
                                                  identity=identb.t[:]), [xn.r, identb.r], [tp.r])
                c0 = c0s[j]
                src3 = tpb.rearrange("p (k t) -> p k t", k=8)
                if cnt["cp"] % 2 == 0:
                    A(lambda: nc.scalar.copy(out=dst[:, :, c0:c0 + 128], in_=src3), [tp.r], [dst_res])
                else:
                    V(lambda: nc.vector.tensor_copy(out=dst[:, :, c0:c0 + 128], in_=src3), [tp.r], [dst_res])
                cnt["cp"] += 1
            return xl

        def front_end_multi(tile_aps, dst, dst_res, c0s):
            return fe_b(fe_a(tile_aps), dst, dst_res, c0s)

        def bias_row(dst_ap, wfn, M, wres, dres):
            ps = pnext()
            for kc in range(8):
                P(lambda: nc.tensor.matmul(ps.t[0:1, 0:M], lhsT=sh1b.t[:, kc:kc + 1], rhs=wfn(kc), start=(kc == 0), stop=(kc == 7)),
                  [sh1b.r, wres], [ps.r])
            V(lambda: nc.vector.tensor_copy(out=dst_ap, in_=ps.t[0:1, 0:M]), [ps.r], [dres])

        s1col = lambda kc: s1c.t[:, kc:kc + 1]

        Aall = sbt(es, "Aall", [128, NOWN, 32], BF16)
        A0all = sbt(es, "A0all", [128, NOWN, 32], BF16)
        A1all = sbt(es, "A1all", [128, NOWN, 32], BF16)
        wts = sbt(es, "wts", [128, NOWN, 2], F32)
        att = es.enter_context(ExitStack())
        ckvnT = sbt(att, "ckvnT", [128, S], BF16); ckvn_r = [Res() for _ in range(NG)]
        KT = sbt(att, "KT", [128, S], BF16); ktn_r = [Res() for _ in range(NG)]; ktr_r = [Res() for _ in range(NG)]
        vh_r = [Res() for _ in range(NG)]
        cqnT = sbt(att, "cqnT", [128, 2, TOK], BF16); cqn_r = [Res() for _ in range(NGO)]
        qT_r = [Res() for _ in range(NGO)]
        CQ = sbt(att, "CQ", [128, TOK], BF16); SQ = sbt(att, "SQ", [128, TOK], BF16)
        cs_r = [Res() for _ in range(NGO)]

        def rope_tables(pos_ap_512, Cdst, Sdst, wres, lp, width):
            pi_ = lp["posi"]; R = slice(64, 96)
            T.dma(SP, pi_.l, lambda: nc.sync.dma_start(out=pi_.t[R, 0:width], in_=pos_ap_512.partition_broadcast(32)),
                  writes=[pi_.r])
            pf = lp["pf"]; u = lp["u"]; ki = lp["ki"]; kf = lp["kf"]; f2 = lp["f2"]
            V(lambda: nc.vector.tensor_copy(out=pf.t[R, 0:width], in_=pi_.t[R, 0:width]), [pi_.r], [pf.r])
            for which, off, dstb in ((0, 0.5, Sdst), (1, 0.75, Cdst)):
                V(lambda: nc.vector.tensor_scalar(out=u.t[R, 0:width], in0=pf.t[R, 0:width], scalar1=fq.t[R, 0:1],
                                                  scalar2=off, op0=ALU.mult, op1=ALU.add), [pf.r, fq.r], [u.r])
                V(lambda: nc.vector.tensor_copy(out=ki.t[R, 0:width], in_=u.t[R, 0:width]), [u.r], [ki.r])
                V(lambda: nc.vector.tensor_copy(out=kf.t[R, 0:width], in_=ki.t[R, 0:width]), [ki.r], [kf.r])
                V(lambda: nc.vector.scalar_tensor_tensor(out=f2.t[R, 0:width], in0=u.t[R, 0:width], scalar=-0.5,
                                                         in1=kf.t[R, 0:width], op0=ALU.add, op1=ALU.subtract),
                  [u.r, kf.r], [f2.r])
                V(lambda: nc.vector.scalar_tensor_tensor(out=u.t[R, 0:width], in0=f2.t[R, 0:width], scalar=-0.5,
                                                         in1=f2.t[R, 0:width], op0=ALU.is_lt, op1=ALU.add),
                  [f2.r], [u.r])
                A(lambda: nc.scalar.activation(out=dstb, in_=u.t[R, 0:width], func=AF.Sin, scale=6.28318),
                  [u.r], [wres])

        with ExitStack() as lp_:
            make_fe(lp_, 4, 4)
            lp = {}
            lp["posi"] = sbt(lp_, "posi", [128, 512], I32, True)
            for nm in ("pf", "u", "kf", "f2"):
                lp[nm] = sbt(lp_, "lp_" + nm, [128, 512], F32)
            lp["t1"] = lp["pf"]; lp["t2"] = lp["kf"]
            lp["ki"] = sbt(lp_, "lp_ki", [128, 512], I32)
            Ctmp = sbt(lp_, "Ctmp", [128, 512], F32); Stmp = sbt(lp_, "Stmp", [128, 512], F32)
            h1Ts = [sbt(lp_, f"h1T{i}", [128, 8, 512], BF16) for i in range(2)]
            sq = sbt(lp_, "sq", [128, 2, 512], BF16)
            rs = sbt(lp_, "rs", [128, 512], F32); rs2 = sbt(lp_, "rs2", [128, 512], F32)
            wkv = sbt(lp_, "wkv", [128, 8, 128], BF16, True)
            wkr = sbt(lp_, "wkr", [128, 8, 96], BF16, True)
            wkrr = sbt(lp_, "wkrr", [128, 8, 96], BF16)
            wq = sbt(lp_, "wq", [128, 8, 256], BF16, True)
            win_v = w_in.rearrange("(kc p) n -> p kc n", p=128)
            load(wkv, win_v[:, :, 256:384], POOL)
            V(lambda: nc.vector.memset(wkr.t[:], 0.0), [], [wkr.r])
            V(lambda: nc.vector.memset(wkrr.t[:], 0.0), [], [wkrr.r])
            load_to(wkr, wkr.t[:, :, 64:96], win_v[:, :, 384:416], POOL)
            load(wq, win_v[:, :, 0:256], POOL)
            V(lambda: nc.vector.tensor_scalar(out=wkrr.t[:, :, 64:80], in0=wkr.t[:, :, 80:96], scalar1=-1.0, scalar2=None,
                                              op0=ALU.mult), [wkr.r, wkrr.r], [wkrr.r])
            V(lambda: nc.vector.tensor_copy(out=wkrr.t[:, :, 80:96], in_=wkr.t[:, :, 64:80]), [wkr.r, wkrr.r], [wkrr.r])
            set_rot(range(8))
            brows = sbt(lp_, "brows", [1, 640], BF16)
            bias_row(brows.t[0:1, 0:128], lambda kc: wkv.t[:, kc, :], 128, wkv.r, brows.r)
            bias_row(brows.t[0:1, 128:224], lambda kc: wkr.t[:, kc, :], 96, wkr.r, brows.r)
            bias_row(brows.t[0:1, 224:320], lambda kc: wkrr.t[:, kc, :], 96, wkrr.r, brows.r)
            bias_row(brows.t[0:1, 320:576], lambda kc: wq.t[:, kc, :], 256, wq.r, brows.r)
            hcnt = {"i": 0}
            h1_of = {}

            fe_state = {}

            def latent_fe_a(mode, g, width):
                ntl = width // 128
                src = xb if mode == "k" else xo
                fe_state[(mode, g)] = fe_a([src[(g * 4 + tl) * 128:(g * 4 + tl + 1) * 128, :] for tl in range(ntl)])

            def latent_fe_b(mode, g, width):
                ntl = width // 128
                h1 = h1Ts[hcnt["i"] % 2]; hcnt["i"] += 1
                h1_of[(mode, g)] = h1
                fe_b(fe_state.pop((mode, g)), h1.t, h1.r, [tl * 128 for tl in range(ntl)])

            mm_out = {}

            def latent_mm(mode, g, width):
                h1 = h1_of.pop((mode, g))
                W = slice(0, width)
                if mode == "k":
                    pk = pnext(); pr = pnext(); prr = pnext()
                    for kc in range(8):
                        P(lambda: nc.tensor.matmul(pk.t[:, W], lhsT=wkv.t[:, kc, :], rhs=h1.t[:, kc, W],
                                                   start=(kc == 0), stop=False), [wkv.r, h1.r], [pk.r])
                    P(lambda: nc.tensor.matmul(pk.t[:, W], lhsT=brows.t[0:1, 0:128], rhs=ones512.t[0:1, W], start=False, stop=True),
                      [brows.r, ones512.r], [pk.r])
                    for kc in range(8):
                        P(lambda: nc.tensor.matmul(pr.t[0:96, W], lhsT=wkr.t[:, kc, :], rhs=h1.t[:, kc, W],
                                                   start=(kc == 0), stop=False), [wkr.r, h1.r], [pr.r])
                    P(lambda: nc.tensor.matmul(pr.t[0:96, W], lhsT=brows.t[0:1, 128:224], rhs=ones512.t[0:1, W], start=False, stop=True),
                      [brows.r, ones512.r], [pr.r])
                    for kc in range(8):
                        P(lambda: nc.tensor.matmul(prr.t[0:96, W], lhsT=wkrr.t[:, kc, :], rhs=h1.t[:, kc, W],
                                                   start=(kc == 0), stop=False), [wkrr.r, h1.r], [prr.r])
                    P(lambda: nc.tensor.matmul(prr.t[0:96, W], lhsT=brows.t[0:1, 224:320], rhs=ones512.t[0:1, W], start=False, stop=True),
                      [brows.r, ones512.r], [prr.r])
                    chunks = [pk]
                    gcol = [gkvc.t[:, 0:1]]
                    nfeat = 128
                else:
                    p0 = pnext(); p1 = pnext()
                    for ci, pp in enumerate((p0, p1)):
                        for kc in range(8):
                            P(lambda: nc.tensor.matmul(pp.t[:, W], lhsT=wq.t[:, kc, ci * 128:(ci + 1) * 128],
                                                       rhs=h1.t[:, kc, W], start=(kc == 0), stop=False),
                              [wq.r, h1.r], [pp.r])
                        P(lambda: nc.tensor.matmul(pp.t[:, W], lhsT=brows.t[0:1, 320 + ci * 128:320 + (ci + 1) * 128],
                                                   rhs=ones512.t[0:1, W], start=False, stop=True), [brows.r, ones512.r], [pp.r])
                    chunks = [p0, p1]
                    gcol = [gqc.t[:, 0:1], gqc.t[:, 1:2]]
                    nfeat = 256
                mm_out[(mode, g)] = (chunks, gcol, nfeat, (pr, prr) if mode == "k" else None)

            def latent_rest(mode, g, width):
                W = slice(0, width)
                chunks, gcol, nfeat, prs = mm_out.pop((mode, g))
                if prs is not None:
                    pr, prr = prs
                for ci, pp in enumerate(chunks):
                    A(lambda: nc.scalar.activation(out=sq.t[:, ci, W], in_=pp.t[:, W], func=AF.Square), [pp.r], [sq.r])
                pss = pnext()
                for ci in range(len(chunks)):
                    P(lambda: nc.tensor.matmul(pss.t[:, W], lhsT=onesb.t[:], rhs=sq.t[:, ci, W], start=(ci == 0),
                                               stop=(ci == len(chunks) - 1)), [onesb.r, sq.r], [pss.r])
                V(lambda: nc.vector.tensor_scalar(out=rs.t[:, W], in0=pss.t[:, W], scalar1=1.0 / nfeat, scalar2=EPS,
                                                  op0=ALU.mult, op1=ALU.add), [pss.r], [rs.r])
                A(lambda: nc.scalar.activation(out=rs2.t[:, W], in_=rs.t[:, W], func=AF.Sqrt), [rs.r], [rs2.r])
                V(lambda: nc.vector.reciprocal(out=rs.t[:, W], in_=rs2.t[:, W]), [rs2.r], [rs.r])
                c0 = g * 512
                for ci, pp in enumerate(chunks):
                    if mode == "k":
                        dst = ckvnT.t[:, c0:c0 + width]; dres = ckvn_r[g]
                    else:
                        dst = cqnT.t[:, ci, c0:c0 + width]; dres = cqn_r[g]
                    V(lambda: nc.vector.scalar_tensor_tensor(out=dst, in0=pp.t[:, W], scalar=gcol[ci], in1=rs.t[:, W],
                                                             op0=ALU.mult, op1=ALU.mult),
                      [pp.r, rs.r, gkvc.r, gqc.r], [dres])
                R = slice(64, 96)
                if mode == "k":
                    rope_tables(posb[0:1, c0:c0 + width], Ctmp.t[R, W], Stmp.t[R, W], Ctmp.r, lp, width)
                    t1 = lp["t1"]; t2 = lp["t2"]
                    V(lambda: nc.vector.tensor_tensor(out=t1.t[R, W], in0=pr.t[R, W], in1=Ctmp.t[R, W], op=ALU.mult),
                      [pr.r, Ctmp.r], [t1.r])
                    V(lambda: nc.vector.tensor_tensor(out=t2.t[R, W], in0=prr.t[R, W], in1=Stmp.t[R, W], op=ALU.mult),
                      [prr.r, Ctmp.r], [t2.r])
                    V(lambda: nc.vector.tensor_tensor(out=KT.t[R, c0:c0 + width], in0=t1.t[R, W], in1=t2.t[R, W],
                                                      op=ALU.add), [t1.r, t2.r], [ktr_r[g]])
                else:
                    rope_tables(poso[0:1, c0:c0 + width], CQ.t[R, c0:c0 + width], SQ.t[R, c0:c0 + width], cs_r[g], lp, width)

            work = [("k", g, 512) for g in range(NG)] + [("q", g, GW) for g in range(NGO)]
            latent_fe_a(*work[0]); latent_fe_b(*work[0])
            for wi, w_ in enumerate(work):
                nxt = work[wi + 1] if wi + 1 < len(work) else None
                if nxt:
                    latent_fe_a(*nxt)
                latent_mm(*w_)
                if nxt:
                    latent_fe_b(*nxt)
                latent_rest(*w_)
            T.barrier()
            if stop <= 1:
                return nc

        with ExitStack() as ap_:
            qT = sbt(ap_, "qT", [128, TOK], BF16)
            Vh = sbt(ap_, "Vh", [128, NT, 65], BF16)
            V(lambda: nc.vector.memset(Vh.t[:, :, 64:65], 1.0), [], vh_r)
            wuqb = sbt(ap_, "wuqb", [128, 2, 768], BF16, True)
            wuqr = sbt(ap_, "wuqr", [128, 2, NH, 96], BF16)
            wukvb = sbt(ap_, "wukvb", [128, 1024], BF16, True)
            load(wuqb, w_uq.rearrange("(kc p) n -> p kc n", p=128), POOL)
            load(wukvb, w_ukv, POOL)
            V(lambda: nc.vector.memset(wuqr.t[:], 0.0), [], [wuqr.r])
            wv = wuqb.t[:].rearrange("p k (h d) -> p k h d", h=NH)
            V(lambda: nc.vector.tensor_scalar(out=wuqr.t[:, :, :, 64:80], in0=wv[:, :, :, 80:96], scalar1=-1.0, scalar2=None,
                                              op0=ALU.mult), [wuqb.r, wuqr.r], [wuqr.r])
            V(lambda: nc.vector.tensor_copy(out=wuqr.t[:, :, :, 80:96], in_=wv[:, :, :, 64:80]), [wuqb.r, wuqr.r], [wuqr.r])

            PTs = [sbt(ap_, f"PT{i}", [128, 512], BF16) for i in range(4)]
            rec = [sbt(ap_, f"rec{i}", [128, 1], F32) for i in range(4)]
            ost = [sbt(ap_, f"ost{i}", [128, 64], BF16, True) for i in range(4)]
            osc = [0]
            t1q = sbt(ap_, "t1q", [128, 512], F32); t2q = sbt(ap_, "t2q", [128, 512], F32)
            set_rot([4, 5, 6, 7])
            oacc_i = 0
            ptc = 0
            for h in range(NH):
                for g in range(NG):
                    ps = pnext()
                    P(lambda: nc.tensor.matmul(ps.t[0:64, :], lhsT=wukvb.t[:, h * 128:h * 128 + 64],
                                               rhs=ckvnT.t[:, g * 512:(g + 1) * 512], start=True, stop=True),
                      [wukvb.r, ckvn_r[g]], [ps.r])
                    if g % 2 == 0:
                        V(lambda: nc.vector.tensor_copy(out=KT.t[0:64, g * 512:(g + 1) * 512], in_=ps.t[0:64, :]),
                          [ps.r], [ktn_r[g]])
                    else:
                        A(lambda: nc.scalar.copy(out=KT.t[0:64, g * 512:(g + 1) * 512], in_=ps.t[0:64, :]),
                          [ps.r], [ktn_r[g]])
                for g8 in range(NT // 8):
                    ps = pnext()
                    for k8 in range(8):
                        kt = g8 * 8 + k8
                        P(lambda: nc.tensor.matmul(ps.t[:, k8 * 64:(k8 + 1) * 64], lhsT=ckvnT.t[:, kt * 128:(kt + 1) * 128],
                                                   rhs=wukvb.t[:, h * 128 + 64:h * 128 + 128], start=True, stop=True),
                          [wukvb.r, ckvn_r[kt // 4]], [ps.r])
                    wr_ = [vh_r[2 * g8], vh_r[2 * g8 + 1]]
                    src3 = ps.t[:].rearrange("p (a d) -> p a d", a=8)
                    if g8 % 2 == 0:
                        A(lambda: nc.scalar.copy(out=Vh.t[:, g8 * 8:(g8 + 1) * 8, 0:64], in_=src3), [ps.r], wr_)
                    else:
                        V(lambda: nc.vector.tensor_copy(out=Vh.t[:, g8 * 8:(g8 + 1) * 8, 0:64], in_=src3), [ps.r], wr_)
                for g in range(NGO):
                    W = slice(0, GW); c0 = g * 512; R = slice(64, 96)
                    pa = pnext(); pr = pnext()
                    for kc in range(2):
                        P(lambda: nc.tensor.matmul(pa.t[0:96, W], lhsT=wuqb.t[:, kc, h * 96:(h + 1) * 96],
                                                   rhs=cqnT.t[:, kc, c0:c0 + GW], start=(kc == 0), stop=(kc == 1)),
                          [wuqb.r, cqn_r[g]], [pa.r])
                    for kc in range(2):
                        P(lambda: nc.tensor.matmul(pr.t[0:96, W], lhsT=wuqr.t[:, kc, h, :],
                                                   rhs=cqnT.t[:, kc, c0:c0 + GW], start=(kc == 0), stop=(kc == 1)),
                          [wuqr.r, cqn_r[g]], [pr.r])
                    V(lambda: nc.vector.tensor_copy(out=qT.t[0:64, c0:c0 + GW], in_=pa.t[0:64, W]), [pa.r], [qT_r[g]])
                    V(lambda: nc.vector.tensor_tensor(out=t1q.t[R, W], in0=pa.t[R, W], in1=CQ.t[R, c0:c0 + GW], op=ALU.mult),
                      [pa.r, cs_r[g]], [t1q.r])
                    V(lambda: nc.vector.tensor_tensor(out=t2q.t[R, W], in0=pr.t[R, W], in1=SQ.t[R, c0:c0 + GW], op=ALU.mult),
                      [pr.r, cs_r[g]], [t2q.r])
                    V(lambda: nc.vector.tensor_tensor(out=qT.t[R, c0:c0 + GW], in0=t1q.t[R, W], in1=t2q.t[R, W], op=ALU.add),
                      [t1q.r, t2q.r], [qT_r[g]])
                steps = [(m, kp) for m in range(NP) for kp in range((8 * m + 8) // 2)]
                oa_of = {}
                for m in range(NP):
                    oa_of[m] = [banks[(oacc_i * 2) % 4], banks[(oacc_i * 2 + 1) % 4]]
                    oacc_i += 1
                pt_of = {}

                def emit_qk(si):
                    m, kp = steps[si]
                    qg = qT_r[(m * 256) // 512]
                    qs = qT.t[0:96, m * 256:(m + 1) * 256]
                    ps = pnext()
                    for kl in range(2):
                        kt = 2 * kp + kl
                        P(lambda: nc.tensor.matmul(ps.t[:, kl * 256:(kl + 1) * 256], lhsT=KT.t[0:96, kt * 128:(kt + 1) * 128],
                                                   rhs=qs, start=True, stop=True),
                          [ktn_r[kt // 4], ktr_r[kt // 4], qg], [ps.r])
                    PT = PTs[si % len(PTs)]
                    pt_of[si] = PT
                    A(lambda: nc.scalar.activation(out=PT.t[:], in_=ps.t[:], func=AF.Exp, scale=SCALE), [ps.r], [PT.r])
                    for kl in range(2):
                        kt = 2 * kp + kl
                        o = kt - 8 * m
                        if o >= 0:
                            for a in range(2):
                                cs_ = kl * 256 + a * 128
                                V(lambda: nc.vector.scalar_tensor_tensor(
                                    out=PT.t[:, cs_:cs_ + 128], in0=kqb.t[:], scalar=dtab.t[:, a * 8 + o:a * 8 + o + 1],
                                    in1=PT.t[:, cs_:cs_ + 128], op0=ALU.is_le, op1=ALU.mult),
                                  [PT.r, kqb.r, dtab.r], [PT.r])

                def emit_pv(si):
                    nonlocal_osc = None
                    m, kp = steps[si]
                    nk = 8 * m + 8
                    oa = oa_of[m]
                    PT = pt_of.pop(si)
                    for kl in range(2):
                        kt = 2 * kp + kl
                        for a in range(2):
                            cs_ = kl * 256 + a * 128
                            P(lambda: nc.tensor.matmul(oa[a].t[:, 0:65], lhsT=PT.t[:, cs_:cs_ + 128], rhs=Vh.t[:, kt, :],
                                                       start=(kt == 0), stop=(kt == nk - 1)),
                              [PT.r, vh_r[kt // 4]], [oa[a].r])
                    if kp == nk // 2 - 1:
                        for a in range(2):
                            i_own = 2 * m + a
                            rc = rec[(2 * m + a) % 4]
                            V(lambda: nc.vector.reciprocal(out=rc.t[:], in_=oa[a].t[:, 64:65]), [oa[a].r], [rc.r])
                            os_ = ost[osc[0] % 4]; osc[0] += 1
                            V(lambda: nc.vector.tensor_scalar(out=os_.t[:], in0=oa[a].t[:, 0:64],
                                                              scalar1=rc.t[:, 0:1], scalar2=None, op0=ALU.mult),
                              [oa[a].r, rc.r], [os_.r])
                            T.dma(SP, os_.l, lambda: nc.sync.dma_start(out=OS[i_own * 128:(i_own + 1) * 128, h * 64:(h + 1) * 64],
                                                                       in_=os_.t[:]), reads=[os_.r])

                LA = 2
                for si in range(len(steps) + LA):
                    if si < len(steps):
                        emit_qk(si)
                    if si - LA >= 0:
                        emit_pv(si - LA)
            T.barrier()
            if stop <= 2:
                return nc
        att.close()

        pc_ = es.enter_context(ExitStack())
        set_rot(range(8))
        win_v = w_in.rearrange("(kc p) n -> p kc n", p=128)
        wsg = sbt(pc_, "wsg", [128, 8, 1024], BF16, True); load(wsg, win_v[:, :, 416:1440], POOL)
        wgl = sbt(pc_, "wgl", [128, 8, 2048], BF16, True)
        for kc in range(8):
            load_to(wgl, wgl.t[:, kc, :], win_v[:, kc, 1440:3488], POOL)
        wbm = sbt(pc_, "wbm", [128, 4, D], BF16, True); load(wbm, w_br_mla.rearrange("(kc p) n -> p kc n", p=128), POOL)
        wbs = sbt(pc_, "wbs", [128, 4, D], BF16, True); load(wbs, w_br_sgu.rearrange("(kc p) n -> p kc n", p=128), POOL)
        wo = sbt(pc_, "wo", [128, 8, D], BF16, True); load(wo, w_out.rearrange("(kc p) n -> p kc n", p=128), POOL)
        wrf = sbt(pc_, "wrf", [128, 8, 36], F32, True); load(wrf, wr.rearrange("(kc p) n -> p kc n", p=128))
        brb = sbt(pc_, "brb", [128, 36], F32, True); load(brb, br_row.partition_broadcast(128))
        gvb = sbt(pc_, "gvb", [128, 512], F32, True); load(gvb, gv_row.partition_broadcast(128))
        bvb = sbt(pc_, "bvb", [128, 512], F32, True); load(bvb, bv_row.partition_broadcast(128))
        bst = sbt(pc_, "bst", [128, 8], F32, True); load(bst, bsT)
        make_fe(pc_, 3)
        browc = sbt(pc_, "browc", [1, 3072], BF16)
        for q_ in range(2):
            bias_row(browc.t[0:1, q_ * 512:(q_ + 1) * 512], lambda kc: wsg.t[:, kc, q_ * 512:(q_ + 1) * 512], 512, wsg.r, browc.r)
        for q_ in range(4):
            bias_row(browc.t[0:1, 1024 + q_ * 512:1024 + (q_ + 1) * 512], lambda kc: wgl.t[:, kc, q_ * 512:(q_ + 1) * 512], 512,
                     wgl.r, browc.r)
        wsT = sbt(pc_, "wsT", [128, 8, 128], BF16)
        with ExitStack() as tmp_:
            g2rb = sbt(tmp_, "g2rb", [128, D], F32, True); load(g2rb, g2row.partition_broadcast(128))
            V(lambda: nc.vector.scalar_tensor_tensor(out=s2b.t[:], in0=s2b.t[:], scalar=1.0, in1=g2rb.t[:], op0=ALU.add,
                                                     op1=ALU.mult), [s2b.r, g2rb.r], [s2b.r])
            wsf = sbt(tmp_, "wsf", [128, 8, 128], F32, True); load(wsf, w_s.rearrange("g t s -> t g s"))
            trl = sbt(tmp_, "trl", [128, 128], F32, True); load(trl, c_tril)
            wsm = sbt(tmp_, "wsm", [128, 8, 128], BF16)
            V(lambda: nc.vector.tensor_tensor(out=wsm.t[:], in0=wsf.t[:], in1=trl.t[:].unsqueeze(1).to_broadcast([128, 8, 128]),
                                              op=ALU.mult), [wsf.r, trl.r], [wsm.r])
            tp = pnext(); tpb = tp.t[:].bitcast(BF16)
            for g in range(8):
                P(lambda: nc.tensor.transpose(out=tpb[:, g * 128:(g + 1) * 128], in_=wsm.t[:, g, :], identity=identb.t[:]),
                  [wsm.r, identb.r], [tp.r])
            V(lambda: nc.vector.tensor_copy(out=wsT.t[:].rearrange("p g t -> p (g t)"), in_=tpb[:, :]), [tp.r], [wsT.r])
            T.barrier()


        with ExitStack() as cw:
            h1c = [sbt(cw, f"h1c{i}", [128, 8, 128], BF16) for i in range(1)]
            uv = sbt(cw, "uv", [128, 1024], F32)
            x2 = sbt(cw, "x2t", [128, 1024], F32)
            inner = sbt(cw, "inner", [128, 1024], F32)
            gel = sbt(cw, "gel", [128, 512], F32)
            otile = [sbt(cw, f"otile{i}", [128, 512], BF16, True) for i in range(2)]
            sgoB = [sbt(cw, f"sgo{i}", [128, 512], BF16) for i in range(1)]
            lst = sbt(cw, "lst", [128, 8], F32)
            sigB = [sbt(cw, f"sig{i}", [128, 2048], BF16) for i in range(2)]
            OT = sbt(cw, "OT", [128, 4, 128], BF16)
            SGT = sbt(cw, "SGT", [128, 4, 128], BF16)
            m1 = sbt(cw, "m1", [128, D], F32); m2 = sbt(cw, "m2", [128, D], F32)
            mrg = sbt(cw, "mrg", [128, D], BF16)
            mT = sbt(cw, "mT", [128, 8, 128], BF16)
            x1 = [sbt(cw, f"x1_{i}", [128, D], F32, True) for i in range(2)]
            x1n = m1
            h2T = sbt(cw, "h2T", [128, 8, 128], F32)
            h2f = m2
            h2p = [sbt(cw, f"h2p{i}", [128, D], BF16, True) for i in range(1)]
            st2 = sbt(cw, "st2", [128, 4], F32)
            lgall = sbt(cw, "lgall", [128, NOWN, 36], F32)
            rtb = sbt(cw, "rtb", [128, 8, NOWN], F32)
            s1out = {}

            vnbB = [sbt(cw, f"vnbB{i}", [128, 512], BF16) for i in range(2)]
            gubB = [sbt(cw, f"gubB{i}", [128, 512], BF16) for i in range(2)]
            fe_st = {}

            def S1(i):
                h1 = h1c[0]; sig = sigB[i % 2]; vnb_ = vnbB[i % 2]; gub = gubB[i % 2]
                if i not in fe_st:
                    fe_st[i] = fe_a([xo[i * 128:(i + 1) * 128, :]])
                xt = fe_b(fe_st.pop(i), h1.t, h1.r, [0])[0]
                s1out[i] = (xt, vnb_, gub, sig)
                load(otile[i % 2], OS[i * 128:(i + 1) * 128, :])
                pu = pnext(); pv = pnext()
                for hh, pp in enumerate((pu, pv)):
                    for kc in range(8):
                        P(lambda: nc.tensor.matmul(pp.t[:], lhsT=h1.t[:, kc, :], rhs=wsg.t[:, kc, hh * 512:(hh + 1) * 512],
                                                   start=(kc == 0), stop=False), [h1.r, wsg.r], [pp.r])
                    P(lambda: nc.tensor.matmul(pp.t[:], lhsT=onesb.t[0:1, :], rhs=browc.t[0:1, hh * 512:(hh + 1) * 512],
                                               start=False, stop=True), [onesb.r, browc.r], [pp.r])
                pg = [pnext() for _ in range(4)]
                for q4 in range(4):
                    for kc in range(8):
                        P(lambda: nc.tensor.matmul(pg[q4].t[:], lhsT=h1.t[:, kc, :], rhs=wgl.t[:, kc, q4 * 512:(q4 + 1) * 512],
                                                   start=(kc == 0), stop=False), [h1.r, wgl.r], [pg[q4].r])
                    P(lambda: nc.tensor.matmul(pg[q4].t[:], lhsT=onesb.t[0:1, :], rhs=browc.t[0:1, 1024 + q4 * 512:1024 + (q4 + 1) * 512],
                                               start=False, stop=True), [onesb.r, browc.r], [pg[q4].r])
                for hh, pp in enumerate((pu, pv)):
                    cs_ = slice(hh * 512, (hh + 1) * 512)
                    A(lambda: nc.scalar.copy(out=uv.t[:, cs_], in_=pp.t[:]), [pp.r], [uv.r])
                    A(lambda: nc.scalar.activation(out=x2.t[:, cs_], in_=pp.t[:], func=AF.Square, scale=0.044715 ** 0.5),
                      [pp.r], [x2.r])
                V(lambda: nc.vector.scalar_tensor_tensor(out=inner.t[:], in0=x2.t[:], scalar=1.0, in1=uv.t[:], op0=ALU.add,
                                                         op1=ALU.mult), [x2.r, uv.r], [inner.r])
                for q4 in range(4):
                    A(lambda: nc.scalar.activation(out=sig.t[:, q4 * 512:(q4 + 1) * 512], in_=pg[q4].t[:], func=AF.Sigmoid),
                      [pg[q4].r], [sig.r])
                A(lambda: nc.scalar.activation(out=inner.t[:], in_=inner.t[:], func=AF.Sigmoid, scale=GELU_C), [inner.r], [inner.r])
                if i + 1 < NOWN:
                    fe_st[i + 1] = fe_a([xo[(i + 1) * 128:(i + 2) * 128, :]])
                G(lambda: nc.gpsimd.tensor_tensor(out=gel.t[:, 0:512], in0=inner.t[:, 512:1024], in1=uv.t[:, 512:1024], op=ALU.mult),
                  [inner.r, uv.r], [gel.r])
                G(lambda: nc.gpsimd.tensor_tensor(out=gub.t[:], in0=inner.t[:, 0:512], in1=uv.t[:, 0:512], op=ALU.mult),
                  [inner.r, uv.r], [gub.r])
                gv_ = gel.t[:, 0:512]
                V(lambda: nc.vector.tensor_reduce(out=lst.t[:, 0:1], in_=gv_, axis=AX.X, op=ALU.add), [gel.r], [lst.r])
                V(lambda: nc.vector.tensor_scalar(out=lst.t[:, 1:2], in0=lst.t[:, 0:1], scalar1=1.0 / 512, scalar2=None,
                                                  op0=ALU.mult), [lst.r], [lst.r])
                V(lambda: nc.vector.tensor_scalar(out=x2.t[:, 0:512], in0=gv_, scalar1=lst.t[:, 1:2], scalar2=None,
                                                  op0=ALU.subtract), [gel.r, lst.r], [x2.r])
                A(lambda: nc.scalar.activation(out=x2.t[:, 512:1024], in_=x2.t[:, 0:512], func=AF.Square, accum_out=lst.t[:, 2:3]),
                  [x2.r], [x2.r, lst.r])
                V(lambda: nc.vector.tensor_scalar(out=lst.t[:, 3:4], in0=lst.t[:, 2:3], scalar1=1.0 / 512, scalar2=EPS,
                                                  op0=ALU.mult, op1=ALU.add), [lst.r], [lst.r])
                A(lambda: nc.scalar.activation(out=lst.t[:, 4:5], in_=lst.t[:, 3:4], func=AF.Sqrt), [lst.r], [lst.r])
                V(lambda: nc.vector.reciprocal(out=lst.t[:, 5:6], in_=lst.t[:, 4:5]), [lst.r], [lst.r])
                V(lambda: nc.vector.scalar_tensor_tensor(out=x2.t[:, 0:512], in0=x2.t[:, 0:512], scalar=lst.t[:, 5:6],
                                                         in1=gvb.t[:], op0=ALU.mult, op1=ALU.mult), [x2.r, lst.r, gvb.r], [x2.r])
                V(lambda: nc.vector.tensor_tensor(out=vnb_.t[:], in0=x2.t[:, 0:512], in1=bvb.t[:], op=ALU.add),
                  [x2.r, bvb.r], [vnb_.r])

            def S2(i):
                xt, vnb_, gub, sig = s1out.pop(i)
                sgo = sgoB[0]
                psg = pnext()
                for g in range(8):
                    P(lambda: nc.tensor.matmul(psg.t[:, g * 64:(g + 1) * 64], lhsT=wsT.t[:, g, :], rhs=vnb_.t[:, g * 64:(g + 1) * 64],
                                               start=True, stop=True), [wsT.r, vnb_.r], [psg.r])
                ot_ = otile[i % 2]
                tp = pnext(); tpb = tp.t[:].bitcast(BF16)
                for kc in range(4):
                    P(lambda: nc.tensor.transpose(out=tpb[:, kc * 128:(kc + 1) * 128], in_=ot_.t[:, kc * 128:(kc + 1) * 128],
                                                  identity=identb.t[:]), [ot_.r, identb.r], [tp.r])
                A(lambda: nc.scalar.copy(out=OT.t[:].rearrange("p k t -> p (k t)"), in_=tpb[:, 0:512]), [tp.r], [OT.r])
                V(lambda: nc.vector.tensor_tensor(out=m2.t[:, 0:512].rearrange("p (g c) -> p g c", g=8),
                                                  in0=psg.t[:].rearrange("p (g c) -> p g c", g=8),
                                                  in1=bst.t[:].unsqueeze(2).to_broadcast([128, 8, 64]), op=ALU.add),
                  [psg.r, bst.r], [m2.r])
                V(lambda: nc.vector.tensor_tensor(out=sgo.t[:], in0=m2.t[:, 0:512], in1=gub.t[:], op=ALU.mult),
                  [m2.r, gub.r], [sgo.r])
                for hh in range(2):
                    ps = pnext()
                    for kc in range(4):
                        P(lambda: nc.tensor.matmul(ps.t[:], lhsT=OT.t[:, kc, :], rhs=wbm.t[:, kc, hh * 512:(hh + 1) * 512],
                                                   start=(kc == 0), stop=(kc == 3)), [OT.r, wbm.r], [ps.r])
                    V(lambda: nc.vector.tensor_tensor(out=m1.t[:, hh * 512:(hh + 1) * 512], in0=ps.t[:],
                                                      in1=sig.t[:, hh * 512:(hh + 1) * 512], op=ALU.mult), [ps.r, sig.r], [m1.r])
                tp2 = pnext(); tpb2 = tp2.t[:].bitcast(BF16)
                for kc in range(4):
                    P(lambda: nc.tensor.transpose(out=tpb2[:, kc * 128:(kc + 1) * 128], in_=sgo.t[:, kc * 128:(kc + 1) * 128],
                                                  identity=identb.t[:]), [sgo.r, identb.r], [tp2.r])
                A(lambda: nc.scalar.copy(out=SGT.t[:].rearrange("p k t -> p (k t)"), in_=tpb2[:, 0:512]), [tp2.r], [SGT.r])
                for hh in range(2):
                    ps = pnext()
                    for kc in range(4):
                        P(lambda: nc.tensor.matmul(ps.t[:], lhsT=SGT.t[:, kc, :], rhs=wbs.t[:, kc, hh * 512:(hh + 1) * 512],
                                                   start=(kc == 0), stop=(kc == 3)), [SGT.r, wbs.r], [ps.r])
                    V(lambda: nc.vector.tensor_tensor(out=m2.t[:, hh * 512:(hh + 1) * 512], in0=ps.t[:],
                                                      in1=sig.t[:, 1024 + hh * 512:1024 + (hh + 1) * 512], op=ALU.mult),
                      [ps.r, sig.r], [m2.r])
                    V(lambda: nc.vector.tensor_tensor(out=mrg.t[:, hh * 512:(hh + 1) * 512], in0=m1.t[:, hh * 512:(hh + 1) * 512],
                                                      in1=m2.t[:, hh * 512:(hh + 1) * 512], op=ALU.add), [m1.r, m2.r], [mrg.r])
                tp = pnext(); tpb = tp.t[:].bitcast(BF16)
                for kc in range(8):
                    P(lambda: nc.tensor.transpose(out=tpb[:, kc * 128:(kc + 1) * 128], in_=mrg.t[:, kc * 128:(kc + 1) * 128],
                                                  identity=identb.t[:]), [mrg.r, identb.r], [tp.r])
                A(lambda: nc.scalar.copy(out=mT.t[:].rearrange("p k t -> p (k t)"), in_=tpb[:, :]), [tp.r], [mT.r])
                xx = x1[i % 2]
                for hh in range(2):
                    ps = pnext()
                    for kc in range(8):
                        P(lambda: nc.tensor.matmul(ps.t[:], lhsT=mT.t[:, kc, :], rhs=wo.t[:, kc, hh * 512:(hh + 1) * 512],
                                                   start=(kc == 0), stop=(kc == 7)), [mT.r, wo.r], [ps.r])
                    cs_ = slice(hh * 512, (hh + 1) * 512)
                    V(lambda: nc.vector.tensor_tensor(out=m1.t[:, cs_], in0=ps.t[:], in1=gate1b.t[:, cs_], op=ALU.mult),
                      [ps.r, gate1b.r], [m1.r])
                V(lambda: nc.vector.tensor_tensor(out=xx.t[:], in0=m1.t[:], in1=xt.t[:], op=ALU.add), [m1.r, xt.r], [xx.r])
                T.dma(SP, xx.l, lambda: nc.sync.dma_start(out=X1[i * 128:(i + 1) * 128, :], in_=xx.t[:]), reads=[xx.r])

            def S3(i):
                xx = x1[i % 2]
                junk = FE["junk"]
                A(lambda: nc.scalar.activation(out=junk.t[:], in_=xx.t[:], func=AF.Square, accum_out=st2.t[:, 0:1]),
                  [xx.r], [junk.r, st2.r])
                V(lambda: nc.vector.tensor_scalar(out=st2.t[:, 1:2], in0=st2.t[:, 0:1], scalar1=1.0 / D, scalar2=EPS,
                                                  op0=ALU.mult, op1=ALU.add), [st2.r], [st2.r])
                A(lambda: nc.scalar.activation(out=st2.t[:, 2:3], in_=st2.t[:, 1:2], func=AF.Sqrt), [st2.r], [st2.r])
                V(lambda: nc.vector.reciprocal(out=st2.t[:, 3:4], in_=st2.t[:, 2:3]), [st2.r], [st2.r])
                V(lambda: nc.vector.tensor_scalar(out=x1n.t[:], in0=xx.t[:], scalar1=st2.t[:, 3:4], scalar2=None, op0=ALU.mult),
                  [xx.r, st2.r], [x1n.r])
                G(lambda: nc.gpsimd.tensor_tensor(out=h2f.t[:], in0=x1n.t[:], in1=s2b.t[:], op=ALU.mult), [x1n.r, s2b.r], [h2f.r])
                hp = h2p[0]
                G(lambda: nc.gpsimd.tensor_tensor(out=hp.t[:].rearrange("t (kc p) -> t p kc", kc=8),
                                                  in0=h2f.t[:].rearrange("t (p kc) -> t p kc", kc=8),
                                                  in1=sh2b.t[:].rearrange("t (p kc) -> t p kc", kc=8), op=ALU.add),
                  [h2f.r, sh2b.r], [hp.r])
                T.dma(SP, hp.l, lambda: nc.sync.dma_start(out=H2[i * 128:(i + 1) * 128, :], in_=hp.t[:]), reads=[hp.r])
                tA = pnext(); tB = pnext()
                for kc in range(8):
                    tt = tA if kc < 4 else tB
                    P(lambda: nc.tensor.transpose(out=tt.t[:, (kc % 4) * 128:(kc % 4 + 1) * 128], in_=x1n.t[:, kc * 128:(kc + 1) * 128],
                                                  identity=identf.t[:]), [x1n.r, identf.r], [tt.r])
                for kc in range(8):
                    tt = tA if kc < 4 else tB
                    V(lambda: nc.vector.tensor_scalar(out=h2T.t[:, kc, :], in0=tt.t[:, (kc % 4) * 128:(kc % 4 + 1) * 128],
                                                      scalar1=s2c.t[:, kc:kc + 1], scalar2=sh2(kc), op0=ALU.mult, op1=ALU.add),
                      [tt.r, s2c.r, modT.r], [h2T.r])
                pl = pnext()
                for kc in range(8):
                    P(lambda: nc.tensor.matmul(pl.t[:, 0:36], lhsT=h2T.t[:, kc, :], rhs=wrf.t[:, kc, :], start=(kc == 0), stop=(kc == 7)),
                      [h2T.r, wrf.r], [pl.r])
                V(lambda: nc.vector.tensor_tensor(out=lgall.t[:, i, :], in0=pl.t[:, 0:36], in1=brb.t[:], op=ALU.add), [pl.r, brb.r], [lgall.r])

            for step in range(NOWN + 2):
                if step < NOWN:
                    S1(step)
                if 0 <= step - 1 < NOWN:
                    S2(step - 1)
                if 0 <= step - 2 < NOWN:
                    S3(step - 2)

            TT = NOWN
            Lg = lgall.t[:, :, 0:4]
            Le4 = lgall.t[:, :, 4:36].rearrange("p t (g e) -> p t g e", g=4)
            gm = rtb.t[:, 0, :]; se = rtb.t[:, 1, :]; pgr = rtb.t[:, 2, :]; mv1 = rtb.t[:, 3, :]; mv2 = rtb.t[:, 4, :]
            dd_ = rtb.t[:, 5, :]; ee_ = rtb.t[:, 6, :]; rr_ = rtb.t[:, 7, :]
            ex = uv.t[:, 0:TT * 4].rearrange("p (t g) -> p t g", g=4)
            pen3 = x2.t[:, 0:TT * 4].rearrange("p (t g) -> p t g", g=4)
            Mx3 = inner.t[:, 0:TT * 32].rearrange("p (t e) -> p t e", e=32)
            Mx4 = inner.t[:, 0:TT * 32].rearrange("p (t g e) -> p t g e", g=4, e=8)
            My3 = m1.t[:, 0:TT * 32].rearrange("p (t e) -> p t e", e=32)
            bc3 = lambda ap2, n: ap2.unsqueeze(2).to_broadcast([128, TT, n])
            V(lambda: nc.vector.tensor_reduce(out=gm, in_=Lg, axis=AX.X, op=ALU.max), [lgall.r], [rtb.r])
            V(lambda: nc.vector.tensor_tensor(out=ex, in0=Lg, in1=bc3(gm, 4), op=ALU.subtract), [lgall.r, rtb.r], [uv.r])
            A(lambda: nc.scalar.activation(out=ex, in_=ex, func=AF.Exp), [uv.r], [uv.r])
            V(lambda: nc.vector.tensor_reduce(out=se, in_=ex, axis=AX.X, op=ALU.add), [uv.r], [rtb.r])
            V(lambda: nc.vector.reciprocal(out=pgr, in_=se), [rtb.r], [rtb.r])
            V(lambda: nc.vector.tensor_tensor(out=pen3, in0=Lg, in1=bc3(gm, 4), op=ALU.is_equal), [lgall.r, rtb.r], [x2.r])
            V(lambda: nc.vector.tensor_scalar(out=pen3, in0=pen3, scalar1=-1.0, scalar2=1e30, op0=ALU.add, op1=ALU.mult),
              [x2.r], [x2.r])
            V(lambda: nc.vector.tensor_tensor(out=Mx4, in0=Le4, in1=pen3.unsqueeze(3).to_broadcast([128, TT, 4, 8]), op=ALU.add),
              [lgall.r, x2.r], [inner.r])
            V(lambda: nc.vector.tensor_reduce(out=mv1, in_=Mx3, axis=AX.X, op=ALU.max), [inner.r], [rtb.r])
            V(lambda: nc.vector.tensor_tensor(out=A0all.t[:], in0=Mx3, in1=bc3(mv1, 32), op=ALU.is_equal), [inner.r, rtb.r], [A0all.r])
            V(lambda: nc.vector.scalar_tensor_tensor(out=My3, in0=A0all.t[:], scalar=-1e30, in1=Mx3, op0=ALU.mult, op1=ALU.add),
              [A0all.r, inner.r], [m1.r])
            V(lambda: nc.vector.tensor_reduce(out=mv2, in_=My3, axis=AX.X, op=ALU.max), [m1.r], [rtb.r])
            V(lambda: nc.vector.tensor_tensor(out=A1all.t[:], in0=My3, in1=bc3(mv2, 32), op=ALU.is_equal), [m1.r, rtb.r], [A1all.r])
            V(lambda: nc.vector.tensor_tensor(out=Aall.t[:], in0=A0all.t[:], in1=A1all.t[:], op=ALU.add), [A0all.r, A1all.r], [Aall.r])
            V(lambda: nc.vector.tensor_tensor(out=dd_, in0=mv2, in1=mv1, op=ALU.subtract), [rtb.r], [rtb.r])
            A(lambda: nc.scalar.activation(out=ee_, in_=dd_, func=AF.Exp), [rtb.r], [rtb.r])
            V(lambda: nc.vector.tensor_scalar(out=ee_, in0=ee_, scalar1=1.0, scalar2=None, op0=ALU.add), [rtb.r], [rtb.r])
            V(lambda: nc.vector.reciprocal(out=rr_, in_=ee_), [rtb.r], [rtb.r])
            V(lambda: nc.vector.tensor_tensor(out=wts.t[:, :, 0], in0=rr_, in1=pgr, op=ALU.mult), [rtb.r], [wts.r])
            V(lambda: nc.vector.tensor_tensor(out=wts.t[:, :, 1], in0=pgr, in1=wts.t[:, :, 0], op=ALU.subtract), [rtb.r, wts.r], [wts.r])
            T.barrier()
            if stop <= 3:
                return nc
        pc_.close()

        NA = NOWN * 32
        REG_SLOT = nc.gpsimd.to_reg(NSLOT - 1)
        REG_W = nc.gpsimd.to_reg(NEXP * 128 - 1)
        d0i = sbt(es, "d0i", [128, NOWN], I32); d1i = sbt(es, "d1i", [128, NOWN], I32)
        idxw = sbt(es, "idxw", [128, NBLK], I32)
        with ExitStack() as dp:
            set_rot(range(8))
            Ab_t = Aall.t[:].rearrange("p t e -> p (t e)")
            lsf = sbt(dp, "lsf", [128, 128], F32, True); load(lsf, c_lstrict)
            lsb = sbt(dp, "lsb", [128, 128], BF16)
            thr = sbt(dp, "thr", [128, NBLK], F32, True); load(thr, c_thr)
            pidx = sbt(dp, "pidx", [128, 1], F32, True); load(pidx, c_pidx)
            V(lambda: nc.vector.tensor_copy(out=lsb.t[:], in_=lsf.t[:]), [lsf.r], [lsb.r])
            tsum = sbt(dp, "tsum", [128, NOWN, 32], F32)
            rloc = sbt(dp, "rloc", [128, NOWN, 32], F32)
            for c0 in range(0, NA, 512):
                w_ = min(512, NA - c0)
                ps = pnext()
                P(lambda: nc.tensor.matmul(ps.t[:, 0:w_], lhsT=onesb.t[:], rhs=Ab_t[:, c0:c0 + w_], start=True, stop=True),
                  [onesb.r, Aall.r], [ps.r])
                V(lambda: nc.vector.tensor_copy(out=tsum.t[:].rearrange("p t e -> p (t e)")[:, c0:c0 + w_], in_=ps.t[:, 0:w_]),
                  [ps.r], [tsum.r])
                ps2 = pnext()
                P(lambda: nc.tensor.matmul(ps2.t[:, 0:w_], lhsT=lsb.t[:], rhs=Ab_t[:, c0:c0 + w_], start=True, stop=True),
                  [lsb.r, Aall.r], [ps2.r])
                V(lambda: nc.vector.tensor_copy(out=rloc.t[:].rearrange("p t e -> p (t e)")[:, c0:c0 + w_], in_=ps2.t[:, 0:w_]),
                  [ps2.r], [rloc.r])
            cum = sbt(dp, "cum", [128, NOWN + 1, 32], F32)
            V(lambda: nc.vector.memset(cum.t[:, 0, :], 0.0), [], [cum.r])
            for t in range(NOWN):
                V(lambda: nc.vector.tensor_tensor(out=cum.t[:, t + 1, :], in0=cum.t[:, t, :], in1=tsum.t[:, t, :], op=ALU.add),
                  [cum.r, tsum.r], [cum.r])
            ci = sbt(dp, "ci", [128, 32], I32)
            pad = sbt(dp, "pad", [128, 32], F32)
            pend = sbt(dp, "pend", [128, 32], F32)
            pst = sbt(dp, "pst", [128, 32], F32)
            V(lambda: nc.vector.tensor_scalar(out=pad.t[:], in0=cum.t[:, NOWN, :], scalar1=float(BLK - 1), scalar2=None,
                                              op0=ALU.add), [cum.r], [pad.r])
            V(lambda: nc.vector.tensor_copy(out=ci.t[:], in_=pad.t[:]), [pad.r], [ci.r])
            V(lambda: nc.vector.tensor_scalar(out=ci.t[:], in0=ci.t[:], scalar1=8, scalar2=8, op0=ALU.arith_shift_right,
                                              op1=ALU.logical_shift_left), [ci.r], [ci.r])
            V(lambda: nc.vector.tensor_copy(out=pad.t[:], in_=ci.t[:]), [ci.r], [pad.r])
            V(lambda: nc.vector.tensor_tensor_scan(out=pend.t[:], data0=onesf.t[:, 0:32], data1=pad.t[:], initial=0.0,
                                                   op0=ALU.mult, op1=ALU.add), [onesf.r, pad.r], [pend.r])
            V(lambda: nc.vector.tensor_tensor(out=pst.t[:], in0=pend.t[:], in1=pad.t[:], op=ALU.subtract), [pend.r, pad.r], [pst.r])
            V(lambda: nc.vector.tensor_tensor(out=rloc.t[:], in0=rloc.t[:], in1=cum.t[:, 0:NOWN, :], op=ALU.add),
              [rloc.r, cum.r], [rloc.r])
            V(lambda: nc.vector.tensor_tensor(out=rloc.t[:], in0=rloc.t[:], in1=pst.t[:].unsqueeze(1).to_broadcast([128, NOWN, 32]),
                                              op=ALU.add), [rloc.r, pst.r], [rloc.r])
            df = sbt(dp, "df", [128, NOWN], F32)
            for Ak, dk in ((A0all, d0i), (A1all, d1i)):
                V(lambda: nc.vector.tensor_tensor(out=tsum.t[:], in0=Ak.t[:], in1=rloc.t[:], op=ALU.mult), [Ak.r, rloc.r, tsum.r], [tsum.r])
                V(lambda: nc.vector.tensor_reduce(out=df.t[:], in_=tsum.t[:], axis=AX.X, op=ALU.add), [tsum.r], [df.r])
                V(lambda: nc.vector.tensor_copy(out=dk.t[:], in_=df.t[:]), [df.r], [dk.r])
            cmp_ = sbt(dp, "cmp", [128, NBLK, 32], F32)
            bef = sbt(dp, "bef", [128, NBLK], F32)
            V(lambda: nc.vector.tensor_tensor(out=cmp_.t[:], in0=pend.t[:].unsqueeze(1).to_broadcast([128, NBLK, 32]),
                                              in1=thr.t[:].unsqueeze(2).to_broadcast([128, NBLK, 32]), op=ALU.is_le),
              [pend.r, thr.r], [cmp_.r])
            V(lambda: nc.vector.tensor_reduce(out=bef.t[:], in_=cmp_.t[:], axis=AX.X, op=ALU.add), [cmp_.r], [bef.r])
            V(lambda: nc.vector.tensor_scalar(out=bef.t[:], in0=bef.t[:], scalar1=float(NEXP - 1), scalar2=128.0, op0=ALU.min,
                                              op1=ALU.mult), [bef.r], [bef.r])
            V(lambda: nc.vector.tensor_scalar(out=bef.t[:], in0=bef.t[:], scalar1=pidx.t[:, 0:1], scalar2=None, op0=ALU.add),
              [bef.r, pidx.r], [bef.r])
            skp = sbt(dp, "skp", [128, NBLK], F32)
            V(lambda: nc.vector.tensor_scalar(out=skp.t[:], in0=thr.t[:], scalar1=pend.t[:, 31:32], scalar2=1.0e6, op0=ALU.is_ge,
                                              op1=ALU.mult), [thr.r, pend.r], [skp.r])
            V(lambda: nc.vector.tensor_tensor(out=bef.t[:], in0=bef.t[:], in1=skp.t[:], op=ALU.add), [bef.r, skp.r], [bef.r])
            V(lambda: nc.vector.tensor_copy(out=idxw.t[:], in_=bef.t[:]), [bef.r], [idxw.r])
            hb = [sbt(dp, f"hb{i}", [128, D], BF16, True) for i in range(3)]
            xs_res = Res("XS")
            for t in range(NOWN):
                b_ = hb[t % 3]
                load(b_, H2[t * 128:(t + 1) * 128, :])
                for dk in (d0i, d1i):
                    T.dma(POOL, b_.l, lambda: nc.gpsimd.indirect_dma_start(
                        out=XS, out_offset=bass.IndirectOffsetOnAxis(ap=dk.t[:, t:t + 1], axis=0), in_=b_.t[:], in_offset=None,
                        bounds_check=REG_SLOT, oob_is_err=False),
                        reads=[b_.r, dk.r])
            T.barrier()
            if stop <= 4:
                return nc

        with ExitStack() as dd:
            set_rot(range(8))
            stg = [[sbt(dd, f"stg{i}_{k}", [128, 4096], F32, True) for k in range(3)] for i in range(2)]
            w1b = [sbt(dd, f"w1b{i}", [128, 8, 4, 128], BF16) for i in range(2)]
            w3b = [sbt(dd, f"w3b{i}", [128, 8, 4, 128], BF16) for i in range(2)]
            w2b = [sbt(dd, f"w2b{i}", [128, 4, D], BF16) for i in range(2)]
            xbk = [sbt(dd, f"xbk{i}", [128, 2, D], BF16, True) for i in range(2)]
            xTb = [sbt(dd, f"xTb{i}", [128, 8, 256], BF16) for i in range(1)]
            sil = sbt(dd, "sil", [128, 256], F32)
            gTb = [sbt(dd, f"gTb{i}", [128, 4, 256], BF16) for i in range(2)]
            ysb = [sbt(dd, f"ysb{i}", [128, D], F32, True) for i in range(2)]
            yc = 0

            def issue_w(j):
                s_ = stg[j % 2]
                for k, wsrc in enumerate((w1, w3, w2)):
                    T.dma(POOL, s_[k].l, lambda: nc.gpsimd.indirect_dma_start(
                        out=s_[k].t[:], out_offset=None, in_=wsrc,
                        in_offset=bass.IndirectOffsetOnAxis(ap=idxw.t[:, j:j + 1], axis=0),
                        bounds_check=REG_W, oob_is_err=False),
                        reads=[idxw.r], writes=[s_[k].r])

            def issue_x(j):
                load(xbk[j % 2], XS[j * BLK:(j + 1) * BLK, :].rearrange("(a p) d -> p a d", p=128))

            issue_w(0)
            issue_x(0)
            for j in range(NBLK):
                if j + 1 < NBLK:
                    issue_w(j + 1)
                    issue_x(j + 1)
                s_ = stg[j % 2]; b1 = w1b[j % 2]; b3 = w3b[j % 2]; b2 = w2b[j % 2]
                V(lambda: nc.vector.tensor_copy(out=b1.t[:], in_=s_[0].t[:].rearrange("p (kc m fc) -> p kc fc m", kc=8, m=128, fc=4)),
                  [s_[0].r], [b1.r])
                V(lambda: nc.vector.tensor_copy(out=b3.t[:], in_=s_[1].t[:].rearrange("p (kc m fc) -> p kc fc m", kc=8, m=128, fc=4)),
                  [s_[1].r], [b3.r])
                A(lambda: nc.scalar.copy(out=b2.t[:].rearrange("p f n -> p (f n)"), in_=s_[2].t[:]), [s_[2].r], [b2.r])
                xk = xbk[j % 2]; xT_ = xTb[0]; gT_ = gTb[j % 2]
                for st_ in range(2):
                    tp = pnext(); tpb = tp.t[:].bitcast(BF16)
                    for kc in range(8):
                        P(lambda: nc.tensor.transpose(out=tpb[:, kc * 128:(kc + 1) * 128], in_=xk.t[:, st_, kc * 128:(kc + 1) * 128],
                                                      identity=identb.t[:]), [xk.r, identb.r], [tp.r])
                    cp = (lambda f: A(f, [tp.r], [xT_.r])) if st_ == 0 else (lambda f: V(f, [tp.r], [xT_.r]))
                    if st_ == 0:
                        A(lambda: nc.scalar.copy(out=xT_.t[:, :, 0:128], in_=tpb.rearrange("p (k t) -> p k t", k=8)), [tp.r], [xT_.r])
                    else:
                        V(lambda: nc.vector.tensor_copy(out=xT_.t[:, :, 128:256], in_=tpb.rearrange("p (k t) -> p k t", k=8)),
                          [tp.r], [xT_.r])
                for fc in range(4):
                    pa = pnext(); pb_ = pnext()
                    for kc in range(8):
                        P(lambda: nc.tensor.matmul(pa.t[:, 0:256], lhsT=b1.t[:, kc, fc, :], rhs=xT_.t[:, kc, :], start=(kc == 0),
                                                   stop=(kc == 7)), [b1.r, xT_.r], [pa.r])
                    for kc in range(8):
                        P(lambda: nc.tensor.matmul(pb_.t[:, 0:256], lhsT=b3.t[:, kc, fc, :], rhs=xT_.t[:, kc, :], start=(kc == 0),
                                                   stop=(kc == 7)), [b3.r, xT_.r], [pb_.r])
                    A(lambda: nc.scalar.activation(out=sil.t[:], in_=pa.t[:, 0:256], func=AF.Silu), [pa.r], [sil.r])
                    V(lambda: nc.vector.tensor_tensor(out=gT_.t[:, fc, :], in0=pb_.t[:, 0:256], in1=sil.t[:], op=ALU.mult),
                      [pb_.r, sil.r], [gT_.r])
                for st_ in range(2):
                    y_ = ysb[yc % 2]; yc += 1
                    for hh in range(2):
                        ps = pnext()
                        for fc in range(4):
                            P(lambda: nc.tensor.matmul(ps.t[:], lhsT=gT_.t[:, fc, st_ * 128:(st_ + 1) * 128],
                                                       rhs=b2.t[:, fc, hh * 512:(hh + 1) * 512], start=(fc == 0), stop=(fc == 3)),
                              [gT_.r, b2.r], [ps.r])
                        if hh == 0:
                            A(lambda: nc.scalar.copy(out=y_.t[:, 0:512], in_=ps.t[:]), [ps.r], [y_.r])
                        else:
                            V(lambda: nc.vector.tensor_copy(out=y_.t[:, 512:1024], in_=ps.t[:]), [ps.r], [y_.r])
                    r0 = j * BLK + st_ * 128
                    T.dma(SP, y_.l, lambda: nc.sync.dma_start(out=YS[r0:r0 + 128, :], in_=y_.t[:]), reads=[y_.r])
            T.barrier()
            if stop <= 5:
                return nc

        with ExitStack() as ee:
            set_rot(range(8))
            gfb = sbt(ee, "gfb", [128, D], F32, True); load(gfb, gf_row.partition_broadcast(128))
            y0 = [sbt(ee, f"y0_{i}", [128, D], F32, True) for i in range(2)]
            y1 = [sbt(ee, f"y1_{i}", [128, D], F32, True) for i in range(2)]
            xr = [sbt(ee, f"xr{i}", [128, D], F32, True) for i in range(2)]
            acc = sbt(ee, "acc", [128, D], F32)
            jk = sbt(ee, "jk", [128, D], F32)
            ob = [sbt(ee, f"ob{i}", [128, D], F32, True) for i in range(2)]
            st3 = sbt(ee, "st3", [128, 4], F32)
            def issue_e(t):
                a0 = y0[t % 2]; a1 = y1[t % 2]; xx = xr[t % 2]
                T.dma(POOL, a0.l, lambda: nc.gpsimd.indirect_dma_start(
                    out=a0.t[:], out_offset=None, in_=YS, in_offset=bass.IndirectOffsetOnAxis(ap=d0i.t[:, t:t + 1], axis=0),
                    bounds_check=REG_SLOT, oob_is_err=False),
                    reads=[d0i.r], writes=[a0.r])
                T.dma(POOL, a1.l, lambda: nc.gpsimd.indirect_dma_start(
                    out=a1.t[:], out_offset=None, in_=YS, in_offset=bass.IndirectOffsetOnAxis(ap=d1i.t[:, t:t + 1], axis=0),
                    bounds_check=REG_SLOT, oob_is_err=False),
                    reads=[d1i.r], writes=[a1.r])
                load(xx, X1[t * 128:(t + 1) * 128, :])

            issue_e(0)
            for t in range(NOWN):
                a0 = y0[t % 2]; a1 = y1[t % 2]; xx = xr[t % 2]; o_ = ob[t % 2]
                if t + 1 < NOWN:
                    issue_e(t + 1)
                V(lambda: nc.vector.tensor_scalar(out=acc.t[:], in0=a0.t[:], scalar1=wts.t[:, t, 0:1], scalar2=None, op0=ALU.mult),
                  [a0.r, wts.r], [acc.r])
                V(lambda: nc.vector.scalar_tensor_tensor(out=acc.t[:], in0=a1.t[:], scalar=wts.t[:, t, 1:2], in1=acc.t[:],
                                                         op0=ALU.mult, op1=ALU.add), [a1.r, wts.r, acc.r], [acc.r])
                V(lambda: nc.vector.tensor_tensor(out=acc.t[:], in0=acc.t[:], in1=gate2b.t[:], op=ALU.mult), [acc.r, gate2b.r], [acc.r])
                V(lambda: nc.vector.tensor_tensor(out=acc.t[:], in0=acc.t[:], in1=xx.t[:], op=ALU.add), [acc.r, xx.r], [acc.r])
                A(lambda: nc.scalar.activation(out=jk.t[:], in_=acc.t[:], func=AF.Square, accum_out=st3.t[:, 0:1]),
                  [acc.r], [jk.r, st3.r])
                V(lambda: nc.vector.tensor_scalar(out=st3.t[:, 1:2], in0=st3.t[:, 0:1], scalar1=1.0 / D, scalar2=EPS,
                                                  op0=ALU.mult, op1=ALU.add), [st3.r], [st3.r])
                A(lambda: nc.scalar.activation(out=st3.t[:, 2:3], in_=st3.t[:, 1:2], func=AF.Sqrt), [st3.r], [st3.r])
                V(lambda: nc.vector.reciprocal(out=st3.t[:, 3:4], in_=st3.t[:, 2:3]), [st3.r], [st3.r])
                V(lambda: nc.vector.scalar_tensor_tensor(out=o_.t[:], in0=acc.t[:], scalar=st3.t[:, 3:4], in1=gfb.t[:],
                                                         op0=ALU.mult, op1=ALU.mult), [acc.r, st3.r, gfb.r], [o_.r])
                T.dma(SP, o_.l, lambda: nc.sync.dma_start(out=out_d[t * 128:(t + 1) * 128, :], in_=o_.t[:]), reads=[o_.r])
            T.barrier()
    return nc


def _own_tiles(j, NP):
    t = []
    for m in range(NP):
        t += [8 * m + j, 8 * m + 7 - j]
    return t


def make_in_maps(inp, S):
    f32 = np.float32
    NT = S // 128; NP = NT // 8; NOWN = 2 * NP; TOK = NOWN * 128
    NBLK = (TOK * 2) // BLK + NEXP
    g = lambda k: np.asarray(inp[k])
    col = lambda v, n: np.ascontiguousarray(np.asarray(v, f32).reshape(n, 128).T)
    x = g("x"); c = g("c"); pos = g("positions")
    p = np.arange(128)
    fq = np.zeros((128, 1), f32)
    freqs = (np.float32(10000.0) ** (-np.arange(16, dtype=f32) / np.float32(16))).astype(f32)
    fq[64:96, 0] = np.tile(freqs, 2) / np.float32(2 * np.pi)
    shared = dict(
        w_ada=np.ascontiguousarray(g("w_ada")[0]), b_adaT=col(g("b_ada")[0], 48), b_ada_row=g("b_ada")[0].reshape(1, -1).astype(f32),
        g1col=col(g("norm1_g")[0], 8), g1row=g("norm1_g")[0].reshape(1, -1).astype(f32), w_in=np.ascontiguousarray(g("w_in")[0]),
        gqcol=col(g("q_norm_g")[0], 2), w_uq=np.ascontiguousarray(g("w_uq")[0]),
        gkvcol=col(g("kv_norm_g")[0], 1), w_ukv=np.ascontiguousarray(g("w_ukv")[0]),
        gv_row=g("v_norm_g")[0].reshape(1, -1).astype(f32), bv_row=g("v_norm_b")[0].reshape(1, -1).astype(f32),
        w_s=np.ascontiguousarray(g("w_s")[0]), bsT=np.ascontiguousarray(g("b_s")[0].T),
        w_br_mla=np.ascontiguousarray(g("w_br_mla")[0]), w_br_sgu=np.ascontiguousarray(g("w_br_sgu")[0]),
        w_out=np.ascontiguousarray(g("w_out")[0]),
        g2col=col(g("norm2_g")[0], 8), g2row=g("norm2_g")[0].reshape(1, -1).astype(f32),
        wr=np.ascontiguousarray(np.concatenate([g("w_rg")[0], g("w_re")[0]], axis=1)),
        br_row=np.concatenate([g("b_rg")[0], g("b_re")[0]]).reshape(1, -1).astype(f32),
        w1=np.ascontiguousarray(g("w1")[0]).reshape(NEXP * 128, 4096),
        w3=np.ascontiguousarray(g("w3")[0]).reshape(NEXP * 128, 4096),
        w2=np.ascontiguousarray(g("w2")[0]).reshape(NEXP * 128, 4096),
        gf_row=g("final_g").reshape(1, -1).astype(f32),
        c_ident=np.eye(128, dtype=f32), c_tril=np.tril(np.ones((128, 128), f32)),
        c_lstrict=(p[:, None] < p[None, :]).astype(f32), c_kq=(p[:, None] - p[None, :]).astype(f32),
        c_fq=fq, c_thr=np.tile((np.arange(NBLK, dtype=f32) * BLK)[None, :], (128, 1)).astype(f32),
        c_pidx=p.astype(f32).reshape(128, 1),
    )
    maps = []
    owns = []
    for core in range(8):
        b, j = core // 4, core % 4
        tiles = _own_tiles(j, NP)
        rows = np.concatenate([np.arange(t * 128, (t + 1) * 128) for t in tiles])
        owns.append((b, rows))
        dt_ = np.zeros((128, 16), f32)
        for o in range(8):
            dt_[:, o] = (j - o) * 128
            dt_[:, 8 + o] = (7 - j - o) * 128
        m = dict(shared)
        m.update(
            xb=np.ascontiguousarray(x[b, :S]), xo=np.ascontiguousarray(x[b][rows]),
            posb=np.ascontiguousarray(pos[b, :S].reshape(1, -1).astype(np.int32)),
            poso=np.ascontiguousarray(pos[b][rows].reshape(1, -1).astype(np.int32)),
            ccol=col(c[b], 8), c_dtab=dt_,
        )
        maps.append(m)
    return maps, owns


_NC_CACHE = {}


def kernel(**inputs):
    S = int(np.asarray(inputs["x"]).shape[1])
    if S not in _NC_CACHE:
        _NC_CACHE[S] = build_nc(S)
    nc = _NC_CACHE[S]
    maps, owns = make_in_maps(inputs, S)
    res = run_bass_kernel_spmd(nc, maps, core_ids=list(range(8)))
    B = np.asarray(inputs["x"]).shape[0]
    out = np.zeros((B, S, D), np.float32)
    for core, (b, rows) in enumerate(owns):
        out[b, rows] = np.asarray(res.results[core]["out"], dtype=np.float32)
    return out
```

```python
import numpy as np
from contextlib import ExitStack
import concourse.bass as bass
import concourse.mybir as mybir
from concourse.bass_utils import run_bass_kernel_spmd

F32 = mybir.dt.float32
BF16 = mybir.dt.bfloat16
I32 = mybir.dt.int32
AF = mybir.ActivationFunctionType
ALU = mybir.AluOpType
AX = mybir.AxisListType

D = 1024
NH = 8
EPS = 1e-6
NEXP = 32
FF = 512
BLK = 256
IN_W = 3488
SCALE = 96 ** -0.5
GELU_C = 1.5957691216057308


class Res:
    __slots__ = ("name", "w", "r")

    def __init__(self, name=""):
        self.name = name
        self.w = None
        self.r = {}


class Lane:
    def __init__(self, T, name):
        self.sem = T.newsem(name)
        self.count = 0


class Eng:
    def __init__(self, T, name, eng, is_pe=False):
        self.name = name
        self.eng = eng
        self.is_pe = is_pe
        self.sem = T.newsem(name + "_s0")
        self.own = [self.sem]
        self.count = 0
        self.waited = {}


class Tracker:
    CAP = 12000

    def __init__(self, nc, es):
        self.nc = nc
        self.es = es
        self.nsems = 0
        self.lanes = []
        self.pe = Eng(self, "pe", nc.tensor, True)
        self.act = Eng(self, "act", nc.scalar)
        self.dve = Eng(self, "dve", nc.vector)
        self.pool = Eng(self, "pool", nc.gpsimd)
        self.sp = Eng(self, "sp", nc.sync)
        self.engs = [self.pe, self.act, self.dve, self.pool, self.sp]

    def newsem(self, name):
        self.nsems += 1
        return self.es.enter_context(self.nc.semaphore(f"{name}_{self.nsems}"))

    def lane(self, name="l"):
        l = Lane(self, name)
        self.lanes.append(l)
        return l

    def _wait(self, E, deps):
        for sem, val in deps:
            if E.is_pe and any(sem is s for s in E.own):
                continue
            k = id(sem)
            if E.waited.get(k, 0) < val:
                E.eng.wait_ge(sem, val)
                E.waited[k] = val

    @staticmethod
    def _deps(reads, writes):
        deps = []
        for r in reads:
            if r.w is not None:
                deps.append(r.w)
        for w in writes:
            if w.w is not None:
                deps.append(w.w)
            deps.extend(w.r.values())
        return deps

    @staticmethod
    def _record(ev, reads, writes):
        for r in reads:
            r.r[id(ev[0])] = ev
        for w in writes:
            w.w = ev
            w.r = {}

    def op(self, E, fn, reads=(), writes=()):
        self._wait(E, self._deps(reads, writes))
        if E.count >= self.CAP:
            E.sem = self.newsem(E.name + "_s")
            E.own.append(E.sem)
            E.count = 0
        ins = fn()
        ins.then_inc(E.sem, 1)
        E.count += 1
        self._record((E.sem, E.count), reads, writes)

    def dma(self, E, lane, fn, reads=(), writes=()):
        self._wait(E, self._deps(reads, writes))
        ins = fn()
        ins.then_inc(lane.sem, 16)
        lane.count += 16
        self._record((lane.sem, lane.count), reads, writes)

    def barrier(self):
        evs = [(e.sem, e.count) for e in self.engs if e.count > 0]
        evs += [(l.sem, l.count) for l in self.lanes if l.count > 0]
        for E in self.engs:
            self._wait(E, evs)


class Buf:
    def __init__(self, T, tile, name, lane=False):
        self.t = tile
        self.r = Res(name)
        self.l = T.lane(name) if lane else None


def build_nc(S, stop=99):
    NT = S // 128
    NP = NT // 8
    NOWN = 2 * NP
    TOK = NOWN * 128
    NG = NT // 4
    NGO = max(NOWN // 4, 1)
    GW = min(512, TOK)
    NBLK = (TOK * 2) // BLK + NEXP
    NSLOT = NBLK * BLK

    nc = bass.Bass("TRN2", target_bir_lowering=False)

    def din(name, shape, dt=F32):
        return nc.dram_tensor(name, list(shape), dt, kind="ExternalInput").ap()

    xb = din("xb", [S, D]); xo = din("xo", [TOK, D])
    posb = din("posb", [1, S], I32); poso = din("poso", [1, TOK], I32)
    ccol = din("ccol", [128, 8])
    w_ada = din("w_ada", [D, 6 * D]); b_adaT = din("b_adaT", [128, 48]); b_ada_row = din("b_ada_row", [1, 6 * D])
    g1col = din("g1col", [128, 8]); g1row = din("g1row", [1, D]); w_in = din("w_in", [D, IN_W])
    gqcol = din("gqcol", [128, 2]); w_uq = din("w_uq", [256, 768])
    gkvcol = din("gkvcol", [128, 1]); w_ukv = din("w_ukv", [128, 1024])
    gv_row = din("gv_row", [1, 512]); bv_row = din("bv_row", [1, 512])
    w_s = din("w_s", [8, 128, 128]); bsT = din("bsT", [128, 8])
    w_br_mla = din("w_br_mla", [512, D]); w_br_sgu = din("w_br_sgu", [512, D]); w_out = din("w_out", [D, D])
    g2col = din("g2col", [128, 8]); g2row = din("g2row", [1, D])
    wr = din("wr", [D, 36]); br_row = din("br_row", [1, 36])
    w1 = din("w1", [NEXP * 128, 4096]); w3 = din("w3", [NEXP * 128, 4096]); w2 = din("w2", [NEXP * 128, 4096])
    gf_row = din("gf_row", [1, D])
    c_ident = din("c_ident", [128, 128]); c_tril = din("c_tril", [128, 128]); c_lstrict = din("c_lstrict", [128, 128])
    c_kq = din("c_kq", [128, 128]); c_fq = din("c_fq", [128, 1]); c_dtab = din("c_dtab", [128, 16])
    c_thr = din("c_thr", [128, NBLK]); c_pidx = din("c_pidx", [128, 1])

    out_d = nc.dram_tensor("out", [TOK, D], F32, kind="ExternalOutput").ap()
    X1 = nc.dram_tensor("X1s", [TOK, D], F32, kind="Internal").ap()
    H2 = nc.dram_tensor("H2s", [TOK, D], BF16, kind="Internal").ap()
    XS = nc.dram_tensor("XSs", [NSLOT, D], BF16, kind="Internal").ap()
    YS = nc.dram_tensor("YSs", [NSLOT, D], F32, kind="Internal").ap()
    OS = nc.dram_tensor("OSs", [TOK, 512], BF16, kind="Internal").ap()

    with ExitStack() as es:
        T = Tracker(nc, es)
        PE, ACT, DVE, POOL, SP = T.pe, T.act, T.dve, T.pool, T.sp

        def sbt(stack, name, shape, dt, lane=False):
            t = stack.enter_context(nc.sbuf_tensor(name, list(shape), dt))
            return Buf(T, t, name, lane)

        banks = []
        for i in range(8):
            t = es.enter_context(nc.psum_tensor(f"pb{i}", [128, 512], F32))
            banks.append(Buf(T, t, f"pb{i}"))
        rot = {"i": 0, "lst": list(range(8))}

        def pnext():
            b = banks[rot["lst"][rot["i"] % len(rot["lst"])]]
            rot["i"] += 1
            return b

        def set_rot(lst):
            rot["lst"] = list(lst)
            rot["i"] = 0

        def V(fn, reads=(), writes=()):
            T.op(DVE, fn, reads, writes)

        def A(fn, reads=(), writes=()):
            T.op(ACT, fn, reads, writes)

        def G(fn, reads=(), writes=()):
            T.op(POOL, fn, reads, writes)

        def P(fn, reads=(), writes=()):
            T.op(PE, fn, reads, writes)

        def load(buf, src, eng=None):
            E = SP if eng is None else eng
            T.dma(E, buf.l, lambda: E.eng.dma_start(out=buf.t[:], in_=src), writes=[buf.r])

        def load_to(buf, dst_ap, src, eng=None):
            E = SP if eng is None else eng
            T.dma(E, buf.l, lambda: E.eng.dma_start(out=dst_ap, in_=src), writes=[buf.r])

        identf = sbt(es, "identf", [128, 128], F32, True); load(identf, c_ident)
        identb = sbt(es, "identb", [128, 128], BF16)
        V(lambda: nc.vector.tensor_copy(out=identb.t[:], in_=identf.t[:]), [identf.r], [identb.r])
        onesb = sbt(es, "onesb", [128, 128], BF16)
        V(lambda: nc.vector.memset(onesb.t[:], 1.0), [], [onesb.r])
        onesf = sbt(es, "onesf", [128, 128], F32)
        V(lambda: nc.vector.memset(onesf.t[:], 1.0), [], [onesf.r])
        kqf = sbt(es, "kqf", [128, 128], F32, True); load(kqf, c_kq)
        kqb = sbt(es, "kqb", [128, 128], BF16)
        V(lambda: nc.vector.tensor_copy(out=kqb.t[:], in_=kqf.t[:]), [kqf.r], [kqb.r])
        fq = sbt(es, "fq", [128, 1], F32, True); load(fq, c_fq)
        dtab = sbt(es, "dtab", [128, 16], F32, True); load(dtab, c_dtab)
        g1c = sbt(es, "g1c", [128, 8], F32, True); load(g1c, g1col)
        g2c = sbt(es, "g2c", [128, 8], F32, True); load(g2c, g2col)
        gqc = sbt(es, "gqc", [128, 2], F32, True); load(gqc, gqcol)
        gkvc = sbt(es, "gkvc", [128, 1], F32, True); load(gkvc, gkvcol)
        cc = sbt(es, "cc", [128, 8], F32, True); load(cc, ccol)
        badT = sbt(es, "badT", [128, 48], F32, True); load(badT, b_adaT)
        modT = sbt(es, "modT", [128, 48], F32)
        gate1b = sbt(es, "gate1b", [128, D], F32)
        s2b = sbt(es, "s2b", [128, D], F32)
        sh2b = sbt(es, "sh2b", [128, D], F32)
        gate2b = sbt(es, "gate2b", [128, D], F32)
        s1b = sbt(es, "s1b", [128, D], F32)
        sh1b = sbt(es, "sh1b", [128, 8], BF16)
        ones512 = sbt(es, "ones512", [1, 512], BF16)
        V(lambda: nc.vector.memset(ones512.t[:], 1.0), [], [ones512.r])
        s1c = sbt(es, "s1c", [128, 8], F32); s2c = sbt(es, "s2c", [128, 8], F32)

        with ExitStack() as ph:
            sc = sbt(ph, "sc", [128, 8], F32)
            A(lambda: nc.scalar.activation(out=sc.t[:], in_=cc.t[:], func=AF.Silu), [cc.r], [sc.r])
            brow = [sbt(ph, f"brow{i}", [1, 512], F32, True) for i in range(2)]
            rowdst = {2: (s1b, 0), 3: (s1b, 512), 4: (gate1b, 0), 5: (gate1b, 512), 6: (sh2b, 0), 7: (sh2b, 512), 8: (s2b, 0), 9: (s2b, 512),
                      10: (gate2b, 0), 11: (gate2b, 512)}
            wp = [sbt(ph, f"wada{i}", [128, 8, 512], F32, True) for i in range(2)]
            pcol = banks[7]
            set_rot(range(7))
            rowtmp = sbt(ph, "rowtmp", [1, 512], F32)
            for pc in range(12):
                wb_ = wp[pc % 2]
                load(wb_, w_ada[:, pc * 512:(pc + 1) * 512].rearrange("(kc p) n -> p kc n", p=128))
                if pc in rowdst:
                    prow = pnext()
                    br_ = brow[pc % 2]
                    load(br_, b_ada_row[0:1, pc * 512:(pc + 1) * 512])
                    for kc in range(8):
                        P(lambda: nc.tensor.matmul(prow.t[0:1, :], lhsT=sc.t[:, kc:kc + 1], rhs=wb_.t[:, kc, :],
                                                   start=(kc == 0), stop=(kc == 7)), [sc.r, wb_.r], [prow.r])
                    V(lambda: nc.vector.tensor_tensor(out=rowtmp.t[0:1, :], in0=prow.t[0:1, :], in1=br_.t[0:1, :], op=ALU.add),
                      [prow.r, br_.r], [rowtmp.r])
                    pbc = pnext()
                    P(lambda: nc.tensor.matmul(pbc.t[:], lhsT=onesf.t[0:1, :], rhs=rowtmp.t[0:1, :], start=True, stop=True),
                      [onesf.r, rowtmp.r], [pbc.r])
                    dtile, dc0 = rowdst[pc]
                    V(lambda: nc.vector.tensor_copy(out=dtile.t[:, dc0:dc0 + 512], in_=pbc.t[:]), [pbc.r], [dtile.r])
                for oc in range(4):
                    col = pc * 4 + oc
                    for kc in range(8):
                        P(lambda: nc.tensor.matmul(pcol.t[:, col:col + 1], lhsT=wb_.t[:, kc, oc * 128:(oc + 1) * 128],
                                                   rhs=sc.t[:, kc:kc + 1], start=(kc == 0), stop=(kc == 7)),
                          [sc.r, wb_.r], [pcol.r])
            V(lambda: nc.vector.tensor_tensor(out=modT.t[:], in0=pcol.t[:, 0:48], in1=badT.t[:], op=ALU.add),
              [pcol.r, badT.r], [modT.r])
            V(lambda: nc.vector.scalar_tensor_tensor(out=s1c.t[:], in0=modT.t[:, 8:16], scalar=1.0, in1=g1c.t[:],
                                                     op0=ALU.add, op1=ALU.mult), [modT.r, g1c.r], [s1c.r])
            V(lambda: nc.vector.scalar_tensor_tensor(out=s2c.t[:], in0=modT.t[:, 32:40], scalar=1.0, in1=g2c.t[:],
                                                     op0=ALU.add, op1=ALU.mult), [modT.r, g2c.r], [s2c.r])
            g1rb = sbt(ph, "g1rb", [128, D], F32, True); load(g1rb, g1row.partition_broadcast(128))
            V(lambda: nc.vector.scalar_tensor_tensor(out=s1b.t[:], in0=s1b.t[:], scalar=1.0, in1=g1rb.t[:], op0=ALU.add,
                                                     op1=ALU.mult), [s1b.r, g1rb.r], [s1b.r])
            V(lambda: nc.vector.tensor_copy(out=sh1b.t[:], in_=modT.t[:, 0:8]), [modT.r], [sh1b.r])
            T.barrier()
        if stop <= 0:
            return nc
        sh1 = lambda kc: modT.t[:, kc:kc + 1]
        sh2 = lambda kc: modT.t[:, 24 + kc:25 + kc]

        FE = {}
        cnt = {"x": 0, "fe": 0, "st": 0, "xn": 0, "cp": 0}

        def make_fe(stack, nx=3, nxn=2):
            k = cnt["fe"]; cnt["fe"] += 1
            FE["nx"] = nx
            FE["xts"] = [sbt(stack, f"xt{k}_{i}", [128, D], F32, True) for i in range(nx)]
            FE["junk"] = sbt(stack, f"junk{k}", [128, D], BF16)
            FE["stat"] = [sbt(stack, f"stat{k}_{i}", [128, 16], F32) for i in range(3)]
            FE["xns"] = [sbt(stack, f"xn{k}_{i}", [128, D], BF16) for i in range(nxn)]

        def fe_a(tile_aps):
            n = len(tile_aps)
            st = FE["stat"][cnt["st"] % 3]; cnt["st"] += 1
            junk = FE["junk"]
            xl = []
            for j in range(n):
                xt = FE["xts"][cnt["x"] % FE["nx"]]; cnt["x"] += 1
                load(xt, tile_aps[j])
                xl.append(xt)
            for j in range(n):
                A(lambda: nc.scalar.activation(out=junk.t[:], in_=xl[j].t[:], func=AF.Square, accum_out=st.t[:, j:j + 1]),
                  [xl[j].r], [junk.r, st.r])
            V(lambda: nc.vector.tensor_scalar(out=st.t[:, 4:4 + n], in0=st.t[:, 0:n], scalar1=1.0 / D, scalar2=EPS,
                                              op0=ALU.mult, op1=ALU.add), [st.r], [st.r])
            A(lambda: nc.scalar.activation(out=st.t[:, 8:8 + n], in_=st.t[:, 4:4 + n], func=AF.Sqrt), [st.r], [st.r])
            V(lambda: nc.vector.reciprocal(out=st.t[:, 12:12 + n], in_=st.t[:, 8:8 + n]), [st.r], [st.r])
            xnl = []
            for j in range(n):
                xn = FE["xns"][cnt["xn"] % len(FE["xns"])]; cnt["xn"] += 1
                V(lambda: nc.vector.scalar_tensor_tensor(out=xn.t[:], in0=xl[j].t[:], scalar=st.t[:, 12 + j:13 + j], in1=s1b.t[:],
                                                         op0=ALU.mult, op1=ALU.mult), [xl[j].r, st.r, s1b.r], [xn.r])
                xnl.append(xn)
            return xl, xnl

        def fe_b(state, dst, dst_res, c0s):
            xl, xnl = state
            for j, xn in enumerate(xnl):
                tp = pnext()
                tpb = tp.t[:].bitcast(BF16)
                for kc in range(8):
                    P(lambda: nc.tensor.transpose(out=tpb[:, kc * 128:(kc + 1) * 128], in_=xn.t[:, kc * 128:(kc + 1) * 128],
                                                  identity=identb.t[:]), [xn.r, identb.r], [tp.r])
                c0 = c0s[j]
                src3 = tpb.rearrange("p (k t) -> p k t", k=8)
                if cnt["cp"] % 2 == 0:
                    A(lambda: nc.scalar.copy(out=dst[:, :, c0:c0 + 128], in_=src3), [tp.r], [dst_res])
                else:
                    V(lambda: nc.vector.tensor_copy(out=dst[:, :, c0:c0 + 128], in_=src3), [tp.r], [dst_res])
                cnt["cp"] += 1
            return xl

        def front_end_multi(tile_aps, dst, dst_res, c0s):
            return fe_b(fe_a(tile_aps), dst, dst_res, c0s)

        def bias_row(dst_ap, wfn, M, wres, dres):
            ps = pnext()
            for kc in range(8):
                P(lambda: nc.tensor.matmul(ps.t[0:1, 0:M], lhsT=sh1b.t[:, kc:kc + 1], rhs=wfn(kc), start=(kc == 0), stop=(kc == 7)),
                  [sh1b.r, wres], [ps.r])
            V(lambda: nc.vector.tensor_copy(out=dst_ap, in_=ps.t[0:1, 0:M]), [ps.r], [dres])

        s1col = lambda kc: s1c.t[:, kc:kc + 1]

        Aall = sbt(es, "Aall", [128, NOWN, 32], BF16)
        A0all = sbt(es, "A0all", [128, NOWN, 32], BF16)
        A1all = sbt(es, "A1all", [128, NOWN, 32], BF16)
        wts = sbt(es, "wts", [128, NOWN, 2], F32)
        att = es.enter_context(ExitStack())
        ckvnT = sbt(att, "ckvnT", [128, S], BF16); ckvn_r = [Res() for _ in range(NG)]
        KT = sbt(att, "KT", [128, S], BF16); ktn_r = [Res() for _ in range(NG)]; ktr_r = [Res() for _ in range(NG)]
        vh_r = [Res() for _ in range(NG)]
        cqnT = sbt(att, "cqnT", [128, 2, TOK], BF16); cqn_r = [Res() for _ in range(NGO)]
        qT_r = [Res() for _ in range(NGO)]
        CQ = sbt(att, "CQ", [128, TOK], BF16); SQ = sbt(att, "SQ", [128, TOK], BF16)
        cs_r = [Res() for _ in range(NGO)]

        def rope_tables(pos_ap_512, Cdst, Sdst, wres, lp, width):
            pi_ = lp["posi"]; R = slice(64, 96)
            T.dma(SP, pi_.l, lambda: nc.sync.dma_start(out=pi_.t[R, 0:width], in_=pos_ap_512.partition_broadcast(32)),
                  writes=[pi_.r])
            pf = lp["pf"]; u = lp["u"]; ki = lp["ki"]; kf = lp["kf"]; f2 = lp["f2"]
            V(lambda: nc.vector.tensor_copy(out=pf.t[R, 0:width], in_=pi_.t[R, 0:width]), [pi_.r], [pf.r])
            for which, off, dstb in ((0, 0.5, Sdst), (1, 0.75, Cdst)):
                V(lambda: nc.vector.tensor_scalar(out=u.t[R, 0:width], in0=pf.t[R, 0:width], scalar1=fq.t[R, 0:1],
                                                  scalar2=off, op0=ALU.mult, op1=ALU.add), [pf.r, fq.r], [u.r])
                V(lambda: nc.vector.tensor_copy(out=ki.t[R, 0:width], in_=u.t[R, 0:width]), [u.r], [ki.r])
                V(lambda: nc.vector.tensor_copy(out=kf.t[R, 0:width], in_=ki.t[R, 0:width]), [ki.r], [kf.r])
                V(lambda: nc.vector.scalar_tensor_tensor(out=f2.t[R, 0:width], in0=u.t[R, 0:width], scalar=-0.5,
                                                         in1=kf.t[R, 0:width], op0=ALU.add, op1=ALU.subtract),
                  [u.r, kf.r], [f2.r])
                V(lambda: nc.vector.scalar_tensor_tensor(out=u.t[R, 0:width], in0=f2.t[R, 0:width], scalar=-0.5,
                                                         in1=f2.t[R, 0:width], op0=ALU.is_lt, op1=ALU.add),
                  [f2.r], [u.r])
                A(lambda: nc.scalar.activation(out=dstb, in_=u.t[R, 0:width], func=AF.Sin, scale=6.28318),
                  [u.r], [wres])

        with ExitStack() as lp_:
            make_fe(lp_, 4, 4)
            lp = {}
            lp["posi"] = sbt(lp_, "posi", [128, 512], I32, True)
            for nm in ("pf", "u", "kf", "f2"):
                lp[nm] = sbt(lp_, "lp_" + nm, [128, 512], F32)
            lp["t1"] = lp["pf"]; lp["t2"] = lp["kf"]
            lp["ki"] = sbt(lp_, "lp_ki", [128, 512], I32)
            Ctmp = sbt(lp_, "Ctmp", [128, 512], F32); Stmp = sbt(lp_, "Stmp", [128, 512], F32)
            h1Ts = [sbt(lp_, f"h1T{i}", [128, 8, 512], BF16) for i in range(2)]
            sq = sbt(lp_, "sq", [128, 2, 512], BF16)
            rs = sbt(lp_, "rs", [128, 512], F32); rs2 = sbt(lp_, "rs2", [128, 512], F32)
            wkv = sbt(lp_, "wkv", [128, 8, 128], BF16, True)
            wkr = sbt(lp_, "wkr", [128, 8, 96], BF16, True)
            wkrr = sbt(lp_, "wkrr", [128, 8, 96], BF16)
            wq = sbt(lp_, "wq", [128, 8, 256], BF16, True)
            win_v = w_in.rearrange("(kc p) n -> p kc n", p=128)
            load(wkv, win_v[:, :, 256:384], POOL)
            V(lambda: nc.vector.memset(wkr.t[:], 0.0), [], [wkr.r])
            V(lambda: nc.vector.memset(wkrr.t[:], 0.0), [], [wkrr.r])
            load_to(wkr, wkr.t[:, :, 64:96], win_v[:, :, 384:416], POOL)
            load(wq, win_v[:, :, 0:256], POOL)
            V(lambda: nc.vector.tensor_scalar(out=wkrr.t[:, :, 64:80], in0=wkr.t[:, :, 80:96], scalar1=-1.0, scalar2=None,
                                              op0=ALU.mult), [wkr.r, wkrr.r], [wkrr.r])
            V(lambda: nc.vector.tensor_copy(out=wkrr.t[:, :, 80:96], in_=wkr.t[:, :, 64:80]), [wkr.r, wkrr.r], [wkrr.r])
            set_rot(range(8))
            brows = sbt(lp_, "brows", [1, 640], BF16)
            bias_row(brows.t[0:1, 0:128], lambda kc: wkv.t[:, kc, :], 128, wkv.r, brows.r)
            bias_row(brows.t[0:1, 128:224], lambda kc: wkr.t[:, kc, :], 96, wkr.r, brows.r)
            bias_row(brows.t[0:1, 224:320], lambda kc: wkrr.t[:, kc, :], 96, wkrr.r, brows.r)
            bias_row(brows.t[0:1, 320:576], lambda kc: wq.t[:, kc, :], 256, wq.r, brows.r)
            hcnt = {"i": 0}
            h1_of = {}

            fe_state = {}

            def latent_fe_a(mode, g, width):
                ntl = width // 128
                src = xb if mode == "k" else xo
                fe_state[(mode, g)] = fe_a([src[(g * 4 + tl) * 128:(g * 4 + tl + 1) * 128, :] for tl in range(ntl)])

            def latent_fe_b(mode, g, width):
                ntl = width // 128
                h1 = h1Ts[hcnt["i"] % 2]; hcnt["i"] += 1
                h1_of[(mode, g)] = h1
                fe_b(fe_state.pop((mode, g)), h1.t, h1.r, [tl * 128 for tl in range(ntl)])

            mm_out = {}

            def latent_mm(mode, g, width):
                h1 = h1_of.pop((mode, g))
                W = slice(0, width)
                if mode == "k":
                    pk = pnext(); pr = pnext(); prr = pnext()
                    for kc in range(8):
                        P(lambda: nc.tensor.matmul(pk.t[:, W], lhsT=wkv.t[:, kc, :], rhs=h1.t[:, kc, W],
                                                   start=(kc == 0), stop=False), [wkv.r, h1.r], [pk.r])
                    P(lambda: nc.tensor.matmul(pk.t[:, W], lhsT=brows.t[0:1, 0:128], rhs=ones512.t[0:1, W], start=False, stop=True),
                      [brows.r, ones512.r], [pk.r])
                    for kc in range(8):
                        P(lambda: nc.tensor.matmul(pr.t[0:96, W], lhsT=wkr.t[:, kc, :], rhs=h1.t[:, kc, W],
                                                   start=(kc == 0), stop=False), [wkr.r, h1.r], [pr.r])
                    P(lambda: nc.tensor.matmul(pr.t[0:96, W], lhsT=brows.t[0:1, 128:224], rhs=ones512.t[0:1, W], start=False, stop=True),
                      [brows.r, ones512.r], [pr.r])
                    for kc in range(8):
                        P(lambda: nc.tensor.matmul(prr.t[0:96, W], lhsT=wkrr.t[:, kc, :], rhs=h1.t[:, kc, W],
                                                   start=(kc == 0), stop=False), [wkrr.r, h1.r], [prr.r])
                    P(lambda: nc.tensor.matmul(prr.t[0:96, W], lhsT=brows.t[0:1, 224:320], rhs=ones512.t[0:1, W], start=False, stop=True),
                      [brows.r, ones512.r], [prr.r])
                    chunks = [pk]
                    gcol = [gkvc.t[:, 0:1]]
                    nfeat = 128
                else:
                    p0 = pnext(); p1 = pnext()
                    for ci, pp in enumerate((p0, p1)):
                        for kc in range(8):
                            P(lambda: nc.tensor.matmul(pp.t[:, W], lhsT=wq.t[:, kc, ci * 128:(ci + 1) * 128],
                                                       rhs=h1.t[:, kc, W], start=(kc == 0), stop=False),
                              [wq.r, h1.r], [pp.r])
                        P(lambda: nc.tensor.matmul(pp.t[:, W], lhsT=brows.t[0:1, 320 + ci * 128:320 + (ci + 1) * 128],
                                                   rhs=ones512.t[0:1, W], start=False, stop=True), [brows.r, ones512.r], [pp.r])
                    chunks = [p0, p1]
                    gcol = [gqc.t[:, 0:1], gqc.t[:, 1:2]]
                    nfeat = 256
                mm_out[(mode, g)] = (chunks, gcol, nfeat, (pr, prr) if mode == "k" else None)

            def latent_rest(mode, g, width):
                W = slice(0, width)
                chunks, gcol, nfeat, prs = mm_out.pop((mode, g))
                if prs is not None:
                    pr, prr = prs
                for ci, pp in enumerate(chunks):
                    A(lambda: nc.scalar.activation(out=sq.t[:, ci, W], in_=pp.t[:, W], func=AF.Square), [pp.r], [sq.r])
                pss = pnext()
                for ci in range(len(chunks)):
                    P(lambda: nc.tensor.matmul(pss.t[:, W], lhsT=onesb.t[:], rhs=sq.t[:, ci, W], start=(ci == 0),
                                               stop=(ci == len(chunks) - 1)), [onesb.r, sq.r], [pss.r])
                V(lambda: nc.vector.tensor_scalar(out=rs.t[:, W], in0=pss.t[:, W], scalar1=1.0 / nfeat, scalar2=EPS,
                                                  op0=ALU.mult, op1=ALU.add), [pss.r], [rs.r])
                A(lambda: nc.scalar.activation(out=rs2.t[:, W], in_=rs.t[:, W], func=AF.Sqrt), [rs.r], [rs2.r])
                V(lambda: nc.vector.reciprocal(out=rs.t[:, W], in_=rs2.t[:, W]), [rs2.r], [rs.r])
                c0 = g * 512
                for ci, pp in enumerate(chunks):
                    if mode == "k":
                        dst = ckvnT.t[:, c0:c0 + width]; dres = ckvn_r[g]
                    else:
                        dst = cqnT.t[:, ci, c0:c0 + width]; dres = cqn_r[g]
                    V(lambda: nc.vector.scalar_tensor_tensor(out=dst, in0=pp.t[:, W], scalar=gcol[ci], in1=rs.t[:, W],
                                                             op0=ALU.mult, op1=ALU.mult),
                      [pp.r, rs.r, gkvc.r, gqc.r], [dres])
                R = slice(64, 96)
                if mode == "k":
                    rope_tables(posb[0:1, c0:c0 + width], Ctmp.t[R, W], Stmp.t[R, W], Ctmp.r, lp, width)
                    t1 = lp["t1"]; t2 = lp["t2"]
                    V(lambda: nc.vector.tensor_tensor(out=t1.t[R, W], in0=pr.t[R, W], in1=Ctmp.t[R, W], op=ALU.mult),
                      [pr.r, Ctmp.r], [t1.r])
                    V(lambda: nc.vector.tensor_tensor(out=t2.t[R, W], in0=prr.t[R, W], in1=Stmp.t[R, W], op=ALU.mult),
                      [prr.r, Ctmp.r], [t2.r])
                    V(lambda: nc.vector.tensor_tensor(out=KT.t[R, c0:c0 + width], in0=t1.t[R, W], in1=t2.t[R, W],
                                                      op=ALU.add), [t1.r, t2.r], [ktr_r[g]])
                else:
                    rope_tables(poso[0:1, c0:c0 + width], CQ.t[R, c0:c0 + width], SQ.t[R, c0:c0 + width], cs_r[g], lp, width)

            work = [("k", g, 512) for g in range(NG)] + [("q", g, GW) for g in range(NGO)]
            latent_fe_a(*work[0]); latent_fe_b(*work[0])
            for wi, w_ in enumerate(work):
                nxt = work[wi + 1] if wi + 1 < len(work) else None
                if nxt:
                    latent_fe_a(*nxt)
                latent_mm(*w_)
                if nxt:
                    latent_fe_b(*nxt)
                latent_rest(*w_)
            T.barrier()
            if stop <= 1:
                return nc

        with ExitStack() as ap_:
            qT = sbt(ap_, "qT", [128, TOK], BF16)
            Vh = sbt(ap_, "Vh", [128, NT, 65], BF16)
            V(lambda: nc.vector.memset(Vh.t[:, :, 64:65], 1.0), [], vh_r)
            wuqb = sbt(ap_, "wuqb", [128, 2, 768], BF16, True)
            wuqr = sbt(ap_, "wuqr", [128, 2, NH, 96], BF16)
            wukvb = sbt(ap_, "wukvb", [128, 1024], BF16, True)
            load(wuqb, w_uq.rearrange("(kc p) n -> p kc n", p=128), POOL)
            load(wukvb, w_ukv, POOL)
            V(lambda: nc.vector.memset(wuqr.t[:], 0.0), [], [wuqr.r])
            wv = wuqb.t[:].rearrange("p k (h d) -> p k h d", h=NH)
            V(lambda: nc.vector.tensor_scalar(out=wuqr.t[:, :, :, 64:80], in0=wv[:, :, :, 80:96], scalar1=-1.0, scalar2=None,
                                              op0=ALU.mult), [wuqb.r, wuqr.r], [wuqr.r])
            V(lambda: nc.vector.tensor_copy(out=wuqr.t[:, :, :, 80:96], in_=wv[:, :, :, 64:80]), [wuqb.r, wuqr.r], [wuqr.r])

            PTs = [sbt(ap_, f"PT{i}", [128, 512], BF16) for i in range(6)]
            rec = [sbt(ap_, f"rec{i}", [128, 1], F32) for i in range(4)]
            ost = [sbt(ap_, f"ost{i}", [128, 64], BF16, True) for i in range(4)]
            osc = [0]
            t1q = sbt(ap_, "t1q", [128, 512], F32); t2q = sbt(ap_, "t2q", [128, 512], F32)
            set_rot([4, 5, 6, 7])
            oacc_i = 0
            ptc = 0
            for h in range(NH):
                for g in range(NG):
                    ps = pnext()
                    P(lambda: nc.tensor.matmul(ps.t[0:64, :], lhsT=wukvb.t[:, h * 128:h * 128 + 64],
                                               rhs=ckvnT.t[:, g * 512:(g + 1) * 512], start=True, stop=True),
                      [wukvb.r, ckvn_r[g]], [ps.r])
                    if g % 2 == 0:
                        V(lambda: nc.vector.tensor_copy(out=KT.t[0:64, g * 512:(g + 1) * 512], in_=ps.t[0:64, :]),
                          [ps.r], [ktn_r[g]])
                    else:
                        A(lambda: nc.scalar.copy(out=KT.t[0:64, g * 512:(g + 1) * 512], in_=ps.t[0:64, :]),
                          [ps.r], [ktn_r[g]])
                for g8 in range(NT // 8):
                    ps = pnext()
                    for k8 in range(8):
                        kt = g8 * 8 + k8
                        P(lambda: nc.tensor.matmul(ps.t[:, k8 * 64:(k8 + 1) * 64], lhsT=ckvnT.t[:, kt * 128:(kt + 1) * 128],
                                                   rhs=wukvb.t[:, h * 128 + 64:h * 128 + 128], start=True, stop=True),
                          [wukvb.r, ckvn_r[kt // 4]], [ps.r])
                    wr_ = [vh_r[2 * g8], vh_r[2 * g8 + 1]]
                    src3 = ps.t[:].rearrange("p (a d) -> p a d", a=8)
                    if g8 % 2 == 0:
                        A(lambda: nc.scalar.copy(out=Vh.t[:, g8 * 8:(g8 + 1) * 8, 0:64], in_=src3), [ps.r], wr_)
                    else:
                        V(lambda: nc.vector.tensor_copy(out=Vh.t[:, g8 * 8:(g8 + 1) * 8, 0:64], in_=src3), [ps.r], wr_)
                for g in range(NGO):
                    W = slice(0, GW); c0 = g * 512; R = slice(64, 96)
                    pa = pnext(); pr = pnext()
                    for kc in range(2):
                        P(lambda: nc.tensor.matmul(pa.t[0:96, W], lhsT=wuqb.t[:, kc, h * 96:(h + 1) * 96],
                                                   rhs=cqnT.t[:, kc, c0:c0 + GW], start=(kc == 0), stop=(kc == 1)),
                          [wuqb.r, cqn_r[g]], [pa.r])
                    for kc in range(2):
                        P(lambda: nc.tensor.matmul(pr.t[0:96, W], lhsT=wuqr.t[:, kc, h, :],
                                                   rhs=cqnT.t[:, kc, c0:c0 + GW], start=(kc == 0), stop=(kc == 1)),
                          [wuqr.r, cqn_r[g]], [pr.r])
                    V(lambda: nc.vector.tensor_copy(out=qT.t[0:64, c0:c0 + GW], in_=pa.t[0:64, W]), [pa.r], [qT_r[g]])
                    V(lambda: nc.vector.tensor_tensor(out=t1q.t[R, W], in0=pa.t[R, W], in1=CQ.t[R, c0:c0 + GW], op=ALU.mult),
                      [pa.r, cs_r[g]], [t1q.r])
                    V(lambda: nc.vector.tensor_tensor(out=t2q.t[R, W], in0=pr.t[R, W], in1=SQ.t[R, c0:c0 + GW], op=ALU.mult),
                      [pr.r, cs_r[g]], [t2q.r])
                    V(lambda: nc.vector.tensor_tensor(out=qT.t[R, c0:c0 + GW], in0=t1q.t[R, W], in1=t2q.t[R, W], op=ALU.add),
                      [t1q.r, t2q.r], [qT_r[g]])
                steps = [(m, kp) for m in range(NP) for kp in range((8 * m + 8) // 2)]
                oa_of = {}
                for m in range(NP):
                    oa_of[m] = [banks[(oacc_i * 2) % 4], banks[(oacc_i * 2 + 1) % 4]]
                    oacc_i += 1
                pt_of = {}

                def emit_qk(si):
                    m, kp = steps[si]
                    qg = qT_r[(m * 256) // 512]
                    qs = qT.t[0:96, m * 256:(m + 1) * 256]
                    ps = pnext()
                    for kl in range(2):
                        kt = 2 * kp + kl
                        P(lambda: nc.tensor.matmul(ps.t[:, kl * 256:(kl + 1) * 256], lhsT=KT.t[0:96, kt * 128:(kt + 1) * 128],
                                                   rhs=qs, start=True, stop=True),
                          [ktn_r[kt // 4], ktr_r[kt // 4], qg], [ps.r])
                    PT = PTs[si % len(PTs)]
                    pt_of[si] = PT
                    A(lambda: nc.scalar.activation(out=PT.t[:], in_=ps.t[:], func=AF.Exp, scale=SCALE), [ps.r], [PT.r])
                    for kl in range(2):
                        kt = 2 * kp + kl
                        o = kt - 8 * m
                        if o >= 0:
                            for a in range(2):
                                cs_ = kl * 256 + a * 128
                                V(lambda: nc.vector.scalar_tensor_tensor(
                                    out=PT.t[:, cs_:cs_ + 128], in0=kqb.t[:], scalar=dtab.t[:, a * 8 + o:a * 8 + o + 1],
                                    in1=PT.t[:, cs_:cs_ + 128], op0=ALU.is_le, op1=ALU.mult),
                                  [PT.r, kqb.r, dtab.r], [PT.r])

                def emit_pv(si):
                    nonlocal_osc = None
                    m, kp = steps[si]
                    nk = 8 * m + 8
                    oa = oa_of[m]
                    PT = pt_of.pop(si)
                    for kl in range(2):
                        kt = 2 * kp + kl
                        for a in range(2):
                            cs_ = kl * 256 + a * 128
                            P(lambda: nc.tensor.matmul(oa[a].t[:, 0:65], lhsT=PT.t[:, cs_:cs_ + 128], rhs=Vh.t[:, kt, :],
                                                       start=(kt == 0), stop=(kt == nk - 1)),
                              [PT.r, vh_r[kt // 4]], [oa[a].r])
                    if kp == nk // 2 - 1:
                        for a in range(2):
                            i_own = 2 * m + a
                            rc = rec[(2 * m + a) % 4]
                            V(lambda: nc.vector.reciprocal(out=rc.t[:], in_=oa[a].t[:, 64:65]), [oa[a].r], [rc.r])
                            os_ = ost[osc[0] % 4]; osc[0] += 1
                            V(lambda: nc.vector.tensor_scalar(out=os_.t[:], in0=oa[a].t[:, 0:64],
                                                              scalar1=rc.t[:, 0:1], scalar2=None, op0=ALU.mult),
                              [oa[a].r, rc.r], [os_.r])
                            T.dma(SP, os_.l, lambda: nc.sync.dma_start(out=OS[i_own * 128:(i_own + 1) * 128, h * 64:(h + 1) * 64],
                                                                       in_=os_.t[:]), reads=[os_.r])

                LA = 3
                for si in range(len(steps) + LA):
                    if si < len(steps):
                        emit_qk(si)
                    if si - LA >= 0:
                        emit_pv(si - LA)
            T.barrier()
            if stop <= 2:
                return nc
        att.close()

        pc_ = es.enter_context(ExitStack())
        set_rot(range(8))
        win_v = w_in.rearrange("(kc p) n -> p kc n", p=128)
        wsg = sbt(pc_, "wsg", [128, 8, 1024], BF16, True); load(wsg, win_v[:, :, 416:1440], POOL)
        wgl = sbt(pc_, "wgl", [128, 8, 2048], BF16, True)
        for kc in range(8):
            load_to(wgl, wgl.t[:, kc, :], win_v[:, kc, 1440:3488], POOL)
        wbm = sbt(pc_, "wbm", [128, 4, D], BF16, True); load(wbm, w_br_mla.rearrange("(kc p) n -> p kc n", p=128), POOL)
        wbs = sbt(pc_, "wbs", [128, 4, D], BF16, True); load(wbs, w_br_sgu.rearrange("(kc p) n -> p kc n", p=128), POOL)
        wo = sbt(pc_, "wo", [128, 8, D], BF16, True); load(wo, w_out.rearrange("(kc p) n -> p kc n", p=128), POOL)
        wrf = sbt(pc_, "wrf", [128, 8, 36], F32, True); load(wrf, wr.rearrange("(kc p) n -> p kc n", p=128))
        brb = sbt(pc_, "brb", [128, 36], F32, True); load(brb, br_row.partition_broadcast(128))
        gvb = sbt(pc_, "gvb", [128, 512], F32, True); load(gvb, gv_row.partition_broadcast(128))
        bvb = sbt(pc_, "bvb", [128, 512], F32, True); load(bvb, bv_row.partition_broadcast(128))
        bst = sbt(pc_, "bst", [128, 8], F32, True); load(bst, bsT)
        make_fe(pc_, 3)
        browc = sbt(pc_, "browc", [1, 3072], BF16)
        for q_ in range(2):
            bias_row(browc.t[0:1, q_ * 512:(q_ + 1) * 512], lambda kc: wsg.t[:, kc, q_ * 512:(q_ + 1) * 512], 512, wsg.r, browc.r)
        for q_ in range(4):
            bias_row(browc.t[0:1, 1024 + q_ * 512:1024 + (q_ + 1) * 512], lambda kc: wgl.t[:, kc, q_ * 512:(q_ + 1) * 512], 512,
                     wgl.r, browc.r)
        wsT = sbt(pc_, "wsT", [128, 8, 128], BF16)
        with ExitStack() as tmp_:
            g2rb = sbt(tmp_, "g2rb", [128, D], F32, True); load(g2rb, g2row.partition_broadcast(128))
            V(lambda: nc.vector.scalar_tensor_tensor(out=s2b.t[:], in0=s2b.t[:], scalar=1.0, in1=g2rb.t[:], op0=ALU.add,
                                                     op1=ALU.mult), [s2b.r, g2rb.r], [s2b.r])
            wsf = sbt(tmp_, "wsf", [128, 8, 128], F32, True); load(wsf, w_s.rearrange("g t s -> t g s"))
            trl = sbt(tmp_, "trl", [128, 128], F32, True); load(trl, c_tril)
            wsm = sbt(tmp_, "wsm", [128, 8, 128], BF16)
            V(lambda: nc.vector.tensor_tensor(out=wsm.t[:], in0=wsf.t[:], in1=trl.t[:].unsqueeze(1).to_broadcast([128, 8, 128]),
                                              op=ALU.mult), [wsf.r, trl.r], [wsm.r])
            tp = pnext(); tpb = tp.t[:].bitcast(BF16)
            for g in range(8):
                P(lambda: nc.tensor.transpose(out=tpb[:, g * 128:(g + 1) * 128], in_=wsm.t[:, g, :], identity=identb.t[:]),
                  [wsm.r, identb.r], [tp.r])
            V(lambda: nc.vector.tensor_copy(out=wsT.t[:].rearrange("p g t -> p (g t)"), in_=tpb[:, :]), [tp.r], [wsT.r])
            T.barrier()


        with ExitStack() as cw:
            h1c = [sbt(cw, f"h1c{i}", [128, 8, 128], BF16) for i in range(1)]
            uv = sbt(cw, "uv", [128, 1024], F32)
            x2 = sbt(cw, "x2t", [128, 1024], F32)
            inner = sbt(cw, "inner", [128, 1024], F32)
            gel = sbt(cw, "gel", [128, 512], F32)
            otile = [sbt(cw, f"otile{i}", [128, 512], BF16, True) for i in range(2)]
            sgoB = [sbt(cw, f"sgo{i}", [128, 512], BF16) for i in range(1)]
            lst = sbt(cw, "lst", [128, 8], F32)
            sigB = [sbt(cw, f"sig{i}", [128, 2048], BF16) for i in range(2)]
            OT = sbt(cw, "OT", [128, 4, 128], BF16)
            SGT = sbt(cw, "SGT", [128, 4, 128], BF16)
            m1 = sbt(cw, "m1", [128, D], F32); m2 = sbt(cw, "m2", [128, D], F32)
            mrg = sbt(cw, "mrg", [128, D], BF16)
            mT = sbt(cw, "mT", [128, 8, 128], BF16)
            x1 = [sbt(cw, f"x1_{i}", [128, D], F32, True) for i in range(2)]
            x1n = m1
            h2T = sbt(cw, "h2T", [128, 8, 128], F32)
            h2f = m2
            h2p = [sbt(cw, f"h2p{i}", [128, D], BF16, True) for i in range(1)]
            st2 = sbt(cw, "st2", [128, 4], F32)
            lgall = sbt(cw, "lgall", [128, NOWN, 36], F32)
            rtb = sbt(cw, "rtb", [128, 8, NOWN], F32)
            s1out = {}

            vnbB = [sbt(cw, f"vnbB{i}", [128, 512], BF16) for i in range(2)]
            gubB = [sbt(cw, f"gubB{i}", [128, 512], BF16) for i in range(2)]
            fe_st = {}

            def S1(i):
                h1 = h1c[0]; sig = sigB[i % 2]; vnb_ = vnbB[i % 2]; gub = gubB[i % 2]
                if i not in fe_st:
                    fe_st[i] = fe_a([xo[i * 128:(i + 1) * 128, :]])
                xt = fe_b(fe_st.pop(i), h1.t, h1.r, [0])[0]
                s1out[i] = (xt, vnb_, gub, sig)
                load(otile[i % 2], OS[i * 128:(i + 1) * 128, :])
                pu = pnext(); pv = pnext()
                for hh, pp in enumerate((pu, pv)):
                    for kc in range(8):
                        P(lambda: nc.tensor.matmul(pp.t[:], lhsT=h1.t[:, kc, :], rhs=wsg.t[:, kc, hh * 512:(hh + 1) * 512],
                                                   start=(kc == 0), stop=False), [h1.r, wsg.r], [pp.r])
                    P(lambda: nc.tensor.matmul(pp.t[:], lhsT=onesb.t[0:1, :], rhs=browc.t[0:1, hh * 512:(hh + 1) * 512],
                                               start=False, stop=True), [onesb.r, browc.r], [pp.r])
                pg = [pnext() for _ in range(4)]
                for q4 in range(4):
                    for kc in range(8):
                        P(lambda: nc.tensor.matmul(pg[q4].t[:], lhsT=h1.t[:, kc, :], rhs=wgl.t[:, kc, q4 * 512:(q4 + 1) * 512],
                                                   start=(kc == 0), stop=False), [h1.r, wgl.r], [pg[q4].r])
                    P(lambda: nc.tensor.matmul(pg[q4].t[:], lhsT=onesb.t[0:1, :], rhs=browc.t[0:1, 1024 + q4 * 512:1024 + (q4 + 1) * 512],
                                               start=False, stop=True), [onesb.r, browc.r], [pg[q4].r])
                for hh, pp in enumerate((pu, pv)):
                    cs_ = slice(hh * 512, (hh + 1) * 512)
                    A(lambda: nc.scalar.copy(out=uv.t[:, cs_], in_=pp.t[:]), [pp.r], [uv.r])
                    A(lambda: nc.scalar.activation(out=x2.t[:, cs_], in_=pp.t[:], func=AF.Square, scale=0.044715 ** 0.5),
                      [pp.r], [x2.r])
                V(lambda: nc.vector.scalar_tensor_tensor(out=inner.t[:], in0=x2.t[:], scalar=1.0, in1=uv.t[:], op0=ALU.add,
                                                         op1=ALU.mult), [x2.r, uv.r], [inner.r])
                for q4 in range(4):
                    A(lambda: nc.scalar.activation(out=sig.t[:, q4 * 512:(q4 + 1) * 512], in_=pg[q4].t[:], func=AF.Sigmoid),
                      [pg[q4].r], [sig.r])
                A(lambda: nc.scalar.activation(out=inner.t[:], in_=inner.t[:], func=AF.Sigmoid, scale=GELU_C), [inner.r], [inner.r])
                if i + 1 < NOWN:
                    fe_st[i + 1] = fe_a([xo[(i + 1) * 128:(i + 2) * 128, :]])
                G(lambda: nc.gpsimd.tensor_tensor(out=gel.t[:, 0:512], in0=inner.t[:, 512:1024], in1=uv.t[:, 512:1024], op=ALU.mult),
                  [inner.r, uv.r], [gel.r])
                G(lambda: nc.gpsimd.tensor_tensor(out=gub.t[:], in0=inner.t[:, 0:512], in1=uv.t[:, 0:512], op=ALU.mult),
                  [inner.r, uv.r], [gub.r])
                gv_ = gel.t[:, 0:512]
                V(lambda: nc.vector.tensor_reduce(out=lst.t[:, 0:1], in_=gv_, axis=AX.X, op=ALU.add), [gel.r], [lst.r])
                V(lambda: nc.vector.tensor_scalar(out=lst.t[:, 1:2], in0=lst.t[:, 0:1], scalar1=1.0 / 512, scalar2=None,
                                                  op0=ALU.mult), [lst.r], [lst.r])
                V(lambda: nc.vector.tensor_scalar(out=x2.t[:, 0:512], in0=gv_, scalar1=lst.t[:, 1:2], scalar2=None,
                                                  op0=ALU.subtract), [gel.r, lst.r], [x2.r])
                A(lambda: nc.scalar.activation(out=x2.t[:, 512:1024], in_=x2.t[:, 0:512], func=AF.Square, accum_out=lst.t[:, 2:3]),
                  [x2.r], [x2.r, lst.r])
                V(lambda: nc.vector.tensor_scalar(out=lst.t[:, 3:4], in0=lst.t[:, 2:3], scalar1=1.0 / 512, scalar2=EPS,
                                                  op0=ALU.mult, op1=ALU.add), [lst.r], [lst.r])
                A(lambda: nc.scalar.activation(out=lst.t[:, 4:5], in_=lst.t[:, 3:4], func=AF.Sqrt), [lst.r], [lst.r])
                V(lambda: nc.vector.reciprocal(out=lst.t[:, 5:6], in_=lst.t[:, 4:5]), [lst.r], [lst.r])
                V(lambda: nc.vector.scalar_tensor_tensor(out=x2.t[:, 0:512], in0=x2.t[:, 0:512], scalar=lst.t[:, 5:6],
                                                         in1=gvb.t[:], op0=ALU.mult, op1=ALU.mult), [x2.r, lst.r, gvb.r], [x2.r])
                V(lambda: nc.vector.tensor_tensor(out=vnb_.t[:], in0=x2.t[:, 0:512], in1=bvb.t[:], op=ALU.add),
                  [x2.r, bvb.r], [vnb_.r])

            def S2(i):
                xt, vnb_, gub, sig = s1out.pop(i)
                sgo = sgoB[0]
                psg = pnext()
                for g in range(8):
                    P(lambda: nc.tensor.matmul(psg.t[:, g * 64:(g + 1) * 64], lhsT=wsT.t[:, g, :], rhs=vnb_.t[:, g * 64:(g + 1) * 64],
                                               start=True, stop=True), [wsT.r, vnb_.r], [psg.r])
                ot_ = otile[i % 2]
                tp = pnext(); tpb = tp.t[:].bitcast(BF16)
                for kc in range(4):
                    P(lambda: nc.tensor.transpose(out=tpb[:, kc * 128:(kc + 1) * 128], in_=ot_.t[:, kc * 128:(kc + 1) * 128],
                                                  identity=identb.t[:]), [ot_.r, identb.r], [tp.r])
                A(lambda: nc.scalar.copy(out=OT.t[:].rearrange("p k t -> p (k t)"), in_=tpb[:, 0:512]), [tp.r], [OT.r])
                V(lambda: nc.vector.tensor_tensor(out=m2.t[:, 0:512].rearrange("p (g c) -> p g c", g=8),
                                                  in0=psg.t[:].rearrange("p (g c) -> p g c", g=8),
                                                  in1=bst.t[:].unsqueeze(2).to_broadcast([128, 8, 64]), op=ALU.add),
                  [psg.r, bst.r], [m2.r])
                V(lambda: nc.vector.tensor_tensor(out=sgo.t[:], in0=m2.t[:, 0:512], in1=gub.t[:], op=ALU.mult),
                  [m2.r, gub.r], [sgo.r])
                for hh in range(2):
                    ps = pnext()
                    for kc in range(4):
                        P(lambda: nc.tensor.matmul(ps.t[:], lhsT=OT.t[:, kc, :], rhs=wbm.t[:, kc, hh * 512:(hh + 1) * 512],
                                                   start=(kc == 0), stop=(kc == 3)), [OT.r, wbm.r], [ps.r])
                    V(lambda: nc.vector.tensor_tensor(out=m1.t[:, hh * 512:(hh + 1) * 512], in0=ps.t[:],
                                                      in1=sig.t[:, hh * 512:(hh + 1) * 512], op=ALU.mult), [ps.r, sig.r], [m1.r])
                tp2 = pnext(); tpb2 = tp2.t[:].bitcast(BF16)
                for kc in range(4):
                    P(lambda: nc.tensor.transpose(out=tpb2[:, kc * 128:(kc + 1) * 128], in_=sgo.t[:, kc * 128:(kc + 1) * 128],
                                                  identity=identb.t[:]), [sgo.r, identb.r], [tp2.r])
                A(lambda: nc.scalar.copy(out=SGT.t[:].rearrange("p k t -> p (k t)"), in_=tpb2[:, 0:512]), [tp2.r], [SGT.r])
                for hh in range(2):
                    ps = pnext()
                    for kc in range(4):
                        P(lambda: nc.tensor.matmul(ps.t[:], lhsT=SGT.t[:, kc, :], rhs=wbs.t[:, kc, hh * 512:(hh + 1) * 512],
                                                   start=(kc == 0), stop=(kc == 3)), [SGT.r, wbs.r], [ps.r])
                    V(lambda: nc.vector.tensor_tensor(out=m2.t[:, hh * 512:(hh + 1) * 512], in0=ps.t[:],
                                                      in1=sig.t[:, 1024 + hh * 512:1024 + (hh + 1) * 512], op=ALU.mult),
                      [ps.r, sig.r], [m2.r])
                    V(lambda: nc.vector.tensor_tensor(out=mrg.t[:, hh * 512:(hh + 1) * 512], in0=m1.t[:, hh * 512:(hh + 1) * 512],
                                                      in1=m2.t[:, hh * 512:(hh + 1) * 512], op=ALU.add), [m1.r, m2.r], [mrg.r])
                tp = pnext(); tpb = tp.t[:].bitcast(BF16)
                for kc in range(8):
                    P(lambda: nc.tensor.transpose(out=tpb[:, kc * 128:(kc + 1) * 128], in_=mrg.t[:, kc * 128:(kc + 1) * 128],
                                                  identity=identb.t[:]), [mrg.r, identb.r], [tp.r])
                A(lambda: nc.scalar.copy(out=mT.t[:].rearrange("p k t -> p (k t)"), in_=tpb[:, :]), [tp.r], [mT.r])
                xx = x1[i % 2]
                for hh in range(2):
                    ps = pnext()
                    for kc in range(8):
                        P(lambda: nc.tensor.matmul(ps.t[:], lhsT=mT.t[:, kc, :], rhs=wo.t[:, kc, hh * 512:(hh + 1) * 512],
                                                   start=(kc == 0), stop=(kc == 7)), [mT.r, wo.r], [ps.r])
                    cs_ = slice(hh * 512, (hh + 1) * 512)
                    V(lambda: nc.vector.tensor_tensor(out=m1.t[:, cs_], in0=ps.t[:], in1=gate1b.t[:, cs_], op=ALU.mult),
                      [ps.r, gate1b.r], [m1.r])
                V(lambda: nc.vector.tensor_tensor(out=xx.t[:], in0=m1.t[:], in1=xt.t[:], op=ALU.add), [m1.r, xt.r], [xx.r])
                T.dma(SP, xx.l, lambda: nc.sync.dma_start(out=X1[i * 128:(i + 1) * 128, :], in_=xx.t[:]), reads=[xx.r])

            def S3(i):
                xx = x1[i % 2]
                junk = FE["junk"]
                A(lambda: nc.scalar.activation(out=junk.t[:], in_=xx.t[:], func=AF.Square, accum_out=st2.t[:, 0:1]),
                  [xx.r], [junk.r, st2.r])
                V(lambda: nc.vector.tensor_scalar(out=st2.t[:, 1:2], in0=st2.t[:, 0:1], scalar1=1.0 / D, scalar2=EPS,
                                                  op0=ALU.mult, op1=ALU.add), [st2.r], [st2.r])
                A(lambda: nc.scalar.activation(out=st2.t[:, 2:3], in_=st2.t[:, 1:2], func=AF.Sqrt), [st2.r], [st2.r])
                V(lambda: nc.vector.reciprocal(out=st2.t[:, 3:4], in_=st2.t[:, 2:3]), [st2.r], [st2.r])
                V(lambda: nc.vector.tensor_scalar(out=x1n.t[:], in0=xx.t[:], scalar1=st2.t[:, 3:4], scalar2=None, op0=ALU.mult),
                  [xx.r, st2.r], [x1n.r])
                G(lambda: nc.gpsimd.tensor_tensor(out=h2f.t[:], in0=x1n.t[:], in1=s2b.t[:], op=ALU.mult), [x1n.r, s2b.r], [h2f.r])
                hp = h2p[0]
                G(lambda: nc.gpsimd.tensor_tensor(out=hp.t[:].rearrange("t (kc p) -> t p kc", kc=8),
                                                  in0=h2f.t[:].rearrange("t (p kc) -> t p kc", kc=8),
                                                  in1=sh2b.t[:].rearrange("t (p kc) -> t p kc", kc=8), op=ALU.add),
                  [h2f.r, sh2b.r], [hp.r])
                T.dma(SP, hp.l, lambda: nc.sync.dma_start(out=H2[i * 128:(i + 1) * 128, :], in_=hp.t[:]), reads=[hp.r])
                tA = pnext(); tB = pnext()
                for kc in range(8):
                    tt = tA if kc < 4 else tB
                    P(lambda: nc.tensor.transpose(out=tt.t[:, (kc % 4) * 128:(kc % 4 + 1) * 128], in_=x1n.t[:, kc * 128:(kc + 1) * 128],
                                                  identity=identf.t[:]), [x1n.r, identf.r], [tt.r])
                for kc in range(8):
                    tt = tA if kc < 4 else tB
                    V(lambda: nc.vector.tensor_scalar(out=h2T.t[:, kc, :], in0=tt.t[:, (kc % 4) * 128:(kc % 4 + 1) * 128],
                                                      scalar1=s2c.t[:, kc:kc + 1], scalar2=sh2(kc), op0=ALU.mult, op1=ALU.add),
                      [tt.r, s2c.r, modT.r], [h2T.r])
                pl = pnext()
                for kc in range(8):
                    P(lambda: nc.tensor.matmul(pl.t[:, 0:36], lhsT=h2T.t[:, kc, :], rhs=wrf.t[:, kc, :], start=(kc == 0), stop=(kc == 7)),
                      [h2T.r, wrf.r], [pl.r])
                V(lambda: nc.vector.tensor_tensor(out=lgall.t[:, i, :], in0=pl.t[:, 0:36], in1=brb.t[:], op=ALU.add), [pl.r, brb.r], [lgall.r])

            for step in range(NOWN + 2):
                if step < NOWN:
                    S1(step)
                if 0 <= step - 1 < NOWN:
                    S2(step - 1)
                if 0 <= step - 2 < NOWN:
                    S3(step - 2)

            TT = NOWN
            Lg = lgall.t[:, :, 0:4]
            Le4 = lgall.t[:, :, 4:36].rearrange("p t (g e) -> p t g e", g=4)
            gm = rtb.t[:, 0, :]; se = rtb.t[:, 1, :]; pgr = rtb.t[:, 2, :]; mv1 = rtb.t[:, 3, :]; mv2 = rtb.t[:, 4, :]
            dd_ = rtb.t[:, 5, :]; ee_ = rtb.t[:, 6, :]; rr_ = rtb.t[:, 7, :]
            ex = uv.t[:, 0:TT * 4].rearrange("p (t g) -> p t g", g=4)
            pen3 = x2.t[:, 0:TT * 4].rearrange("p (t g) -> p t g", g=4)
            Mx3 = inner.t[:, 0:TT * 32].rearrange("p (t e) -> p t e", e=32)
            Mx4 = inner.t[:, 0:TT * 32].rearrange("p (t g e) -> p t g e", g=4, e=8)
            My3 = m1.t[:, 0:TT * 32].rearrange("p (t e) -> p t e", e=32)
            bc3 = lambda ap2, n: ap2.unsqueeze(2).to_broadcast([128, TT, n])
            V(lambda: nc.vector.tensor_reduce(out=gm, in_=Lg, axis=AX.X, op=ALU.max), [lgall.r], [rtb.r])
            V(lambda: nc.vector.tensor_tensor(out=ex, in0=Lg, in1=bc3(gm, 4), op=ALU.subtract), [lgall.r, rtb.r], [uv.r])
            A(lambda: nc.scalar.activation(out=ex, in_=ex, func=AF.Exp), [uv.r], [uv.r])
            V(lambda: nc.vector.tensor_reduce(out=se, in_=ex, axis=AX.X, op=ALU.add), [uv.r], [rtb.r])
            V(lambda: nc.vector.reciprocal(out=pgr, in_=se), [rtb.r], [rtb.r])
            V(lambda: nc.vector.tensor_tensor(out=pen3, in0=Lg, in1=bc3(gm, 4), op=ALU.is_equal), [lgall.r, rtb.r], [x2.r])
            V(lambda: nc.vector.tensor_scalar(out=pen3, in0=pen3, scalar1=-1.0, scalar2=1e30, op0=ALU.add, op1=ALU.mult),
              [x2.r], [x2.r])
            V(lambda: nc.vector.tensor_tensor(out=Mx4, in0=Le4, in1=pen3.unsqueeze(3).to_broadcast([128, TT, 4, 8]), op=ALU.add),
              [lgall.r, x2.r], [inner.r])
            V(lambda: nc.vector.tensor_reduce(out=mv1, in_=Mx3, axis=AX.X, op=ALU.max), [inner.r], [rtb.r])
            V(lambda: nc.vector.tensor_tensor(out=A0all.t[:], in0=Mx3, in1=bc3(mv1, 32), op=ALU.is_equal), [inner.r, rtb.r], [A0all.r])
            V(lambda: nc.vector.scalar_tensor_tensor(out=My3, in0=A0all.t[:], scalar=-1e30, in1=Mx3, op0=ALU.mult, op1=ALU.add),
              [A0all.r, inner.r], [m1.r])
            V(lambda: nc.vector.tensor_reduce(out=mv2, in_=My3, axis=AX.X, op=ALU.max), [m1.r], [rtb.r])
            V(lambda: nc.vector.tensor_tensor(out=A1all.t[:], in0=My3, in1=bc3(mv2, 32), op=ALU.is_equal), [m1.r, rtb.r], [A1all.r])
            V(lambda: nc.vector.tensor_tensor(out=Aall.t[:], in0=A0all.t[:], in1=A1all.t[:], op=ALU.add), [A0all.r, A1all.r], [Aall.r])
            V(lambda: nc.vector.tensor_tensor(out=dd_, in0=mv2, in1=mv1, op=ALU.subtract), [rtb.r], [rtb.r])
            A(lambda: nc.scalar.activation(out=ee_, in_=dd_, func=AF.Exp), [rtb.r], [rtb.r])
            V(lambda: nc.vector.tensor_scalar(out=ee_, in0=ee_, scalar1=1.0, scalar2=None, op0=ALU.add), [rtb.r], [rtb.r])
            V(lambda: nc.vector.reciprocal(out=rr_, in_=ee_), [rtb.r], [rtb.r])
            V(lambda: nc.vector.tensor_tensor(out=wts.t[:, :, 0], in0=rr_, in1=pgr, op=ALU.mult), [rtb.r], [wts.r])
            V(lambda: nc.vector.tensor_tensor(out=wts.t[:, :, 1], in0=pgr, in1=wts.t[:, :, 0], op=ALU.subtract), [rtb.r, wts.r], [wts.r])
            T.barrier()
            if stop <= 3:
                return nc
        pc_.close()

        NA = NOWN * 32
        REG_SLOT = nc.gpsimd.to_reg(NSLOT - 1)
        REG_W = nc.gpsimd.to_reg(NEXP * 128 - 1)
        d0i = sbt(es, "d0i", [128, NOWN], I32); d1i = sbt(es, "d1i", [128, NOWN], I32)
        idxw = sbt(es, "idxw", [128, NBLK], I32)
        with ExitStack() as dp:
            set_rot(range(8))
            Ab_t = Aall.t[:].rearrange("p t e -> p (t e)")
            lsf = sbt(dp, "lsf", [128, 128], F32, True); load(lsf, c_lstrict)
            lsb = sbt(dp, "lsb", [128, 128], BF16)
            thr = sbt(dp, "thr", [128, NBLK], F32, True); load(thr, c_thr)
            pidx = sbt(dp, "pidx", [128, 1], F32, True); load(pidx, c_pidx)
            V(lambda: nc.vector.tensor_copy(out=lsb.t[:], in_=lsf.t[:]), [lsf.r], [lsb.r])
            tsum = sbt(dp, "tsum", [128, NOWN, 32], F32)
            rloc = sbt(dp, "rloc", [128, NOWN, 32], F32)
            for c0 in range(0, NA, 512):
                w_ = min(512, NA - c0)
                ps = pnext()
                P(lambda: nc.tensor.matmul(ps.t[:, 0:w_], lhsT=onesb.t[:], rhs=Ab_t[:, c0:c0 + w_], start=True, stop=True),
                  [onesb.r, Aall.r], [ps.r])
                V(lambda: nc.vector.tensor_copy(out=tsum.t[:].rearrange("p t e -> p (t e)")[:, c0:c0 + w_], in_=ps.t[:, 0:w_]),
                  [ps.r], [tsum.r])
                ps2 = pnext()
                P(lambda: nc.tensor.matmul(ps2.t[:, 0:w_], lhsT=lsb.t[:], rhs=Ab_t[:, c0:c0 + w_], start=True, stop=True),
                  [lsb.r, Aall.r], [ps2.r])
                V(lambda: nc.vector.tensor_copy(out=rloc.t[:].rearrange("p t e -> p (t e)")[:, c0:c0 + w_], in_=ps2.t[:, 0:w_]),
                  [ps2.r], [rloc.r])
            cum = sbt(dp, "cum", [128, NOWN + 1, 32], F32)
            V(lambda: nc.vector.memset(cum.t[:, 0, :], 0.0), [], [cum.r])
            for t in range(NOWN):
                V(lambda: nc.vector.tensor_tensor(out=cum.t[:, t + 1, :], in0=cum.t[:, t, :], in1=tsum.t[:, t, :], op=ALU.add),
                  [cum.r, tsum.r], [cum.r])
            ci = sbt(dp, "ci", [128, 32], I32)
            pad = sbt(dp, "pad", [128, 32], F32)
            pend = sbt(dp, "pend", [128, 32], F32)
            pst = sbt(dp, "pst", [128, 32], F32)
            V(lambda: nc.vector.tensor_scalar(out=pad.t[:], in0=cum.t[:, NOWN, :], scalar1=float(BLK - 1), scalar2=None,
                                              op0=ALU.add), [cum.r], [pad.r])
            V(lambda: nc.vector.tensor_copy(out=ci.t[:], in_=pad.t[:]), [pad.r], [ci.r])
            V(lambda: nc.vector.tensor_scalar(out=ci.t[:], in0=ci.t[:], scalar1=8, scalar2=8, op0=ALU.arith_shift_right,
                                              op1=ALU.logical_shift_left), [ci.r], [ci.r])
            V(lambda: nc.vector.tensor_copy(out=pad.t[:], in_=ci.t[:]), [ci.r], [pad.r])
            V(lambda: nc.vector.tensor_tensor_scan(out=pend.t[:], data0=onesf.t[:, 0:32], data1=pad.t[:], initial=0.0,
                                                   op0=ALU.mult, op1=ALU.add), [onesf.r, pad.r], [pend.r])
            V(lambda: nc.vector.tensor_tensor(out=pst.t[:], in0=pend.t[:], in1=pad.t[:], op=ALU.subtract), [pend.r, pad.r], [pst.r])
            V(lambda: nc.vector.tensor_tensor(out=rloc.t[:], in0=rloc.t[:], in1=cum.t[:, 0:NOWN, :], op=ALU.add),
              [rloc.r, cum.r], [rloc.r])
            V(lambda: nc.vector.tensor_tensor(out=rloc.t[:], in0=rloc.t[:], in1=pst.t[:].unsqueeze(1).to_broadcast([128, NOWN, 32]),
                                              op=ALU.add), [rloc.r, pst.r], [rloc.r])
            df = sbt(dp, "df", [128, NOWN], F32)
            for Ak, dk in ((A0all, d0i), (A1all, d1i)):
                V(lambda: nc.vector.tensor_tensor(out=tsum.t[:], in0=Ak.t[:], in1=rloc.t[:], op=ALU.mult), [Ak.r, rloc.r, tsum.r], [tsum.r])
                V(lambda: nc.vector.tensor_reduce(out=df.t[:], in_=tsum.t[:], axis=AX.X, op=ALU.add), [tsum.r], [df.r])
                V(lambda: nc.vector.tensor_copy(out=dk.t[:], in_=df.t[:]), [df.r], [dk.r])
            cmp_ = sbt(dp, "cmp", [128, NBLK, 32], F32)
            bef = sbt(dp, "bef", [128, NBLK], F32)
            V(lambda: nc.vector.tensor_tensor(out=cmp_.t[:], in0=pend.t[:].unsqueeze(1).to_broadcast([128, NBLK, 32]),
                                              in1=thr.t[:].unsqueeze(2).to_broadcast([128, NBLK, 32]), op=ALU.is_le),
              [pend.r, thr.r], [cmp_.r])
            V(lambda: nc.vector.tensor_reduce(out=bef.t[:], in_=cmp_.t[:], axis=AX.X, op=ALU.add), [cmp_.r], [bef.r])
            V(lambda: nc.vector.tensor_scalar(out=bef.t[:], in0=bef.t[:], scalar1=float(NEXP - 1), scalar2=128.0, op0=ALU.min,
                                              op1=ALU.mult), [bef.r], [bef.r])
            V(lambda: nc.vector.tensor_scalar(out=bef.t[:], in0=bef.t[:], scalar1=pidx.t[:, 0:1], scalar2=None, op0=ALU.add),
              [bef.r, pidx.r], [bef.r])
            skp = sbt(dp, "skp", [128, NBLK], F32)
            V(lambda: nc.vector.tensor_scalar(out=skp.t[:], in0=thr.t[:], scalar1=pend.t[:, 31:32], scalar2=1.0e6, op0=ALU.is_ge,
                                              op1=ALU.mult), [thr.r, pend.r], [skp.r])
            V(lambda: nc.vector.tensor_tensor(out=bef.t[:], in0=bef.t[:], in1=skp.t[:], op=ALU.add), [bef.r, skp.r], [bef.r])
            V(lambda: nc.vector.tensor_copy(out=idxw.t[:], in_=bef.t[:]), [bef.r], [idxw.r])
            hb = [sbt(dp, f"hb{i}", [128, D], BF16, True) for i in range(3)]
            xs_res = Res("XS")
            for t in range(NOWN):
                b_ = hb[t % 3]
                load(b_, H2[t * 128:(t + 1) * 128, :])
                for dk in (d0i, d1i):
                    T.dma(POOL, b_.l, lambda: nc.gpsimd.indirect_dma_start(
                        out=XS, out_offset=bass.IndirectOffsetOnAxis(ap=dk.t[:, t:t + 1], axis=0), in_=b_.t[:], in_offset=None,
                        bounds_check=REG_SLOT, oob_is_err=False),
                        reads=[b_.r, dk.r])
            T.barrier()
            if stop <= 4:
                return nc

        with ExitStack() as dd:
            set_rot(range(8))
            stg = [[sbt(dd, f"stg{i}_{k}", [128, 4096], F32, True) for k in range(3)] for i in range(2)]
            w1b = [sbt(dd, f"w1b{i}", [128, 8, 4, 128], BF16) for i in range(2)]
            w3b = [sbt(dd, f"w3b{i}", [128, 8, 4, 128], BF16) for i in range(2)]
            w2b = [sbt(dd, f"w2b{i}", [128, 4, D], BF16) for i in range(2)]
            xbk = [sbt(dd, f"xbk{i}", [128, 2, D], BF16, True) for i in range(2)]
            xTb = [sbt(dd, f"xTb{i}", [128, 8, 256], BF16) for i in range(1)]
            sil = sbt(dd, "sil", [128, 256], F32)
            gTb = [sbt(dd, f"gTb{i}", [128, 4, 256], BF16) for i in range(2)]
            ysb = [sbt(dd, f"ysb{i}", [128, D], F32, True) for i in range(2)]
            yc = 0

            def issue_w(j):
                s_ = stg[j % 2]
                for k, wsrc in enumerate((w1, w3, w2)):
                    T.dma(POOL, s_[k].l, lambda: nc.gpsimd.indirect_dma_start(
                        out=s_[k].t[:], out_offset=None, in_=wsrc,
                        in_offset=bass.IndirectOffsetOnAxis(ap=idxw.t[:, j:j + 1], axis=0),
                        bounds_check=REG_W, oob_is_err=False),
                        reads=[idxw.r], writes=[s_[k].r])

            def issue_x(j):
                load(xbk[j % 2], XS[j * BLK:(j + 1) * BLK, :].rearrange("(a p) d -> p a d", p=128))

            issue_w(0)
            issue_x(0)
            for j in range(NBLK):
                if j + 1 < NBLK:
                    issue_w(j + 1)
                    issue_x(j + 1)
                s_ = stg[j % 2]; b1 = w1b[j % 2]; b3 = w3b[j % 2]; b2 = w2b[j % 2]
                V(lambda: nc.vector.tensor_copy(out=b1.t[:], in_=s_[0].t[:].rearrange("p (kc m fc) -> p kc fc m", kc=8, m=128, fc=4)),
                  [s_[0].r], [b1.r])
                V(lambda: nc.vector.tensor_copy(out=b3.t[:], in_=s_[1].t[:].rearrange("p (kc m fc) -> p kc fc m", kc=8, m=128, fc=4)),
                  [s_[1].r], [b3.r])
                A(lambda: nc.scalar.copy(out=b2.t[:].rearrange("p f n -> p (f n)"), in_=s_[2].t[:]), [s_[2].r], [b2.r])
                xk = xbk[j % 2]; xT_ = xTb[0]; gT_ = gTb[j % 2]
                for st_ in range(2):
                    tp = pnext(); tpb = tp.t[:].bitcast(BF16)
                    for kc in range(8):
                        P(lambda: nc.tensor.transpose(out=tpb[:, kc * 128:(kc + 1) * 128], in_=xk.t[:, st_, kc * 128:(kc + 1) * 128],
                                                      identity=identb.t[:]), [xk.r, identb.r], [tp.r])
                    cp = (lambda f: A(f, [tp.r], [xT_.r])) if st_ == 0 else (lambda f: V(f, [tp.r], [xT_.r]))
                    if st_ == 0:
                        A(lambda: nc.scalar.copy(out=xT_.t[:, :, 0:128], in_=tpb.rearrange("p (k t) -> p k t", k=8)), [tp.r], [xT_.r])
                    else:
                        V(lambda: nc.vector.tensor_copy(out=xT_.t[:, :, 128:256], in_=tpb.rearrange("p (k t) -> p k t", k=8)),
                          [tp.r], [xT_.r])
                for fc in range(4):
                    pa = pnext(); pb_ = pnext()
                    for kc in range(8):
                        P(lambda: nc.tensor.matmul(pa.t[:, 0:256], lhsT=b1.t[:, kc, fc, :], rhs=xT_.t[:, kc, :], start=(kc == 0),
                                                   stop=(kc == 7)), [b1.r, xT_.r], [pa.r])
                    for kc in range(8):
                        P(lambda: nc.tensor.matmul(pb_.t[:, 0:256], lhsT=b3.t[:, kc, fc, :], rhs=xT_.t[:, kc, :], start=(kc == 0),
                                                   stop=(kc == 7)), [b3.r, xT_.r], [pb_.r])
                    A(lambda: nc.scalar.activation(out=sil.t[:], in_=pa.t[:, 0:256], func=AF.Silu), [pa.r], [sil.r])
                    V(lambda: nc.vector.tensor_tensor(out=gT_.t[:, fc, :], in0=pb_.t[:, 0:256], in1=sil.t[:], op=ALU.mult),
                      [pb_.r, sil.r], [gT_.r])
                for st_ in range(2):
                    y_ = ysb[yc % 2]; yc += 1
                    for hh in range(2):
                        ps = pnext()
                        for fc in range(4):
                            P(lambda: nc.tensor.matmul(ps.t[:], lhsT=gT_.t[:, fc, st_ * 128:(st_ + 1) * 128],
                                                       rhs=b2.t[:, fc, hh * 512:(hh + 1) * 512], start=(fc == 0), stop=(fc == 3)),
                              [gT_.r, b2.r], [ps.r])
                        if hh == 0:
                            A(lambda: nc.scalar.copy(out=y_.t[:, 0:512], in_=ps.t[:]), [ps.r], [y_.r])
                        else:
                            V(lambda: nc.vector.tensor_copy(out=y_.t[:, 512:1024], in_=ps.t[:]), [ps.r], [y_.r])
                    r0 = j * BLK + st_ * 128
                    T.dma(SP, y_.l, lambda: nc.sync.dma_start(out=YS[r0:r0 + 128, :], in_=y_.t[:]), reads=[y_.r])
            T.barrier()
            if stop <= 5:
                return nc

        with ExitStack() as ee:
            set_rot(range(8))
            gfb = sbt(ee, "gfb", [128, D], F32, True); load(gfb, gf_row.partition_broadcast(128))
            y0 = [sbt(ee, f"y0_{i}", [128, D], F32, True) for i in range(2)]
            y1 = [sbt(ee, f"y1_{i}", [128, D], F32, True) for i in range(2)]
            xr = [sbt(ee, f"xr{i}", [128, D], F32, True) for i in range(2)]
            acc = sbt(ee, "acc", [128, D], F32)
            jk = sbt(ee, "jk", [128, D], F32)
            ob = [sbt(ee, f"ob{i}", [128, D], F32, True) for i in range(2)]
            st3 = sbt(ee, "st3", [128, 4], F32)
            def issue_e(t):
                a0 = y0[t % 2]; a1 = y1[t % 2]; xx = xr[t % 2]
                T.dma(POOL, a0.l, lambda: nc.gpsimd.indirect_dma_start(
                    out=a0.t[:], out_offset=None, in_=YS, in_offset=bass.IndirectOffsetOnAxis(ap=d0i.t[:, t:t + 1], axis=0),
                    bounds_check=REG_SLOT, oob_is_err=False),
                    reads=[d0i.r], writes=[a0.r])
                T.dma(POOL, a1.l, lambda: nc.gpsimd.indirect_dma_start(
                    out=a1.t[:], out_offset=None, in_=YS, in_offset=bass.IndirectOffsetOnAxis(ap=d1i.t[:, t:t + 1], axis=0),
                    bounds_check=REG_SLOT, oob_is_err=False),
                    reads=[d1i.r], writes=[a1.r])
                load(xx, X1[t * 128:(t + 1) * 128, :])

            issue_e(0)
            for t in range(NOWN):
                a0 = y0[t % 2]; a1 = y1[t % 2]; xx = xr[t % 2]; o_ = ob[t % 2]
                if t + 1 < NOWN:
                    issue_e(t + 1)
                V(lambda: nc.vector.tensor_scalar(out=acc.t[:], in0=a0.t[:], scalar1=wts.t[:, t, 0:1], scalar2=None, op0=ALU.mult),
                  [a0.r, wts.r], [acc.r])
                V(lambda: nc.vector.scalar_tensor_tensor(out=acc.t[:], in0=a1.t[:], scalar=wts.t[:, t, 1:2], in1=acc.t[:],
                                                         op0=ALU.mult, op1=ALU.add), [a1.r, wts.r, acc.r], [acc.r])
                V(lambda: nc.vector.tensor_tensor(out=acc.t[:], in0=acc.t[:], in1=gate2b.t[:], op=ALU.mult), [acc.r, gate2b.r], [acc.r])
                V(lambda: nc.vector.tensor_tensor(out=acc.t[:], in0=acc.t[:], in1=xx.t[:], op=ALU.add), [acc.r, xx.r], [acc.r])
                A(lambda: nc.scalar.activation(out=jk.t[:], in_=acc.t[:], func=AF.Square, accum_out=st3.t[:, 0:1]),
                  [acc.r], [jk.r, st3.r])
                V(lambda: nc.vector.tensor_scalar(out=st3.t[:, 1:2], in0=st3.t[:, 0:1], scalar1=1.0 / D, scalar2=EPS,
                                                  op0=ALU.mult, op1=ALU.add), [st3.r], [st3.r])
                A(lambda: nc.scalar.activation(out=st3.t[:, 2:3], in_=st3.t[:, 1:2], func=AF.Sqrt), [st3.r], [st3.r])
                V(lambda: nc.vector.reciprocal(out=st3.t[:, 3:4], in_=st3.t[:, 2:3]), [st3.r], [st3.r])
                V(lambda: nc.vector.scalar_tensor_tensor(out=o_.t[:], in0=acc.t[:], scalar=st3.t[:, 3:4], in1=gfb.t[:],
                                                         op0=ALU.mult, op1=ALU.mult), [acc.r, st3.r, gfb.r], [o_.r])
                T.dma(SP, o_.l, lambda: nc.sync.dma_start(out=out_d[t * 128:(t + 1) * 128, :], in_=o_.t[:]), reads=[o_.r])
            T.barrier()
    return nc


def _own_tiles(j, NP):
    t = []
    for m in range(NP):
        t += [8 * m + j, 8 * m + 7 - j]
    return t


def make_in_maps(inp, S):
    f32 = np.float32
    NT = S // 128; NP = NT // 8; NOWN = 2 * NP; TOK = NOWN * 128
    NBLK = (TOK * 2) // BLK + NEXP
    g = lambda k: np.asarray(inp[k])
    col = lambda v, n: np.ascontiguousarray(np.asarray(v, f32).reshape(n, 128).T)
    x = g("x"); c = g("c"); pos = g("positions")
    p = np.arange(128)
    fq = np.zeros((128, 1), f32)
    freqs = (np.float32(10000.0) ** (-np.arange(16, dtype=f32) / np.float32(16))).astype(f32)
    fq[64:96, 0] = np.tile(freqs, 2) / np.float32(2 * np.pi)
    shared = dict(
        w_ada=np.ascontiguousarray(g("w_ada")[0]), b_adaT=col(g("b_ada")[0], 48), b_ada_row=g("b_ada")[0].reshape(1, -1).astype(f32),
        g1col=col(g("norm1_g")[0], 8), g1row=g("norm1_g")[0].reshape(1, -1).astype(f32), w_in=np.ascontiguousarray(g("w_in")[0]),
        gqcol=col(g("q_norm_g")[0], 2), w_uq=np.ascontiguousarray(g("w_uq")[0]),
        gkvcol=col(g("kv_norm_g")[0], 1), w_ukv=np.ascontiguousarray(g("w_ukv")[0]),
        gv_row=g("v_norm_g")[0].reshape(1, -1).astype(f32), bv_row=g("v_norm_b")[0].reshape(1, -1).astype(f32),
        w_s=np.ascontiguousarray(g("w_s")[0]), bsT=np.ascontiguousarray(g("b_s")[0].T),
        w_br_mla=np.ascontiguousarray(g("w_br_mla")[0]), w_br_sgu=np.ascontiguousarray(g("w_br_sgu")[0]),
        w_out=np.ascontiguousarray(g("w_out")[0]),
        g2col=col(g("norm2_g")[0], 8), g2row=g("norm2_g")[0].reshape(1, -1).astype(f32),
        wr=np.ascontiguousarray(np.concatenate([g("w_rg")[0], g("w_re")[0]], axis=1)),
        br_row=np.concatenate([g("b_rg")[0], g("b_re")[0]]).reshape(1, -1).astype(f32),
        w1=np.ascontiguousarray(g("w1")[0]).reshape(NEXP * 128, 4096),
        w3=np.ascontiguousarray(g("w3")[0]).reshape(NEXP * 128, 4096),
        w2=np.ascontiguousarray(g("w2")[0]).reshape(NEXP * 128, 4096),
        gf_row=g("final_g").reshape(1, -1).astype(f32),
        c_ident=np.eye(128, dtype=f32), c_tril=np.tril(np.ones((128, 128), f32)),
        c_lstrict=(p[:, None] < p[None, :]).astype(f32), c_kq=(p[:, None] - p[None, :]).astype(f32),
        c_fq=fq, c_thr=np.tile((np.arange(NBLK, dtype=f32) * BLK)[None, :], (128, 1)).astype(f32),
        c_pidx=p.astype(f32).reshape(128, 1),
    )
    maps = []
    owns = []
    for core in range(8):
        b, j = core // 4, core % 4
        tiles = _own_tiles(j, NP)
        rows = np.concatenate([np.arange(t * 128, (t + 1) * 128) for t in tiles])
        owns.append((b, rows))
        dt_ = np.zeros((128, 16), f32)
        for o in range(8):
            dt_[:, o] = (j - o) * 128
            dt_[:, 8 + o] = (7 - j - o) * 128
        m = dict(shared)
        m.update(
            xb=np.ascontiguousarray(x[b, :S]), xo=np.ascontiguousarray(x[b][rows]),
            posb=np.ascontiguousarray(pos[b, :S].reshape(1, -1).astype(np.int32)),
            poso=np.ascontiguousarray(pos[b][rows].reshape(1, -1).astype(np.int32)),
            ccol=col(c[b], 8), c_dtab=dt_,
        )
        maps.append(m)
    return maps, owns


_NC_CACHE = {}


def kernel(**inputs):
    S = int(np.asarray(inputs["x"]).shape[1])
    if S not in _NC_CACHE:
        _NC_CACHE[S] = build_nc(S)
    nc = _NC_CACHE[S]
    maps, owns = make_in_maps(inputs, S)
    res = run_bass_kernel_spmd(nc, maps, core_ids=list(range(8)))
    B = np.asarray(inputs["x"]).shape[0]
    out = np.zeros((B, S, D), np.float32)
    for core, (b, rows) in enumerate(owns):
        out[b, rows] = np.asarray(res.results[core]["out"], dtype=np.float32)
    return out
```

```python
import numpy as np
from contextlib import ExitStack
import concourse.bass as bass
import concourse.mybir as mybir
from concourse.bass_utils import run_bass_kernel_spmd

F32 = mybir.dt.float32
BF16 = mybir.dt.bfloat16
I32 = mybir.dt.int32
AF = mybir.ActivationFunctionType
ALU = mybir.AluOpType
AX = mybir.AxisListType

D = 1024
NH = 8
EPS = 1e-6
NEXP = 32
FF = 512
BLK = 256
IN_W = 3488
SCALE = 96 ** -0.5
GELU_C = 1.5957691216057308


class Res:
    __slots__ = ("name", "w", "r")

    def __init__(self, name=""):
        self.name = name
        self.w = None
        self.r = {}


class Lane:
    def __init__(self, T, name):
        self.sem = T.newsem(name)
        self.count = 0


class Eng:
    def __init__(self, T, name, eng, is_pe=False):
        self.name = name
        self.eng = eng
        self.is_pe = is_pe
        self.sem = T.newsem(name + "_s0")
        self.own = [self.sem]
        self.count = 0
        self.waited = {}


class Tracker:
    CAP = 12000

    def __init__(self, nc, es):
        self.nc = nc
        self.es = es
        self.nsems = 0
        self.lanes = []
        self.pe = Eng(self, "pe", nc.tensor, True)
        self.act = Eng(self, "act", nc.scalar)
        self.dve = Eng(self, "dve", nc.vector)
        self.pool = Eng(self, "pool", nc.gpsimd)
        self.sp = Eng(self, "sp", nc.sync)
        self.engs = [self.pe, self.act, self.dve, self.pool, self.sp]

    def newsem(self, name):
        self.nsems += 1
        return self.es.enter_context(self.nc.semaphore(f"{name}_{self.nsems}"))

    def lane(self, name="l"):
        l = Lane(self, name)
        self.lanes.append(l)
        return l

    def _wait(self, E, deps):
        for sem, val in deps:
            if E.is_pe and any(sem is s for s in E.own):
                continue
            k = id(sem)
            if E.waited.get(k, 0) < val:
                E.eng.wait_ge(sem, val)
                E.waited[k] = val

    @staticmethod
    def _deps(reads, writes):
        deps = []
        for r in reads:
            if r.w is not None:
                deps.append(r.w)
        for w in writes:
            if w.w is not None:
                deps.append(w.w)
            deps.extend(w.r.values())
        return deps

    @staticmethod
    def _record(ev, reads, writes):
        for r in reads:
            r.r[id(ev[0])] = ev
        for w in writes:
            w.w = ev
            w.r = {}

    def op(self, E, fn, reads=(), writes=()):
        self._wait(E, self._deps(reads, writes))
        if E.count >= self.CAP:
            E.sem = self.newsem(E.name + "_s")
            E.own.append(E.sem)
            E.count = 0
        ins = fn()
        ins.then_inc(E.sem, 1)
        E.count += 1
        self._record((E.sem, E.count), reads, writes)

    def dma(self, E, lane, fn, reads=(), writes=()):
        self._wait(E, self._deps(reads, writes))
        ins = fn()
        ins.then_inc(lane.sem, 16)
        lane.count += 16
        self._record((lane.sem, lane.count), reads, writes)

    def barrier(self):
        evs = [(e.sem, e.count) for e in self.engs if e.count > 0]
        evs += [(l.sem, l.count) for l in self.lanes if l.count > 0]
        for E in self.engs:
            self._wait(E, evs)


class Buf:
    def __init__(self, T, tile, name, lane=False):
        self.t = tile
        self.r = Res(name)
        self.l = T.lane(name) if lane else None


def build_nc(S, stop=99):
    NT = S // 128
    NP = NT // 8
    NOWN = 2 * NP
    TOK = NOWN * 128
    NG = NT // 4
    NGO = max(NOWN // 4, 1)
    GW = min(512, TOK)
    NBLK = (TOK * 2) // BLK + NEXP
    NSLOT = NBLK * BLK

    nc = bass.Bass("TRN2", target_bir_lowering=False)

    def din(name, shape, dt=F32):
        return nc.dram_tensor(name, list(shape), dt, kind="ExternalInput").ap()

    xb = din("xb", [S, D]); xo = din("xo", [TOK, D])
    posb = din("posb", [1, S], I32); poso = din("poso", [1, TOK], I32)
    ccol = din("ccol", [128, 8])
    w_ada = din("w_ada", [D, 6 * D]); b_adaT = din("b_adaT", [128, 48]); b_ada_row = din("b_ada_row", [1, 6 * D])
    g1col = din("g1col", [128, 8]); g1row = din("g1row", [1, D]); w_in = din("w_in", [D, IN_W])
    gqcol = din("gqcol", [128, 2]); w_uq = din("w_uq", [256, 768])
    gkvcol = din("gkvcol", [128, 1]); w_ukv = din("w_ukv", [128, 1024])
    gv_row = din("gv_row", [1, 512]); bv_row = din("bv_row", [1, 512])
    w_s = din("w_s", [8, 128, 128]); bsT = din("bsT", [128, 8])
    w_br_mla = din("w_br_mla", [512, D]); w_br_sgu = din("w_br_sgu", [512, D]); w_out = din("w_out", [D, D])
    g2col = din("g2col", [128, 8]); g2row = din("g2row", [1, D])
    wr = din("wr", [D, 36]); br_row = din("br_row", [1, 36])
    w1 = din("w1", [NEXP * 128, 4096]); w3 = din("w3", [NEXP * 128, 4096]); w2 = din("w2", [NEXP * 128, 4096])
    gf_row = din("gf_row", [1, D])
    c_ident = din("c_ident", [128, 128]); c_tril = din("c_tril", [128, 128]); c_lstrict = din("c_lstrict", [128, 128])
    c_kq = din("c_kq", [128, 128]); c_fq = din("c_fq", [128, 1]); c_dtab = din("c_dtab", [128, 16])
    c_thr = din("c_thr", [128, NBLK]); c_pidx = din("c_pidx", [128, 1])

    out_d = nc.dram_tensor("out", [TOK, D], F32, kind="ExternalOutput").ap()
    X1 = nc.dram_tensor("X1s", [TOK, D], F32, kind="Internal").ap()
    H2 = nc.dram_tensor("H2s", [TOK, D], BF16, kind="Internal").ap()
    XS = nc.dram_tensor("XSs", [NSLOT, D], BF16, kind="Internal").ap()
    YS = nc.dram_tensor("YSs", [NSLOT, D], F32, kind="Internal").ap()
    OS = nc.dram_tensor("OSs", [TOK, 512], BF16, kind="Internal").ap()

    with ExitStack() as es:
        T = Tracker(nc, es)
        PE, ACT, DVE, POOL, SP = T.pe, T.act, T.dve, T.pool, T.sp

        def sbt(stack, name, shape, dt, lane=False):
            t = stack.enter_context(nc.sbuf_tensor(name, list(shape), dt))
            return Buf(T, t, name, lane)

        banks = []
        for i in range(8):
            t = es.enter_context(nc.psum_tensor(f"pb{i}", [128, 512], F32))
            banks.append(Buf(T, t, f"pb{i}"))
        rot = {"i": 0, "lst": list(range(8))}

        def pnext():
            b = banks[rot["lst"][rot["i"] % len(rot["lst"])]]
            rot["i"] += 1
            return b

        def set_rot(lst):
            rot["lst"] = list(lst)
            rot["i"] = 0

        def V(fn, reads=(), writes=()):
            T.op(DVE, fn, reads, writes)

        def A(fn, reads=(), writes=()):
            T.op(ACT, fn, reads, writes)

        def G(fn, reads=(), writes=()):
            T.op(POOL, fn, reads, writes)

        def P(fn, reads=(), writes=()):
            T.op(PE, fn, reads, writes)

        def load(buf, src, eng=None):
            E = SP if eng is None else eng
            T.dma(E, buf.l, lambda: E.eng.dma_start(out=buf.t[:], in_=src), writes=[buf.r])

        def load_to(buf, dst_ap, src, eng=None):
            E = SP if eng is None else eng
            T.dma(E, buf.l, lambda: E.eng.dma_start(out=dst_ap, in_=src), writes=[buf.r])

        identf = sbt(es, "identf", [128, 128], F32, True); load(identf, c_ident)
        identb = sbt(es, "identb", [128, 128], BF16)
        V(lambda: nc.vector.tensor_copy(out=identb.t[:], in_=identf.t[:]), [identf.r], [identb.r])
        onesb = sbt(es, "onesb", [128, 128], BF16)
        V(lambda: nc.vector.memset(onesb.t[:], 1.0), [], [onesb.r])
        onesf = sbt(es, "onesf", [128, 128], F32)
        V(lambda: nc.vector.memset(onesf.t[:], 1.0), [], [onesf.r])
        kqf = sbt(es, "kqf", [128, 128], F32, True); load(kqf, c_kq)
        kqb = sbt(es, "kqb", [128, 128], BF16)
        V(lambda: nc.vector.tensor_copy(out=kqb.t[:], in_=kqf.t[:]), [kqf.r], [kqb.r])
        fq = sbt(es, "fq", [128, 1], F32, True); load(fq, c_fq)
        dtab = sbt(es, "dtab", [128, 16], F32, True); load(dtab, c_dtab)
        g1c = sbt(es, "g1c", [128, 8], F32, True); load(g1c, g1col)
        g2c = sbt(es, "g2c", [128, 8], F32, True); load(g2c, g2col)
        gqc = sbt(es, "gqc", [128, 2], F32, True); load(gqc, gqcol)
        gkvc = sbt(es, "gkvc", [128, 1], F32, True); load(gkvc, gkvcol)
        cc = sbt(es, "cc", [128, 8], F32, True); load(cc, ccol)
        badT = sbt(es, "badT", [128, 48], F32, True); load(badT, b_adaT)
        modT = sbt(es, "modT", [128, 48], F32)
        gate1b = sbt(es, "gate1b", [128, D], F32)
        s2b = sbt(es, "s2b", [128, D], F32)
        sh2b = sbt(es, "sh2b", [128, D], F32)
        gate2b = sbt(es, "gate2b", [128, D], F32)
        s1b = sbt(es, "s1b", [128, D], F32)
        sh1b = sbt(es, "sh1b", [128, 8], BF16)
        ones512 = sbt(es, "ones512", [1, 512], BF16)
        V(lambda: nc.vector.memset(ones512.t[:], 1.0), [], [ones512.r])
        s1c = sbt(es, "s1c", [128, 8], F32); s2c = sbt(es, "s2c", [128, 8], F32)

        with ExitStack() as ph:
            sc = sbt(ph, "sc", [128, 8], F32)
            A(lambda: nc.scalar.activation(out=sc.t[:], in_=cc.t[:], func=AF.Silu), [cc.r], [sc.r])
            brow = [sbt(ph, f"brow{i}", [1, 512], F32, True) for i in range(2)]
            rowdst = {2: (s1b, 0), 3: (s1b, 512), 4: (gate1b, 0), 5: (gate1b, 512), 6: (sh2b, 0), 7: (sh2b, 512), 8: (s2b, 0), 9: (s2b, 512),
                      10: (gate2b, 0), 11: (gate2b, 512)}
            wp = [sbt(ph, f"wada{i}", [128, 8, 512], F32, True) for i in range(2)]
            pcol = banks[7]
            set_rot(range(7))
            rowtmp = sbt(ph, "rowtmp", [1, 512], F32)
            for pc in range(12):
                wb_ = wp[pc % 2]
                load(wb_, w_ada[:, pc * 512:(pc + 1) * 512].rearrange("(kc p) n -> p kc n", p=128))
                if pc in rowdst:
                    prow = pnext()
                    br_ = brow[pc % 2]
                    load(br_, b_ada_row[0:1, pc * 512:(pc + 1) * 512])
                    for kc in range(8):
                        P(lambda: nc.tensor.matmul(prow.t[0:1, :], lhsT=sc.t[:, kc:kc + 1], rhs=wb_.t[:, kc, :],
                                                   start=(kc == 0), stop=(kc == 7)), [sc.r, wb_.r], [prow.r])
                    V(lambda: nc.vector.tensor_tensor(out=rowtmp.t[0:1, :], in0=prow.t[0:1, :], in1=br_.t[0:1, :], op=ALU.add),
                      [prow.r, br_.r], [rowtmp.r])
                    pbc = pnext()
                    P(lambda: nc.tensor.matmul(pbc.t[:], lhsT=onesf.t[0:1, :], rhs=rowtmp.t[0:1, :], start=True, stop=True),
                      [onesf.r, rowtmp.r], [pbc.r])
                    dtile, dc0 = rowdst[pc]
                    V(lambda: nc.vector.tensor_copy(out=dtile.t[:, dc0:dc0 + 512], in_=pbc.t[:]), [pbc.r], [dtile.r])
                for oc in range(4):
                    col = pc * 4 + oc
                    if pc not in (0, 1, 6, 7, 8, 9):
                        continue
                    for kc in range(8):
                        P(lambda: nc.tensor.matmul(pcol.t[:, col:col + 1], lhsT=wb_.t[:, kc, oc * 128:(oc + 1) * 128],
                                                   rhs=sc.t[:, kc:kc + 1], start=(kc == 0), stop=(kc == 7)),
                          [sc.r, wb_.r], [pcol.r])
            V(lambda: nc.vector.memset(modT.t[:], 0.0), [], [modT.r])
            V(lambda: nc.vector.tensor_tensor(out=modT.t[:, 0:8], in0=pcol.t[:, 0:8], in1=badT.t[:, 0:8], op=ALU.add),
              [pcol.r, badT.r], [modT.r])
            V(lambda: nc.vector.tensor_tensor(out=modT.t[:, 24:40], in0=pcol.t[:, 24:40], in1=badT.t[:, 24:40], op=ALU.add),
              [pcol.r, badT.r], [modT.r])
            V(lambda: nc.vector.scalar_tensor_tensor(out=s2c.t[:], in0=modT.t[:, 32:40], scalar=1.0, in1=g2c.t[:],
                                                     op0=ALU.add, op1=ALU.mult), [modT.r, g2c.r], [s2c.r])
            g1rb = sbt(ph, "g1rb", [128, D], F32, True); load(g1rb, g1row.partition_broadcast(128))
            V(lambda: nc.vector.scalar_tensor_tensor(out=s1b.t[:], in0=s1b.t[:], scalar=1.0, in1=g1rb.t[:], op0=ALU.add,
                                                     op1=ALU.mult), [s1b.r, g1rb.r], [s1b.r])
            V(lambda: nc.vector.tensor_copy(out=sh1b.t[:], in_=modT.t[:, 0:8]), [modT.r], [sh1b.r])
            T.barrier()
        if stop <= 0:
            return nc
        sh1 = lambda kc: modT.t[:, kc:kc + 1]
        sh2 = lambda kc: modT.t[:, 24 + kc:25 + kc]

        FE = {}
        cnt = {"x": 0, "fe": 0, "st": 0, "xn": 0, "cp": 0}

        def make_fe(stack, nx=3, nxn=2):
            k = cnt["fe"]; cnt["fe"] += 1
            FE["nx"] = nx
            FE["xts"] = [sbt(stack, f"xt{k}_{i}", [128, D], F32, True) for i in range(nx)]
            FE["junk"] = sbt(stack, f"junk{k}", [128, D], BF16)
            FE["stat"] = [sbt(stack, f"stat{k}_{i}", [128, 16], F32) for i in range(3)]
            FE["xns"] = [sbt(stack, f"xn{k}_{i}", [128, D], BF16) for i in range(nxn)]

        def fe_a(tile_aps):
            n = len(tile_aps)
            st = FE["stat"][cnt["st"] % 3]; cnt["st"] += 1
            junk = FE["junk"]
            xl = []
            for j in range(n):
                xt = FE["xts"][cnt["x"] % FE["nx"]]; cnt["x"] += 1
                load(xt, tile_aps[j])
                xl.append(xt)
            for j in range(n):
                A(lambda: nc.scalar.activation(out=junk.t[:], in_=xl[j].t[:], func=AF.Square, accum_out=st.t[:, j:j + 1]),
                  [xl[j].r], [junk.r, st.r])
            V(lambda: nc.vector.tensor_scalar(out=st.t[:, 4:4 + n], in0=st.t[:, 0:n], scalar1=1.0 / D, scalar2=EPS,
                                              op0=ALU.mult, op1=ALU.add), [st.r], [st.r])
            A(lambda: nc.scalar.activation(out=st.t[:, 8:8 + n], in_=st.t[:, 4:4 + n], func=AF.Sqrt), [st.r], [st.r])
            V(lambda: nc.vector.reciprocal(out=st.t[:, 12:12 + n], in_=st.t[:, 8:8 + n]), [st.r], [st.r])
            xnl = []
            for j in range(n):
                xn = FE["xns"][cnt["xn"] % len(FE["xns"])]; cnt["xn"] += 1
                V(lambda: nc.vector.scalar_tensor_tensor(out=xn.t[:], in0=xl[j].t[:], scalar=st.t[:, 12 + j:13 + j], in1=s1b.t[:],
                                                         op0=ALU.mult, op1=ALU.mult), [xl[j].r, st.r, s1b.r], [xn.r])
                xnl.append(xn)
            return xl, xnl

        def fe_b(state, dst, dst_res, c0s):
            xl, xnl = state
            for j, xn in enumerate(xnl):
                tp = pnext()
                tpb = tp.t[:].bitcast(BF16)
                for kc in range(8):
                    P(lambda: nc.tensor.transpose(out=tpb[:, kc * 128:(kc + 1) * 128], in_=xn.t[:, kc * 128:(kc + 1) * 128],
                                                  identity=identb.t[:]), [xn.r, identb.r], [tp.r])
                c0 = c0s[j]
                src3 = tpb.rearrange("p (k t) -> p k t", k=8)
                if cnt["cp"] % 2 == 0:
                    A(lambda: nc.scalar.copy(out=dst[:, :, c0:c0 + 128], in_=src3), [tp.r], [dst_res])
                else:
                    V(lambda: nc.vector.tensor_copy(out=dst[:, :, c0:c0 + 128], in_=src3), [tp.r], [dst_res])
                cnt["cp"] += 1
            return xl

        def front_end_multi(tile_aps, dst, dst_res, c0s):
            return fe_b(fe_a(tile_aps), dst, dst_res, c0s)

        def bias_row(dst_ap, wfn, M, wres, dres):
            ps = pnext()
            for kc in range(8):
                P(lambda: nc.tensor.matmul(ps.t[0:1, 0:M], lhsT=sh1b.t[:, kc:kc + 1], rhs=wfn(kc), start=(kc == 0), stop=(kc == 7)),
                  [sh1b.r, wres], [ps.r])
            V(lambda: nc.vector.tensor_copy(out=dst_ap, in_=ps.t[0:1, 0:M]), [ps.r], [dres])

        s1col = lambda kc: s1c.t[:, kc:kc + 1]

        Aall = sbt(es, "Aall", [128, NOWN, 32], BF16)
        A0all = sbt(es, "A0all", [128, NOWN, 32], BF16)
        A1all = sbt(es, "A1all", [128, NOWN, 32], BF16)
        wts = sbt(es, "wts", [128, NOWN, 2], F32)
        att = es.enter_context(ExitStack())
        ckvnT = sbt(att, "ckvnT", [128, S], BF16); ckvn_r = [Res() for _ in range(NG)]
        KT = sbt(att, "KT", [128, S], BF16); ktn_r = [Res() for _ in range(NG)]; ktr_r = [Res() for _ in range(NG)]
        vh_r = [Res() for _ in range(NG)]
        cqnT = sbt(att, "cqnT", [128, 2, TOK], BF16); cqn_r = [Res() for _ in range(NGO)]
        qT_r = [Res() for _ in range(NGO)]
        CQ = sbt(att, "CQ", [128, TOK], BF16); SQ = sbt(att, "SQ", [128, TOK], BF16)
        cs_r = [Res() for _ in range(NGO)]

        def rope_tables(pos_ap_512, Cdst, Sdst, wres, lp, width):
            pi_ = lp["posi"]; R = slice(64, 96)
            T.dma(SP, pi_.l, lambda: nc.sync.dma_start(out=pi_.t[R, 0:width], in_=pos_ap_512.partition_broadcast(32)),
                  writes=[pi_.r])
            pf = lp["pf"]; u = lp["u"]; ki = lp["ki"]; kf = lp["kf"]; f2 = lp["f2"]
            V(lambda: nc.vector.tensor_copy(out=pf.t[R, 0:width], in_=pi_.t[R, 0:width]), [pi_.r], [pf.r])
            for which, off, dstb in ((0, 0.5, Sdst), (1, 0.75, Cdst)):
                V(lambda: nc.vector.tensor_scalar(out=u.t[R, 0:width], in0=pf.t[R, 0:width], scalar1=fq.t[R, 0:1],
                                                  scalar2=off, op0=ALU.mult, op1=ALU.add), [pf.r, fq.r], [u.r])
                V(lambda: nc.vector.tensor_copy(out=ki.t[R, 0:width], in_=u.t[R, 0:width]), [u.r], [ki.r])
                V(lambda: nc.vector.tensor_copy(out=kf.t[R, 0:width], in_=ki.t[R, 0:width]), [ki.r], [kf.r])
                V(lambda: nc.vector.scalar_tensor_tensor(out=f2.t[R, 0:width], in0=u.t[R, 0:width], scalar=-0.5,
                                                         in1=kf.t[R, 0:width], op0=ALU.add, op1=ALU.subtract),
                  [u.r, kf.r], [f2.r])
                V(lambda: nc.vector.scalar_tensor_tensor(out=u.t[R, 0:width], in0=f2.t[R, 0:width], scalar=-0.5,
                                                         in1=f2.t[R, 0:width], op0=ALU.is_lt, op1=ALU.add),
                  [f2.r], [u.r])
                A(lambda: nc.scalar.activation(out=dstb, in_=u.t[R, 0:width], func=AF.Sin, scale=6.28318),
                  [u.r], [wres])

        with ExitStack() as lp_:
            make_fe(lp_, 4, 4)
            lp = {}
            lp["posi"] = sbt(lp_, "posi", [128, 512], I32, True)
            for nm in ("pf", "u", "kf", "f2"):
                lp[nm] = sbt(lp_, "lp_" + nm, [128, 512], F32)
            lp["t1"] = lp["pf"]; lp["t2"] = lp["kf"]
            lp["ki"] = sbt(lp_, "lp_ki", [128, 512], I32)
            Ctmp = sbt(lp_, "Ctmp", [128, 512], F32); Stmp = sbt(lp_, "Stmp", [128, 512], F32)
            h1Ts = [sbt(lp_, f"h1T{i}", [128, 8, 512], BF16) for i in range(2)]
            sq = sbt(lp_, "sq", [128, 2, 512], BF16)
            rs = sbt(lp_, "rs", [128, 512], F32); rs2 = sbt(lp_, "rs2", [128, 512], F32)
            wkv = sbt(lp_, "wkv", [128, 8, 128], BF16, True)
            wkr = sbt(lp_, "wkr", [128, 8, 96], BF16, True)
            wkrr = sbt(lp_, "wkrr", [128, 8, 96], BF16)
            wq = sbt(lp_, "wq", [128, 8, 256], BF16, True)
            win_v = w_in.rearrange("(kc p) n -> p kc n", p=128)
            load(wkv, win_v[:, :, 256:384], POOL)
            V(lambda: nc.vector.memset(wkr.t[:], 0.0), [], [wkr.r])
            V(lambda: nc.vector.memset(wkrr.t[:], 0.0), [], [wkrr.r])
            load_to(wkr, wkr.t[:, :, 64:96], win_v[:, :, 384:416], POOL)
            load(wq, win_v[:, :, 0:256], POOL)
            V(lambda: nc.vector.tensor_scalar(out=wkrr.t[:, :, 64:80], in0=wkr.t[:, :, 80:96], scalar1=-1.0, scalar2=None,
                                              op0=ALU.mult), [wkr.r, wkrr.r], [wkrr.r])
            V(lambda: nc.vector.tensor_copy(out=wkrr.t[:, :, 80:96], in_=wkr.t[:, :, 64:80]), [wkr.r, wkrr.r], [wkrr.r])
            set_rot(range(8))
            brows = sbt(lp_, "brows", [1, 640], BF16)
            bias_row(brows.t[0:1, 0:128], lambda kc: wkv.t[:, kc, :], 128, wkv.r, brows.r)
            bias_row(brows.t[0:1, 128:224], lambda kc: wkr.t[:, kc, :], 96, wkr.r, brows.r)
            bias_row(brows.t[0:1, 224:320], lambda kc: wkrr.t[:, kc, :], 96, wkrr.r, brows.r)
            bias_row(brows.t[0:1, 320:576], lambda kc: wq.t[:, kc, :], 256, wq.r, brows.r)
            hcnt = {"i": 0}
            h1_of = {}

            fe_state = {}

            def latent_fe_a(mode, g, width):
                ntl = width // 128
                src = xb if mode == "k" else xo
                fe_state[(mode, g)] = fe_a([src[(g * 4 + tl) * 128:(g * 4 + tl + 1) * 128, :] for tl in range(ntl)])

            def latent_fe_b(mode, g, width):
                ntl = width // 128
                h1 = h1Ts[hcnt["i"] % 2]; hcnt["i"] += 1
                h1_of[(mode, g)] = h1
                fe_b(fe_state.pop((mode, g)), h1.t, h1.r, [tl * 128 for tl in range(ntl)])

            mm_out = {}

            def latent_mm(mode, g, width):
                h1 = h1_of.pop((mode, g))
                W = slice(0, width)
                if mode == "k":
                    pk = pnext(); pr = pnext(); prr = pnext()
                    for kc in range(8):
                        P(lambda: nc.tensor.matmul(pk.t[:, W], lhsT=wkv.t[:, kc, :], rhs=h1.t[:, kc, W],
                                                   start=(kc == 0), stop=False), [wkv.r, h1.r], [pk.r])
                    P(lambda: nc.tensor.matmul(pk.t[:, W], lhsT=brows.t[0:1, 0:128], rhs=ones512.t[0:1, W], start=False, stop=True),
                      [brows.r, ones512.r], [pk.r])
                    for kc in range(8):
                        P(lambda: nc.tensor.matmul(pr.t[0:96, W], lhsT=wkr.t[:, kc, :], rhs=h1.t[:, kc, W],
                                                   start=(kc == 0), stop=False), [wkr.r, h1.r], [pr.r])
                    P(lambda: nc.tensor.matmul(pr.t[0:96, W], lhsT=brows.t[0:1, 128:224], rhs=ones512.t[0:1, W], start=False, stop=True),
                      [brows.r, ones512.r], [pr.r])
                    for kc in range(8):
                        P(lambda: nc.tensor.matmul(prr.t[0:96, W], lhsT=wkrr.t[:, kc, :], rhs=h1.t[:, kc, W],
                                                   start=(kc == 0), stop=False), [wkrr.r, h1.r], [prr.r])
                    P(lambda: nc.tensor.matmul(prr.t[0:96, W], lhsT=brows.t[0:1, 224:320], rhs=ones512.t[0:1, W], start=False, stop=True),
                      [brows.r, ones512.r], [prr.r])
                    chunks = [pk]
                    gcol = [gkvc.t[:, 0:1]]
                    nfeat = 128
                else:
                    p0 = pnext(); p1 = pnext()
                    for ci, pp in enumerate((p0, p1)):
                        for kc in range(8):
                            P(lambda: nc.tensor.matmul(pp.t[:, W], lhsT=wq.t[:, kc, ci * 128:(ci + 1) * 128],
                                                       rhs=h1.t[:, kc, W], start=(kc == 0), stop=False),
                              [wq.r, h1.r], [pp.r])
                        P(lambda: nc.tensor.matmul(pp.t[:, W], lhsT=brows.t[0:1, 320 + ci * 128:320 + (ci + 1) * 128],
                                                   rhs=ones512.t[0:1, W], start=False, stop=True), [brows.r, ones512.r], [pp.r])
                    chunks = [p0, p1]
                    gcol = [gqc.t[:, 0:1], gqc.t[:, 1:2]]
                    nfeat = 256
                mm_out[(mode, g)] = (chunks, gcol, nfeat, (pr, prr) if mode == "k" else None)

            def latent_rest(mode, g, width):
                W = slice(0, width)
                chunks, gcol, nfeat, prs = mm_out.pop((mode, g))
                if prs is not None:
                    pr, prr = prs
                for ci, pp in enumerate(chunks):
                    A(lambda: nc.scalar.activation(out=sq.t[:, ci, W], in_=pp.t[:, W], func=AF.Square), [pp.r], [sq.r])
                pss = pnext()
                for ci in range(len(chunks)):
                    P(lambda: nc.tensor.matmul(pss.t[:, W], lhsT=onesb.t[:], rhs=sq.t[:, ci, W], start=(ci == 0),
                                               stop=(ci == len(chunks) - 1)), [onesb.r, sq.r], [pss.r])
                V(lambda: nc.vector.tensor_scalar(out=rs.t[:, W], in0=pss.t[:, W], scalar1=1.0 / nfeat, scalar2=EPS,
                                                  op0=ALU.mult, op1=ALU.add), [pss.r], [rs.r])
                A(lambda: nc.scalar.activation(out=rs2.t[:, W], in_=rs.t[:, W], func=AF.Sqrt), [rs.r], [rs2.r])
                V(lambda: nc.vector.reciprocal(out=rs.t[:, W], in_=rs2.t[:, W]), [rs2.r], [rs.r])
                c0 = g * 512
                for ci, pp in enumerate(chunks):
                    if mode == "k":
                        dst = ckvnT.t[:, c0:c0 + width]; dres = ckvn_r[g]
                    else:
                        dst = cqnT.t[:, ci, c0:c0 + width]; dres = cqn_r[g]
                    V(lambda: nc.vector.scalar_tensor_tensor(out=dst, in0=pp.t[:, W], scalar=gcol[ci], in1=rs.t[:, W],
                                                             op0=ALU.mult, op1=ALU.mult),
                      [pp.r, rs.r, gkvc.r, gqc.r], [dres])
                R = slice(64, 96)
                if mode == "k":
                    rope_tables(posb[0:1, c0:c0 + width], Ctmp.t[R, W], Stmp.t[R, W], Ctmp.r, lp, width)
                    t1 = lp["t1"]; t2 = lp["t2"]
                    V(lambda: nc.vector.tensor_tensor(out=t1.t[R, W], in0=pr.t[R, W], in1=Ctmp.t[R, W], op=ALU.mult),
                      [pr.r, Ctmp.r], [t1.r])
                    V(lambda: nc.vector.tensor_tensor(out=t2.t[R, W], in0=prr.t[R, W], in1=Stmp.t[R, W], op=ALU.mult),
                      [prr.r, Ctmp.r], [t2.r])
                    V(lambda: nc.vector.tensor_tensor(out=KT.t[R, c0:c0 + width], in0=t1.t[R, W], in1=t2.t[R, W],
                                                      op=ALU.add), [t1.r, t2.r], [ktr_r[g]])
                else:
                    rope_tables(poso[0:1, c0:c0 + width], CQ.t[R, c0:c0 + width], SQ.t[R, c0:c0 + width], cs_r[g], lp, width)

            work = [("k", g, 512) for g in range(NG)] + [("q", g, GW) for g in range(NGO)]
            latent_fe_a(*work[0]); latent_fe_b(*work[0])
            for wi, w_ in enumerate(work):
                nxt = work[wi + 1] if wi + 1 < len(work) else None
                if nxt:
                    latent_fe_a(*nxt)
                latent_mm(*w_)
                if nxt:
                    latent_fe_b(*nxt)
                latent_rest(*w_)
            T.barrier()
            if stop <= 1:
                return nc

        with ExitStack() as ap_:
            qT = sbt(ap_, "qT", [128, TOK], BF16)
            Vh = sbt(ap_, "Vh", [128, NT, 65], BF16)
            V(lambda: nc.vector.memset(Vh.t[:, :, 64:65], 1.0), [], vh_r)
            wuqb = sbt(ap_, "wuqb", [128, 2, 768], BF16, True)
            wuqr = sbt(ap_, "wuqr", [128, 2, NH, 96], BF16)
            wukvb = sbt(ap_, "wukvb", [128, 1024], BF16, True)
            load(wuqb, w_uq.rearrange("(kc p) n -> p kc n", p=128), POOL)
            load(wukvb, w_ukv, POOL)
            V(lambda: nc.vector.memset(wuqr.t[:], 0.0), [], [wuqr.r])
            wv = wuqb.t[:].rearrange("p k (h d) -> p k h d", h=NH)
            V(lambda: nc.vector.tensor_scalar(out=wuqr.t[:, :, :, 64:80], in0=wv[:, :, :, 80:96], scalar1=-1.0, scalar2=None,
                                              op0=ALU.mult), [wuqb.r, wuqr.r], [wuqr.r])
            V(lambda: nc.vector.tensor_copy(out=wuqr.t[:, :, :, 80:96], in_=wv[:, :, :, 64:80]), [wuqb.r, wuqr.r], [wuqr.r])

            PTs = [sbt(ap_, f"PT{i}", [128, 512], BF16) for i in range(8)]
            rec = [sbt(ap_, f"rec{i}", [128, 1], F32) for i in range(4)]
            ost = [sbt(ap_, f"ost{i}", [128, 64], BF16, True) for i in range(4)]
            osc = [0]
            t1q = sbt(ap_, "t1q", [128, 512], F32); t2q = sbt(ap_, "t2q", [128, 512], F32)
            set_rot([4, 5, 6, 7])
            oacc_i = 0
            ptc = 0
            for h in range(NH):
                for g in range(NG):
                    ps = pnext()
                    P(lambda: nc.tensor.matmul(ps.t[0:64, :], lhsT=wukvb.t[:, h * 128:h * 128 + 64],
                                               rhs=ckvnT.t[:, g * 512:(g + 1) * 512], start=True, stop=True),
                      [wukvb.r, ckvn_r[g]], [ps.r])
                    if g % 2 == 0:
                        V(lambda: nc.vector.tensor_copy(out=KT.t[0:64, g * 512:(g + 1) * 512], in_=ps.t[0:64, :]),
                          [ps.r], [ktn_r[g]])
                    else:
                        A(lambda: nc.scalar.copy(out=KT.t[0:64, g * 512:(g + 1) * 512], in_=ps.t[0:64, :]),
                          [ps.r], [ktn_r[g]])
                for g8 in range(NT // 8):
                    ps = pnext()
                    for k8 in range(8):
                        kt = g8 * 8 + k8
                        P(lambda: nc.tensor.matmul(ps.t[:, k8 * 64:(k8 + 1) * 64], lhsT=ckvnT.t[:, kt * 128:(kt + 1) * 128],
                                                   rhs=wukvb.t[:, h * 128 + 64:h * 128 + 128], start=True, stop=True),
                          [wukvb.r, ckvn_r[kt // 4]], [ps.r])
                    wr_ = [vh_r[2 * g8], vh_r[2 * g8 + 1]]
                    src3 = ps.t[:].rearrange("p (a d) -> p a d", a=8)
                    if g8 % 2 == 0:
                        A(lambda: nc.scalar.copy(out=Vh.t[:, g8 * 8:(g8 + 1) * 8, 0:64], in_=src3), [ps.r], wr_)
                    else:
                        V(lambda: nc.vector.tensor_copy(out=Vh.t[:, g8 * 8:(g8 + 1) * 8, 0:64], in_=src3), [ps.r], wr_)
                for g in range(NGO):
                    W = slice(0, GW); c0 = g * 512; R = slice(64, 96)
                    pa = pnext(); pr = pnext()
                    for kc in range(2):
                        P(lambda: nc.tensor.matmul(pa.t[0:96, W], lhsT=wuqb.t[:, kc, h * 96:(h + 1) * 96],
                                                   rhs=cqnT.t[:, kc, c0:c0 + GW], start=(kc == 0), stop=(kc == 1)),
                          [wuqb.r, cqn_r[g]], [pa.r])
                    for kc in range(2):
                        P(lambda: nc.tensor.matmul(pr.t[0:96, W], lhsT=wuqr.t[:, kc, h, :],
                                                   rhs=cqnT.t[:, kc, c0:c0 + GW], start=(kc == 0), stop=(kc == 1)),
                          [wuqr.r, cqn_r[g]], [pr.r])
                    V(lambda: nc.vector.tensor_copy(out=qT.t[0:64, c0:c0 + GW], in_=pa.t[0:64, W]), [pa.r], [qT_r[g]])
                    V(lambda: nc.vector.tensor_tensor(out=t1q.t[R, W], in0=pa.t[R, W], in1=CQ.t[R, c0:c0 + GW], op=ALU.mult),
                      [pa.r, cs_r[g]], [t1q.r])
                    V(lambda: nc.vector.tensor_tensor(out=t2q.t[R, W], in0=pr.t[R, W], in1=SQ.t[R, c0:c0 + GW], op=ALU.mult),
                      [pr.r, cs_r[g]], [t2q.r])
                    V(lambda: nc.vector.tensor_tensor(out=qT.t[R, c0:c0 + GW], in0=t1q.t[R, W], in1=t2q.t[R, W], op=ALU.add),
                      [t1q.r, t2q.r], [qT_r[g]])
                steps = [(m, kp) for m in range(NP) for kp in range((8 * m + 8) // 2)]
                oa_of = {}
                for m in range(NP):
                    oa_of[m] = [banks[(oacc_i * 2) % 4], banks[(oacc_i * 2 + 1) % 4]]
                    oacc_i += 1
                pt_of = {}

                def emit_qk(si):
                    m, kp = steps[si]
                    qg = qT_r[(m * 256) // 512]
                    qs = qT.t[0:96, m * 256:(m + 1) * 256]
                    ps = pnext()
                    for kl in range(2):
                        kt = 2 * kp + kl
                        P(lambda: nc.tensor.matmul(ps.t[:, kl * 256:(kl + 1) * 256], lhsT=KT.t[0:96, kt * 128:(kt + 1) * 128],
                                                   rhs=qs, start=True, stop=True),
                          [ktn_r[kt // 4], ktr_r[kt // 4], qg], [ps.r])
                    PT = PTs[si % len(PTs)]
                    pt_of[si] = PT
                    A(lambda: nc.scalar.activation(out=PT.t[:], in_=ps.t[:], func=AF.Exp, scale=SCALE), [ps.r], [PT.r])
                    for kl in range(2):
                        kt = 2 * kp + kl
                        o = kt - 8 * m
                        if o >= 0:
                            for a in range(2):
                                cs_ = kl * 256 + a * 128
                                V(lambda: nc.vector.scalar_tensor_tensor(
                                    out=PT.t[:, cs_:cs_ + 128], in0=kqb.t[:], scalar=dtab.t[:, a * 8 + o:a * 8 + o + 1],
                                    in1=PT.t[:, cs_:cs_ + 128], op0=ALU.is_le, op1=ALU.mult),
                                  [PT.r, kqb.r, dtab.r], [PT.r])

                def emit_pv(si):
                    nonlocal_osc = None
                    m, kp = steps[si]
                    nk = 8 * m + 8
                    oa = oa_of[m]
                    PT = pt_of.pop(si)
                    for kl in range(2):
                        kt = 2 * kp + kl
                        for a in range(2):
                            cs_ = kl * 256 + a * 128
                            P(lambda: nc.tensor.matmul(oa[a].t[:, 0:65], lhsT=PT.t[:, cs_:cs_ + 128], rhs=Vh.t[:, kt, :],
                                                       start=(kt == 0), stop=(kt == nk - 1)),
                              [PT.r, vh_r[kt // 4]], [oa[a].r])
                    if kp == nk // 2 - 1:
                        for a in range(2):
                            i_own = 2 * m + a
                            rc = rec[(2 * m + a) % 4]
                            V(lambda: nc.vector.reciprocal(out=rc.t[:], in_=oa[a].t[:, 64:65]), [oa[a].r], [rc.r])
                            os_ = ost[osc[0] % 4]; osc[0] += 1
                            V(lambda: nc.vector.tensor_scalar(out=os_.t[:], in0=oa[a].t[:, 0:64],
                                                              scalar1=rc.t[:, 0:1], scalar2=None, op0=ALU.mult),
                              [oa[a].r, rc.r], [os_.r])
                            T.dma(SP, os_.l, lambda: nc.sync.dma_start(out=OS[i_own * 128:(i_own + 1) * 128, h * 64:(h + 1) * 64],
                                                                       in_=os_.t[:]), reads=[os_.r])

                LA = 4
                for si in range(len(steps) + LA):
                    if si < len(steps):
                        emit_qk(si)
                    if si - LA >= 0:
                        emit_pv(si - LA)
            T.barrier()
            if stop <= 2:
                return nc
        att.close()

        pc_ = es.enter_context(ExitStack())
        set_rot(range(8))
        win_v = w_in.rearrange("(kc p) n -> p kc n", p=128)
        wsg = sbt(pc_, "wsg", [128, 8, 1024], BF16, True); load(wsg, win_v[:, :, 416:1440], POOL)
        wgl = sbt(pc_, "wgl", [128, 8, 2048], BF16, True)
        for kc in range(8):
            load_to(wgl, wgl.t[:, kc, :], win_v[:, kc, 1440:3488], POOL)
        wbm = sbt(pc_, "wbm", [128, 4, D], BF16, True); load(wbm, w_br_mla.rearrange("(kc p) n -> p kc n", p=128), POOL)
        wbs = sbt(pc_, "wbs", [128, 4, D], BF16, True); load(wbs, w_br_sgu.rearrange("(kc p) n -> p kc n", p=128), POOL)
        wo = sbt(pc_, "wo", [128, 8, D], BF16, True); load(wo, w_out.rearrange("(kc p) n -> p kc n", p=128), POOL)
        wrf = sbt(pc_, "wrf", [128, 8, 36], F32, True); load(wrf, wr.rearrange("(kc p) n -> p kc n", p=128))
        brb = sbt(pc_, "brb", [128, 36], F32, True); load(brb, br_row.partition_broadcast(128))
        gvb = sbt(pc_, "gvb", [128, 512], F32, True); load(gvb, gv_row.partition_broadcast(128))
        bvb = sbt(pc_, "bvb", [128, 512], F32, True); load(bvb, bv_row.partition_broadcast(128))
        bst = sbt(pc_, "bst", [128, 8], F32, True); load(bst, bsT)
        make_fe(pc_, 3)
        browc = sbt(pc_, "browc", [1, 3072], BF16)
        for q_ in range(2):
            bias_row(browc.t[0:1, q_ * 512:(q_ + 1) * 512], lambda kc: wsg.t[:, kc, q_ * 512:(q_ + 1) * 512], 512, wsg.r, browc.r)
        for q_ in range(4):
            bias_row(browc.t[0:1, 1024 + q_ * 512:1024 + (q_ + 1) * 512], lambda kc: wgl.t[:, kc, q_ * 512:(q_ + 1) * 512], 512,
                     wgl.r, browc.r)
        wsT = sbt(pc_, "wsT", [128, 8, 128], BF16)
        with ExitStack() as tmp_:
            g2rb = sbt(tmp_, "g2rb", [128, D], F32, True); load(g2rb, g2row.partition_broadcast(128))
            V(lambda: nc.vector.scalar_tensor_tensor(out=s2b.t[:], in0=s2b.t[:], scalar=1.0, in1=g2rb.t[:], op0=ALU.add,
                                                     op1=ALU.mult), [s2b.r, g2rb.r], [s2b.r])
            wsf = sbt(tmp_, "wsf", [128, 8, 128], F32, True); load(wsf, w_s.rearrange("g t s -> t g s"))
            trl = sbt(tmp_, "trl", [128, 128], F32, True); load(trl, c_tril)
            wsm = sbt(tmp_, "wsm", [128, 8, 128], BF16)
            V(lambda: nc.vector.tensor_tensor(out=wsm.t[:], in0=wsf.t[:], in1=trl.t[:].unsqueeze(1).to_broadcast([128, 8, 128]),
                                              op=ALU.mult), [wsf.r, trl.r], [wsm.r])
            tp = pnext(); tpb = tp.t[:].bitcast(BF16)
            for g in range(8):
                P(lambda: nc.tensor.transpose(out=tpb[:, g * 128:(g + 1) * 128], in_=wsm.t[:, g, :], identity=identb.t[:]),
                  [wsm.r, identb.r], [tp.r])
            V(lambda: nc.vector.tensor_copy(out=wsT.t[:].rearrange("p g t -> p (g t)"), in_=tpb[:, :]), [tp.r], [wsT.r])
            T.barrier()


        with ExitStack() as cw:
            h1c = [sbt(cw, f"h1c{i}", [128, 8, 128], BF16) for i in range(1)]
            uv = sbt(cw, "uv", [128, 1024], F32)
            x2 = sbt(cw, "x2t", [128, 1024], F32)
            inner = sbt(cw, "inner", [128, 1024], F32)
            gel = sbt(cw, "gel", [128, 512], F32)
            otile = [sbt(cw, f"otile{i}", [128, 512], BF16, True) for i in range(2)]
            sgoB = [sbt(cw, f"sgo{i}", [128, 512], BF16) for i in range(1)]
            lst = sbt(cw, "lst", [128, 8], F32)
            sigB = [sbt(cw, f"sig{i}", [128, 2048], BF16) for i in range(2)]
            OT = sbt(cw, "OT", [128, 4, 128], BF16)
            SGT = sbt(cw, "SGT", [128, 4, 128], BF16)
            m1 = sbt(cw, "m1", [128, D], F32); m2 = sbt(cw, "m2", [128, D], F32)
            mrg = sbt(cw, "mrg", [128, D], BF16)
            mT = sbt(cw, "mT", [128, 8, 128], BF16)
            x1 = [sbt(cw, f"x1_{i}", [128, D], F32, True) for i in range(2)]
            x1n = m1
            h2T = sbt(cw, "h2T", [128, 8, 128], F32)
            h2f = m2
            h2p = [sbt(cw, f"h2p{i}", [128, D], BF16, True) for i in range(1)]
            st2 = sbt(cw, "st2", [128, 4], F32)
            lgall = sbt(cw, "lgall", [128, NOWN, 36], F32)
            rtb = sbt(cw, "rtb", [128, 8, NOWN], F32)
            s1out = {}

            vnbB = [sbt(cw, f"vnbB{i}", [128, 512], BF16) for i in range(2)]
            gubB = [sbt(cw, f"gubB{i}", [128, 512], BF16) for i in range(2)]
            fe_st = {}

            def S1(i):
                h1 = h1c[0]; sig = sigB[i % 2]; vnb_ = vnbB[i % 2]; gub = gubB[i % 2]
                if i not in fe_st:
                    fe_st[i] = fe_a([xo[i * 128:(i + 1) * 128, :]])
                xt = fe_b(fe_st.pop(i), h1.t, h1.r, [0])[0]
                s1out[i] = (xt, vnb_, gub, sig)
                load(otile[i % 2], OS[i * 128:(i + 1) * 128, :])
                pu = pnext(); pv = pnext()
                for hh, pp in enumerate((pu, pv)):
                    for kc in range(8):
                        P(lambda: nc.tensor.matmul(pp.t[:], lhsT=h1.t[:, kc, :], rhs=wsg.t[:, kc, hh * 512:(hh + 1) * 512],
                                                   start=(kc == 0), stop=False), [h1.r, wsg.r], [pp.r])
                    P(lambda: nc.tensor.matmul(pp.t[:], lhsT=onesb.t[0:1, :], rhs=browc.t[0:1, hh * 512:(hh + 1) * 512],
                                               start=False, stop=True), [onesb.r, browc.r], [pp.r])
                pg = [pnext() for _ in range(4)]
                for q4 in range(4):
                    for kc in range(8):
                        P(lambda: nc.tensor.matmul(pg[q4].t[:], lhsT=h1.t[:, kc, :], rhs=wgl.t[:, kc, q4 * 512:(q4 + 1) * 512],
                                                   start=(kc == 0), stop=False), [h1.r, wgl.r], [pg[q4].r])
                    P(lambda: nc.tensor.matmul(pg[q4].t[:], lhsT=onesb.t[0:1, :], rhs=browc.t[0:1, 1024 + q4 * 512:1024 + (q4 + 1) * 512],
                                               start=False, stop=True), [onesb.r, browc.r], [pg[q4].r])
                for hh, pp in enumerate((pu, pv)):
                    cs_ = slice(hh * 512, (hh + 1) * 512)
                    A(lambda: nc.scalar.copy(out=uv.t[:, cs_], in_=pp.t[:]), [pp.r], [uv.r])
                    A(lambda: nc.scalar.activation(out=x2.t[:, cs_], in_=pp.t[:], func=AF.Square, scale=0.044715 ** 0.5),
                      [pp.r], [x2.r])
                V(lambda: nc.vector.scalar_tensor_tensor(out=inner.t[:], in0=x2.t[:], scalar=1.0, in1=uv.t[:], op0=ALU.add,
                                                         op1=ALU.mult), [x2.r, uv.r], [inner.r])
                for q4 in range(4):
                    A(lambda: nc.scalar.activation(out=sig.t[:, q4 * 512:(q4 + 1) * 512], in_=pg[q4].t[:], func=AF.Sigmoid),
                      [pg[q4].r], [sig.r])
                A(lambda: nc.scalar.activation(out=inner.t[:], in_=inner.t[:], func=AF.Sigmoid, scale=GELU_C), [inner.r], [inner.r])
                if i + 1 < NOWN:
                    fe_st[i + 1] = fe_a([xo[(i + 1) * 128:(i + 2) * 128, :]])
                G(lambda: nc.gpsimd.tensor_tensor(out=gel.t[:, 0:512], in0=inner.t[:, 512:1024], in1=uv.t[:, 512:1024], op=ALU.mult),
                  [inner.r, uv.r], [gel.r])
                G(lambda: nc.gpsimd.tensor_tensor(out=gub.t[:], in0=inner.t[:, 0:512], in1=uv.t[:, 0:512], op=ALU.mult),
                  [inner.r, uv.r], [gub.r])
                gv_ = gel.t[:, 0:512]
                V(lambda: nc.vector.tensor_reduce(out=lst.t[:, 0:1], in_=gv_, axis=AX.X, op=ALU.add), [gel.r], [lst.r])
                V(lambda: nc.vector.tensor_scalar(out=lst.t[:, 1:2], in0=lst.t[:, 0:1], scalar1=1.0 / 512, scalar2=None,
                                                  op0=ALU.mult), [lst.r], [lst.r])
                V(lambda: nc.vector.tensor_scalar(out=x2.t[:, 0:512], in0=gv_, scalar1=lst.t[:, 1:2], scalar2=None,
                                                  op0=ALU.subtract), [gel.r, lst.r], [x2.r])
                A(lambda: nc.scalar.activation(out=x2.t[:, 512:1024], in_=x2.t[:, 0:512], func=AF.Square, accum_out=lst.t[:, 2:3]),
                  [x2.r], [x2.r, lst.r])
                V(lambda: nc.vector.tensor_scalar(out=lst.t[:, 3:4], in0=lst.t[:, 2:3], scalar1=1.0 / 512, scalar2=EPS,
                                                  op0=ALU.mult, op1=ALU.add), [lst.r], [lst.r])
                A(lambda: nc.scalar.activation(out=lst.t[:, 4:5], in_=lst.t[:, 3:4], func=AF.Sqrt), [lst.r], [lst.r])
                V(lambda: nc.vector.reciprocal(out=lst.t[:, 5:6], in_=lst.t[:, 4:5]), [lst.r], [lst.r])
                V(lambda: nc.vector.scalar_tensor_tensor(out=x2.t[:, 0:512], in0=x2.t[:, 0:512], scalar=lst.t[:, 5:6],
                                                         in1=gvb.t[:], op0=ALU.mult, op1=ALU.mult), [x2.r, lst.r, gvb.r], [x2.r])
                V(lambda: nc.vector.tensor_tensor(out=vnb_.t[:], in0=x2.t[:, 0:512], in1=bvb.t[:], op=ALU.add),
                  [x2.r, bvb.r], [vnb_.r])

            def S2(i):
                xt, vnb_, gub, sig = s1out.pop(i)
                sgo = sgoB[0]
                psg = pnext()
                for g in range(8):
                    P(lambda: nc.tensor.matmul(psg.t[:, g * 64:(g + 1) * 64], lhsT=wsT.t[:, g, :], rhs=vnb_.t[:, g * 64:(g + 1) * 64],
                                               start=True, stop=True), [wsT.r, vnb_.r], [psg.r])
                ot_ = otile[i % 2]
                tp = pnext(); tpb = tp.t[:].bitcast(BF16)
                for kc in range(4):
                    P(lambda: nc.tensor.transpose(out=tpb[:, kc * 128:(kc + 1) * 128], in_=ot_.t[:, kc * 128:(kc + 1) * 128],
                                                  identity=identb.t[:]), [ot_.r, identb.r], [tp.r])
                A(lambda: nc.scalar.copy(out=OT.t[:].rearrange("p k t -> p (k t)"), in_=tpb[:, 0:512]), [tp.r], [OT.r])
                V(lambda: nc.vector.tensor_tensor(out=m2.t[:, 0:512].rearrange("p (g c) -> p g c", g=8),
                                                  in0=psg.t[:].rearrange("p (g c) -> p g c", g=8),
                                                  in1=bst.t[:].unsqueeze(2).to_broadcast([128, 8, 64]), op=ALU.add),
                  [psg.r, bst.r], [m2.r])
                V(lambda: nc.vector.tensor_tensor(out=sgo.t[:], in0=m2.t[:, 0:512], in1=gub.t[:], op=ALU.mult),
                  [m2.r, gub.r], [sgo.r])
                for hh in range(2):
                    ps = pnext()
                    for kc in range(4):
                        P(lambda: nc.tensor.matmul(ps.t[:], lhsT=OT.t[:, kc, :], rhs=wbm.t[:, kc, hh * 512:(hh + 1) * 512],
                                                   start=(kc == 0), stop=(kc == 3)), [OT.r, wbm.r], [ps.r])
                    V(lambda: nc.vector.tensor_tensor(out=m1.t[:, hh * 512:(hh + 1) * 512], in0=ps.t[:],
                                                      in1=sig.t[:, hh * 512:(hh + 1) * 512], op=ALU.mult), [ps.r, sig.r], [m1.r])
                tp2 = pnext(); tpb2 = tp2.t[:].bitcast(BF16)
                for kc in range(4):
                    P(lambda: nc.tensor.transpose(out=tpb2[:, kc * 128:(kc + 1) * 128], in_=sgo.t[:, kc * 128:(kc + 1) * 128],
                                                  identity=identb.t[:]), [sgo.r, identb.r], [tp2.r])
                A(lambda: nc.scalar.copy(out=SGT.t[:].rearrange("p k t -> p (k t)"), in_=tpb2[:, 0:512]), [tp2.r], [SGT.r])
                for hh in range(2):
                    ps = pnext()
                    for kc in range(4):
                        P(lambda: nc.tensor.matmul(ps.t[:], lhsT=SGT.t[:, kc, :], rhs=wbs.t[:, kc, hh * 512:(hh + 1) * 512],
                                                   start=(kc == 0), stop=(kc == 3)), [SGT.r, wbs.r], [ps.r])
                    V(lambda: nc.vector.tensor_tensor(out=m2.t[:, hh * 512:(hh + 1) * 512], in0=ps.t[:],
                                                      in1=sig.t[:, 1024 + hh * 512:1024 + (hh + 1) * 512], op=ALU.mult),
                      [ps.r, sig.r], [m2.r])
                    V(lambda: nc.vector.tensor_tensor(out=mrg.t[:, hh * 512:(hh + 1) * 512], in0=m1.t[:, hh * 512:(hh + 1) * 512],
                                                      in1=m2.t[:, hh * 512:(hh + 1) * 512], op=ALU.add), [m1.r, m2.r], [mrg.r])
                tp = pnext(); tpb = tp.t[:].bitcast(BF16)
                for kc in range(8):
                    P(lambda: nc.tensor.transpose(out=tpb[:, kc * 128:(kc + 1) * 128], in_=mrg.t[:, kc * 128:(kc + 1) * 128],
                                                  identity=identb.t[:]), [mrg.r, identb.r], [tp.r])
                A(lambda: nc.scalar.copy(out=mT.t[:].rearrange("p k t -> p (k t)"), in_=tpb[:, :]), [tp.r], [mT.r])
                xx = x1[i % 2]
                for hh in range(2):
                    ps = pnext()
                    for kc in range(8):
                        P(lambda: nc.tensor.matmul(ps.t[:], lhsT=mT.t[:, kc, :], rhs=wo.t[:, kc, hh * 512:(hh + 1) * 512],
                                                   start=(kc == 0), stop=(kc == 7)), [mT.r, wo.r], [ps.r])
                    cs_ = slice(hh * 512, (hh + 1) * 512)
                    V(lambda: nc.vector.tensor_tensor(out=m1.t[:, cs_], in0=ps.t[:], in1=gate1b.t[:, cs_], op=ALU.mult),
                      [ps.r, gate1b.r], [m1.r])
                V(lambda: nc.vector.tensor_tensor(out=xx.t[:], in0=m1.t[:], in1=xt.t[:], op=ALU.add), [m1.r, xt.r], [xx.r])
                T.dma(SP, xx.l, lambda: nc.sync.dma_start(out=X1[i * 128:(i + 1) * 128, :], in_=xx.t[:]), reads=[xx.r])

            def S3(i):
                xx = x1[i % 2]
                junk = FE["junk"]
                A(lambda: nc.scalar.activation(out=junk.t[:], in_=xx.t[:], func=AF.Square, accum_out=st2.t[:, 0:1]),
                  [xx.r], [junk.r, st2.r])
                V(lambda: nc.vector.tensor_scalar(out=st2.t[:, 1:2], in0=st2.t[:, 0:1], scalar1=1.0 / D, scalar2=EPS,
                                                  op0=ALU.mult, op1=ALU.add), [st2.r], [st2.r])
                A(lambda: nc.scalar.activation(out=st2.t[:, 2:3], in_=st2.t[:, 1:2], func=AF.Sqrt), [st2.r], [st2.r])
                V(lambda: nc.vector.reciprocal(out=st2.t[:, 3:4], in_=st2.t[:, 2:3]), [st2.r], [st2.r])
                V(lambda: nc.vector.tensor_scalar(out=x1n.t[:], in0=xx.t[:], scalar1=st2.t[:, 3:4], scalar2=None, op0=ALU.mult),
                  [xx.r, st2.r], [x1n.r])
                G(lambda: nc.gpsimd.tensor_tensor(out=h2f.t[:], in0=x1n.t[:], in1=s2b.t[:], op=ALU.mult), [x1n.r, s2b.r], [h2f.r])
                hp = h2p[0]
                G(lambda: nc.gpsimd.tensor_tensor(out=hp.t[:].rearrange("t (kc p) -> t p kc", kc=8),
                                                  in0=h2f.t[:].rearrange("t (p kc) -> t p kc", kc=8),
                                                  in1=sh2b.t[:].rearrange("t (p kc) -> t p kc", kc=8), op=ALU.add),
                  [h2f.r, sh2b.r], [hp.r])
                T.dma(SP, hp.l, lambda: nc.sync.dma_start(out=H2[i * 128:(i + 1) * 128, :], in_=hp.t[:]), reads=[hp.r])
                tA = pnext(); tB = pnext()
                for kc in range(8):
                    tt = tA if kc < 4 else tB
                    P(lambda: nc.tensor.transpose(out=tt.t[:, (kc % 4) * 128:(kc % 4 + 1) * 128], in_=x1n.t[:, kc * 128:(kc + 1) * 128],
                                                  identity=identf.t[:]), [x1n.r, identf.r], [tt.r])
                for kc in range(8):
                    tt = tA if kc < 4 else tB
                    V(lambda: nc.vector.tensor_scalar(out=h2T.t[:, kc, :], in0=tt.t[:, (kc % 4) * 128:(kc % 4 + 1) * 128],
                                                      scalar1=s2c.t[:, kc:kc + 1], scalar2=sh2(kc), op0=ALU.mult, op1=ALU.add),
                      [tt.r, s2c.r, modT.r], [h2T.r])
                pl = pnext()
                for kc in range(8):
                    P(lambda: nc.tensor.matmul(pl.t[:, 0:36], lhsT=h2T.t[:, kc, :], rhs=wrf.t[:, kc, :], start=(kc == 0), stop=(kc == 7)),
                      [h2T.r, wrf.r], [pl.r])
                V(lambda: nc.vector.tensor_tensor(out=lgall.t[:, i, :], in0=pl.t[:, 0:36], in1=brb.t[:], op=ALU.add), [pl.r, brb.r], [lgall.r])

            for step in range(NOWN + 2):
                if step < NOWN:
                    S1(step)
                if 0 <= step - 1 < NOWN:
                    S2(step - 1)
                if 0 <= step - 2 < NOWN:
                    S3(step - 2)

            TT = NOWN
            Lg = lgall.t[:, :, 0:4]
            Le4 = lgall.t[:, :, 4:36].rearrange("p t (g e) -> p t g e", g=4)
            gm = rtb.t[:, 0, :]; se = rtb.t[:, 1, :]; pgr = rtb.t[:, 2, :]; mv1 = rtb.t[:, 3, :]; mv2 = rtb.t[:, 4, :]
            dd_ = rtb.t[:, 5, :]; ee_ = rtb.t[:, 6, :]; rr_ = rtb.t[:, 7, :]
            ex = uv.t[:, 0:TT * 4].rearrange("p (t g) -> p t g", g=4)
            pen3 = x2.t[:, 0:TT * 4].rearrange("p (t g) -> p t g", g=4)
            Mx3 = inner.t[:, 0:TT * 32].rearrange("p (t e) -> p t e", e=32)
            Mx4 = inner.t[:, 0:TT * 32].rearrange("p (t g e) -> p t g e", g=4, e=8)
            My3 = m1.t[:, 0:TT * 32].rearrange("p (t e) -> p t e", e=32)
            bc3 = lambda ap2, n: ap2.unsqueeze(2).to_broadcast([128, TT, n])
            V(lambda: nc.vector.tensor_reduce(out=gm, in_=Lg, axis=AX.X, op=ALU.max), [lgall.r], [rtb.r])
            V(lambda: nc.vector.tensor_tensor(out=ex, in0=Lg, in1=bc3(gm, 4), op=ALU.subtract), [lgall.r, rtb.r], [uv.r])
            A(lambda: nc.scalar.activation(out=ex, in_=ex, func=AF.Exp), [uv.r], [uv.r])
            V(lambda: nc.vector.tensor_reduce(out=se, in_=ex, axis=AX.X, op=ALU.add), [uv.r], [rtb.r])
            V(lambda: nc.vector.reciprocal(out=pgr, in_=se), [rtb.r], [rtb.r])
            V(lambda: nc.vector.tensor_tensor(out=pen3, in0=Lg, in1=bc3(gm, 4), op=ALU.is_equal), [lgall.r, rtb.r], [x2.r])
            V(lambda: nc.vector.tensor_scalar(out=pen3, in0=pen3, scalar1=-1.0, scalar2=1e30, op0=ALU.add, op1=ALU.mult),
              [x2.r], [x2.r])
            V(lambda: nc.vector.tensor_tensor(out=Mx4, in0=Le4, in1=pen3.unsqueeze(3).to_broadcast([128, TT, 4, 8]), op=ALU.add),
              [lgall.r, x2.r], [inner.r])
            V(lambda: nc.vector.tensor_reduce(out=mv1, in_=Mx3, axis=AX.X, op=ALU.max), [inner.r], [rtb.r])
            V(lambda: nc.vector.tensor_tensor(out=A0all.t[:], in0=Mx3, in1=bc3(mv1, 32), op=ALU.is_equal), [inner.r, rtb.r], [A0all.r])
            V(lambda: nc.vector.scalar_tensor_tensor(out=My3, in0=A0all.t[:], scalar=-1e30, in1=Mx3, op0=ALU.mult, op1=ALU.add),
              [A0all.r, inner.r], [m1.r])
            V(lambda: nc.vector.tensor_reduce(out=mv2, in_=My3, axis=AX.X, op=ALU.max), [m1.r], [rtb.r])
            V(lambda: nc.vector.tensor_tensor(out=A1all.t[:], in0=My3, in1=bc3(mv2, 32), op=ALU.is_equal), [m1.r, rtb.r], [A1all.r])
            V(lambda: nc.vector.tensor_tensor(out=Aall.t[:], in0=A0all.t[:], in1=A1all.t[:], op=ALU.add), [A0all.r, A1all.r], [Aall.r])
            V(lambda: nc.vector.tensor_tensor(out=dd_, in0=mv2, in1=mv1, op=ALU.subtract), [rtb.r], [rtb.r])
            A(lambda: nc.scalar.activation(out=ee_, in_=dd_, func=AF.Exp), [rtb.r], [rtb.r])
            V(lambda: nc.vector.tensor_scalar(out=ee_, in0=ee_, scalar1=1.0, scalar2=None, op0=ALU.add), [rtb.r], [rtb.r])
            V(lambda: nc.vector.reciprocal(out=rr_, in_=ee_), [rtb.r], [rtb.r])
            V(lambda: nc.vector.tensor_tensor(out=wts.t[:, :, 0], in0=rr_, in1=pgr, op=ALU.mult), [rtb.r], [wts.r])
            V(lambda: nc.vector.tensor_tensor(out=wts.t[:, :, 1], in0=pgr, in1=wts.t[:, :, 0], op=ALU.subtract), [rtb.r, wts.r], [wts.r])
            T.barrier()
            if stop <= 3:
                return nc
        pc_.close()

        NA = NOWN * 32
        REG_SLOT = nc.gpsimd.to_reg(NSLOT - 1)
        REG_W = nc.gpsimd.to_reg(NEXP * 128 - 1)
        d0i = sbt(es, "d0i", [128, NOWN], I32); d1i = sbt(es, "d1i", [128, NOWN], I32)
        idxw = sbt(es, "idxw", [128, NBLK], I32)
        with ExitStack() as dp:
            set_rot(range(8))
            Ab_t = Aall.t[:].rearrange("p t e -> p (t e)")
            lsf = sbt(dp, "lsf", [128, 128], F32, True); load(lsf, c_lstrict)
            lsb = sbt(dp, "lsb", [128, 128], BF16)
            thr = sbt(dp, "thr", [128, NBLK], F32, True); load(thr, c_thr)
            pidx = sbt(dp, "pidx", [128, 1], F32, True); load(pidx, c_pidx)
            V(lambda: nc.vector.tensor_copy(out=lsb.t[:], in_=lsf.t[:]), [lsf.r], [lsb.r])
            tsum = sbt(dp, "tsum", [128, NOWN, 32], F32)
            rloc = sbt(dp, "rloc", [128, NOWN, 32], F32)
            for c0 in range(0, NA, 512):
                w_ = min(512, NA - c0)
                ps = pnext()
                P(lambda: nc.tensor.matmul(ps.t[:, 0:w_], lhsT=onesb.t[:], rhs=Ab_t[:, c0:c0 + w_], start=True, stop=True),
                  [onesb.r, Aall.r], [ps.r])
                V(lambda: nc.vector.tensor_copy(out=tsum.t[:].rearrange("p t e -> p (t e)")[:, c0:c0 + w_], in_=ps.t[:, 0:w_]),
                  [ps.r], [tsum.r])
                ps2 = pnext()
                P(lambda: nc.tensor.matmul(ps2.t[:, 0:w_], lhsT=lsb.t[:], rhs=Ab_t[:, c0:c0 + w_], start=True, stop=True),
                  [lsb.r, Aall.r], [ps2.r])
                V(lambda: nc.vector.tensor_copy(out=rloc.t[:].rearrange("p t e -> p (t e)")[:, c0:c0 + w_], in_=ps2.t[:, 0:w_]),
                  [ps2.r], [rloc.r])
            cum = sbt(dp, "cum", [128, NOWN + 1, 32], F32)
            V(lambda: nc.vector.memset(cum.t[:, 0, :], 0.0), [], [cum.r])
            for t in range(NOWN):
                V(lambda: nc.vector.tensor_tensor(out=cum.t[:, t + 1, :], in0=cum.t[:, t, :], in1=tsum.t[:, t, :], op=ALU.add),
                  [cum.r, tsum.r], [cum.r])
            ci = sbt(dp, "ci", [128, 32], I32)
            pad = sbt(dp, "pad", [128, 32], F32)
            pend = sbt(dp, "pend", [128, 32], F32)
            pst = sbt(dp, "pst", [128, 32], F32)
            V(lambda: nc.vector.tensor_scalar(out=pad.t[:], in0=cum.t[:, NOWN, :], scalar1=float(BLK - 1), scalar2=None,
                                              op0=ALU.add), [cum.r], [pad.r])
            V(lambda: nc.vector.tensor_copy(out=ci.t[:], in_=pad.t[:]), [pad.r], [ci.r])
            V(lambda: nc.vector.tensor_scalar(out=ci.t[:], in0=ci.t[:], scalar1=8, scalar2=8, op0=ALU.arith_shift_right,
                                              op1=ALU.logical_shift_left), [ci.r], [ci.r])
            V(lambda: nc.vector.tensor_copy(out=pad.t[:], in_=ci.t[:]), [ci.r], [pad.r])
            V(lambda: nc.vector.tensor_tensor_scan(out=pend.t[:], data0=onesf.t[:, 0:32], data1=pad.t[:], initial=0.0,
                                                   op0=ALU.mult, op1=ALU.add), [onesf.r, pad.r], [pend.r])
            V(lambda: nc.vector.tensor_tensor(out=pst.t[:], in0=pend.t[:], in1=pad.t[:], op=ALU.subtract), [pend.r, pad.r], [pst.r])
            V(lambda: nc.vector.tensor_tensor(out=rloc.t[:], in0=rloc.t[:], in1=cum.t[:, 0:NOWN, :], op=ALU.add),
              [rloc.r, cum.r], [rloc.r])
            V(lambda: nc.vector.tensor_tensor(out=rloc.t[:], in0=rloc.t[:], in1=pst.t[:].unsqueeze(1).to_broadcast([128, NOWN, 32]),
                                              op=ALU.add), [rloc.r, pst.r], [rloc.r])
            df = sbt(dp, "df", [128, NOWN], F32)
            for Ak, dk in ((A0all, d0i), (A1all, d1i)):
                V(lambda: nc.vector.tensor_tensor(out=tsum.t[:], in0=Ak.t[:], in1=rloc.t[:], op=ALU.mult), [Ak.r, rloc.r, tsum.r], [tsum.r])
                V(lambda: nc.vector.tensor_reduce(out=df.t[:], in_=tsum.t[:], axis=AX.X, op=ALU.add), [tsum.r], [df.r])
                V(lambda: nc.vector.tensor_copy(out=dk.t[:], in_=df.t[:]), [df.r], [dk.r])
            cmp_ = sbt(dp, "cmp", [128, NBLK, 32], F32)
            bef = sbt(dp, "bef", [128, NBLK], F32)
            V(lambda: nc.vector.tensor_tensor(out=cmp_.t[:], in0=pend.t[:].unsqueeze(1).to_broadcast([128, NBLK, 32]),
                                              in1=thr.t[:].unsqueeze(2).to_broadcast([128, NBLK, 32]), op=ALU.is_le),
              [pend.r, thr.r], [cmp_.r])
            V(lambda: nc.vector.tensor_reduce(out=bef.t[:], in_=cmp_.t[:], axis=AX.X, op=ALU.add), [cmp_.r], [bef.r])
            V(lambda: nc.vector.tensor_scalar(out=bef.t[:], in0=bef.t[:], scalar1=float(NEXP - 1), scalar2=128.0, op0=ALU.min,
                                              op1=ALU.mult), [bef.r], [bef.r])
            V(lambda: nc.vector.tensor_scalar(out=bef.t[:], in0=bef.t[:], scalar1=pidx.t[:, 0:1], scalar2=None, op0=ALU.add),
              [bef.r, pidx.r], [bef.r])
            skp = sbt(dp, "skp", [128, NBLK], F32)
            V(lambda: nc.vector.tensor_scalar(out=skp.t[:], in0=thr.t[:], scalar1=pend.t[:, 31:32], scalar2=1.0e6, op0=ALU.is_ge,
                                              op1=ALU.mult), [thr.r, pend.r], [skp.r])
            V(lambda: nc.vector.tensor_tensor(out=bef.t[:], in0=bef.t[:], in1=skp.t[:], op=ALU.add), [bef.r, skp.r], [bef.r])
            V(lambda: nc.vector.tensor_copy(out=idxw.t[:], in_=bef.t[:]), [bef.r], [idxw.r])
            hb = [sbt(dp, f"hb{i}", [128, D], BF16, True) for i in range(3)]
            xs_res = Res("XS")
            for t in range(NOWN):
                b_ = hb[t % 3]
                load(b_, H2[t * 128:(t + 1) * 128, :])
                for dk in (d0i, d1i):
                    T.dma(POOL, b_.l, lambda: nc.gpsimd.indirect_dma_start(
                        out=XS, out_offset=bass.IndirectOffsetOnAxis(ap=dk.t[:, t:t + 1], axis=0), in_=b_.t[:], in_offset=None,
                        bounds_check=REG_SLOT, oob_is_err=False),
                        reads=[b_.r, dk.r])
            T.barrier()
            if stop <= 4:
                return nc

        with ExitStack() as dd:
            set_rot(range(8))
            stg = [[sbt(dd, f"stg{i}_{k}", [128, 4096], F32, True) for k in range(3)] for i in range(2)]
            w1b = [sbt(dd, f"w1b{i}", [128, 8, 4, 128], BF16) for i in range(2)]
            w3b = [sbt(dd, f"w3b{i}", [128, 8, 4, 128], BF16) for i in range(2)]
            w2b = [sbt(dd, f"w2b{i}", [128, 4, D], BF16) for i in range(2)]
            xbk = [sbt(dd, f"xbk{i}", [128, 2, D], BF16, True) for i in range(2)]
            xTb = [sbt(dd, f"xTb{i}", [128, 8, 256], BF16) for i in range(1)]
            sil = sbt(dd, "sil", [128, 256], F32)
            gTb = [sbt(dd, f"gTb{i}", [128, 4, 256], BF16) for i in range(2)]
            ysb = [sbt(dd, f"ysb{i}", [128, D], F32, True) for i in range(2)]
            yc = 0

            def issue_w(j):
                s_ = stg[j % 2]
                for k, wsrc in enumerate((w1, w3, w2)):
                    T.dma(POOL, s_[k].l, lambda: nc.gpsimd.indirect_dma_start(
                        out=s_[k].t[:], out_offset=None, in_=wsrc,
                        in_offset=bass.IndirectOffsetOnAxis(ap=idxw.t[:, j:j + 1], axis=0),
                        bounds_check=REG_W, oob_is_err=False),
                        reads=[idxw.r], writes=[s_[k].r])

            def issue_x(j):
                load(xbk[j % 2], XS[j * BLK:(j + 1) * BLK, :].rearrange("(a p) d -> p a d", p=128))

            issue_w(0)
            issue_x(0)
            for j in range(NBLK):
                if j + 1 < NBLK:
                    issue_w(j + 1)
                    issue_x(j + 1)
                s_ = stg[j % 2]; b1 = w1b[j % 2]; b3 = w3b[j % 2]; b2 = w2b[j % 2]
                V(lambda: nc.vector.tensor_copy(out=b1.t[:], in_=s_[0].t[:].rearrange("p (kc m fc) -> p kc fc m", kc=8, m=128, fc=4)),
                  [s_[0].r], [b1.r])
                V(lambda: nc.vector.tensor_copy(out=b3.t[:], in_=s_[1].t[:].rearrange("p (kc m fc) -> p kc fc m", kc=8, m=128, fc=4)),
                  [s_[1].r], [b3.r])
                A(lambda: nc.scalar.copy(out=b2.t[:].rearrange("p f n -> p (f n)"), in_=s_[2].t[:]), [s_[2].r], [b2.r])
                xk = xbk[j % 2]; xT_ = xTb[0]; gT_ = gTb[j % 2]
                for st_ in range(2):
                    tp = pnext(); tpb = tp.t[:].bitcast(BF16)
                    for kc in range(8):
                        P(lambda: nc.tensor.transpose(out=tpb[:, kc * 128:(kc + 1) * 128], in_=xk.t[:, st_, kc * 128:(kc + 1) * 128],
                                                      identity=identb.t[:]), [xk.r, identb.r], [tp.r])
                    cp = (lambda f: A(f, [tp.r], [xT_.r])) if st_ == 0 else (lambda f: V(f, [tp.r], [xT_.r]))
                    if st_ == 0:
                        A(lambda: nc.scalar.copy(out=xT_.t[:, :, 0:128], in_=tpb.rearrange("p (k t) -> p k t", k=8)), [tp.r], [xT_.r])
                    else:
                        V(lambda: nc.vector.tensor_copy(out=xT_.t[:, :, 128:256], in_=tpb.rearrange("p (k t) -> p k t", k=8)),
                          [tp.r], [xT_.r])
                for fc in range(4):
                    pa = pnext(); pb_ = pnext()
                    for kc in range(8):
                        P(lambda: nc.tensor.matmul(pa.t[:, 0:256], lhsT=b1.t[:, kc, fc, :], rhs=xT_.t[:, kc, :], start=(kc == 0),
                                                   stop=(kc == 7)), [b1.r, xT_.r], [pa.r])
                    for kc in range(8):
                        P(lambda: nc.tensor.matmul(pb_.t[:, 0:256], lhsT=b3.t[:, kc, fc, :], rhs=xT_.t[:, kc, :], start=(kc == 0),
                                                   stop=(kc == 7)), [b3.r, xT_.r], [pb_.r])
                    A(lambda: nc.scalar.activation(out=sil.t[:], in_=pa.t[:, 0:256], func=AF.Silu), [pa.r], [sil.r])
                    V(lambda: nc.vector.tensor_tensor(out=gT_.t[:, fc, :], in0=pb_.t[:, 0:256], in1=sil.t[:], op=ALU.mult),
                      [pb_.r, sil.r], [gT_.r])
                for st_ in range(2):
                    y_ = ysb[yc % 2]; yc += 1
                    for hh in range(2):
                        ps = pnext()
                        for fc in range(4):
                            P(lambda: nc.tensor.matmul(ps.t[:], lhsT=gT_.t[:, fc, st_ * 128:(st_ + 1) * 128],
                                                       rhs=b2.t[:, fc, hh * 512:(hh + 1) * 512], start=(fc == 0), stop=(fc == 3)),
                              [gT_.r, b2.r], [ps.r])
                        if hh == 0:
                            A(lambda: nc.scalar.copy(out=y_.t[:, 0:512], in_=ps.t[:]), [ps.r], [y_.r])
                        else:
                            V(lambda: nc.vector.tensor_copy(out=y_.t[:, 512:1024], in_=ps.t[:]), [ps.r], [y_.r])
                    r0 = j * BLK + st_ * 128
                    T.dma(SP, y_.l, lambda: nc.sync.dma_start(out=YS[r0:r0 + 128, :], in_=y_.t[:]), reads=[y_.r])
            T.barrier()
            if stop <= 5:
                return nc

        with ExitStack() as ee:
            set_rot(range(8))
            gfb = sbt(ee, "gfb", [128, D], F32, True); load(gfb, gf_row.partition_broadcast(128))
            y0 = [sbt(ee, f"y0_{i}", [128, D], F32, True) for i in range(2)]
            y1 = [sbt(ee, f"y1_{i}", [128, D], F32, True) for i in range(2)]
            xr = [sbt(ee, f"xr{i}", [128, D], F32, True) for i in range(2)]
            acc = sbt(ee, "acc", [128, D], F32)
            jk = sbt(ee, "jk", [128, D], F32)
            ob = [sbt(ee, f"ob{i}", [128, D], F32, True) for i in range(2)]
            st3 = sbt(ee, "st3", [128, 4], F32)
            def issue_e(t):
                a0 = y0[t % 2]; a1 = y1[t % 2]; xx = xr[t % 2]
                T.dma(POOL, a0.l, lambda: nc.gpsimd.indirect_dma_start(
                    out=a0.t[:], out_offset=None, in_=YS, in_offset=bass.IndirectOffsetOnAxis(ap=d0i.t[:, t:t + 1], axis=0),
                    bounds_check=REG_SLOT, oob_is_err=False),
                    reads=[d0i.r], writes=[a0.r])
                T.dma(POOL, a1.l, lambda: nc.gpsimd.indirect_dma_start(
                    out=a1.t[:], out_offset=None, in_=YS, in_offset=bass.IndirectOffsetOnAxis(ap=d1i.t[:, t:t + 1], axis=0),
                    bounds_check=REG_SLOT, oob_is_err=False),
                    reads=[d1i.r], writes=[a1.r])
                load(xx, X1[t * 128:(t + 1) * 128, :])

            issue_e(0)
            for t in range(NOWN):
                a0 = y0[t % 2]; a1 = y1[t % 2]; xx = xr[t % 2]; o_ = ob[t % 2]
                if t + 1 < NOWN:
                    issue_e(t + 1)
                V(lambda: nc.vector.tensor_scalar(out=acc.t[:], in0=a0.t[:], scalar1=wts.t[:, t, 0:1], scalar2=None, op0=ALU.mult),
                  [a0.r, wts.r], [acc.r])
                V(lambda: nc.vector.scalar_tensor_tensor(out=acc.t[:], in0=a1.t[:], scalar=wts.t[:, t, 1:2], in1=acc.t[:],
                                                         op0=ALU.mult, op1=ALU.add), [a1.r, wts.r, acc.r], [acc.r])
                V(lambda: nc.vector.tensor_tensor(out=acc.t[:], in0=acc.t[:], in1=gate2b.t[:], op=ALU.mult), [acc.r, gate2b.r], [acc.r])
                V(lambda: nc.vector.tensor_tensor(out=acc.t[:], in0=acc.t[:], in1=xx.t[:], op=ALU.add), [acc.r, xx.r], [acc.r])
                A(lambda: nc.scalar.activation(out=jk.t[:], in_=acc.t[:], func=AF.Square, accum_out=st3.t[:, 0:1]),
                  [acc.r], [jk.r, st3.r])
                V(lambda: nc.vector.tensor_scalar(out=st3.t[:, 1:2], in0=st3.t[:, 0:1], scalar1=1.0 / D, scalar2=EPS,
                                                  op0=ALU.mult, op1=ALU.add), [st3.r], [st3.r])
                A(lambda: nc.scalar.activation(out=st3.t[:, 2:3], in_=st3.t[:, 1:2], func=AF.Sqrt), [st3.r], [st3.r])
                V(lambda: nc.vector.reciprocal(out=st3.t[:, 3:4], in_=st3.t[:, 2:3]), [st3.r], [st3.r])
                V(lambda: nc.vector.scalar_tensor_tensor(out=o_.t[:], in0=acc.t[:], scalar=st3.t[:, 3:4], in1=gfb.t[:],
                                                         op0=ALU.mult, op1=ALU.mult), [acc.r, st3.r, gfb.r], [o_.r])
                T.dma(SP, o_.l, lambda: nc.sync.dma_start(out=out_d[t * 128:(t + 1) * 128, :], in_=o_.t[:]), reads=[o_.r])
            T.barrier()
    return nc


def _own_tiles(j, NP):
    t = []
    for m in range(NP):
        t += [8 * m + j, 8 * m + 7 - j]
    return t


def make_in_maps(inp, S):
    f32 = np.float32
    NT = S // 128; NP = NT // 8; NOWN = 2 * NP; TOK = NOWN * 128
    NBLK = (TOK * 2) // BLK + NEXP
    g = lambda k: np.asarray(inp[k])
    col = lambda v, n: np.ascontiguousarray(np.asarray(v, f32).reshape(n, 128).T)
    x = g("x"); c = g("c"); pos = g("positions")
    p = np.arange(128)
    fq = np.zeros((128, 1), f32)
    freqs = (np.float32(10000.0) ** (-np.arange(16, dtype=f32) / np.float32(16))).astype(f32)
    fq[64:96, 0] = np.tile(freqs, 2) / np.float32(2 * np.pi)
    shared = dict(
        w_ada=np.ascontiguousarray(g("w_ada")[0]), b_adaT=col(g("b_ada")[0], 48), b_ada_row=g("b_ada")[0].reshape(1, -1).astype(f32),
        g1col=col(g("norm1_g")[0], 8), g1row=g("norm1_g")[0].reshape(1, -1).astype(f32), w_in=np.ascontiguousarray(g("w_in")[0]),
        gqcol=col(g("q_norm_g")[0], 2), w_uq=np.ascontiguousarray(g("w_uq")[0]),
        gkvcol=col(g("kv_norm_g")[0], 1), w_ukv=np.ascontiguousarray(g("w_ukv")[0]),
        gv_row=g("v_norm_g")[0].reshape(1, -1).astype(f32), bv_row=g("v_norm_b")[0].reshape(1, -1).astype(f32),
        w_s=np.ascontiguousarray(g("w_s")[0]), bsT=np.ascontiguousarray(g("b_s")[0].T),
        w_br_mla=np.ascontiguousarray(g("w_br_mla")[0]), w_br_sgu=np.ascontiguousarray(g("w_br_sgu")[0]),
        w_out=np.ascontiguousarray(g("w_out")[0]),
        g2col=col(g("norm2_g")[0], 8), g2row=g("norm2_g")[0].reshape(1, -1).astype(f32),
        wr=np.ascontiguousarray(np.concatenate([g("w_rg")[0], g("w_re")[0]], axis=1)),
        br_row=np.concatenate([g("b_rg")[0], g("b_re")[0]]).reshape(1, -1).astype(f32),
        w1=np.ascontiguousarray(g("w1")[0]).reshape(NEXP * 128, 4096),
        w3=np.ascontiguousarray(g("w3")[0]).reshape(NEXP * 128, 4096),
        w2=np.ascontiguousarray(g("w2")[0]).reshape(NEXP * 128, 4096),
        gf_row=g("final_g").reshape(1, -1).astype(f32),
        c_ident=np.eye(128, dtype=f32), c_tril=np.tril(np.ones((128, 128), f32)),
        c_lstrict=(p[:, None] < p[None, :]).astype(f32), c_kq=(p[:, None] - p[None, :]).astype(f32),
        c_fq=fq, c_thr=np.tile((np.arange(NBLK, dtype=f32) * BLK)[None, :], (128, 1)).astype(f32),
        c_pidx=p.astype(f32).reshape(128, 1),
    )
    maps = []
    owns = []
    for core in range(8):
        b, j = core // 4, core % 4
        tiles = _own_tiles(j, NP)
        rows = np.concatenate([np.arange(t * 128, (t + 1) * 128) for t in tiles])
        owns.append((b, rows))
        dt_ = np.zeros((128, 16), f32)
        for o in range(8):
            dt_[:, o] = (j - o) * 128
            dt_[:, 8 + o] = (7 - j - o) * 128
        m = dict(shared)
        m.update(
            xb=np.ascontiguousarray(x[b, :S]), xo=np.ascontiguousarray(x[b][rows]),
            posb=np.ascontiguousarray(pos[b, :S].reshape(1, -1).astype(np.int32)),
            poso=np.ascontiguousarray(pos[b][rows].reshape(1, -1).astype(np.int32)),
            ccol=col(c[b], 8), c_dtab=dt_,
        )
        maps.append(m)
    return maps, owns


_NC_CACHE = {}


def kernel(**inputs):
    S = int(np.asarray(inputs["x"]).shape[1])
    if S not in _NC_CACHE:
        _NC_CACHE[S] = build_nc(S)
    nc = _NC_CACHE[S]
    maps, owns = make_in_maps(inputs, S)
    res = run_bass_kernel_spmd(nc, maps, core_ids=list(range(8)))
    B = np.asarray(inputs["x"]).shape[0]
    out = np.zeros((B, S, D), np.float32)
    for core, (b, rows) in enumerate(owns):
        out[b, rows] = np.asarray(res.results[core]["out"], dtype=np.float32)
    return out
```

```python
import numpy as np
from contextlib import ExitStack
import concourse.bass as bass
import concourse.mybir as mybir
from concourse.bass_utils import run_bass_kernel_spmd

F32 = mybir.dt.float32
BF16 = mybir.dt.bfloat16
I32 = mybir.dt.int32
AF = mybir.ActivationFunctionType
ALU = mybir.AluOpType
AX = mybir.AxisListType

D = 1024
NH = 8
EPS = 1e-6
NEXP = 32
FF = 512
BLK = 256
IN_W = 3488
SCALE = 96 ** -0.5
GELU_C = 1.5957691216057308


class Res:
    __slots__ = ("name", "w", "r")

    def __init__(self, name=""):
        self.name = name
        self.w = None
        self.r = {}


class Lane:
    def __init__(self, T, name):
        self.sem = T.newsem(name)
        self.count = 0


class Eng:
    def __init__(self, T, name, eng, is_pe=False):
        self.name = name
        self.eng = eng
        self.is_pe = is_pe
        self.sem = T.newsem(name + "_s0")
        self.own = [self.sem]
        self.count = 0
        self.waited = {}


class Tracker:
    CAP = 12000

    def __init__(self, nc, es):
        self.nc = nc
        self.es = es
        self.nsems = 0
        self.lanes = []
        self.pe = Eng(self, "pe", nc.tensor, True)
        self.act = Eng(self, "act", nc.scalar)
        self.dve = Eng(self, "dve", nc.vector)
        self.pool = Eng(self, "pool", nc.gpsimd)
        self.sp = Eng(self, "sp", nc.sync)
        self.engs = [self.pe, self.act, self.dve, self.pool, self.sp]

    def newsem(self, name):
        self.nsems += 1
        return self.es.enter_context(self.nc.semaphore(f"{name}_{self.nsems}"))

    def lane(self, name="l"):
        l = Lane(self, name)
        self.lanes.append(l)
        return l

    def _wait(self, E, deps):
        for sem, val in deps:
            if E.is_pe and any(sem is s for s in E.own):
                continue
            k = id(sem)
            if E.waited.get(k, 0) < val:
                E.eng.wait_ge(sem, val)
                E.waited[k] = val

    @staticmethod
    def _deps(reads, writes):
        deps = []
        for r in reads:
            if r.w is not None:
                deps.append(r.w)
        for w in writes:
            if w.w is not None:
                deps.append(w.w)
            deps.extend(w.r.values())
        return deps

    @staticmethod
    def _record(ev, reads, writes):
        for r in reads:
            r.r[id(ev[0])] = ev
        for w in writes:
            w.w = ev
            w.r = {}

    def op(self, E, fn, reads=(), writes=()):
        self._wait(E, self._deps(reads, writes))
        if E.count >= self.CAP:
            E.sem = self.newsem(E.name + "_s")
            E.own.append(E.sem)
            E.count = 0
        ins = fn()
        ins.then_inc(E.sem, 1)
        E.count += 1
        self._record((E.sem, E.count), reads, writes)

    def dma(self, E, lane, fn, reads=(), writes=()):
        self._wait(E, self._deps(reads, writes))
        ins = fn()
        ins.then_inc(lane.sem, 16)
        lane.count += 16
        self._record((lane.sem, lane.count), reads, writes)

    def barrier(self):
        evs = [(e.sem, e.count) for e in self.engs if e.count > 0]
        evs += [(l.sem, l.count) for l in self.lanes if l.count > 0]
        for E in self.engs:
            self._wait(E, evs)


class Buf:
    def __init__(self, T, tile, name, lane=False):
        self.t = tile
        self.r = Res(name)
        self.l = T.lane(name) if lane else None


def build_nc(S, stop=99):
    NT = S // 128
    NP = NT // 8
    NOWN = 2 * NP
    TOK = NOWN * 128
    NG = NT // 4
    NGO = max(NOWN // 4, 1)
    GW = min(512, TOK)
    NBLK = (TOK * 2) // BLK + NEXP
    NSLOT = NBLK * BLK

    nc = bass.Bass("TRN2", target_bir_lowering=False)

    def din(name, shape, dt=F32):
        return nc.dram_tensor(name, list(shape), dt, kind="ExternalInput").ap()

    xb = din("xb", [S, D]); xo = din("xo", [TOK, D])
    posb = din("posb", [1, S], I32); poso = din("poso", [1, TOK], I32)
    ccol = din("ccol", [128, 8])
    w_ada = din("w_ada", [D, 6 * D]); b_adaT = din("b_adaT", [128, 48]); b_ada_row = din("b_ada_row", [1, 6 * D])
    g1col = din("g1col", [128, 8]); g1row = din("g1row", [1, D]); w_in = din("w_in", [D, IN_W])
    gqcol = din("gqcol", [128, 2]); w_uq = din("w_uq", [256, 768])
    gkvcol = din("gkvcol", [128, 1]); w_ukv = din("w_ukv", [128, 1024])
    gv_row = din("gv_row", [1, 512]); bv_row = din("bv_row", [1, 512])
    w_s = din("w_s", [8, 128, 128]); bsT = din("bsT", [128, 8])
    w_br_mla = din("w_br_mla", [512, D]); w_br_sgu = din("w_br_sgu", [512, D]); w_out = din("w_out", [D, D])
    g2col = din("g2col", [128, 8]); g2row = din("g2row", [1, D])
    wr = din("wr", [D, 36]); br_row = din("br_row", [1, 36])
    w1 = din("w1", [NEXP * 128, 4096]); w3 = din("w3", [NEXP * 128, 4096]); w2 = din("w2", [NEXP * 128, 4096])
    gf_row = din("gf_row", [1, D])
    c_ident = din("c_ident", [128, 128]); c_tril = din("c_tril", [128, 128]); c_lstrict = din("c_lstrict", [128, 128])
    c_kq = din("c_kq", [128, 128]); c_fq = din("c_fq", [128, 1]); c_dtab = din("c_dtab", [128, 16])
    c_thr = din("c_thr", [128, NBLK]); c_pidx = din("c_pidx", [128, 1])

    out_d = nc.dram_tensor("out", [TOK, D], F32, kind="ExternalOutput").ap()
    X1 = nc.dram_tensor("X1s", [TOK, D], F32, kind="Internal").ap()
    H2 = nc.dram_tensor("H2s", [TOK, D], BF16, kind="Internal").ap()
    XS = nc.dram_tensor("XSs", [NSLOT, D], BF16, kind="Internal").ap()
    YS = nc.dram_tensor("YSs", [NSLOT, D], F32, kind="Internal").ap()
    OS = nc.dram_tensor("OSs", [TOK, 512], BF16, kind="Internal").ap()

    with ExitStack() as es:
        T = Tracker(nc, es)
        PE, ACT, DVE, POOL, SP = T.pe, T.act, T.dve, T.pool, T.sp

        def sbt(stack, name, shape, dt, lane=False):
            t = stack.enter_context(nc.sbuf_tensor(name, list(shape), dt))
            return Buf(T, t, name, lane)

        banks = []
        for i in range(8):
            t = es.enter_context(nc.psum_tensor(f"pb{i}", [128, 512], F32))
            banks.append(Buf(T, t, f"pb{i}"))
        rot = {"i": 0, "lst": list(range(8))}

        def pnext():
            b = banks[rot["lst"][rot["i"] % len(rot["lst"])]]
            rot["i"] += 1
            return b

        def set_rot(lst):
            rot["lst"] = list(lst)
            rot["i"] = 0

        def V(fn, reads=(), writes=()):
            T.op(DVE, fn, reads, writes)

        def A(fn, reads=(), writes=()):
            T.op(ACT, fn, reads, writes)

        def G(fn, reads=(), writes=()):
            T.op(POOL, fn, reads, writes)

        def P(fn, reads=(), writes=()):
            T.op(PE, fn, reads, writes)

        def load(buf, src, eng=None):
            E = SP if eng is None else eng
            T.dma(E, buf.l, lambda: E.eng.dma_start(out=buf.t[:], in_=src), writes=[buf.r])

        def load_to(buf, dst_ap, src, eng=None):
            E = SP if eng is None else eng
            T.dma(E, buf.l, lambda: E.eng.dma_start(out=dst_ap, in_=src), writes=[buf.r])

        identf = sbt(es, "identf", [128, 128], F32, True); load(identf, c_ident)
        identb = sbt(es, "identb", [128, 128], BF16)
        V(lambda: nc.vector.tensor_copy(out=identb.t[:], in_=identf.t[:]), [identf.r], [identb.r])
        onesb = sbt(es, "onesb", [128, 128], BF16)
        V(lambda: nc.vector.memset(onesb.t[:], 1.0), [], [onesb.r])
        onesf = sbt(es, "onesf", [128, 128], F32)
        V(lambda: nc.vector.memset(onesf.t[:], 1.0), [], [onesf.r])
        kqf = sbt(es, "kqf", [128, 128], F32, True); load(kqf, c_kq)
        kqb = sbt(es, "kqb", [128, 128], BF16)
        V(lambda: nc.vector.tensor_copy(out=kqb.t[:], in_=kqf.t[:]), [kqf.r], [kqb.r])
        fq = sbt(es, "fq", [128, 1], F32, True); load(fq, c_fq)
        dtab = sbt(es, "dtab", [128, 16], F32, True); load(dtab, c_dtab)
        g1c = sbt(es, "g1c", [128, 8], F32, True); load(g1c, g1col)
        g2c = sbt(es, "g2c", [128, 8], F32, True); load(g2c, g2col)
        gqc = sbt(es, "gqc", [128, 2], F32, True); load(gqc, gqcol)
        gkvc = sbt(es, "gkvc", [128, 1], F32, True); load(gkvc, gkvcol)
        cc = sbt(es, "cc", [128, 8], F32, True); load(cc, ccol)
        badT = sbt(es, "badT", [128, 48], F32, True); load(badT, b_adaT)
        modT = sbt(es, "modT", [128, 48], F32)
        gate1b = sbt(es, "gate1b", [128, D], F32)
        s2b = sbt(es, "s2b", [128, D], F32)
        sh2b = sbt(es, "sh2b", [128, D], F32)
        gate2b = sbt(es, "gate2b", [128, D], F32)
        s1b = sbt(es, "s1b", [128, D], F32)
        sh1b = sbt(es, "sh1b", [128, 8], BF16)
        ones512 = sbt(es, "ones512", [1, 512], BF16)
        V(lambda: nc.vector.memset(ones512.t[:], 1.0), [], [ones512.r])
        s1c = sbt(es, "s1c", [128, 8], F32); s2c = sbt(es, "s2c", [128, 8], F32)

        with ExitStack() as ph:
            sc = sbt(ph, "sc", [128, 8], F32)
            A(lambda: nc.scalar.activation(out=sc.t[:], in_=cc.t[:], func=AF.Silu), [cc.r], [sc.r])
            brow = [sbt(ph, f"brow{i}", [1, 512], F32, True) for i in range(2)]
            rowdst = {2: (s1b, 0), 3: (s1b, 512), 4: (gate1b, 0), 5: (gate1b, 512), 6: (sh2b, 0), 7: (sh2b, 512), 8: (s2b, 0), 9: (s2b, 512),
                      10: (gate2b, 0), 11: (gate2b, 512)}
            wp = [sbt(ph, f"wada{i}", [128, 8, 512], F32, True) for i in range(2)]
            pcol = banks[7]
            set_rot(range(7))
            rowtmp = sbt(ph, "rowtmp", [1, 512], F32)
            for pc in range(12):
                wb_ = wp[pc % 2]
                load(wb_, w_ada[:, pc * 512:(pc + 1) * 512].rearrange("(kc p) n -> p kc n", p=128))
                if pc in rowdst:
                    prow = pnext()
                    br_ = brow[pc % 2]
                    load(br_, b_ada_row[0:1, pc * 512:(pc + 1) * 512])
                    for kc in range(8):
                        P(lambda: nc.tensor.matmul(prow.t[0:1, :], lhsT=sc.t[:, kc:kc + 1], rhs=wb_.t[:, kc, :],
                                                   start=(kc == 0), stop=(kc == 7)), [sc.r, wb_.r], [prow.r])
                    V(lambda: nc.vector.tensor_tensor(out=rowtmp.t[0:1, :], in0=prow.t[0:1, :], in1=br_.t[0:1, :], op=ALU.add),
                      [prow.r, br_.r], [rowtmp.r])
                    pbc = pnext()
                    P(lambda: nc.tensor.matmul(pbc.t[:], lhsT=onesf.t[0:1, :], rhs=rowtmp.t[0:1, :], start=True, stop=True),
                      [onesf.r, rowtmp.r], [pbc.r])
                    dtile, dc0 = rowdst[pc]
                    V(lambda: nc.vector.tensor_copy(out=dtile.t[:, dc0:dc0 + 512], in_=pbc.t[:]), [pbc.r], [dtile.r])
                for oc in range(4):
                    col = pc * 4 + oc
                    if pc not in (0, 1, 6, 7, 8, 9):
                        continue
                    for kc in range(8):
                        P(lambda: nc.tensor.matmul(pcol.t[:, col:col + 1], lhsT=wb_.t[:, kc, oc * 128:(oc + 1) * 128],
                                                   rhs=sc.t[:, kc:kc + 1], start=(kc == 0), stop=(kc == 7)),
                          [sc.r, wb_.r], [pcol.r])
            V(lambda: nc.vector.memset(modT.t[:], 0.0), [], [modT.r])
            V(lambda: nc.vector.tensor_tensor(out=modT.t[:, 0:8], in0=pcol.t[:, 0:8], in1=badT.t[:, 0:8], op=ALU.add),
              [pcol.r, badT.r], [modT.r])
            V(lambda: nc.vector.tensor_tensor(out=modT.t[:, 24:40], in0=pcol.t[:, 24:40], in1=badT.t[:, 24:40], op=ALU.add),
              [pcol.r, badT.r], [modT.r])
            V(lambda: nc.vector.scalar_tensor_tensor(out=s2c.t[:], in0=modT.t[:, 32:40], scalar=1.0, in1=g2c.t[:],
                                                     op0=ALU.add, op1=ALU.mult), [modT.r, g2c.r], [s2c.r])
            g1rb = sbt(ph, "g1rb", [128, D], F32, True); load(g1rb, g1row.partition_broadcast(128))
            V(lambda: nc.vector.scalar_tensor_tensor(out=s1b.t[:], in0=s1b.t[:], scalar=1.0, in1=g1rb.t[:], op0=ALU.add,
                                                     op1=ALU.mult), [s1b.r, g1rb.r], [s1b.r])
            V(lambda: nc.vector.tensor_copy(out=sh1b.t[:], in_=modT.t[:, 0:8]), [modT.r], [sh1b.r])
            T.barrier()
        if stop <= 0:
            return nc
        sh1 = lambda kc: modT.t[:, kc:kc + 1]
        sh2 = lambda kc: modT.t[:, 24 + kc:25 + kc]

        FE = {}
        cnt = {"x": 0, "fe": 0, "st": 0, "xn": 0, "cp": 0}

        def make_fe(stack, nx=3, nxn=2):
            k = cnt["fe"]; cnt["fe"] += 1
            FE["nx"] = nx
            FE["xts"] = [sbt(stack, f"xt{k}_{i}", [128, D], F32, True) for i in range(nx)]
            FE["junk"] = sbt(stack, f"junk{k}", [128, D], BF16)
            FE["stat"] = [sbt(stack, f"stat{k}_{i}", [128, 16], F32) for i in range(3)]
            FE["xns"] = [sbt(stack, f"xn{k}_{i}", [128, D], BF16) for i in range(nxn)]

        def fe_a(tile_aps):
            n = len(tile_aps)
            st = FE["stat"][cnt["st"] % 3]; cnt["st"] += 1
            junk = FE["junk"]
            xl = []
            for j in range(n):
                xt = FE["xts"][cnt["x"] % FE["nx"]]; cnt["x"] += 1
                load(xt, tile_aps[j])
                xl.append(xt)
            for j in range(n):
                A(lambda: nc.scalar.activation(out=junk.t[:], in_=xl[j].t[:], func=AF.Square, accum_out=st.t[:, j:j + 1]),
                  [xl[j].r], [junk.r, st.r])
            V(lambda: nc.vector.tensor_scalar(out=st.t[:, 4:4 + n], in0=st.t[:, 0:n], scalar1=1.0 / D, scalar2=EPS,
                                              op0=ALU.mult, op1=ALU.add), [st.r], [st.r])
            A(lambda: nc.scalar.activation(out=st.t[:, 8:8 + n], in_=st.t[:, 4:4 + n], func=AF.Sqrt), [st.r], [st.r])
            V(lambda: nc.vector.reciprocal(out=st.t[:, 12:12 + n], in_=st.t[:, 8:8 + n]), [st.r], [st.r])
            xnl = []
            for j in range(n):
                xn = FE["xns"][cnt["xn"] % len(FE["xns"])]; cnt["xn"] += 1
                V(lambda: nc.vector.scalar_tensor_tensor(out=xn.t[:], in0=xl[j].t[:], scalar=st.t[:, 12 + j:13 + j], in1=s1b.t[:],
                                                         op0=ALU.mult, op1=ALU.mult), [xl[j].r, st.r, s1b.r], [xn.r])
                xnl.append(xn)
            return xl, xnl

        def fe_b(state, dst, dst_res, c0s):
            xl, xnl = state
            for j, xn in enumerate(xnl):
                tp = pnext()
                tpb = tp.t[:].bitcast(BF16)
                for kc in range(8):
                    P(lambda: nc.tensor.transpose(out=tpb[:, kc * 128:(kc + 1) * 128], in_=xn.t[:, kc * 128:(kc + 1) * 128],
                                                  identity=identb.t[:]), [xn.r, identb.r], [tp.r])
                c0 = c0s[j]
                src3 = tpb.rearrange("p (k t) -> p k t", k=8)
                if cnt["cp"] % 2 == 0:
                    A(lambda: nc.scalar.copy(out=dst[:, :, c0:c0 + 128], in_=src3), [tp.r], [dst_res])
                else:
                    V(lambda: nc.vector.tensor_copy(out=dst[:, :, c0:c0 + 128], in_=src3), [tp.r], [dst_res])
                cnt["cp"] += 1
            return xl

        def front_end_multi(tile_aps, dst, dst_res, c0s):
            return fe_b(fe_a(tile_aps), dst, dst_res, c0s)

        def bias_row(dst_ap, wfn, M, wres, dres):
            ps = pnext()
            for kc in range(8):
                P(lambda: nc.tensor.matmul(ps.t[0:1, 0:M], lhsT=sh1b.t[:, kc:kc + 1], rhs=wfn(kc), start=(kc == 0), stop=(kc == 7)),
                  [sh1b.r, wres], [ps.r])
            V(lambda: nc.vector.tensor_copy(out=dst_ap, in_=ps.t[0:1, 0:M]), [ps.r], [dres])

        s1col = lambda kc: s1c.t[:, kc:kc + 1]

        Aall = sbt(es, "Aall", [128, NOWN, 32], BF16)
        A0all = sbt(es, "A0all", [128, NOWN, 32], BF16)
        A1all = sbt(es, "A1all", [128, NOWN, 32], BF16)
        wts = sbt(es, "wts", [128, NOWN, 2], F32)
        att = es.enter_context(ExitStack())
        ckvnT = sbt(att, "ckvnT", [128, S], BF16); ckvn_r = [Res() for _ in range(NG)]
        KT = sbt(att, "KT", [128, S], BF16); ktn_r = [Res() for _ in range(NG)]; ktr_r = [Res() for _ in range(NG)]
        vh_r = [Res() for _ in range(NG)]
        cqnT = sbt(att, "cqnT", [128, 2, TOK], BF16); cqn_r = [Res() for _ in range(NGO)]
        qT_r = [Res() for _ in range(NGO)]
        CQ = sbt(att, "CQ", [128, TOK], BF16); SQ = sbt(att, "SQ", [128, TOK], BF16)
        cs_r = [Res() for _ in range(NGO)]

        def rope_tables(pos_ap_512, Cdst, Sdst, wres, lp, width):
            pi_ = lp["posi"]; R = slice(64, 96)
            T.dma(SP, pi_.l, lambda: nc.sync.dma_start(out=pi_.t[R, 0:width], in_=pos_ap_512.partition_broadcast(32)),
                  writes=[pi_.r])
            pf = lp["pf"]; u = lp["u"]; ki = lp["ki"]; kf = lp["kf"]; f2 = lp["f2"]
            V(lambda: nc.vector.tensor_copy(out=pf.t[R, 0:width], in_=pi_.t[R, 0:width]), [pi_.r], [pf.r])
            for which, off, dstb in ((0, 0.5, Sdst), (1, 0.75, Cdst)):
                V(lambda: nc.vector.tensor_scalar(out=u.t[R, 0:width], in0=pf.t[R, 0:width], scalar1=fq.t[R, 0:1],
                                                  scalar2=off, op0=ALU.mult, op1=ALU.add), [pf.r, fq.r], [u.r])
                V(lambda: nc.vector.tensor_copy(out=ki.t[R, 0:width], in_=u.t[R, 0:width]), [u.r], [ki.r])
                V(lambda: nc.vector.tensor_copy(out=kf.t[R, 0:width], in_=ki.t[R, 0:width]), [ki.r], [kf.r])
                V(lambda: nc.vector.scalar_tensor_tensor(out=f2.t[R, 0:width], in0=u.t[R, 0:width], scalar=-0.5,
                                                         in1=kf.t[R, 0:width], op0=ALU.add, op1=ALU.subtract),
                  [u.r, kf.r], [f2.r])
                V(lambda: nc.vector.scalar_tensor_tensor(out=u.t[R, 0:width], in0=f2.t[R, 0:width], scalar=-0.5,
                                                         in1=f2.t[R, 0:width], op0=ALU.is_lt, op1=ALU.add),
                  [f2.r], [u.r])
                A(lambda: nc.scalar.activation(out=dstb, in_=u.t[R, 0:width], func=AF.Sin, scale=6.28318),
                  [u.r], [wres])

        with ExitStack() as lp_:
            make_fe(lp_, 4, 4)
            lp = {}
            lp["posi"] = sbt(lp_, "posi", [128, 512], I32, True)
            for nm in ("pf", "u", "kf", "f2"):
                lp[nm] = sbt(lp_, "lp_" + nm, [128, 512], F32)
            lp["t1"] = lp["pf"]; lp["t2"] = lp["kf"]
            lp["ki"] = sbt(lp_, "lp_ki", [128, 512], I32)
            Ctmp = sbt(lp_, "Ctmp", [128, 512], F32); Stmp = sbt(lp_, "Stmp", [128, 512], F32)
            h1Ts = [sbt(lp_, f"h1T{i}", [128, 8, 512], BF16) for i in range(2)]
            sq = sbt(lp_, "sq", [128, 2, 512], BF16)
            rs = sbt(lp_, "rs", [128, 512], F32); rs2 = sbt(lp_, "rs2", [128, 512], F32)
            wkv = sbt(lp_, "wkv", [128, 8, 128], BF16, True)
            wkr = sbt(lp_, "wkr", [128, 8, 96], BF16, True)
            wkrr = sbt(lp_, "wkrr", [128, 8, 96], BF16)
            wq = sbt(lp_, "wq", [128, 8, 256], BF16, True)
            win_v = w_in.rearrange("(kc p) n -> p kc n", p=128)
            load(wkv, win_v[:, :, 256:384], POOL)
            V(lambda: nc.vector.memset(wkr.t[:], 0.0), [], [wkr.r])
            V(lambda: nc.vector.memset(wkrr.t[:], 0.0), [], [wkrr.r])
            load_to(wkr, wkr.t[:, :, 64:96], win_v[:, :, 384:416], POOL)
            load(wq, win_v[:, :, 0:256], POOL)
            V(lambda: nc.vector.tensor_scalar(out=wkrr.t[:, :, 64:80], in0=wkr.t[:, :, 80:96], scalar1=-1.0, scalar2=None,
                                              op0=ALU.mult), [wkr.r, wkrr.r], [wkrr.r])
            V(lambda: nc.vector.tensor_copy(out=wkrr.t[:, :, 80:96], in_=wkr.t[:, :, 64:80]), [wkr.r, wkrr.r], [wkrr.r])
            set_rot(range(8))
            brows = sbt(lp_, "brows", [1, 640], BF16)
            bias_row(brows.t[0:1, 0:128], lambda kc: wkv.t[:, kc, :], 128, wkv.r, brows.r)
            bias_row(brows.t[0:1, 128:224], lambda kc: wkr.t[:, kc, :], 96, wkr.r, brows.r)
            bias_row(brows.t[0:1, 224:320], lambda kc: wkrr.t[:, kc, :], 96, wkrr.r, brows.r)
            bias_row(brows.t[0:1, 320:576], lambda kc: wq.t[:, kc, :], 256, wq.r, brows.r)
            hcnt = {"i": 0}
            h1_of = {}

            fe_state = {}

            def latent_fe_a(mode, g, width):
                ntl = width // 128
                src = xb if mode == "k" else xo
                fe_state[(mode, g)] = fe_a([src[(g * 4 + tl) * 128:(g * 4 + tl + 1) * 128, :] for tl in range(ntl)])

            def latent_fe_b(mode, g, width):
                ntl = width // 128
                h1 = h1Ts[hcnt["i"] % 2]; hcnt["i"] += 1
                h1_of[(mode, g)] = h1
                fe_b(fe_state.pop((mode, g)), h1.t, h1.r, [tl * 128 for tl in range(ntl)])

            mm_out = {}

            def latent_mm(mode, g, width):
                h1 = h1_of.pop((mode, g))
                W = slice(0, width)
                if mode == "k":
                    pk = pnext(); pr = pnext(); prr = pnext()
                    for kc in range(8):
                        P(lambda: nc.tensor.matmul(pk.t[:, W], lhsT=wkv.t[:, kc, :], rhs=h1.t[:, kc, W],
                                                   start=(kc == 0), stop=False), [wkv.r, h1.r], [pk.r])
                    P(lambda: nc.tensor.matmul(pk.t[:, W], lhsT=brows.t[0:1, 0:128], rhs=ones512.t[0:1, W], start=False, stop=True),
                      [brows.r, ones512.r], [pk.r])
                    for kc in range(8):
                        P(lambda: nc.tensor.matmul(pr.t[0:96, W], lhsT=wkr.t[:, kc, :], rhs=h1.t[:, kc, W],
                                                   start=(kc == 0), stop=False), [wkr.r, h1.r], [pr.r])
                    P(lambda: nc.tensor.matmul(pr.t[0:96, W], lhsT=brows.t[0:1, 128:224], rhs=ones512.t[0:1, W], start=False, stop=True),
                      [brows.r, ones512.r], [pr.r])
                    for kc in range(8):
                        P(lambda: nc.tensor.matmul(prr.t[0:96, W], lhsT=wkrr.t[:, kc, :], rhs=h1.t[:, kc, W],
                                                   start=(kc == 0), stop=False), [wkrr.r, h1.r], [prr.r])
                    P(lambda: nc.tensor.matmul(prr.t[0:96, W], lhsT=brows.t[0:1, 224:320], rhs=ones512.t[0:1, W], start=False, stop=True),
                      [brows.r, ones512.r], [prr.r])
                    chunks = [pk]
                    gcol = [gkvc.t[:, 0:1]]
                    nfeat = 128
                else:
                    p0 = pnext(); p1 = pnext()
                    for ci, pp in enumerate((p0, p1)):
                        for kc in range(8):
                            P(lambda: nc.tensor.matmul(pp.t[:, W], lhsT=wq.t[:, kc, ci * 128:(ci + 1) * 128],
                                                       rhs=h1.t[:, kc, W], start=(kc == 0), stop=False),
                              [wq.r, h1.r], [pp.r])
                        P(lambda: nc.tensor.matmul(pp.t[:, W], lhsT=brows.t[0:1, 320 + ci * 128:320 + (ci + 1) * 128],
                                                   rhs=ones512.t[0:1, W], start=False, stop=True), [brows.r, ones512.r], [pp.r])
                    chunks = [p0, p1]
                    gcol = [gqc.t[:, 0:1], gqc.t[:, 1:2]]
                    nfeat = 256
                mm_out[(mode, g)] = (chunks, gcol, nfeat, (pr, prr) if mode == "k" else None)

            def latent_rest(mode, g, width):
                W = slice(0, width)
                chunks, gcol, nfeat, prs = mm_out.pop((mode, g))
                if prs is not None:
                    pr, prr = prs
                for ci, pp in enumerate(chunks):
                    A(lambda: nc.scalar.activation(out=sq.t[:, ci, W], in_=pp.t[:, W], func=AF.Square), [pp.r], [sq.r])
                pss = pnext()
                for ci in range(len(chunks)):
                    P(lambda: nc.tensor.matmul(pss.t[:, W], lhsT=onesb.t[:], rhs=sq.t[:, ci, W], start=(ci == 0),
                                               stop=(ci == len(chunks) - 1)), [onesb.r, sq.r], [pss.r])
                V(lambda: nc.vector.tensor_scalar(out=rs.t[:, W], in0=pss.t[:, W], scalar1=1.0 / nfeat, scalar2=EPS,
                                                  op0=ALU.mult, op1=ALU.add), [pss.r], [rs.r])
                A(lambda: nc.scalar.activation(out=rs2.t[:, W], in_=rs.t[:, W], func=AF.Sqrt), [rs.r], [rs2.r])
                V(lambda: nc.vector.reciprocal(out=rs.t[:, W], in_=rs2.t[:, W]), [rs2.r], [rs.r])
                c0 = g * 512
                for ci, pp in enumerate(chunks):
                    if mode == "k":
                        dst = ckvnT.t[:, c0:c0 + width]; dres = ckvn_r[g]
                    else:
                        dst = cqnT.t[:, ci, c0:c0 + width]; dres = cqn_r[g]
                    V(lambda: nc.vector.scalar_tensor_tensor(out=dst, in0=pp.t[:, W], scalar=gcol[ci], in1=rs.t[:, W],
                                                             op0=ALU.mult, op1=ALU.mult),
                      [pp.r, rs.r, gkvc.r, gqc.r], [dres])
                R = slice(64, 96)
                if mode == "k":
                    rope_tables(posb[0:1, c0:c0 + width], Ctmp.t[R, W], Stmp.t[R, W], Ctmp.r, lp, width)
                    t1 = lp["t1"]; t2 = lp["t2"]
                    V(lambda: nc.vector.tensor_tensor(out=t1.t[R, W], in0=pr.t[R, W], in1=Ctmp.t[R, W], op=ALU.mult),
                      [pr.r, Ctmp.r], [t1.r])
                    V(lambda: nc.vector.tensor_tensor(out=t2.t[R, W], in0=prr.t[R, W], in1=Stmp.t[R, W], op=ALU.mult),
                      [prr.r, Ctmp.r], [t2.r])
                    V(lambda: nc.vector.tensor_tensor(out=KT.t[R, c0:c0 + width], in0=t1.t[R, W], in1=t2.t[R, W],
                                                      op=ALU.add), [t1.r, t2.r], [ktr_r[g]])
                else:
                    rope_tables(poso[0:1, c0:c0 + width], CQ.t[R, c0:c0 + width], SQ.t[R, c0:c0 + width], cs_r[g], lp, width)

            work = [("k", g, 512) for g in range(NG)] + [("q", g, GW) for g in range(NGO)]
            latent_fe_a(*work[0]); latent_fe_b(*work[0])
            for wi, w_ in enumerate(work):
                nxt = work[wi + 1] if wi + 1 < len(work) else None
                if nxt:
                    latent_fe_a(*nxt)
                latent_mm(*w_)
                if nxt:
                    latent_fe_b(*nxt)
                latent_rest(*w_)
            T.barrier()
            if stop <= 1:
                return nc

        with ExitStack() as ap_:
            qT = sbt(ap_, "qT", [128, TOK], BF16)
            Vh = sbt(ap_, "Vh", [128, NT, 65], BF16)
            V(lambda: nc.vector.memset(Vh.t[:, :, 64:65], 1.0), [], vh_r)
            wuqb = sbt(ap_, "wuqb", [128, 2, 768], BF16, True)
            wuqr = sbt(ap_, "wuqr", [128, 2, NH, 96], BF16)
            wukvb = sbt(ap_, "wukvb", [128, 1024], BF16, True)
            load(wuqb, w_uq.rearrange("(kc p) n -> p kc n", p=128), POOL)
            load(wukvb, w_ukv, POOL)
            V(lambda: nc.vector.memset(wuqr.t[:], 0.0), [], [wuqr.r])
            wv = wuqb.t[:].rearrange("p k (h d) -> p k h d", h=NH)
            V(lambda: nc.vector.tensor_scalar(out=wuqr.t[:, :, :, 64:80], in0=wv[:, :, :, 80:96], scalar1=-1.0, scalar2=None,
                                              op0=ALU.mult), [wuqb.r, wuqr.r], [wuqr.r])
            V(lambda: nc.vector.tensor_copy(out=wuqr.t[:, :, :, 80:96], in_=wv[:, :, :, 64:80]), [wuqb.r, wuqr.r], [wuqr.r])

            PTs = [sbt(ap_, f"PT{i}", [128, 512], BF16) for i in range(8)]
            rec = [sbt(ap_, f"rec{i}", [128, 1], F32) for i in range(4)]
            ost = [sbt(ap_, f"ost{i}", [128, 64], BF16, True) for i in range(4)]
            osc = [0]
            t1q = sbt(ap_, "t1q", [128, 512], F32); t2q = sbt(ap_, "t2q", [128, 512], F32)
            set_rot([4, 5, 6, 7])
            oacc_i = 0
            ptc = 0
            for h in range(NH):
                for g in range(NG):
                    ps = pnext()
                    P(lambda: nc.tensor.matmul(ps.t[0:64, :], lhsT=wukvb.t[:, h * 128:h * 128 + 64],
                                               rhs=ckvnT.t[:, g * 512:(g + 1) * 512], start=True, stop=True),
                      [wukvb.r, ckvn_r[g]], [ps.r])
                    if g % 2 == 0:
                        V(lambda: nc.vector.tensor_copy(out=KT.t[0:64, g * 512:(g + 1) * 512], in_=ps.t[0:64, :]),
                          [ps.r], [ktn_r[g]])
                    else:
                        A(lambda: nc.scalar.copy(out=KT.t[0:64, g * 512:(g + 1) * 512], in_=ps.t[0:64, :]),
                          [ps.r], [ktn_r[g]])
                for g8 in range(NT // 8):
                    ps = pnext()
                    for k8 in range(8):
                        kt = g8 * 8 + k8
                        P(lambda: nc.tensor.matmul(ps.t[:, k8 * 64:(k8 + 1) * 64], lhsT=ckvnT.t[:, kt * 128:(kt + 1) * 128],
                                                   rhs=wukvb.t[:, h * 128 + 64:h * 128 + 128], start=True, stop=True),
                          [wukvb.r, ckvn_r[kt // 4]], [ps.r])
                    wr_ = [vh_r[2 * g8], vh_r[2 * g8 + 1]]
                    src3 = ps.t[:].rearrange("p (a d) -> p a d", a=8)
                    if g8 % 2 == 0:
                        A(lambda: nc.scalar.copy(out=Vh.t[:, g8 * 8:(g8 + 1) * 8, 0:64], in_=src3), [ps.r], wr_)
                    else:
                        V(lambda: nc.vector.tensor_copy(out=Vh.t[:, g8 * 8:(g8 + 1) * 8, 0:64], in_=src3), [ps.r], wr_)
                for g in range(NGO):
                    W = slice(0, GW); c0 = g * 512; R = slice(64, 96)
                    pa = pnext(); pr = pnext()
                    for kc in range(2):
                        P(lambda: nc.tensor.matmul(pa.t[0:96, W], lhsT=wuqb.t[:, kc, h * 96:(h + 1) * 96],
                                                   rhs=cqnT.t[:, kc, c0:c0 + GW], start=(kc == 0), stop=(kc == 1)),
                          [wuqb.r, cqn_r[g]], [pa.r])
                    for kc in range(2):
                        P(lambda: nc.tensor.matmul(pr.t[0:96, W], lhsT=wuqr.t[:, kc, h, :],
                                                   rhs=cqnT.t[:, kc, c0:c0 + GW], start=(kc == 0), stop=(kc == 1)),
                          [wuqr.r, cqn_r[g]], [pr.r])
                    V(lambda: nc.vector.tensor_copy(out=qT.t[0:64, c0:c0 + GW], in_=pa.t[0:64, W]), [pa.r], [qT_r[g]])
                    V(lambda: nc.vector.tensor_tensor(out=t1q.t[R, W], in0=pa.t[R, W], in1=CQ.t[R, c0:c0 + GW], op=ALU.mult),
                      [pa.r, cs_r[g]], [t1q.r])
                    V(lambda: nc.vector.tensor_tensor(out=t2q.t[R, W], in0=pr.t[R, W], in1=SQ.t[R, c0:c0 + GW], op=ALU.mult),
                      [pr.r, cs_r[g]], [t2q.r])
                    V(lambda: nc.vector.tensor_tensor(out=qT.t[R, c0:c0 + GW], in0=t1q.t[R, W], in1=t2q.t[R, W], op=ALU.add),
                      [t1q.r, t2q.r], [qT_r[g]])
                steps = [(m, kp) for m in range(NP) for kp in range((8 * m + 8) // 2)]
                oa_of = {}
                for m in range(NP):
                    oa_of[m] = [banks[(oacc_i * 2) % 4], banks[(oacc_i * 2 + 1) % 4]]
                    oacc_i += 1
                pt_of = {}

                def emit_qk(si):
                    m, kp = steps[si]
                    qg = qT_r[(m * 256) // 512]
                    qs = qT.t[0:96, m * 256:(m + 1) * 256]
                    ps = pnext()
                    for kl in range(2):
                        kt = 2 * kp + kl
                        P(lambda: nc.tensor.matmul(ps.t[:, kl * 256:(kl + 1) * 256], lhsT=KT.t[0:96, kt * 128:(kt + 1) * 128],
                                                   rhs=qs, start=True, stop=True),
                          [ktn_r[kt // 4], ktr_r[kt // 4], qg], [ps.r])
                    PT = PTs[si % len(PTs)]
                    pt_of[si] = PT
                    A(lambda: nc.scalar.activation(out=PT.t[:], in_=ps.t[:], func=AF.Exp, scale=SCALE), [ps.r], [PT.r])
                    for kl in range(2):
                        kt = 2 * kp + kl
                        o = kt - 8 * m
                        if o >= 0:
                            for a in range(2):
                                cs_ = kl * 256 + a * 128
                                V(lambda: nc.vector.scalar_tensor_tensor(
                                    out=PT.t[:, cs_:cs_ + 128], in0=kqb.t[:], scalar=dtab.t[:, a * 8 + o:a * 8 + o + 1],
                                    in1=PT.t[:, cs_:cs_ + 128], op0=ALU.is_le, op1=ALU.mult),
                                  [PT.r, kqb.r, dtab.r], [PT.r])

                def emit_pv(si):
                    nonlocal_osc = None
                    m, kp = steps[si]
                    nk = 8 * m + 8
                    oa = oa_of[m]
                    PT = pt_of.pop(si)
                    for kl in range(2):
                        kt = 2 * kp + kl
                        for a in range(2):
                            cs_ = kl * 256 + a * 128
                            P(lambda: nc.tensor.matmul(oa[a].t[:, 0:65], lhsT=PT.t[:, cs_:cs_ + 128], rhs=Vh.t[:, kt, :],
                                                       start=(kt == 0), stop=(kt == nk - 1)),
                              [PT.r, vh_r[kt // 4]], [oa[a].r])
                    if kp == nk // 2 - 1:
                        for a in range(2):
                            i_own = 2 * m + a
                            rc = rec[(2 * m + a) % 4]
                            V(lambda: nc.vector.reciprocal(out=rc.t[:], in_=oa[a].t[:, 64:65]), [oa[a].r], [rc.r])
                            os_ = ost[osc[0] % 4]; osc[0] += 1
                            V(lambda: nc.vector.tensor_scalar(out=os_.t[:], in0=oa[a].t[:, 0:64],
                                                              scalar1=rc.t[:, 0:1], scalar2=None, op0=ALU.mult),
                              [oa[a].r, rc.r], [os_.r])
                            T.dma(SP, os_.l, lambda: nc.sync.dma_start(out=OS[i_own * 128:(i_own + 1) * 128, h * 64:(h + 1) * 64],
                                                                       in_=os_.t[:]), reads=[os_.r])

                LA = 4
                for si in range(len(steps) + LA):
                    if si < len(steps):
                        emit_qk(si)
                    if si - LA >= 0:
                        emit_pv(si - LA)
            T.barrier()
            if stop <= 2:
                return nc
        att.close()

        pc_ = es.enter_context(ExitStack())
        set_rot(range(8))
        win_v = w_in.rearrange("(kc p) n -> p kc n", p=128)
        wsg = sbt(pc_, "wsg", [128, 8, 1024], BF16, True); load(wsg, win_v[:, :, 416:1440], POOL)
        wgl = sbt(pc_, "wgl", [128, 8, 2048], BF16, True)
        for kc in range(8):
            load_to(wgl, wgl.t[:, kc, :], win_v[:, kc, 1440:3488], POOL)
        wbm = sbt(pc_, "wbm", [128, 4, D], BF16, True); load(wbm, w_br_mla.rearrange("(kc p) n -> p kc n", p=128), POOL)
        wbs = sbt(pc_, "wbs", [128, 4, D], BF16, True); load(wbs, w_br_sgu.rearrange("(kc p) n -> p kc n", p=128), POOL)
        wo = sbt(pc_, "wo", [128, 8, D], BF16, True); load(wo, w_out.rearrange("(kc p) n -> p kc n", p=128), POOL)
        wrf = sbt(pc_, "wrf", [128, 8, 36], F32, True); load(wrf, wr.rearrange("(kc p) n -> p kc n", p=128))
        brb = sbt(pc_, "brb", [128, 36], F32, True); load(brb, br_row.partition_broadcast(128))
        gvb = sbt(pc_, "gvb", [128, 512], F32, True); load(gvb, gv_row.partition_broadcast(128))
        bvb = sbt(pc_, "bvb", [128, 512], F32, True); load(bvb, bv_row.partition_broadcast(128))
        bst = sbt(pc_, "bst", [128, 8], F32, True); load(bst, bsT)
        make_fe(pc_, 3)
        browc = sbt(pc_, "browc", [1, 3072], BF16)
        for q_ in range(2):
            bias_row(browc.t[0:1, q_ * 512:(q_ + 1) * 512], lambda kc: wsg.t[:, kc, q_ * 512:(q_ + 1) * 512], 512, wsg.r, browc.r)
        for q_ in range(4):
            bias_row(browc.t[0:1, 1024 + q_ * 512:1024 + (q_ + 1) * 512], lambda kc: wgl.t[:, kc, q_ * 512:(q_ + 1) * 512], 512,
                     wgl.r, browc.r)
        wsT = sbt(pc_, "wsT", [128, 8, 128], BF16)
        with ExitStack() as tmp_:
            g2rb = sbt(tmp_, "g2rb", [128, D], F32, True); load(g2rb, g2row.partition_broadcast(128))
            V(lambda: nc.vector.scalar_tensor_tensor(out=s2b.t[:], in0=s2b.t[:], scalar=1.0, in1=g2rb.t[:], op0=ALU.add,
                                                     op1=ALU.mult), [s2b.r, g2rb.r], [s2b.r])
            wsf = sbt(tmp_, "wsf", [128, 8, 128], F32, True); load(wsf, w_s.rearrange("g t s -> t g s"))
            trl = sbt(tmp_, "trl", [128, 128], F32, True); load(trl, c_tril)
            wsm = sbt(tmp_, "wsm", [128, 8, 128], BF16)
            V(lambda: nc.vector.tensor_tensor(out=wsm.t[:], in0=wsf.t[:], in1=trl.t[:].unsqueeze(1).to_broadcast([128, 8, 128]),
                                              op=ALU.mult), [wsf.r, trl.r], [wsm.r])
            tp = pnext(); tpb = tp.t[:].bitcast(BF16)
            for g in range(8):
                P(lambda: nc.tensor.transpose(out=tpb[:, g * 128:(g + 1) * 128], in_=wsm.t[:, g, :], identity=identb.t[:]),
                  [wsm.r, identb.r], [tp.r])
            V(lambda: nc.vector.tensor_copy(out=wsT.t[:].rearrange("p g t -> p (g t)"), in_=tpb[:, :]), [tp.r], [wsT.r])
            T.barrier()


        with ExitStack() as cw:
            h1c = [sbt(cw, f"h1c{i}", [128, 8, 128], BF16) for i in range(1)]
            uv = sbt(cw, "uv", [128, 1024], F32)
            x2 = sbt(cw, "x2t", [128, 1024], F32)
            inner = sbt(cw, "inner", [128, 1024], F32)
            gel = sbt(cw, "gel", [128, 512], F32)
            otile = [sbt(cw, f"otile{i}", [128, 512], BF16, True) for i in range(2)]
            sgoB = [sbt(cw, f"sgo{i}", [128, 512], BF16) for i in range(1)]
            lst = sbt(cw, "lst", [128, 8], F32)
            sigB = [sbt(cw, f"sig{i}", [128, 2048], BF16) for i in range(2)]
            OT = sbt(cw, "OT", [128, 4, 128], BF16)
            SGT = sbt(cw, "SGT", [128, 4, 128], BF16)
            m1 = sbt(cw, "m1", [128, D], F32); m2 = sbt(cw, "m2", [128, D], F32)
            mrg = sbt(cw, "mrg", [128, D], BF16)
            mT = sbt(cw, "mT", [128, 8, 128], BF16)
            x1 = [sbt(cw, f"x1_{i}", [128, D], F32, True) for i in range(2)]
            x1n = m1
            h2T = sbt(cw, "h2T", [128, 8, 128], F32)
            h2f = m2
            h2p = [sbt(cw, f"h2p{i}", [128, D], BF16, True) for i in range(1)]
            st2 = sbt(cw, "st2", [128, 4], F32)
            lgall = sbt(cw, "lgall", [128, NOWN, 36], F32)
            rtb = sbt(cw, "rtb", [128, 8, NOWN], F32)
            s1out = {}

            vnbB = [sbt(cw, f"vnbB{i}", [128, 512], BF16) for i in range(2)]
            gubB = [sbt(cw, f"gubB{i}", [128, 512], BF16) for i in range(2)]
            fe_st = {}

            def S1(i):
                h1 = h1c[0]; sig = sigB[i % 2]; vnb_ = vnbB[i % 2]; gub = gubB[i % 2]
                if i not in fe_st:
                    fe_st[i] = fe_a([xo[i * 128:(i + 1) * 128, :]])
                xt = fe_b(fe_st.pop(i), h1.t, h1.r, [0])[0]
                s1out[i] = (xt, vnb_, gub, sig)
                load(otile[i % 2], OS[i * 128:(i + 1) * 128, :])
                pu = pnext(); pv = pnext()
                for hh, pp in enumerate((pu, pv)):
                    for kc in range(8):
                        P(lambda: nc.tensor.matmul(pp.t[:], lhsT=h1.t[:, kc, :], rhs=wsg.t[:, kc, hh * 512:(hh + 1) * 512],
                                                   start=(kc == 0), stop=False), [h1.r, wsg.r], [pp.r])
                    P(lambda: nc.tensor.matmul(pp.t[:], lhsT=onesb.t[0:1, :], rhs=browc.t[0:1, hh * 512:(hh + 1) * 512],
                                               start=False, stop=True), [onesb.r, browc.r], [pp.r])
                pg = [pnext() for _ in range(4)]
                for q4 in range(4):
                    for kc in range(8):
                        P(lambda: nc.tensor.matmul(pg[q4].t[:], lhsT=h1.t[:, kc, :], rhs=wgl.t[:, kc, q4 * 512:(q4 + 1) * 512],
                                                   start=(kc == 0), stop=False), [h1.r, wgl.r], [pg[q4].r])
                    P(lambda: nc.tensor.matmul(pg[q4].t[:], lhsT=onesb.t[0:1, :], rhs=browc.t[0:1, 1024 + q4 * 512:1024 + (q4 + 1) * 512],
                                               start=False, stop=True), [onesb.r, browc.r], [pg[q4].r])
                for hh, pp in enumerate((pu, pv)):
                    cs_ = slice(hh * 512, (hh + 1) * 512)
                    A(lambda: nc.scalar.copy(out=uv.t[:, cs_], in_=pp.t[:]), [pp.r], [uv.r])
                    A(lambda: nc.scalar.activation(out=x2.t[:, cs_], in_=pp.t[:], func=AF.Square, scale=0.044715 ** 0.5),
                      [pp.r], [x2.r])
                V(lambda: nc.vector.scalar_tensor_tensor(out=inner.t[:], in0=x2.t[:], scalar=1.0, in1=uv.t[:], op0=ALU.add,
                                                         op1=ALU.mult), [x2.r, uv.r], [inner.r])
                for q4 in range(4):
                    A(lambda: nc.scalar.activation(out=sig.t[:, q4 * 512:(q4 + 1) * 512], in_=pg[q4].t[:], func=AF.Sigmoid),
                      [pg[q4].r], [sig.r])
                A(lambda: nc.scalar.activation(out=inner.t[:], in_=inner.t[:], func=AF.Sigmoid, scale=GELU_C), [inner.r], [inner.r])
                if i + 1 < NOWN:
                    fe_st[i + 1] = fe_a([xo[(i + 1) * 128:(i + 2) * 128, :]])
                G(lambda: nc.gpsimd.tensor_tensor(out=gel.t[:, 0:512], in0=inner.t[:, 512:1024], in1=uv.t[:, 512:1024], op=ALU.mult),
                  [inner.r, uv.r], [gel.r])
                G(lambda: nc.gpsimd.tensor_tensor(out=gub.t[:], in0=inner.t[:, 0:512], in1=uv.t[:, 0:512], op=ALU.mult),
                  [inner.r, uv.r], [gub.r])
                gv_ = gel.t[:, 0:512]
                V(lambda: nc.vector.tensor_reduce(out=lst.t[:, 0:1], in_=gv_, axis=AX.X, op=ALU.add), [gel.r], [lst.r])
                V(lambda: nc.vector.tensor_scalar(out=lst.t[:, 1:2], in0=lst.t[:, 0:1], scalar1=1.0 / 512, scalar2=None,
                                                  op0=ALU.mult), [lst.r], [lst.r])
                V(lambda: nc.vector.tensor_scalar(out=x2.t[:, 0:512], in0=gv_, scalar1=lst.t[:, 1:2], scalar2=None,
                                                  op0=ALU.subtract), [gel.r, lst.r], [x2.r])
                A(lambda: nc.scalar.activation(out=x2.t[:, 512:1024], in_=x2.t[:, 0:512], func=AF.Square, accum_out=lst.t[:, 2:3]),
                  [x2.r], [x2.r, lst.r])
                V(lambda: nc.vector.tensor_scalar(out=lst.t[:, 3:4], in0=lst.t[:, 2:3], scalar1=1.0 / 512, scalar2=EPS,
                                                  op0=ALU.mult, op1=ALU.add), [lst.r], [lst.r])
                A(lambda: nc.scalar.activation(out=lst.t[:, 4:5], in_=lst.t[:, 3:4], func=AF.Sqrt), [lst.r], [lst.r])
                V(lambda: nc.vector.reciprocal(out=lst.t[:, 5:6], in_=lst.t[:, 4:5]), [lst.r], [lst.r])
                V(lambda: nc.vector.scalar_tensor_tensor(out=x2.t[:, 0:512], in0=x2.t[:, 0:512], scalar=lst.t[:, 5:6],
                                                         in1=gvb.t[:], op0=ALU.mult, op1=ALU.mult), [x2.r, lst.r, gvb.r], [x2.r])
                V(lambda: nc.vector.tensor_tensor(out=vnb_.t[:], in0=x2.t[:, 0:512], in1=bvb.t[:], op=ALU.add),
                  [x2.r, bvb.r], [vnb_.r])

            def S2(i):
                xt, vnb_, gub, sig = s1out.pop(i)
                sgo = sgoB[0]
                psg = pnext()
                for g in range(8):
                    P(lambda: nc.tensor.matmul(psg.t[:, g * 64:(g + 1) * 64], lhsT=wsT.t[:, g, :], rhs=vnb_.t[:, g * 64:(g + 1) * 64],
                                               start=True, stop=True), [wsT.r, vnb_.r], [psg.r])
                ot_ = otile[i % 2]
                tp = pnext(); tpb = tp.t[:].bitcast(BF16)
                for kc in range(4):
                    P(lambda: nc.tensor.transpose(out=tpb[:, kc * 128:(kc + 1) * 128], in_=ot_.t[:, kc * 128:(kc + 1) * 128],
                                                  identity=identb.t[:]), [ot_.r, identb.r], [tp.r])
                A(lambda: nc.scalar.copy(out=OT.t[:].rearrange("p k t -> p (k t)"), in_=tpb[:, 0:512]), [tp.r], [OT.r])
                V(lambda: nc.vector.tensor_tensor(out=m2.t[:, 0:512].rearrange("p (g c) -> p g c", g=8),
                                                  in0=psg.t[:].rearrange("p (g c) -> p g c", g=8),
                                                  in1=bst.t[:].unsqueeze(2).to_broadcast([128, 8, 64]), op=ALU.add),
                  [psg.r, bst.r], [m2.r])
                V(lambda: nc.vector.tensor_tensor(out=sgo.t[:], in0=m2.t[:, 0:512], in1=gub.t[:], op=ALU.mult),
                  [m2.r, gub.r], [sgo.r])
                for hh in range(2):
                    ps = pnext()
                    for kc in range(4):
                        P(lambda: nc.tensor.matmul(ps.t[:], lhsT=OT.t[:, kc, :], rhs=wbm.t[:, kc, hh * 512:(hh + 1) * 512],
                                                   start=(kc == 0), stop=(kc == 3)), [OT.r, wbm.r], [ps.r])
                    V(lambda: nc.vector.tensor_tensor(out=m1.t[:, hh * 512:(hh + 1) * 512], in0=ps.t[:],
                                                      in1=sig.t[:, hh * 512:(hh + 1) * 512], op=ALU.mult), [ps.r, sig.r], [m1.r])
                tp2 = pnext(); tpb2 = tp2.t[:].bitcast(BF16)
                for kc in range(4):
                    P(lambda: nc.tensor.transpose(out=tpb2[:, kc * 128:(kc + 1) * 128], in_=sgo.t[:, kc * 128:(kc + 1) * 128],
                                                  identity=identb.t[:]), [sgo.r, identb.r], [tp2.r])
                A(lambda: nc.scalar.copy(out=SGT.t[:].rearrange("p k t -> p (k t)"), in_=tpb2[:, 0:512]), [tp2.r], [SGT.r])
                for hh in range(2):
                    ps = pnext()
                    for kc in range(4):
                        P(lambda: nc.tensor.matmul(ps.t[:], lhsT=SGT.t[:, kc, :], rhs=wbs.t[:, kc, hh * 512:(hh + 1) * 512],
                                                   start=(kc == 0), stop=(kc == 3)), [SGT.r, wbs.r], [ps.r])
                    V(lambda: nc.vector.tensor_tensor(out=m2.t[:, hh * 512:(hh + 1) * 512], in0=ps.t[:],
                                                      in1=sig.t[:, 1024 + hh * 512:1024 + (hh + 1) * 512], op=ALU.mult),
                      [ps.r, sig.r], [m2.r])
                    V(lambda: nc.vector.tensor_tensor(out=mrg.t[:, hh * 512:(hh + 1) * 512], in0=m1.t[:, hh * 512:(hh + 1) * 512],
                                                      in1=m2.t[:, hh * 512:(hh + 1) * 512], op=ALU.add), [m1.r, m2.r], [mrg.r])
                tp = pnext(); tpb = tp.t[:].bitcast(BF16)
                for kc in range(8):
                    P(lambda: nc.tensor.transpose(out=tpb[:, kc * 128:(kc + 1) * 128], in_=mrg.t[:, kc * 128:(kc + 1) * 128],
                                                  identity=identb.t[:]), [mrg.r, identb.r], [tp.r])
                A(lambda: nc.scalar.copy(out=mT.t[:].rearrange("p k t -> p (k t)"), in_=tpb[:, :]), [tp.r], [mT.r])
                xx = x1[i % 2]
                for hh in range(2):
                    ps = pnext()
                    for kc in range(8):
                        P(lambda: nc.tensor.matmul(ps.t[:], lhsT=mT.t[:, kc, :], rhs=wo.t[:, kc, hh * 512:(hh + 1) * 512],
                                                   start=(kc == 0), stop=(kc == 7)), [mT.r, wo.r], [ps.r])
                    cs_ = slice(hh * 512, (hh + 1) * 512)
                    V(lambda: nc.vector.tensor_tensor(out=m1.t[:, cs_], in0=ps.t[:], in1=gate1b.t[:, cs_], op=ALU.mult),
                      [ps.r, gate1b.r], [m1.r])
                V(lambda: nc.vector.tensor_tensor(out=xx.t[:], in0=m1.t[:], in1=xt.t[:], op=ALU.add), [m1.r, xt.r], [xx.r])
                T.dma(SP, xx.l, lambda: nc.sync.dma_start(out=X1[i * 128:(i + 1) * 128, :], in_=xx.t[:]), reads=[xx.r])

            def S3(i):
                xx = x1[i % 2]
                junk = FE["junk"]
                A(lambda: nc.scalar.activation(out=junk.t[:], in_=xx.t[:], func=AF.Square, accum_out=st2.t[:, 0:1]),
                  [xx.r], [junk.r, st2.r])
                V(lambda: nc.vector.tensor_scalar(out=st2.t[:, 1:2], in0=st2.t[:, 0:1], scalar1=1.0 / D, scalar2=EPS,
                                                  op0=ALU.mult, op1=ALU.add), [st2.r], [st2.r])
                A(lambda: nc.scalar.activation(out=st2.t[:, 2:3], in_=st2.t[:, 1:2], func=AF.Sqrt), [st2.r], [st2.r])
                V(lambda: nc.vector.reciprocal(out=st2.t[:, 3:4], in_=st2.t[:, 2:3]), [st2.r], [st2.r])
                V(lambda: nc.vector.tensor_scalar(out=x1n.t[:], in0=xx.t[:], scalar1=st2.t[:, 3:4], scalar2=None, op0=ALU.mult),
                  [xx.r, st2.r], [x1n.r])
                G(lambda: nc.gpsimd.tensor_tensor(out=h2f.t[:], in0=x1n.t[:], in1=s2b.t[:], op=ALU.mult), [x1n.r, s2b.r], [h2f.r])
                hp = h2p[0]
                G(lambda: nc.gpsimd.tensor_tensor(out=hp.t[:].rearrange("t (kc p) -> t p kc", kc=8),
                                                  in0=h2f.t[:].rearrange("t (p kc) -> t p kc", kc=8),
                                                  in1=sh2b.t[:].rearrange("t (p kc) -> t p kc", kc=8), op=ALU.add),
                  [h2f.r, sh2b.r], [hp.r])
                T.dma(SP, hp.l, lambda: nc.sync.dma_start(out=H2[i * 128:(i + 1) * 128, :], in_=hp.t[:]), reads=[hp.r])
                tA = pnext(); tB = pnext()
                for kc in range(8):
                    tt = tA if kc < 4 else tB
                    P(lambda: nc.tensor.transpose(out=tt.t[:, (kc % 4) * 128:(kc % 4 + 1) * 128], in_=x1n.t[:, kc * 128:(kc + 1) * 128],
                                                  identity=identf.t[:]), [x1n.r, identf.r], [tt.r])
                for kc in range(8):
                    tt = tA if kc < 4 else tB
                    V(lambda: nc.vector.tensor_scalar(out=h2T.t[:, kc, :], in0=tt.t[:, (kc % 4) * 128:(kc % 4 + 1) * 128],
                                                      scalar1=s2c.t[:, kc:kc + 1], scalar2=sh2(kc), op0=ALU.mult, op1=ALU.add),
                      [tt.r, s2c.r, modT.r], [h2T.r])
                pl = pnext()
                for kc in range(8):
                    P(lambda: nc.tensor.matmul(pl.t[:, 0:36], lhsT=h2T.t[:, kc, :], rhs=wrf.t[:, kc, :], start=(kc == 0), stop=(kc == 7)),
                      [h2T.r, wrf.r], [pl.r])
                V(lambda: nc.vector.tensor_tensor(out=lgall.t[:, i, :], in0=pl.t[:, 0:36], in1=brb.t[:], op=ALU.add), [pl.r, brb.r], [lgall.r])

            for step in range(NOWN + 2):
                if step < NOWN:
                    S1(step)
                if 0 <= step - 1 < NOWN:
                    S2(step - 1)
                if 0 <= step - 2 < NOWN:
                    S3(step - 2)

            TT = NOWN
            Lg = lgall.t[:, :, 0:4]
            Le4 = lgall.t[:, :, 4:36].rearrange("p t (g e) -> p t g e", g=4)
            gm = rtb.t[:, 0, :]; se = rtb.t[:, 1, :]; pgr = rtb.t[:, 2, :]; mv1 = rtb.t[:, 3, :]; mv2 = rtb.t[:, 4, :]
            dd_ = rtb.t[:, 5, :]; ee_ = rtb.t[:, 6, :]; rr_ = rtb.t[:, 7, :]
            ex = uv.t[:, 0:TT * 4].rearrange("p (t g) -> p t g", g=4)
            pen3 = x2.t[:, 0:TT * 4].rearrange("p (t g) -> p t g", g=4)
            Mx3 = inner.t[:, 0:TT * 32].rearrange("p (t e) -> p t e", e=32)
            Mx4 = inner.t[:, 0:TT * 32].rearrange("p (t g e) -> p t g e", g=4, e=8)
            My3 = m1.t[:, 0:TT * 32].rearrange("p (t e) -> p t e", e=32)
            bc3 = lambda ap2, n: ap2.unsqueeze(2).to_broadcast([128, TT, n])
            V(lambda: nc.vector.tensor_reduce(out=gm, in_=Lg, axis=AX.X, op=ALU.max), [lgall.r], [rtb.r])
            V(lambda: nc.vector.tensor_tensor(out=ex, in0=Lg, in1=bc3(gm, 4), op=ALU.subtract), [lgall.r, rtb.r], [uv.r])
            A(lambda: nc.scalar.activation(out=ex, in_=ex, func=AF.Exp), [uv.r], [uv.r])
            V(lambda: nc.vector.tensor_reduce(out=se, in_=ex, axis=AX.X, op=ALU.add), [uv.r], [rtb.r])
            V(lambda: nc.vector.reciprocal(out=pgr, in_=se), [rtb.r], [rtb.r])
            V(lambda: nc.vector.tensor_tensor(out=pen3, in0=Lg, in1=bc3(gm, 4), op=ALU.is_equal), [lgall.r, rtb.r], [x2.r])
            V(lambda: nc.vector.tensor_scalar(out=pen3, in0=pen3, scalar1=-1.0, scalar2=1e30, op0=ALU.add, op1=ALU.mult),
              [x2.r], [x2.r])
            V(lambda: nc.vector.tensor_tensor(out=Mx4, in0=Le4, in1=pen3.unsqueeze(3).to_broadcast([128, TT, 4, 8]), op=ALU.add),
              [lgall.r, x2.r], [inner.r])
            V(lambda: nc.vector.tensor_reduce(out=mv1, in_=Mx3, axis=AX.X, op=ALU.max), [inner.r], [rtb.r])
            V(lambda: nc.vector.tensor_tensor(out=A0all.t[:], in0=Mx3, in1=bc3(mv1, 32), op=ALU.is_equal), [inner.r, rtb.r], [A0all.r])
            V(lambda: nc.vector.scalar_tensor_tensor(out=My3, in0=A0all.t[:], scalar=-1e30, in1=Mx3, op0=ALU.mult, op1=ALU.add),
              [A0all.r, inner.r], [m1.r])
            V(lambda: nc.vector.tensor_reduce(out=mv2, in_=My3, axis=AX.X, op=ALU.max), [m1.r], [rtb.r])
            V(lambda: nc.vector.tensor_tensor(out=A1all.t[:], in0=My3, in1=bc3(mv2, 32), op=ALU.is_equal), [m1.r, rtb.r], [A1all.r])
            V(lambda: nc.vector.tensor_tensor(out=Aall.t[:], in0=A0all.t[:], in1=A1all.t[:], op=ALU.add), [A0all.r, A1all.r], [Aall.r])
            V(lambda: nc.vector.tensor_tensor(out=dd_, in0=mv2, in1=mv1, op=ALU.subtract), [rtb.r], [rtb.r])
            A(lambda: nc.scalar.activation(out=ee_, in_=dd_, func=AF.Exp), [rtb.r], [rtb.r])
            V(lambda: nc.vector.tensor_scalar(out=ee_, in0=ee_, scalar1=1.0, scalar2=None, op0=ALU.add), [rtb.r], [rtb.r])
            V(lambda: nc.vector.reciprocal(out=rr_, in_=ee_), [rtb.r], [rtb.r])
            V(lambda: nc.vector.tensor_tensor(out=wts.t[:, :, 0], in0=rr_, in1=pgr, op=ALU.mult), [rtb.r], [wts.r])
            V(lambda: nc.vector.tensor_tensor(out=wts.t[:, :, 1], in0=pgr, in1=wts.t[:, :, 0], op=ALU.subtract), [rtb.r, wts.r], [wts.r])
            T.barrier()
            if stop <= 3:
                return nc
        pc_.close()

        NA = NOWN * 32
        REG_SLOT = nc.gpsimd.to_reg(NSLOT - 1)
        REG_W = nc.gpsimd.to_reg(NEXP * 128 - 1)
        d0i = sbt(es, "d0i", [128, NOWN], I32); d1i = sbt(es, "d1i", [128, NOWN], I32)
        idxw = sbt(es, "idxw", [128, NBLK], I32)
        with ExitStack() as dp:
            set_rot(range(8))
            Ab_t = Aall.t[:].rearrange("p t e -> p (t e)")
            lsf = sbt(dp, "lsf", [128, 128], F32, True); load(lsf, c_lstrict)
            lsb = sbt(dp, "lsb", [128, 128], BF16)
            thr = sbt(dp, "thr", [128, NBLK], F32, True); load(thr, c_thr)
            pidx = sbt(dp, "pidx", [128, 1], F32, True); load(pidx, c_pidx)
            V(lambda: nc.vector.tensor_copy(out=lsb.t[:], in_=lsf.t[:]), [lsf.r], [lsb.r])
            tsum = sbt(dp, "tsum", [128, NOWN, 32], F32)
            rloc = sbt(dp, "rloc", [128, NOWN, 32], F32)
            for c0 in range(0, NA, 512):
                w_ = min(512, NA - c0)
                ps = pnext()
                P(lambda: nc.tensor.matmul(ps.t[:, 0:w_], lhsT=onesb.t[:], rhs=Ab_t[:, c0:c0 + w_], start=True, stop=True),
                  [onesb.r, Aall.r], [ps.r])
                V(lambda: nc.vector.tensor_copy(out=tsum.t[:].rearrange("p t e -> p (t e)")[:, c0:c0 + w_], in_=ps.t[:, 0:w_]),
                  [ps.r], [tsum.r])
                ps2 = pnext()
                P(lambda: nc.tensor.matmul(ps2.t[:, 0:w_], lhsT=lsb.t[:], rhs=Ab_t[:, c0:c0 + w_], start=True, stop=True),
                  [lsb.r, Aall.r], [ps2.r])
                V(lambda: nc.vector.tensor_copy(out=rloc.t[:].rearrange("p t e -> p (t e)")[:, c0:c0 + w_], in_=ps2.t[:, 0:w_]),
                  [ps2.r], [rloc.r])
            cum = sbt(dp, "cum", [128, NOWN + 1, 32], F32)
            V(lambda: nc.vector.memset(cum.t[:, 0, :], 0.0), [], [cum.r])
            for t in range(NOWN):
                V(lambda: nc.vector.tensor_tensor(out=cum.t[:, t + 1, :], in0=cum.t[:, t, :], in1=tsum.t[:, t, :], op=ALU.add),
                  [cum.r, tsum.r], [cum.r])
            ci = sbt(dp, "ci", [128, 32], I32)
            pad = sbt(dp, "pad", [128, 32], F32)
            pend = sbt(dp, "pend", [128, 32], F32)
            pst = sbt(dp, "pst", [128, 32], F32)
            V(lambda: nc.vector.tensor_scalar(out=pad.t[:], in0=cum.t[:, NOWN, :], scalar1=float(BLK - 1), scalar2=None,
                                              op0=ALU.add), [cum.r], [pad.r])
            V(lambda: nc.vector.tensor_copy(out=ci.t[:], in_=pad.t[:]), [pad.r], [ci.r])
            V(lambda: nc.vector.tensor_scalar(out=ci.t[:], in0=ci.t[:], scalar1=8, scalar2=8, op0=ALU.arith_shift_right,
                                              op1=ALU.logical_shift_left), [ci.r], [ci.r])
            V(lambda: nc.vector.tensor_copy(out=pad.t[:], in_=ci.t[:]), [ci.r], [pad.r])
            V(lambda: nc.vector.tensor_tensor_scan(out=pend.t[:], data0=onesf.t[:, 0:32], data1=pad.t[:], initial=0.0,
                                                   op0=ALU.mult, op1=ALU.add), [onesf.r, pad.r], [pend.r])
            V(lambda: nc.vector.tensor_tensor(out=pst.t[:], in0=pend.t[:], in1=pad.t[:], op=ALU.subtract), [pend.r, pad.r], [pst.r])
            V(lambda: nc.vector.tensor_tensor(out=rloc.t[:], in0=rloc.t[:], in1=cum.t[:, 0:NOWN, :], op=ALU.add),
              [rloc.r, cum.r], [rloc.r])
            V(lambda: nc.vector.tensor_tensor(out=rloc.t[:], in0=rloc.t[:], in1=pst.t[:].unsqueeze(1).to_broadcast([128, NOWN, 32]),
                                              op=ALU.add), [rloc.r, pst.r], [rloc.r])
            df = sbt(dp, "df", [128, NOWN], F32)
            for Ak, dk in ((A0all, d0i), (A1all, d1i)):
                V(lambda: nc.vector.tensor_tensor(out=tsum.t[:], in0=Ak.t[:], in1=rloc.t[:], op=ALU.mult), [Ak.r, rloc.r, tsum.r], [tsum.r])
                V(lambda: nc.vector.tensor_reduce(out=df.t[:], in_=tsum.t[:], axis=AX.X, op=ALU.add), [tsum.r], [df.r])
                V(lambda: nc.vector.tensor_copy(out=dk.t[:], in_=df.t[:]), [df.r], [dk.r])
            cmp_ = sbt(dp, "cmp", [128, NBLK, 32], F32)
            bef = sbt(dp, "bef", [128, NBLK], F32)
            V(lambda: nc.vector.tensor_tensor(out=cmp_.t[:], in0=pend.t[:].unsqueeze(1).to_broadcast([128, NBLK, 32]),
                                              in1=thr.t[:].unsqueeze(2).to_broadcast([128, NBLK, 32]), op=ALU.is_le),
              [pend.r, thr.r], [cmp_.r])
            V(lambda: nc.vector.tensor_reduce(out=bef.t[:], in_=cmp_.t[:], axis=AX.X, op=ALU.add), [cmp_.r], [bef.r])
            V(lambda: nc.vector.tensor_scalar(out=bef.t[:], in0=bef.t[:], scalar1=float(NEXP - 1), scalar2=128.0, op0=ALU.min,
                                              op1=ALU.mult), [bef.r], [bef.r])
            V(lambda: nc.vector.tensor_scalar(out=bef.t[:], in0=bef.t[:], scalar1=pidx.t[:, 0:1], scalar2=None, op0=ALU.add),
              [bef.r, pidx.r], [bef.r])
            skp = sbt(dp, "skp", [128, NBLK], F32)
            V(lambda: nc.vector.tensor_scalar(out=skp.t[:], in0=thr.t[:], scalar1=pend.t[:, 31:32], scalar2=1.0e6, op0=ALU.is_ge,
                                              op1=ALU.mult), [thr.r, pend.r], [skp.r])
            V(lambda: nc.vector.tensor_tensor(out=bef.t[:], in0=bef.t[:], in1=skp.t[:], op=ALU.add), [bef.r, skp.r], [bef.r])
            V(lambda: nc.vector.tensor_copy(out=idxw.t[:], in_=bef.t[:]), [bef.r], [idxw.r])
            hb = [sbt(dp, f"hb{i}", [128, D], BF16, True) for i in range(3)]
            xs_res = Res("XS")
            for t in range(NOWN):
                b_ = hb[t % 3]
                load(b_, H2[t * 128:(t + 1) * 128, :])
                for dk in (d0i, d1i):
                    T.dma(POOL, b_.l, lambda: nc.gpsimd.indirect_dma_start(
                        out=XS, out_offset=bass.IndirectOffsetOnAxis(ap=dk.t[:, t:t + 1], axis=0), in_=b_.t[:], in_offset=None,
                        bounds_check=REG_SLOT, oob_is_err=False),
                        reads=[b_.r, dk.r])
            T.barrier()
            if stop <= 4:
                return nc

        with ExitStack() as dd:
            set_rot(range(8))
            stg = [[sbt(dd, f"stg{i}_{k}", [128, 4096], F32, True) for k in range(3)] for i in range(2)]
            w1b = [sbt(dd, f"w1b{i}", [128, 8, 4, 128], BF16) for i in range(2)]
            w3b = [sbt(dd, f"w3b{i}", [128, 8, 4, 128], BF16) for i in range(2)]
            w2b = [sbt(dd, f"w2b{i}", [128, 4, D], BF16) for i in range(2)]
            xbk = [sbt(dd, f"xbk{i}", [128, 2, D], BF16, True) for i in range(2)]
            xTb = [sbt(dd, f"xTb{i}", [128, 8, 256], BF16) for i in range(1)]
            sil = sbt(dd, "sil", [128, 256], F32)
            gTb = [sbt(dd, f"gTb{i}", [128, 4, 256], BF16) for i in range(2)]
            ysb = [sbt(dd, f"ysb{i}", [128, D], F32, True) for i in range(2)]
            yc = 0

            def issue_w(j):
                s_ = stg[j % 2]
                for k, wsrc in enumerate((w1, w3, w2)):
                    T.dma(POOL, s_[k].l, lambda: nc.gpsimd.indirect_dma_start(
                        out=s_[k].t[:], out_offset=None, in_=wsrc,
                        in_offset=bass.IndirectOffsetOnAxis(ap=idxw.t[:, j:j + 1], axis=0),
                        bounds_check=REG_W, oob_is_err=False),
                        reads=[idxw.r], writes=[s_[k].r])

            def issue_x(j):
                load(xbk[j % 2], XS[j * BLK:(j + 1) * BLK, :].rearrange("(a p) d -> p a d", p=128))

            issue_w(0)
            issue_x(0)
            for j in range(NBLK):
                if j + 1 < NBLK:
                    issue_w(j + 1)
                    issue_x(j + 1)
                s_ = stg[j % 2]; b1 = w1b[j % 2]; b3 = w3b[j % 2]; b2 = w2b[j % 2]
                V(lambda: nc.vector.tensor_copy(out=b1.t[:], in_=s_[0].t[:].rearrange("p (kc m fc) -> p kc fc m", kc=8, m=128, fc=4)),
                  [s_[0].r], [b1.r])
                V(lambda: nc.vector.tensor_copy(out=b3.t[:], in_=s_[1].t[:].rearrange("p (kc m fc) -> p kc fc m", kc=8, m=128, fc=4)),
                  [s_[1].r], [b3.r])
                A(lambda: nc.scalar.copy(out=b2.t[:].rearrange("p f n -> p (f n)"), in_=s_[2].t[:]), [s_[2].r], [b2.r])
                xk = xbk[j % 2]; xT_ = xTb[0]; gT_ = gTb[j % 2]
                for st_ in range(2):
                    tp = pnext(); tpb = tp.t[:].bitcast(BF16)
                    for kc in range(8):
                        P(lambda: nc.tensor.transpose(out=tpb[:, kc * 128:(kc + 1) * 128], in_=xk.t[:, st_, kc * 128:(kc + 1) * 128],
                                                      identity=identb.t[:]), [xk.r, identb.r], [tp.r])
                    cp = (lambda f: A(f, [tp.r], [xT_.r])) if st_ == 0 else (lambda f: V(f, [tp.r], [xT_.r]))
                    if st_ == 0:
                        A(lambda: nc.scalar.copy(out=xT_.t[:, :, 0:128], in_=tpb.rearrange("p (k t) -> p k t", k=8)), [tp.r], [xT_.r])
                    else:
                        V(lambda: nc.vector.tensor_copy(out=xT_.t[:, :, 128:256], in_=tpb.rearrange("p (k t) -> p k t", k=8)),
                          [tp.r], [xT_.r])
                for fc in range(4):
                    pa = pnext(); pb_ = pnext()
                    for kc in range(8):
                        P(lambda: nc.tensor.matmul(pa.t[:, 0:256], lhsT=b1.t[:, kc, fc, :], rhs=xT_.t[:, kc, :], start=(kc == 0),
                                                   stop=(kc == 7)), [b1.r, xT_.r], [pa.r])
                    for kc in range(8):
                        P(lambda: nc.tensor.matmul(pb_.t[:, 0:256], lhsT=b3.t[:, kc, fc, :], rhs=xT_.t[:, kc, :], start=(kc == 0),
                                                   stop=(kc == 7)), [b3.r, xT_.r], [pb_.r])
                    A(lambda: nc.scalar.activation(out=sil.t[:], in_=pa.t[:, 0:256], func=AF.Silu), [pa.r], [sil.r])
                    V(lambda: nc.vector.tensor_tensor(out=gT_.t[:, fc, :], in0=pb_.t[:, 0:256], in1=sil.t[:], op=ALU.mult),
                      [pb_.r, sil.r], [gT_.r])
                for st_ in range(2):
                    y_ = ysb[yc % 2]; yc += 1
                    for hh in range(2):
                        ps = pnext()
                        for fc in range(4):
                            P(lambda: nc.tensor.matmul(ps.t[:], lhsT=gT_.t[:, fc, st_ * 128:(st_ + 1) * 128],
                                                       rhs=b2.t[:, fc, hh * 512:(hh + 1) * 512], start=(fc == 0), stop=(fc == 3)),
                              [gT_.r, b2.r], [ps.r])
                        if hh == 0:
                            A(lambda: nc.scalar.copy(out=y_.t[:, 0:512], in_=ps.t[:]), [ps.r], [y_.r])
                        else:
                            V(lambda: nc.vector.tensor_copy(out=y_.t[:, 512:1024], in_=ps.t[:]), [ps.r], [y_.r])
                    r0 = j * BLK + st_ * 128
                    T.dma(SP, y_.l, lambda: nc.sync.dma_start(out=YS[r0:r0 + 128, :], in_=y_.t[:]), reads=[y_.r])
            T.barrier()
            if stop <= 5:
                return nc

        with ExitStack() as ee:
            set_rot(range(8))
            gfb = sbt(ee, "gfb", [128, D], F32, True); load(gfb, gf_row.partition_broadcast(128))
            y0 = [sbt(ee, f"y0_{i}", [128, D], F32, True) for i in range(2)]
            y1 = [sbt(ee, f"y1_{i}", [128, D], F32, True) for i in range(2)]
            xr = [sbt(ee, f"xr{i}", [128, D], F32, True) for i in range(2)]
            acc = sbt(ee, "acc", [128, D], F32)
            jk = sbt(ee, "jk", [128, D], F32)
            ob = [sbt(ee, f"ob{i}", [128, D], F32, True) for i in range(2)]
            st3 = sbt(ee, "st3", [128, 8], F32)
            accH = [sbt(ee, f"accH{i}", [128, 512], F32) for i in range(2)]
            def issue_e(t):
                a0 = y0[t % 2]; a1 = y1[t % 2]; xx = xr[t % 2]
                T.dma(POOL, a0.l, lambda: nc.gpsimd.indirect_dma_start(
                    out=a0.t[:], out_offset=None, in_=YS, in_offset=bass.IndirectOffsetOnAxis(ap=d0i.t[:, t:t + 1], axis=0),
                    bounds_check=REG_SLOT, oob_is_err=False),
                    reads=[d0i.r], writes=[a0.r])
                T.dma(POOL, a1.l, lambda: nc.gpsimd.indirect_dma_start(
                    out=a1.t[:], out_offset=None, in_=YS, in_offset=bass.IndirectOffsetOnAxis(ap=d1i.t[:, t:t + 1], axis=0),
                    bounds_check=REG_SLOT, oob_is_err=False),
                    reads=[d1i.r], writes=[a1.r])
                load(xx, X1[t * 128:(t + 1) * 128, :])

            issue_e(0)
            for t in range(NOWN):
                a0 = y0[t % 2]; a1 = y1[t % 2]; xx = xr[t % 2]; o_ = ob[t % 2]
                if t + 1 < NOWN:
                    issue_e(t + 1)
                H = [(accH[0], slice(0, 512)), (accH[1], slice(512, 1024))]
                for ac, cs_ in H:
                    V(lambda: nc.vector.tensor_scalar(out=ac.t[:], in0=a0.t[:, cs_], scalar1=wts.t[:, t, 0:1], scalar2=None,
                                                      op0=ALU.mult), [a0.r, wts.r], [ac.r])
                for ac, cs_ in H:
                    V(lambda: nc.vector.scalar_tensor_tensor(out=ac.t[:], in0=a1.t[:, cs_], scalar=wts.t[:, t, 1:2], in1=ac.t[:],
                                                             op0=ALU.mult, op1=ALU.add), [a1.r, wts.r, ac.r], [ac.r])
                for ac, cs_ in H:
                    V(lambda: nc.vector.tensor_tensor(out=ac.t[:], in0=ac.t[:], in1=gate2b.t[:, cs_], op=ALU.mult),
                      [ac.r, gate2b.r], [ac.r])
                for ac, cs_ in H:
                    V(lambda: nc.vector.tensor_tensor(out=ac.t[:], in0=ac.t[:], in1=xx.t[:, cs_], op=ALU.add), [ac.r, xx.r], [ac.r])
                for hi_, (ac, cs_) in enumerate(H):
                    A(lambda: nc.scalar.activation(out=jk.t[:, cs_], in_=ac.t[:], func=AF.Square, accum_out=st3.t[:, 4 + hi_:5 + hi_]),
                      [ac.r], [jk.r, st3.r])
                V(lambda: nc.vector.tensor_tensor(out=st3.t[:, 0:1], in0=st3.t[:, 4:5], in1=st3.t[:, 5:6], op=ALU.add), [st3.r], [st3.r])
                V(lambda: nc.vector.tensor_scalar(out=st3.t[:, 1:2], in0=st3.t[:, 0:1], scalar1=1.0 / D, scalar2=EPS,
                                                  op0=ALU.mult, op1=ALU.add), [st3.r], [st3.r])
                A(lambda: nc.scalar.activation(out=st3.t[:, 2:3], in_=st3.t[:, 1:2], func=AF.Sqrt), [st3.r], [st3.r])
                V(lambda: nc.vector.reciprocal(out=st3.t[:, 3:4], in_=st3.t[:, 2:3]), [st3.r], [st3.r])
                for ac, cs_ in H:
                    V(lambda: nc.vector.scalar_tensor_tensor(out=o_.t[:, cs_], in0=ac.t[:], scalar=st3.t[:, 3:4], in1=gfb.t[:, cs_],
                                                             op0=ALU.mult, op1=ALU.mult), [ac.r, st3.r, gfb.r], [o_.r])
                T.dma(SP, o_.l, lambda: nc.sync.dma_start(out=out_d[t * 128:(t + 1) * 128, :], in_=o_.t[:]), reads=[o_.r])
            T.barrier()
    return nc


def _own_tiles(j, NP):
    t = []
    for m in range(NP):
        t += [8 * m + j, 8 * m + 7 - j]
    return t


def make_in_maps(inp, S):
    f32 = np.float32
    NT = S // 128; NP = NT // 8; NOWN = 2 * NP; TOK = NOWN * 128
    NBLK = (TOK * 2) // BLK + NEXP
    g = lambda k: np.asarray(inp[k])
    col = lambda v, n: np.ascontiguousarray(np.asarray(v, f32).reshape(n, 128).T)
    x = g("x"); c = g("c"); pos = g("positions")
    p = np.arange(128)
    fq = np.zeros((128, 1), f32)
    freqs = (np.float32(10000.0) ** (-np.arange(16, dtype=f32) / np.float32(16))).astype(f32)
    fq[64:96, 0] = np.tile(freqs, 2) / np.float32(2 * np.pi)
    shared = dict(
        w_ada=np.ascontiguousarray(g("w_ada")[0]), b_adaT=col(g("b_ada")[0], 48), b_ada_row=g("b_ada")[0].reshape(1, -1).astype(f32),
        g1col=col(g("norm1_g")[0], 8), g1row=g("norm1_g")[0].reshape(1, -1).astype(f32), w_in=np.ascontiguousarray(g("w_in")[0]),
        gqcol=col(g("q_norm_g")[0], 2), w_uq=np.ascontiguousarray(g("w_uq")[0]),
        gkvcol=col(g("kv_norm_g")[0], 1), w_ukv=np.ascontiguousarray(g("w_ukv")[0]),
        gv_row=g("v_norm_g")[0].reshape(1, -1).astype(f32), bv_row=g("v_norm_b")[0].reshape(1, -1).astype(f32),
        w_s=np.ascontiguousarray(g("w_s")[0]), bsT=np.ascontiguousarray(g("b_s")[0].T),
        w_br_mla=np.ascontiguousarray(g("w_br_mla")[0]), w_br_sgu=np.ascontiguousarray(g("w_br_sgu")[0]),
        w_out=np.ascontiguousarray(g("w_out")[0]),
        g2col=col(g("norm2_g")[0], 8), g2row=g("norm2_g")[0].reshape(1, -1).astype(f32),
        wr=np.ascontiguousarray(np.concatenate([g("w_rg")[0], g("w_re")[0]], axis=1)),
        br_row=np.concatenate([g("b_rg")[0], g("b_re")[0]]).reshape(1, -1).astype(f32),
        w1=np.ascontiguousarray(g("w1")[0]).reshape(NEXP * 128, 4096),
        w3=np.ascontiguousarray(g("w3")[0]).reshape(NEXP * 128, 4096),
        w2=np.ascontiguousarray(g("w2")[0]).reshape(NEXP * 128, 4096),
        gf_row=g("final_g").reshape(1, -1).astype(f32),
        c_ident=np.eye(128, dtype=f32), c_tril=np.tril(np.ones((128, 128), f32)),
        c_lstrict=(p[:, None] < p[None, :]).astype(f32), c_kq=(p[:, None] - p[None, :]).astype(f32),
        c_fq=fq, c_thr=np.tile((np.arange(NBLK, dtype=f32) * BLK)[None, :], (128, 1)).astype(f32),
        c_pidx=p.astype(f32).reshape(128, 1),
    )
    maps = []
    owns = []
    for core in range(8):
        b, j = core // 4, core % 4
        tiles = _own_tiles(j, NP)
        rows = np.concatenate([np.arange(t * 128, (t + 1) * 128) for t in tiles])
        owns.append((b, rows))
        dt_ = np.zeros((128, 16), f32)
        for o in range(8):
            dt_[:, o] = (j - o) * 128
            dt_[:, 8 + o] = (7 - j - o) * 128
        m = dict(shared)
        m.update(
            xb=np.ascontiguousarray(x[b, :S]), xo=np.ascontiguousarray(x[b][rows]),
            posb=np.ascontiguousarray(pos[b, :S].reshape(1, -1).astype(np.int32)),
            poso=np.ascontiguousarray(pos[b][rows].reshape(1, -1).astype(np.int32)),
            ccol=col(c[b], 8), c_dtab=dt_,
        )
        maps.append(m)
    return maps, owns


_NC_CACHE = {}


def kernel(**inputs):
    S = int(np.asarray(inputs["x"]).shape[1])
    if S not in _NC_CACHE:
        _NC_CACHE[S] = build_nc(S)
    nc = _NC_CACHE[S]
    maps, owns = make_in_maps(inputs, S)
    res = run_bass_kernel_spmd(nc, maps, core_ids=list(range(8)))
    B = np.asarray(inputs["x"]).shape[0]
    out = np.zeros((B, S, D), np.float32)
    for core, (b, rows) in enumerate(owns):
        out[b, rows] = np.asarray(res.results[core]["out"], dtype=np.float32)
    return out
```
